# Optimizing a Trainium2 kernel written in Bass

```python
import math
import numpy as np
import jax, jax.numpy as jnp
from jax import lax

D_MODEL = 1024
BATCH = 2
SEQ = 16384
DEPTH = 2

N_MIXERS = 2
S5_GROUP = 16
S5_GROUPS = D_MODEL // S5_GROUP
S5_STATE = 64
HEAD_DIM = 64
N_HEADS = D_MODEL // HEAD_DIM
D_ATTN = N_HEADS * HEAD_DIM
DILATED_PAIRS = ((128, 1), (512, 4), (2048, 16))
N_DIL_GROUPS = len(DILATED_PAIRS)
ATTN_BLOCK = 128
N_BUCKETS = 32
MAX_DISTANCE = 2048
D_FF = ((8 * D_MODEL // 3 + 127) // 128) * 128
N_EXPERTS = 8
TOP_K = 2
D_FF_EXPERT = 7 * D_MODEL // 2
N_S5_LAYERS = (DEPTH + 1) // 2
N_ATTN_LAYERS = DEPTH // 2
DEEPNORM_ALPHA = (2 * DEPTH) ** 0.25
DEEPNORM_BETA = (8 * DEPTH) ** -0.25
LN_EPS = 1e-5
NEG_INF = -1e30

kernel_name = 'hybrid_s5_dilated_attn_moe_deepnorm'


def _layer_norm(x, g, b):
    xf = x.astype(jnp.float32)
    mu = jnp.mean(xf, axis=-1, keepdims=True)
    var = jnp.mean(jnp.square(xf - mu), axis=-1, keepdims=True)
    y = (xf - mu) * lax.rsqrt(var + LN_EPS)
    return (y * g.astype(jnp.float32) + b.astype(jnp.float32)).astype(x.dtype)


def _t5_bucket(dist):
    max_exact = N_BUCKETS // 2
    d = np.maximum(dist, 0)
    large = max_exact + (np.log(np.maximum(d, 1) / max_exact) / math.log(MAX_DISTANCE / max_exact)
                         * (N_BUCKETS - max_exact)).astype(np.int64)
    large = np.minimum(large, N_BUCKETS - 1)
    return np.where(d < max_exact, d, large).astype(np.int32)


def _s5_mixer(x, w_in, a_re, a_im, log_dt, b_re, b_im, c_re, c_im, d_skip, w_glu):
    f32 = jnp.float32
    bsz, seq, _ = x.shape
    u = (x @ w_in).reshape(bsz, seq, S5_GROUPS, S5_GROUP).astype(f32)
    dt = jnp.exp(log_dt.astype(f32))[:, None]
    lam_re = jnp.minimum(a_re.astype(f32), -1e-4)
    lam_im = a_im.astype(f32)
    mag = jnp.exp(lam_re * dt)
    ab_re = mag * jnp.cos(lam_im * dt)
    ab_im = mag * jnp.sin(lam_im * dt)
    den = lam_re * lam_re + lam_im * lam_im
    nr = ab_re - 1.0
    q_re = (nr * lam_re + ab_im * lam_im) / den
    q_im = (ab_im * lam_re - nr * lam_im) / den
    br, bi = b_re.astype(f32), b_im.astype(f32)
    bb_re = q_re[..., None] * br - q_im[..., None] * bi
    bb_im = q_re[..., None] * bi + q_im[..., None] * br
    bu_re = jnp.einsum('bsgc,gpc->bsgp', u, bb_re)
    bu_im = jnp.einsum('bsgc,gpc->bsgp', u, bb_im)
    a_re_t = jnp.broadcast_to(ab_re, bu_re.shape)
    a_im_t = jnp.broadcast_to(ab_im, bu_re.shape)

    def combine(e1, e2):
        a1r, a1i, b1r, b1i = e1
        a2r, a2i, b2r, b2i = e2
        return (a2r * a1r - a2i * a1i,
                a2r * a1i + a2i * a1r,
                a2r * b1r - a2i * b1i + b2r,
                a2r * b1i + a2i * b1r + b2i)

    _, _, h_re, h_im = lax.associative_scan(combine, (a_re_t, a_im_t, bu_re, bu_im), axis=1)
    y = (jnp.einsum('bsgp,gcp->bsgc', h_re, c_re.astype(f32))
         - jnp.einsum('bsgp,gcp->bsgc', h_im, c_im.astype(f32))
         + d_skip.astype(f32).reshape(S5_GROUPS, S5_GROUP) * u)
    y = y.reshape(bsz, seq, D_MODEL).astype(x.dtype)
    g = jax.nn.gelu(y)
    vg = g @ w_glu
    return vg[..., :D_MODEL] * jax.nn.sigmoid(vg[..., D_MODEL:])


def _dilated_branch(q, k, v, rel_bias, window, dilation):
    f32 = jnp.float32
    bsz, seq, nh, hd = q.shape
    span = window // dilation
    L = ATTN_BLOCK
    chunk = dilation * L
    sp = -(-seq // chunk) * chunk
    nb = sp // chunk

    def to_blocks(t):
        t = jnp.pad(t, ((0, 0), (0, sp - seq), (0, 0), (0, 0)))
        t = t.reshape(bsz, sp // dilation, dilation, nh, t.shape[-1]).transpose(0, 2, 1, 3, 4)
        return t.reshape(bsz, dilation, nb, L, nh, t.shape[-1])

    def from_blocks(t):
        t = t.reshape(bsz, dilation, sp // dilation, nh, t.shape[-1]).transpose(0, 2, 1, 3, 4)
        return t.reshape(bsz, sp, nh, t.shape[-1])[:, :seq]

    qb, kb, vb = to_blocks(q), to_blocks(k), to_blocks(v)
    def with_prev(t):
        prev = jnp.concatenate([jnp.zeros_like(t[:, :, :1]), t[:, :, :-1]], axis=2)
        return jnp.concatenate([prev, t], axis=3)
    kk, vv = with_prev(kb), with_prev(vb)

    qi = np.arange(L)[:, None]
    ki = np.arange(2 * L)[None, :]
    off = qi + L - ki
    band = (off >= 0) & (off <= span)
    mask = band[None] & ((np.arange(nb)[:, None, None] > 0) | (ki >= L)[None])
    bucket = _t5_bucket(np.clip(off, 0, None) * dilation)
    bias = jnp.transpose(rel_bias[bucket], (2, 0, 1)).astype(f32)

    logits = jnp.einsum('brnqhd,brnkhd->brnhqk', qb, kk).astype(f32) * (1.0 / math.sqrt(hd)) + bias
    logits = jnp.where(mask[None, None, :, None], logits, NEG_INF)
    m = jnp.max(logits, axis=-1, keepdims=True)
    p = jnp.exp(logits - m)
    s = jnp.sum(p, axis=-1, keepdims=True)
    o = jnp.einsum('brnhqk,brnkhd->brnqhd', p, vv.astype(f32)) / jnp.swapaxes(s, 3, 4)
    lse = jnp.swapaxes(m + jnp.log(s), 3, 4)
    return from_blocks(o), from_blocks(lse)[..., 0]


def _dilated_attention(x, w_qkv, w_o, rel_bias):
    bsz, seq, _ = x.shape
    qkv = (x @ w_qkv).reshape(bsz, seq, N_DIL_GROUPS, 3, N_HEADS, HEAD_DIM)
    outs, lses = [], []
    for g, (window, dilation) in enumerate(DILATED_PAIRS):
        o, lse = _dilated_branch(qkv[:, :, g, 0], qkv[:, :, g, 1], qkv[:, :, g, 2], rel_bias, window, dilation)
        outs.append(o)
        lses.append(lse)
    w = jax.nn.softmax(jnp.stack(lses, axis=0), axis=0)
    merged = jnp.einsum('gbsh,gbshd->bshd', w, jnp.stack(outs, axis=0))
    return merged.reshape(bsz, seq, D_ATTN).astype(x.dtype) @ w_o


def _swiglu(x, w1, w3, w2):
    return (jax.nn.silu(x @ w1) * (x @ w3)) @ w2


def _moe(x, w_router, w1, w3, w2):
    logits = (x @ w_router).astype(jnp.float32)
    top_v, top_i = lax.top_k(logits, TOP_K)
    gates = jax.nn.softmax(top_v, axis=-1)
    gate_full = jnp.sum(jax.nn.one_hot(top_i, N_EXPERTS, dtype=jnp.float32) * gates[..., None], axis=-2)
    out = jnp.zeros_like(x)
    for e in range(N_EXPERTS):
        y = _swiglu(x, w1[e], w3[e], w2[e])
        out = out + gate_full[..., e, None].astype(x.dtype) * y
    return out


def setup_inputs(seed: int = 0) -> dict:
    key = jax.random.key(seed)
    ks = jax.random.split(key, 24)
    f32 = jnp.float32

    def nrm(k, shape, scale):
        return jax.random.normal(k, shape, f32) * scale

    NS, NA = N_S5_LAYERS, N_ATTN_LAYERS
    x = nrm(ks[0], (BATCH, SEQ, D_MODEL), 1.0)
    s5_w_in = nrm(ks[1], (NS, D_MODEL, D_MODEL), D_MODEL ** -0.5)
    s5_a_re = -0.5 + nrm(ks[2], (NS, S5_GROUPS, S5_STATE), 0.01)
    s5_a_im = math.pi * jnp.arange(S5_STATE, dtype=f32) + nrm(ks[3], (NS, S5_GROUPS, S5_STATE), 0.01)
    s5_log_dt = jax.random.uniform(ks[4], (NS, S5_GROUPS), f32, math.log(1e-3), math.log(1e-1))
    s5_b_re = nrm(ks[5], (NS, S5_GROUPS, S5_STATE, S5_GROUP), (2 * S5_GROUP) ** -0.5)
    s5_b_im = nrm(ks[6], (NS, S5_GROUPS, S5_STATE, S5_GROUP), (2 * S5_GROUP) ** -0.5)
    s5_c_re = nrm(ks[7], (NS, S5_GROUPS, S5_GROUP, S5_STATE), (2 * S5_STATE) ** -0.5)
    s5_c_im = nrm(ks[8], (NS, S5_GROUPS, S5_GROUP, S5_STATE), (2 * S5_STATE) ** -0.5)
    s5_d = nrm(ks[9], (NS, D_MODEL), 1.0)
    glu_scale = jnp.concatenate([jnp.full((D_MODEL,), DEEPNORM_BETA, f32), jnp.ones((D_MODEL,), f32)])
    s5_w_glu = nrm(ks[10], (NS, D_MODEL, 2 * D_MODEL), D_MODEL ** -0.5) * glu_scale
    qkv_scale = jnp.array([1.0, 1.0, DEEPNORM_BETA], f32)[:, None]
    attn_w_qkv = (nrm(ks[11], (NA, D_MODEL, N_DIL_GROUPS, 3, D_ATTN), D_MODEL ** -0.5)
                  * qkv_scale).reshape(NA, D_MODEL, N_DIL_GROUPS * 3 * D_ATTN)
    attn_w_o = nrm(ks[12], (NA, D_ATTN, D_MODEL), D_ATTN ** -0.5 * DEEPNORM_BETA)
    rel_bias = nrm(ks[13], (N_BUCKETS, N_HEADS), 0.5)
    ffn_w1 = nrm(ks[14], (NS, D_MODEL, D_FF), D_MODEL ** -0.5)
    ffn_w3 = nrm(ks[15], (NS, D_MODEL, D_FF), D_MODEL ** -0.5)
    ffn_w2 = nrm(ks[16], (NS, D_FF, D_MODEL), D_FF ** -0.5 * DEEPNORM_BETA)
    moe_w_router = nrm(ks[17], (NA, D_MODEL, N_EXPERTS), D_MODEL ** -0.5)
    moe_w1 = nrm(ks[18], (NA, N_EXPERTS, D_MODEL, D_FF_EXPERT), D_MODEL ** -0.5)
    moe_w3 = nrm(ks[19], (NA, N_EXPERTS, D_MODEL, D_FF_EXPERT), D_MODEL ** -0.5)
    moe_w2 = nrm(ks[20], (NA, N_EXPERTS, D_FF_EXPERT, D_MODEL), D_FF_EXPERT ** -0.5 * DEEPNORM_BETA)
    ln_g = 1.0 + nrm(ks[21], (DEPTH, 2, D_MODEL), 0.02)
    ln_b = nrm(ks[22], (DEPTH, 2, D_MODEL), 0.02)
    return {'x': x, 's5_w_in': s5_w_in, 's5_a_re': s5_a_re, 's5_a_im': s5_a_im, 's5_log_dt': s5_log_dt,
            's5_b_re': s5_b_re, 's5_b_im': s5_b_im, 's5_c_re': s5_c_re, 's5_c_im': s5_c_im, 's5_d': s5_d,
            's5_w_glu': s5_w_glu, 'attn_w_qkv': attn_w_qkv, 'attn_w_o': attn_w_o, 'rel_bias': rel_bias,
            'ffn_w1': ffn_w1, 'ffn_w3': ffn_w3, 'ffn_w2': ffn_w2, 'moe_w_router': moe_w_router,
            'moe_w1': moe_w1, 'moe_w3': moe_w3, 'moe_w2': moe_w2, 'ln_g': ln_g, 'ln_b': ln_b}


def reference(x, s5_w_in, s5_a_re, s5_a_im, s5_log_dt, s5_b_re, s5_b_im, s5_c_re, s5_c_im, s5_d,
              s5_w_glu, attn_w_qkv, attn_w_o, rel_bias, ffn_w1, ffn_w3, ffn_w2, moe_w_router,
              moe_w1, moe_w3, moe_w2, ln_g, ln_b):
    for i in range(DEPTH):
        j = i // N_MIXERS
        if i % N_MIXERS == 0:
            h = _s5_mixer(x, s5_w_in[j], s5_a_re[j], s5_a_im[j], s5_log_dt[j], s5_b_re[j], s5_b_im[j],
                          s5_c_re[j], s5_c_im[j], s5_d[j], s5_w_glu[j])
            x = _layer_norm(DEEPNORM_ALPHA * x + h, ln_g[i, 0], ln_b[i, 0])
            f = _swiglu(x, ffn_w1[j], ffn_w3[j], ffn_w2[j])
        else:
            h = _dilated_attention(x, attn_w_qkv[j], attn_w_o[j], rel_bias)
            x = _layer_norm(DEEPNORM_ALPHA * x + h, ln_g[i, 0], ln_b[i, 0])
            f = _moe(x, moe_w_router[j], moe_w1[j], moe_w3[j], moe_w2[j])
        x = _layer_norm(DEEPNORM_ALPHA * x + f, ln_g[i, 1], ln_b[i, 1])
    return x
```

```python
import math
import numpy as np
import concourse.bass as bass
import concourse.mybir as mybir
from concourse.bass_utils import run_bass_kernel_spmd
from contextlib import ExitStack

F32 = mybir.dt.float32
BF16 = mybir.dt.bfloat16
I32 = mybir.dt.int32
ALU = mybir.AluOpType
AF = mybir.ActivationFunctionType
AX = mybir.AxisListType

NCORES = 8
D = 1024
SEQ = 16384
BATCH = 2
NTOK = BATCH * SEQ
TPC = NTOK // NCORES
DEPTH = 2
ALPHA = (2 * DEPTH) ** 0.25
LN_EPS = 1e-5
PI = math.pi


class Buf:
    __slots__ = ("name", "t", "lw", "rd", "dsem", "dcnt", "nodep")

    def __init__(self, name, t):
        self.name = name
        self.t = t
        self.lw = None
        self.rd = {}
        self.dsem = None
        self.dcnt = 0
        self.nodep = False

    def __getitem__(self, idx):
        return self.t[idx]


class KB:
    def __init__(self, same_engine_sync=True):
        self.nc = bass.Bass("TRN2", target_bir_lowering=False)
        self.es = ExitStack()
        nc = self.nc
        self.eng = {"pe": nc.tensor, "dve": nc.vector, "act": nc.scalar, "pool": nc.gpsimd, "sp": nc.sync}
        self.sem = {e: self.es.enter_context(nc.semaphore("s_" + e)) for e in self.eng}
        self.cnt = {e: 0 for e in self.eng}
        self.known = {e: {} for e in self.eng}
        self.ses = same_engine_sync
        self.nsem = 0
        self.prog = {e: [] for e in self.eng}
        self.outs = []
        self.uid = 0

    def sb(self, name, shape, dt):
        self.uid += 1
        return Buf(name, self.es.enter_context(self.nc.sbuf_tensor("%s_%d" % (name, self.uid), list(shape), dt)))

    def ps(self, name, shape=(128, 512), dt=F32):
        self.uid += 1
        return Buf(name, self.es.enter_context(self.nc.psum_tensor("%s_%d" % (name, self.uid), list(shape), dt)))

    def dram(self, name, shape, dt, kind):
        b = Buf(name, self.nc.dram_tensor(name, list(shape), dt, kind=kind).ap())
        if kind == "ExternalOutput":
            self.outs.append(b)
            b.nodep = True
        return b

    def _deps(self, e, reads, writes):
        need = {}

        def add(tag):
            if tag is None:
                return
            s, v = tag
            if need.get(s, 0) < v:
                need[s] = v

        for b in reads:
            add(b.lw)
        for b in writes:
            if b.nodep:
                continue
            add(b.lw)
            for s, v in b.rd.items():
                add((s, v))
        k = self.known[e]
        own = self.sem[e]
        for s, v in need.items():
            if s == own and (e == "pe" or not self.ses):
                continue
            if k.get(s, 0) >= v:
                continue
            self.prog[e].append(("w", s, v))
            k[s] = v

    def op(self, e, fn, reads=(), writes=()):
        self._deps(e, reads, writes)
        self.cnt[e] += 1
        self.prog[e].append(("i", fn, self.sem[e], 1))
        tag = (self.sem[e], self.cnt[e])
        for b in writes:
            b.lw = tag
            b.rd = {}
        for b in reads:
            if b not in writes:
                b.rd[tag[0]] = tag[1]

    def dma(self, q, out_buf, out_ap, in_buf, in_ap, **kw):
        self._deps(q, [in_buf], [out_buf])
        if out_buf.dsem is None:
            self.nsem += 1
            out_buf.dsem = self.es.enter_context(self.nc.semaphore("d%d" % self.nsem))
        self.prog[q].append(("i", (lambda e, o=out_ap, i=in_ap, kw=kw: e.dma_start(out=o, in_=i, **kw)),
                             out_buf.dsem, 16))
        out_buf.dcnt += 16
        tag = (out_buf.dsem, out_buf.dcnt)
        out_buf.lw = tag
        out_buf.rd = {}
        in_buf.rd[tag[0]] = tag[1]

    def finish(self):
        for b in self.outs:
            if b.lw is not None:
                self.prog["sp"].append(("w", b.lw[0], b.lw[1]))
        for e in ("pe", "dve", "act", "pool"):
            if self.cnt[e]:
                self.prog["sp"].append(("w", self.sem[e], self.cnt[e]))
        prog = self.prog

        def replay(e):
            def body(eng):
                for it in prog[e]:
                    if it[0] == "w":
                        eng.wait_ge(it[1], it[2])
                    else:
                        it[1](eng).then_inc(it[2], it[3])
            return body

        with self.nc.Block() as block:
            block.sync(replay("sp"))
            block.tensor(replay("pe"))
            block.vector(replay("dve"))
            block.scalar(replay("act"))
            block.gpsimd(replay("pool"))
        self.es.close()
        return self.nc

    def tt(self, e, out_b, out_ap, a_b, a_ap, b_b, b_ap, op):
        self.op(e, lambda g: g.tensor_tensor(out=out_ap, in0=a_ap, in1=b_ap, op=op), [a_b, b_b], [out_b])

    def ts(self, e, out_b, out_ap, a_b, a_ap, s1, op0, s2=None, op1=None, extra_reads=()):
        if op1 is None:
            self.op(e, lambda g: g.tensor_scalar(out=out_ap, in0=a_ap, scalar1=s1, scalar2=None, op0=op0),
                    [a_b] + list(extra_reads), [out_b])
        else:
            self.op(e, lambda g: g.tensor_scalar(out=out_ap, in0=a_ap, scalar1=s1, scalar2=s2, op0=op0, op1=op1),
                    [a_b] + list(extra_reads), [out_b])

    def stt(self, e, out_b, out_ap, a_b, a_ap, scalar, b_b, b_ap, op0, op1, extra_reads=()):
        self.op(e, lambda g: g.scalar_tensor_tensor(out=out_ap, in0=a_ap, scalar=scalar, in1=b_ap, op0=op0, op1=op1),
                [a_b, b_b] + list(extra_reads), [out_b])

    def act(self, out_b, out_ap, in_b, in_ap, func, scale=1.0, bias=0.0, extra_reads=()):
        self.op("act", lambda g: g.activation(out=out_ap, in_=in_ap, func=func, scale=scale, bias=bias),
                [in_b] + list(extra_reads), [out_b])

    def mm(self, out_b, out_ap, l_b, l_ap, r_b, r_ap, start, stop):
        self.op("pe", lambda g: g.matmul(out_ap, l_ap, r_ap, start=start, stop=stop), [l_b, r_b], [out_b])


def build_uproj():
    k = KB()
    xT = k.dram("xT", [D, TPC], F32, "ExternalInput")
    w = k.dram("w", [D, D], F32, "ExternalInput")
    uT = k.dram("uT", [D, TPC], F32, "ExternalOutput")
    wsb = k.sb("w", [128, 8, D], BF16)
    k.dma("pool", wsb, wsb[:, :, :], w, w.t.rearrange("(kt p) m -> p kt m", p=128))
    xs = [k.sb("xs", [128, 8, 512], BF16) for _ in range(2)]
    pbs = [k.ps("pb") for _ in range(4)]
    sts = [k.sb("st", [128, 512], F32) for _ in range(4)]
    i = 0
    for nb in range(TPC // 512):
        xb = xs[nb % 2]
        k.dma("pool", xb, xb[:, :, :], xT, xT.t[:, nb * 512:(nb + 1) * 512].rearrange("(kt p) n -> p kt n", p=128))
        for m in range(8):
            pb = pbs[i % 4]
            st = sts[i % 4]
            for kt in range(8):
                k.mm(pb, pb[:, :], wsb, wsb[:, kt, m * 128:(m + 1) * 128], xb, xb[:, kt, :], kt == 0, kt == 7)
            if i % 2 == 0:
                k.act(st, st[:, :], pb, pb[:, :], AF.Copy)
            else:
                k.op("dve", lambda g, o=st[:, :], a=pb[:, :]: g.tensor_copy(out=o, in_=a), [pb], [st])
            k.dma("sp", uT, uT.t[m * 128:(m + 1) * 128, nb * 512:(nb + 1) * 512], st, st[:, :])
            i += 1
    return k.finish()


S5_BLK = 512


def build_s5(ntok=NTOK, seq=SEQ):
    k = KB()
    u = k.dram("u", [128, ntok], F32, "ExternalInput")
    are = k.dram("are", [128, 4], F32, "ExternalInput")
    aim = k.dram("aim", [128, 4], F32, "ExternalInput")
    ldt = k.dram("ldt", [128, 4], F32, "ExternalInput")
    bre = k.dram("bre", [128, 4, 128], F32, "ExternalInput")
    bim = k.dram("bim", [128, 4, 128], F32, "ExternalInput")
    cre = k.dram("cre", [128, 4, 32], F32, "ExternalInput")
    cim = k.dram("cim", [128, 4, 32], F32, "ExternalInput")
    dsk = k.dram("dsk", [128, 1], F32, "ExternalInput")
    y = k.dram("y", [128, ntok], F32, "ExternalOutput")
    L = S5_BLK

    def small(name, w=4, dt=F32):
        return k.sb(name, [128, w], dt)

    s_are, s_aim, s_ldt = small("are"), small("aim"), small("ldt")
    k.dma("sp", s_are, s_are[:, :], are, are.t[:, :])
    k.dma("sp", s_aim, s_aim[:, :], aim, aim.t[:, :])
    k.dma("sp", s_ldt, s_ldt[:, :], ldt, ldt.t[:, :])
    s_d = small("dsk", 1)
    k.dma("sp", s_d, s_d[:, :], dsk, dsk.t[:, :])
    b_re = k.sb("bre", [128, 4, 128], BF16)
    b_im = k.sb("bim", [128, 4, 128], BF16)
    k.dma("pool", b_re, b_re[:, :, :], bre, bre.t[:, :, :])
    k.dma("pool", b_im, b_im[:, :, :], bim, bim.t[:, :, :])
    c_re = k.sb("cre", [128, 4, 32], F32)
    c_im = k.sb("cim", [128, 4, 32], F32)
    k.dma("sp", c_re, c_re[:, :, :], cre, cre.t[:, :, :])
    k.dma("sp", c_im, c_im[:, :, :], cim, cim.t[:, :, :])

    dt_ = small("dt")
    k.act(dt_, dt_[:, :], s_ldt, s_ldt[:, :], AF.Exp)
    lre = small("lre")
    k.ts("dve", lre, lre[:, :], s_are, s_are[:, :], -1e-4, ALU.min)
    lrd = small("lrd")
    k.tt("dve", lrd, lrd[:, :], lre, lre[:, :], dt_, dt_[:, :], ALU.mult)
    rho = small("rho")
    k.act(rho, rho[:, :], lrd, lrd[:, :], AF.Exp)
    th = small("th")
    k.tt("dve", th, th[:, :], s_aim, s_aim[:, :], dt_, dt_[:, :], ALU.mult)

    def sin_of(src, shift, name):
        a = small(name + "a")
        k.ts("dve", a, a[:, :], src, src[:, :], float(shift), ALU.add)
        t = small(name + "t")
        k.ts("dve", t, t[:, :], a, a[:, :], 1.0 / (2 * PI), ALU.mult)
        ti = small(name + "ti", dt=I32)
        k.op("dve", lambda g: g.tensor_copy(out=ti[:, :], in_=t[:, :]), [t], [ti])
        tf = small(name + "tf")
        k.op("dve", lambda g: g.tensor_copy(out=tf[:, :], in_=ti[:, :]), [ti], [tf])
        r = small(name + "r")
        k.stt("dve", r, r[:, :], tf, tf[:, :], -2 * PI, a, a[:, :], ALU.mult, ALU.add)
        m1 = small(name + "m1")
        k.ts("dve", m1, m1[:, :], r, r[:, :], PI, ALU.is_gt)
        r2 = small(name + "r2")
        k.stt("dve", r2, r2[:, :], m1, m1[:, :], -2 * PI, r, r[:, :], ALU.mult, ALU.add)
        m2 = small(name + "m2")
        k.ts("dve", m2, m2[:, :], r2, r2[:, :], -PI, ALU.is_lt)
        r3 = small(name + "r3")
        k.stt("dve", r3, r3[:, :], m2, m2[:, :], 2 * PI, r2, r2[:, :], ALU.mult, ALU.add)
        r4 = small(name + "r4")
        k.ts("dve", r4, r4[:, :], r3, r3[:, :], PI, ALU.min, -PI, ALU.max)
        o = small(name + "o")
        k.act(o, o[:, :], r4, r4[:, :], AF.Sin)
        return o

    sn = sin_of(th, 0.0, "sn")
    cs = sin_of(th, PI / 2, "cs")
    abr, abi = small("abr"), small("abi")
    k.tt("dve", abr, abr[:, :], rho, rho[:, :], cs, cs[:, :], ALU.mult)
    k.tt("dve", abi, abi[:, :], rho, rho[:, :], sn, sn[:, :], ALU.mult)
    den, t0 = small("den"), small("t0")
    k.tt("dve", den, den[:, :], lre, lre[:, :], lre, lre[:, :], ALU.mult)
    k.tt("dve", t0, t0[:, :], s_aim, s_aim[:, :], s_aim, s_aim[:, :], ALU.mult)
    den2 = small("den2")
    k.tt("dve", den2, den2[:, :], den, den[:, :], t0, t0[:, :], ALU.add)
    rden = small("rden")
    k.op("dve", lambda g: g.reciprocal(out=rden[:, :], in_=den2[:, :]), [den2], [rden])
    nr = small("nr")
    k.ts("dve", nr, nr[:, :], abr, abr[:, :], -1.0, ALU.add)
    t1, t2, t3, t4 = small("t1"), small("t2"), small("t3"), small("t4")
    k.tt("dve", t1, t1[:, :], nr, nr[:, :], lre, lre[:, :], ALU.mult)
    k.tt("dve", t2, t2[:, :], abi, abi[:, :], s_aim, s_aim[:, :], ALU.mult)
    k.tt("dve", t3, t3[:, :], abi, abi[:, :], lre, lre[:, :], ALU.mult)
    k.tt("dve", t4, t4[:, :], nr, nr[:, :], s_aim, s_aim[:, :], ALU.mult)
    q1, q2 = small("q1"), small("q2")
    k.tt("dve", q1, q1[:, :], t1, t1[:, :], t2, t2[:, :], ALU.add)
    k.tt("dve", q2, q2[:, :], t3, t3[:, :], t4, t4[:, :], ALU.subtract)
    qre, qim = small("qre"), small("qim")
    k.tt("dve", qre, qre[:, :], q1, q1[:, :], rden, rden[:, :], ALU.mult)
    k.tt("dve", qim, qim[:, :], q2, q2[:, :], rden, rden[:, :], ALU.mult)
    nqim = small("nqim")
    k.ts("dve", nqim, nqim[:, :], qim, qim[:, :], -1.0, ALU.mult)
    nqre = small("nqre")
    k.ts("dve", nqre, nqre[:, :], qre, qre[:, :], -1.0, ALU.mult)

    cp_re = k.sb("cpre", [128, 4, 128], BF16)
    cp_imn = k.sb("cpimn", [128, 4, 128], BF16)
    k.op("pool", lambda g: g.memset(cp_re[:, :, :], 0.0), [], [cp_re])
    k.op("pool", lambda g: g.memset(cp_imn[:, :, :], 0.0), [], [cp_imn])
    ctmp = k.sb("ctmp", [128, 4, 32], F32)
    for qq in range(4):
        k.ts("dve", ctmp, ctmp[:, qq, :], c_im, c_im[:, qq, :], nqim[:, qq:qq + 1], ALU.mult, extra_reads=[nqim])
        k.stt("dve", cp_re, cp_re[:, qq, 32 * qq:32 * qq + 32], c_re, c_re[:, qq, :], qre[:, qq:qq + 1], ctmp, ctmp[:, qq, :],
              ALU.mult, ALU.add, extra_reads=[qre])
    ctmp2 = k.sb("ctmp2", [128, 4, 32], F32)
    for qq in range(4):
        k.ts("dve", ctmp2, ctmp2[:, qq, :], c_im, c_im[:, qq, :], nqre[:, qq:qq + 1], ALU.mult, extra_reads=[nqre])
        k.stt("dve", cp_imn, cp_imn[:, qq, 32 * qq:32 * qq + 32], c_re, c_re[:, qq, :], nqim[:, qq:qq + 1], ctmp2, ctmp2[:, qq, :],
              ALU.mult, ALU.add, extra_reads=[nqim])

    COS = k.sb("COS", [128, 4, L], F32)
    SIN = k.sb("SIN", [128, 4, L], F32)
    RHO = k.sb("RHO", [128, 4, L], F32)
    k.op("pool", lambda g: g.memset(COS[:, :, :], 1.0), [], [COS])
    k.op("pool", lambda g: g.memset(SIN[:, :, :], 0.0), [], [SIN])
    k.op("pool", lambda g: g.memset(RHO[:, :, :], 1.0), [], [RHO])
    for qq in range(4):
        k.ts("dve", RHO, RHO[:, qq, :], RHO, RHO[:, qq, :], rho[:, qq:qq + 1], ALU.mult, extra_reads=[rho])
    pr, pi_ = cs, sn
    ttmp = k.sb("ttmp", [128, 4, L // 2], F32)
    w = 1
    lvl = 0
    while w < L:
        npi = small("npi%d" % lvl)
        k.ts("dve", npi, npi[:, :], pi_, pi_[:, :], -1.0, ALU.mult)
        for qq in range(4):
            k.ts("dve", ttmp, ttmp[:, qq, 0:w], SIN, SIN[:, qq, 0:w], npi[:, qq:qq + 1], ALU.mult, extra_reads=[npi])
            k.stt("dve", COS, COS[:, qq, w:2 * w], COS, COS[:, qq, 0:w], pr[:, qq:qq + 1], ttmp, ttmp[:, qq, 0:w],
                  ALU.mult, ALU.add, extra_reads=[pr])
            k.ts("dve", ttmp, ttmp[:, qq, 0:w], SIN, SIN[:, qq, 0:w], pr[:, qq:qq + 1], ALU.mult, extra_reads=[pr])
            k.stt("dve", SIN, SIN[:, qq, w:2 * w], COS, COS[:, qq, 0:w], pi_[:, qq:qq + 1], ttmp, ttmp[:, qq, 0:w],
                  ALU.mult, ALU.add, extra_reads=[pi_])
        a2, b2, ab_ = small("a2_%d" % lvl), small("b2_%d" % lvl), small("ab_%d" % lvl)
        k.tt("dve", a2, a2[:, :], pr, pr[:, :], pr, pr[:, :], ALU.mult)
        k.tt("dve", b2, b2[:, :], pi_, pi_[:, :], pi_, pi_[:, :], ALU.mult)
        k.tt("dve", ab_, ab_[:, :], pr, pr[:, :], pi_, pi_[:, :], ALU.mult)
        npr, npi2 = small("npr%d" % lvl), small("npi2%d" % lvl)
        k.tt("dve", npr, npr[:, :], a2, a2[:, :], b2, b2[:, :], ALU.subtract)
        k.ts("dve", npi2, npi2[:, :], ab_, ab_[:, :], 2.0, ALU.mult)
        pr, pi_ = npr, npi2
        w *= 2
        lvl += 1
    ELr, ELi = pr, pi_
    nELi = small("nELi")
    k.ts("dve", nELi, nELi[:, :], ELi, ELi[:, :], -1.0, ALU.mult)

    ub = [k.sb("ub", [128, L], F32) for _ in range(2)]
    ubf = [k.sb("ubf", [128, L], BF16) for _ in range(2)]
    ps_re = [k.ps("psre") for _ in range(2)]
    ps_im = [k.ps("psim") for _ in range(2)]
    ps_y = [k.ps("psy") for _ in range(2)]
    T = [[k.sb("T%d" % j, [128, L], F32) for j in range(4)] for _ in range(2)]
    gin = [[k.sb("gin%d" % j, [128, L], F32) for j in range(2)] for _ in range(2)]
    gg = [[[k.sb("g%d_%d" % (qq, j), [128, L], F32) for j in range(2)] for qq in range(4)] for _ in range(2)]
    T2 = [[k.sb("U%d" % j, [128, L], F32) for j in range(4)] for _ in range(2)]
    hh = [[k.sb("h%d" % j, [128, L], BF16) for j in range(2)] for _ in range(2)]
    ini = [[k.sb("ini%d_%d" % (qq, j), [128, 1], F32) for j in range(2)] for qq in range(4)]
    itmp = [k.sb("itmp%d" % qq, [128, 1], F32) for qq in range(4)]
    yo = [k.sb("yo", [128, L], F32) for _ in range(2)]
    nblk = ntok // L
    bps = seq // L
    it = 0
    for blk in range(nblk):
        sl = slice(blk * L, (blk + 1) * L)
        uB, uF, pY = ub[blk % 2], ubf[blk % 2], ps_y[blk % 2]
        k.dma("sp", uB, uB[:, :], u, u.t[:, sl])
        k.act(uF, uF[:, :], uB, uB[:, :], AF.Copy)
        first = (blk % bps == 0)
        for qq in range(4):
            par = it % 2
            it += 1
            pr_, pi2_ = ps_re[par], ps_im[par]
            rs = slice(32 * qq, 32 * qq + 32)
            k.mm(pr_, pr_[:, :], b_re, b_re[:, qq, :], uF, uF[:, :], True, True)
            k.mm(pi2_, pi2_[:, :], b_im, b_im[:, qq, :], uF, uF[:, :], True, True)
            t = T[par]
            cq, sq, rq = COS[:, qq, :], SIN[:, qq, :], RHO[:, qq, :]
            k.tt("dve", t[0], t[0][:, :], pr_, pr_[:, :], COS, cq, ALU.mult)
            k.tt("dve", t[1], t[1][:, :], pi2_, pi2_[:, :], SIN, sq, ALU.mult)
            k.tt("dve", t[2], t[2][:, :], pi2_, pi2_[:, :], COS, cq, ALU.mult)
            k.tt("dve", t[3], t[3][:, :], pr_, pr_[:, :], SIN, sq, ALU.mult)
            gi = gin[par]
            k.tt("pool", gi[0], gi[0][:, :], t[0], t[0][:, :], t[1], t[1][:, :], ALU.add)
            k.tt("pool", gi[1], gi[1][:, :], t[2], t[2][:, :], t[3], t[3][:, :], ALU.subtract)
            g_prev = gg[(blk + 1) % 2][qq]
            g_cur = gg[blk % 2][qq]
            if first:
                i_re, i_im = 0.0, 0.0
                xr = []
            else:
                k.ts("dve", itmp[qq], itmp[qq][:, :], g_prev[1], g_prev[1][:, L - 1:L], nELi[:, qq:qq + 1], ALU.mult,
                     extra_reads=[nELi])
                k.stt("dve", ini[qq][0], ini[qq][0][:, :], g_prev[0], g_prev[0][:, L - 1:L], ELr[:, qq:qq + 1],
                      itmp[qq], itmp[qq][:, :], ALU.mult, ALU.add, extra_reads=[ELr])
                k.ts("dve", itmp[qq], itmp[qq][:, :], g_prev[1], g_prev[1][:, L - 1:L], ELr[:, qq:qq + 1], ALU.mult,
                     extra_reads=[ELr])
                k.stt("dve", ini[qq][1], ini[qq][1][:, :], g_prev[0], g_prev[0][:, L - 1:L], ELi[:, qq:qq + 1],
                      itmp[qq], itmp[qq][:, :], ALU.mult, ALU.add, extra_reads=[ELi])
                i_re, i_im = ini[qq][0][:, :], ini[qq][1][:, :]
                xr = [ini[qq][0], ini[qq][1]]
            k.op("dve", lambda g, o=g_cur[0][:, :], a=rq, b=gi[0][:, :], i0=i_re: g.tensor_tensor_scan(
                out=o, data0=a, data1=b, initial=i0, op0=ALU.mult, op1=ALU.add), [RHO, gi[0]] + xr[:1], [g_cur[0]])
            k.op("dve", lambda g, o=g_cur[1][:, :], a=rq, b=gi[1][:, :], i0=i_im: g.tensor_tensor_scan(
                out=o, data0=a, data1=b, initial=i0, op0=ALU.mult, op1=ALU.add), [RHO, gi[1]] + xr[1:], [g_cur[1]])
            t2_ = T2[par]
            k.tt("pool", t2_[0], t2_[0][:, :], g_cur[0], g_cur[0][:, :], COS, cq, ALU.mult)
            k.tt("pool", t2_[1], t2_[1][:, :], g_cur[1], g_cur[1][:, :], SIN, sq, ALU.mult)
            k.tt("pool", t2_[2], t2_[2][:, :], g_cur[1], g_cur[1][:, :], COS, cq, ALU.mult)
            k.tt("pool", t2_[3], t2_[3][:, :], g_cur[0], g_cur[0][:, :], SIN, sq, ALU.mult)
            h = hh[par]
            k.tt("pool", h[0], h[0][:, :], t2_[0], t2_[0][:, :], t2_[1], t2_[1][:, :], ALU.subtract)
            k.tt("pool", h[1], h[1][:, :], t2_[2], t2_[2][:, :], t2_[3], t2_[3][:, :], ALU.add)
            k.mm(pY, pY[:, :], cp_re, cp_re[:, qq, :], h[0], h[0][:, :], qq == 0, False)
            k.mm(pY, pY[:, :], cp_imn, cp_imn[:, qq, :], h[1], h[1][:, :], False, qq == 3)
        yB = yo[blk % 2]
        k.stt("dve", yB, yB[:, :], uB, uB[:, :], s_d[:, 0:1], pY, pY[:, :], ALU.mult, ALU.add, extra_reads=[s_d])
        k.dma("sp", y, y.t[:, sl], yB, yB[:, :])
    return k.finish()


def s5_core_params(c, a_re, a_im, log_dt, b_re, b_im, c_re, c_im, d):
    g0 = 8 * c

    def pq(a):
        return np.ascontiguousarray(a[g0:g0 + 8].reshape(4, 2, 64).transpose(1, 2, 0).reshape(128, 4)).astype(np.float32)

    are = pq(a_re)
    aim = pq(a_im)
    ldt = pq(np.broadcast_to(log_dt[:, None], (64, 64)))
    B_re = np.zeros((4, 2, 16, 4, 2, 64), np.float32)
    B_im = np.zeros((4, 2, 16, 4, 2, 64), np.float32)
    C_re = np.zeros((2, 64, 4, 2, 16), np.float32)
    C_im = np.zeros((2, 64, 4, 2, 16), np.float32)
    for qq in range(4):
        for gp in range(2):
            g = g0 + 2 * qq + gp
            B_re[qq, gp, :, qq, gp, :] = b_re[g].T
            B_im[qq, gp, :, qq, gp, :] = b_im[g].T
            C_re[gp, :, qq, gp, :] = c_re[g].T
            C_im[gp, :, qq, gp, :] = c_im[g].T
    return {"are": are, "aim": aim, "ldt": ldt,
            "bre": B_re.reshape(128, 4, 128), "bim": B_im.reshape(128, 4, 128),
            "cre": C_re.reshape(128, 4, 32), "cim": C_im.reshape(128, 4, 32),
            "dsk": np.ascontiguousarray(d[128 * c:128 * c + 128].reshape(128, 1)).astype(np.float32)}


class Ctx:
    def __init__(self, k, NT, nkt_half):
        self.k = k
        self.NT = NT
        self.ps1 = [k.ps("ps1") for _ in range(2)]
        self.ps3 = [k.ps("ps3") for _ in range(2)]
        self.pso = [k.ps("pso") for _ in range(2)]
        self.pss = [k.ps("pss") for _ in range(2)]
        self.wa = [k.sb("wa", [128, 8, 512], BF16) for _ in range(2)]
        self.wb = [k.sb("wb", [128, 8, 512], BF16) for _ in range(2)]
        self.w2h = k.sb("w2h", [128, nkt_half, 1024], BF16)
        self.hmid = k.sb("hmid", [128, max(nkt_half, 8), NT], BF16)
        self.z = k.sb("z", [128, 8, NT], F32)
        self.xbf = k.sb("xbf", [128, 8, NT], BF16)
        self.sact = [k.sb("sact", [128, 512], F32) for _ in range(2)]
        self.tmp = [k.sb("tmp", [128, 512], F32) for _ in range(2)]
        self.ones = k.sb("ones", [128, 128], F32)
        k.op("pool", lambda g: g.memset(self.ones[:, :], 1.0), [], [self.ones])
        self.sq = [k.sb("sq", [128, 512], F32) for _ in range(2)]
        self.mean = k.sb("mean", [128, 512], F32)
        self.ex2 = k.sb("ex2", [128, 512], F32)
        self.rstd = k.sb("rstd", [128, 512], F32)
        self.nmr = k.sb("nmr", [128, 512], F32)
        self.lng = k.sb("lng", [128, 32], F32)
        self.lnb = k.sb("lnb", [128, 32], F32)
        self.wi = 0
        self.pi = 0
        self.oi = 0


def load_wblock(k, buf, wd, k0, nk, c0, cw):
    k.dma("pool", buf, buf[:, 0:nk, 0:cw], wd, wd.t[k0 * 128:(k0 + nk) * 128, c0:c0 + cw].rearrange("(kt p) m -> p kt m", p=128))


def glu_phase(k, cx, x_in, wa_d, a_c0, wb_d, b_c0, ncols, func, emit):
    NT = cx.NT
    for c0 in range(0, ncols, 512):
        cw = min(512, ncols - c0)
        wa, wb = cx.wa[cx.wi % 2], cx.wb[cx.wi % 2]
        cx.wi += 1
        load_wblock(k, wa, wa_d, 0, 8, a_c0 + c0, cw)
        load_wblock(k, wb, wb_d, 0, 8, b_c0 + c0, cw)
        for fl in range(cw // 128):
            ft = c0 // 128 + fl
            for tb in range(NT // 512):
                p1, p3 = cx.ps1[cx.pi % 2], cx.ps3[cx.pi % 2]
                sa = cx.sact[cx.pi % 2]
                cx.pi += 1
                ts_ = slice(tb * 512, tb * 512 + 512)
                for kt in range(8):
                    k.mm(p1, p1[:, :], wa, wa[:, kt, fl * 128:fl * 128 + 128], x_in, x_in[:, kt, ts_], kt == 0, kt == 7)
                for kt in range(8):
                    k.mm(p3, p3[:, :], wb, wb[:, kt, fl * 128:fl * 128 + 128], x_in, x_in[:, kt, ts_], kt == 0, kt == 7)
                k.act(sa, sa[:, :], p1, p1[:, :], func)
                emit(ft, tb, sa, p3)


def down_phase(k, cx, w2_d, k0, nk, zacc, first_copy=False):
    NT = cx.NT
    k.dma("pool", cx.w2h, cx.w2h[:, 0:nk, :], w2_d, w2_d.t[k0 * 128:(k0 + nk) * 128, :].rearrange("(kt p) m -> p kt m", p=128))
    for m in range(8):
        for tb in range(NT // 512):
            po = cx.pso[cx.oi % 2]
            cx.oi += 1
            ts_ = slice(tb * 512, tb * 512 + 512)
            for kt in range(nk):
                k.mm(po, po[:, :], cx.w2h, cx.w2h[:, kt, m * 128:m * 128 + 128], cx.hmid, cx.hmid[:, kt, ts_], kt == 0, kt == nk - 1)
            k.tt("dve", zacc, zacc[:, m, ts_], po, po[:, :], zacc, zacc[:, m, ts_], ALU.add)


def layer_norm(k, cx, z, ln_idx, xbf_out):
    NT = cx.NT
    for tb in range(NT // 512):
        ts_ = slice(tb * 512, tb * 512 + 512)
        s1, s2 = cx.pss[0], cx.pss[1]
        for kt in range(8):
            k.mm(s1, s1[:, :], cx.ones, cx.ones[:, :], z, z[:, kt, ts_], kt == 0, kt == 7)
        for kt in range(8):
            sq = cx.sq[kt % 2]
            k.act(sq, sq[:, :], z, z[:, kt, ts_], AF.Square)
            k.mm(s2, s2[:, :], cx.ones, cx.ones[:, :], sq, sq[:, :], kt == 0, kt == 7)
        k.act(cx.mean, cx.mean[:, :], s1, s1[:, :], AF.Copy, scale=1.0 / D)
        k.act(cx.ex2, cx.ex2[:, :], s2, s2[:, :], AF.Copy, scale=1.0 / D)
        k.tt("dve", cx.nmr, cx.nmr[:, :], cx.mean, cx.mean[:, :], cx.mean, cx.mean[:, :], ALU.mult)
        k.tt("dve", cx.ex2, cx.ex2[:, :], cx.ex2, cx.ex2[:, :], cx.nmr, cx.nmr[:, :], ALU.subtract)
        k.ts("dve", cx.ex2, cx.ex2[:, :], cx.ex2, cx.ex2[:, :], LN_EPS, ALU.add)
        k.act(cx.ex2, cx.ex2[:, :], cx.ex2, cx.ex2[:, :], AF.Sqrt)
        k.op("dve", lambda g: g.reciprocal(out=cx.rstd[:, :], in_=cx.ex2[:, :]), [cx.ex2], [cx.rstd])
        k.stt("dve", cx.nmr, cx.nmr[:, :], cx.mean, cx.mean[:, :], -1.0, cx.rstd, cx.rstd[:, :], ALU.mult, ALU.mult)
        for kt in range(8):
            t = cx.tmp[kt % 2]
            k.tt("dve", t, t[:, :], z, z[:, kt, ts_], cx.rstd, cx.rstd[:, :], ALU.mult)
            k.tt("dve", t, t[:, :], t, t[:, :], cx.nmr, cx.nmr[:, :], ALU.add)
            col = ln_idx * 8 + kt
            k.op("act", lambda g, o=z[:, kt, ts_], i=t[:, :], c=col: g.activation(
                out=o, in_=i, func=AF.Identity, scale=cx.lng[:, c:c + 1], bias=cx.lnb[:, c:c + 1]),
                [t, cx.lng, cx.lnb], [z])
            if xbf_out is not None:
                k.op("pool", lambda g, o=xbf_out[:, kt, ts_], i=z[:, kt, ts_]: g.tensor_copy(out=o, in_=i), [z], [xbf_out])


def ln_layout(ln_g, ln_b):
    g = np.ascontiguousarray(ln_g.reshape(4, 8, 128).transpose(2, 0, 1).reshape(128, 32)).astype(np.float32)
    b = np.ascontiguousarray(ln_b.reshape(4, 8, 128).transpose(2, 0, 1).reshape(128, 32)).astype(np.float32)
    return g, b


DFF = 2816


def build_layer0_rest(tpc=TPC, NT=1024):
    k = KB()
    yT = k.dram("yT", [D, tpc], F32, "ExternalInput")
    xT = k.dram("xT", [D, tpc], F32, "ExternalInput")
    wglu = k.dram("wglu", [D, 2 * D], F32, "ExternalInput")
    w1 = k.dram("w1", [D, DFF], F32, "ExternalInput")
    w3 = k.dram("w3", [D, DFF], F32, "ExternalInput")
    w2 = k.dram("w2", [DFF, D], F32, "ExternalInput")
    lng = k.dram("lng", [128, 32], F32, "ExternalInput")
    lnb = k.dram("lnb", [128, 32], F32, "ExternalInput")
    oT = k.dram("oT", [D, tpc], F32, "ExternalOutput")
    cx = Ctx(k, NT, 11)
    k.dma("sp", cx.lng, cx.lng[:, :], lng, lng.t[:, :])
    k.dma("sp", cx.lnb, cx.lnb[:, :], lnb, lnb.t[:, :])
    ystage = [k.sb("ystage", [128, NT], F32) for _ in range(2)]
    gbf = cx.hmid
    z = cx.z
    for sb_ in range(tpc // NT):
        tsl = slice(sb_ * NT, (sb_ + 1) * NT)
        for kt in range(8):
            ys = ystage[kt % 2]
            k.dma("sp", ys, ys[:, :], yT, yT.t[kt * 128:(kt + 1) * 128, tsl])
            k.act(gbf, gbf[:, kt, :], ys, ys[:, :], AF.Gelu)
            k.dma("sp", z, z[:, kt, :], xT, xT.t[kt * 128:(kt + 1) * 128, tsl])

        def emit_glu(ft, tb, sa, pB):
            t = cx.tmp[(ft + tb) % 2]
            ts_ = slice(tb * 512, tb * 512 + 512)
            k.tt("dve", t, t[:, :], pB, pB[:, :], sa, sa[:, :], ALU.mult)
            k.stt("dve", z, z[:, ft, ts_], z, z[:, ft, ts_], float(ALPHA), t, t[:, :], ALU.mult, ALU.add)

        glu_phase(k, cx, gbf, wglu, D, wglu, 0, D, AF.Sigmoid, emit_glu)
        layer_norm(k, cx, z, 0, cx.xbf)
        for kt in range(8):
            k.ts("dve", z, z[:, kt, :], z, z[:, kt, :], float(ALPHA), ALU.mult)
        for half in range(2):
            f0 = half * 11

            def emit_ffn(ft, tb, sa, pB):
                ts_ = slice(tb * 512, tb * 512 + 512)
                k.tt("dve", cx.hmid, cx.hmid[:, ft, ts_], pB, pB[:, :], sa, sa[:, :], ALU.mult)

            glu_phase(k, cx, cx.xbf, w1, f0 * 128, w3, f0 * 128, 11 * 128, AF.Silu, emit_ffn)
            down_phase(k, cx, w2, f0, 11, z)
        layer_norm(k, cx, z, 1, None)
        for kt in range(8):
            k.dma("sp", oT, oT.t[kt * 128:(kt + 1) * 128, tsl], z, z[:, kt, :])
    return k.finish()


NQKV = 9216
DILS = (1, 4, 16)


def build_qkv(tpc=TPC):
    k = KB()
    xT = k.dram("xT", [D, tpc], F32, "ExternalInput")
    w = k.dram("w", [D, NQKV], F32, "ExternalInput")
    o = k.dram("qkvT", [NQKV, tpc], BF16, "ExternalOutput")
    xb = k.sb("xb", [128, 8, tpc], BF16)
    for kt in range(8):
        k.dma("pool", xb, xb[:, kt, :], xT, xT.t[kt * 128:(kt + 1) * 128, :])
    wbuf = [k.sb("wq", [128, 8, 512], BF16) for _ in range(2)]
    pbs = [k.ps("pb") for _ in range(4)]
    sts = [k.sb("st", [128, 512], BF16) for _ in range(4)]
    i = 0
    for cb in range(NQKV // 512):
        wb = wbuf[cb % 2]
        load_wblock(k, wb, w, 0, 8, cb * 512, 512)
        for fl in range(4):
            for tb in range(tpc // 512):
                pb, st = pbs[i % 4], sts[i % 4]
                ts_ = slice(tb * 512, tb * 512 + 512)
                for kt in range(8):
                    k.mm(pb, pb[:, :], wb, wb[:, kt, fl * 128:fl * 128 + 128], xb, xb[:, kt, ts_], kt == 0, kt == 7)
                if i % 2 == 0:
                    k.act(st, st[:, :], pb, pb[:, :], AF.Copy)
                else:
                    k.op("dve", lambda g, o_=st[:, :], a=pb[:, :]: g.tensor_copy(out=o_, in_=a), [pb], [st])
                r0 = cb * 512 + fl * 128
                k.dma("sp", o, o.t[r0:r0 + 128, ts_], st, st[:, :])
                i += 1
    return k.finish()


ACH = 2048
NEG = -1e30


def build_attn(tpc=TPC):
    k = KB()
    nblk = tpc // 128
    qb_d = k.dram("qb", [3, 8, 128, nblk * 128], BF16, "ExternalInput")
    kb_d = k.dram("kb", [3, 8, 128, (nblk + 16) * 128], BF16, "ExternalInput")
    vb_d = k.dram("vb", [3, 8, 128, (nblk + 16) * 128], BF16, "ExternalInput")
    bias_d = k.dram("biasT", [3, 8, 128, 512], F32, "ExternalInput")
    negb_d = k.dram("negb", [128, 1], F32, "ExternalInput")
    o = k.dram("attnT", [D, tpc], F32, "ExternalOutput")
    negb = k.sb("negb", [128, 1], F32)
    k.dma("sp", negb, negb[:, :], negb_d, negb_d.t[:, :])
    ones = k.sb("ones", [128, 64], BF16)
    k.op("pool", lambda g: g.memset(ones[:, :], 1.0), [], [ones])
    Qb = [k.sb("Qb", [128, 16 * 128], BF16) for _ in range(2)]
    Kb = [k.sb("Kb", [128, 32 * 128], BF16) for _ in range(2)]
    Vb = [k.sb("Vb", [128, 32, 128], BF16) for _ in range(2)]
    Bs = [k.sb("Bs", [128, 2, 256], F32) for _ in range(2)]
    num = [k.sb("num", [128, ACH], F32) for _ in range(2)]
    den = [k.sb("den", [128, ACH], F32) for _ in range(2)]
    rec = k.sb("rec", [128, ACH], F32)
    ps_s = [k.ps("ps_s") for _ in range(2)]
    ps_o = [k.ps("ps_o") for _ in range(2)]
    ps_d = [k.ps("ps_d") for _ in range(2)]
    tmp = [k.sb("tmpS", [128, 256], F32) for _ in range(3)]
    pT = [k.sb("pT", [128, 256], BF16) for _ in range(3)]
    li = 0
    ui = 0
    ei = 0
    ci = 0
    for ch in range(tpc // ACH):
        for ft in range(8):
            nm, dn = num[ci % 2], den[ci % 2]
            ci += 1
            for g in range(3):
                d = DILS[g]
                qb, kb, vb, bs = Qb[li % 2], Kb[li % 2], Vb[li % 2], Bs[li % 2]
                li += 1
                b0 = ch * 16
                k.dma("sp", qb, qb[:, :], qb_d, qb_d.t[g, ft, :, b0 * 128:(b0 + 16) * 128])
                nkb = 16 + d
                k0 = 16 + b0 - d
                k.dma("sp", kb, kb[:, 0:nkb * 128], kb_d, kb_d.t[g, ft, :, k0 * 128:(k0 + nkb) * 128])
                k.dma("sp", vb, vb[:, 0:nkb, :], vb_d,
                      vb_d.t[g, ft, :, k0 * 128:(k0 + nkb) * 128].rearrange("p (b f) -> p b f", f=128))
                k.dma("sp", bs, bs[:, :, :], bias_d, bias_d.t[g, ft, :, :].rearrange("p (h f) -> p h f", h=2))
                for B in range(16):
                    po, pd = ps_o[ei % 2], ps_d[ei % 2]
                    halo = (ch == 0 and B < d)
                    for hh in range(2):
                        rs = slice(64 * hh, 64 * hh + 64)
                        pss = ps_s[ui % 2]
                        tm, pt = tmp[ui % 3], pT[ui % 3]
                        ui += 1
                        qs = slice(B * 128, B * 128 + 128)
                        kp = slice(B * 128, B * 128 + 128)
                        kc = slice((B + d) * 128, (B + d) * 128 + 128)
                        k.mm(pss, pss[:, 0:128], kb, kb[rs, kp], qb, qb[rs, qs], True, True)
                        k.mm(pss, pss[:, 128:256], kb, kb[rs, kc], qb, qb[rs, qs], True, True)
                        k.stt("dve", tm, tm[:, :], pss, pss[:, 0:256], 0.125, bs, bs[:, hh, :], ALU.mult, ALU.add)
                        if halo:
                            k.op("act", lambda e, o_=pt[:, 0:128], i_=tm[:, 0:128]: e.activation(
                                out=o_, in_=i_, func=AF.Exp, bias=negb[:, 0:1], scale=1.0), [tm, negb], [pt])
                            k.act(pt, pt[:, 128:256], tm, tm[:, 128:256], AF.Exp)
                        else:
                            k.act(pt, pt[:, :], tm, tm[:, :], AF.Exp)
                        cs = slice((B % 4) * 128, (B % 4) * 128 + 128)
                        k.mm(po, po[rs, cs], vb, vb[:, B, rs], pt, pt[:, 0:128], True, False)
                        k.mm(po, po[rs, cs], vb, vb[:, B + d, rs], pt, pt[:, 128:256], False, True)
                        k.mm(pd, pd[rs, cs], ones, ones[:, :], pt, pt[:, 0:128], True, False)
                        k.mm(pd, pd[rs, cs], ones, ones[:, :], pt, pt[:, 128:256], False, True)
                    if B % 4 == 3:
                        ei += 1
                        B0 = B - 3
                        if d == 1:
                            ov = lambda t: t[:, B0 * 128:(B0 + 4) * 128]
                            iv = lambda p: p[:, :]
                        else:
                            n_, r0 = B0 // d, B0 % d
                            base = n_ * 128 * d
                            ov = lambda t, base=base, d=d, r0=r0: t[:, base:base + 128 * d].rearrange(
                                "p (i r) -> p i r", r=d)[:, :, r0:r0 + 4]
                            iv = lambda p: p[:, :].rearrange("p (j i) -> p i j", j=4)
                        if g == 0:
                            k.op("dve", lambda e, o_=ov(nm), i_=iv(po): e.tensor_copy(out=o_, in_=i_), [po], [nm])
                            k.act(dn, ov(dn), pd, iv(pd), AF.Copy)
                        else:
                            k.tt("dve", nm, ov(nm), po, iv(po), nm, ov(nm), ALU.add)
                            k.tt("dve", dn, ov(dn), pd, iv(pd), dn, ov(dn), ALU.add)
            k.op("dve", lambda e, o_=rec[:, :], i_=dn[:, :]: e.reciprocal(out=o_, in_=i_), [dn], [rec])
            k.tt("pool", nm, nm[:, :], nm, nm[:, :], rec, rec[:, :], ALU.mult)
            k.dma("sp", o, o.t[ft * 128:(ft + 1) * 128, ch * ACH:(ch + 1) * ACH], nm, nm[:, :])
    return k.finish()


def t5_bucket(dist):
    n_buckets, max_distance = 32, 2048
    max_exact = n_buckets // 2
    d = np.maximum(dist, 0)
    large = max_exact + (np.log(np.maximum(d, 1) / max_exact) / math.log(max_distance / max_exact)
                         * (n_buckets - max_exact)).astype(np.int64)
    large = np.minimum(large, n_buckets - 1)
    return np.where(d < max_exact, d, large).astype(np.int32)


def attn_bias_table(rel_bias):
    L = 128
    qi = np.arange(L)[:, None]
    ki = np.arange(2 * L)[None, :]
    off = qi + L - ki
    band = (off >= 0) & (off <= L)
    out = np.zeros((3, 8, 128, 2, 2, 128), np.float32)
    for g, d in enumerate(DILS):
        bucket = t5_bucket(np.clip(off, 0, None) * d)
        bias = rel_bias[bucket]
        bias = np.where(band[:, :, None], bias, np.float32(NEG))
        b = bias.reshape(L, 2, L, 8, 2)
        out[g] = b.transpose(3, 2, 4, 1, 0)
    return out.reshape(3, 8, 128, 512)


def attn_host_layout(qkvT, rel_bias, seqlen, tpc):
    ntok = qkvT.shape[1]
    ncore = ntok // tpc
    nblk = tpc // 128
    bias = attn_bias_table(rel_bias)
    perm = {}
    for g, d in enumerate(DILS):
        for j in range(3):
            a = qkvT[(g * 3 + j) * 1024:(g * 3 + j + 1) * 1024]
            a = a.reshape(8, 128, ntok // (128 * d), 128, d).transpose(0, 1, 2, 4, 3)
            perm[g, j] = a.reshape(8, 128, ntok // 128, 128)
    maps = []
    for c in range(ncore):
        first = (c * tpc) % seqlen == 0
        B0 = c * nblk
        qb = np.stack([perm[g, 0][:, :, B0:B0 + nblk].reshape(8, 128, nblk * 128) for g in range(3)])
        kbs, vbs = [], []
        for g in range(3):
            kk = np.zeros((8, 128, nblk + 16, 128), qkvT.dtype)
            vv = np.zeros((8, 128, nblk + 16, 128), qkvT.dtype)
            lo = 0 if first else 16
            kk[:, :, 16 - lo:] = perm[g, 1][:, :, B0 - lo:B0 + nblk]
            vv[:, :, 16 - lo:] = perm[g, 2][:, :, B0 - lo:B0 + nblk]
            kbs.append(kk.reshape(8, 128, -1))
            vbs.append(vv.transpose(0, 3, 2, 1).reshape(8, 128, -1))
        maps.append({"qb": np.ascontiguousarray(qb), "kb": np.ascontiguousarray(np.stack(kbs)),
                     "vb": np.ascontiguousarray(np.stack(vbs)), "biasT": bias,
                     "negb": np.full((128, 1), NEG if first else 0.0, np.float32)})
    return maps


NEXP = 8
DFE = 3584


def build_layer1_rest(tpc=TPC, NT=1024):
    k = KB()
    aT = k.dram("aT", [D, tpc], F32, "ExternalInput")
    xT = k.dram("xT", [D, tpc], F32, "ExternalInput")
    wo = k.dram("wo", [D, D], F32, "ExternalInput")
    wr = k.dram("wr", [128, 8, NEXP], F32, "ExternalInput")
    w1 = k.dram("w1", [NEXP, D, DFE], F32, "ExternalInput")
    w3 = k.dram("w3", [NEXP, D, DFE], F32, "ExternalInput")
    w2 = k.dram("w2", [NEXP, DFE, D], F32, "ExternalInput")
    lng = k.dram("lng", [128, 32], F32, "ExternalInput")
    lnb = k.dram("lnb", [128, 32], F32, "ExternalInput")
    ident_d = k.dram("ident", [128, 128], F32, "ExternalInput")
    oT = k.dram("oT", [D, tpc], F32, "ExternalOutput")
    cx = Ctx(k, NT, 14)
    k.dma("sp", cx.lng, cx.lng[:, :], lng, lng.t[:, :])
    k.dma("sp", cx.lnb, cx.lnb[:, :], lnb, lnb.t[:, :])
    ident = k.sb("ident", [128, 128], F32)
    k.dma("sp", ident, ident[:, :], ident_d, ident_d.t[:, :])
    wr_sb = k.sb("wr", [128, 8, NEXP], F32)
    k.dma("sp", wr_sb, wr_sb[:, :, :], wr, wr.t[:, :, :])
    ones_bf = k.sb("ones_bf", [128, 128], BF16)
    k.op("pool", lambda g: g.memset(ones_bf[:, :], 1.0), [], [ones_bf])
    gate_bc = k.sb("gate_bc", [128, NEXP, NT], BF16)
    ntt = NT // 128
    lg = k.sb("lg", [128, ntt, 8], F32)
    top = k.sb("top", [128, ntt, 8], F32)
    gsm = [k.sb("gsm%d" % i, [128, ntt], F32) for i in range(4)]
    gate = k.sb("gate", [128, ntt, 8], F32)
    gt2 = k.sb("gt2", [128, ntt, 8], F32)
    diag = [k.sb("diag", [128, 128], BF16) for _ in range(2)]
    z = cx.z
    abf = cx.hmid
    for sb_ in range(tpc // NT):
        tsl = slice(sb_ * NT, (sb_ + 1) * NT)
        for kt in range(8):
            k.dma("pool", abf, abf[:, kt, :], aT, aT.t[kt * 128:(kt + 1) * 128, tsl])
            k.dma("sp", z, z[:, kt, :], xT, xT.t[kt * 128:(kt + 1) * 128, tsl])
        for cb in range(2):
            wa = cx.wa[cx.wi % 2]
            cx.wi += 1
            load_wblock(k, wa, wo, 0, 8, cb * 512, 512)
            for fl in range(4):
                m = cb * 4 + fl
                for tb in range(NT // 512):
                    po = cx.pso[cx.oi % 2]
                    cx.oi += 1
                    ts_ = slice(tb * 512, tb * 512 + 512)
                    for kt in range(8):
                        k.mm(po, po[:, :], wa, wa[:, kt, fl * 128:fl * 128 + 128], abf, abf[:, kt, ts_], kt == 0, kt == 7)
                    k.stt("dve", z, z[:, m, ts_], z, z[:, m, ts_], float(ALPHA), po, po[:, :], ALU.mult, ALU.add)
        layer_norm(k, cx, z, 2, cx.xbf)
        pl = cx.pss[0]
        for tt in range(ntt):
            for kt in range(8):
                k.mm(pl, pl[:, tt * 8:tt * 8 + 8], z, z[:, kt, tt * 128:(tt + 1) * 128], wr_sb, wr_sb[:, kt, :], kt == 0, kt == 7)
        k.act(lg, lg[:, :, :], pl, pl[:, 0:ntt * 8].rearrange("p (t e) -> p t e", e=8), AF.Copy)
        nv1, e2, g1, g2 = gsm
        for tt in range(ntt):
            k.op("dve", lambda g, o=top[:, tt, :], i=lg[:, tt, :]: g.max(out=o, in_=i), [lg], [top])
        k.ts("dve", nv1, nv1[:, :], top, top[:, :, 0], -1.0, ALU.mult)
        k.tt("dve", e2, e2[:, :], top, top[:, :, 1], nv1, nv1[:, :], ALU.add)
        k.act(e2, e2[:, :], e2, e2[:, :], AF.Exp)
        k.ts("dve", g1, g1[:, :], e2, e2[:, :], 1.0, ALU.add)
        k.op("dve", lambda g: g.reciprocal(out=g1[:, :], in_=g1[:, :]), [g1], [g1])
        k.tt("dve", g2, g2[:, :], e2, e2[:, :], g1, g1[:, :], ALU.mult)
        for tt in range(ntt):
            k.ts("dve", gate, gate[:, tt, :], lg, lg[:, tt, :], top[:, tt, 0:1], ALU.is_equal, g1[:, tt:tt + 1], ALU.mult,
                 extra_reads=[top, g1])
            k.ts("dve", gt2, gt2[:, tt, :], lg, lg[:, tt, :], top[:, tt, 1:2], ALU.is_equal, g2[:, tt:tt + 1], ALU.mult,
                 extra_reads=[top, g2])
        k.tt("dve", gate, gate[:, :, :], gate, gate[:, :, :], gt2, gt2[:, :, :], ALU.add)
        di = 0
        for e in range(NEXP):
            for hb in range(NT // 512):
                pg = cx.pss[1]
                for t4 in range(4):
                    tt = hb * 4 + t4
                    dg = diag[di % 2]
                    di += 1
                    k.ts("dve", dg, dg[:, :], ident, ident[:, :], gate[:, tt, e:e + 1], ALU.mult, extra_reads=[gate])
                    k.mm(pg, pg[:, t4 * 128:(t4 + 1) * 128], ones_bf, ones_bf[:, :], dg, dg[:, :], True, True)
                k.act(gate_bc, gate_bc[:, e, hb * 512:(hb + 1) * 512], pg, pg[:, :], AF.Copy)
        for kt in range(8):
            k.ts("dve", z, z[:, kt, :], z, z[:, kt, :], float(ALPHA), ALU.mult)
        for e in range(NEXP):
            w1e, w3e, w2e = Buf("w1e", w1.t[e]), Buf("w3e", w3.t[e]), Buf("w2e", w2.t[e])
            for half in range(2):
                f0 = half * 14

                def emit_moe(ft, tb, sa, pB, e=e):
                    ts_ = slice(tb * 512, tb * 512 + 512)
                    t = cx.tmp[(ft + tb) % 2]
                    k.tt("dve", t, t[:, :], pB, pB[:, :], sa, sa[:, :], ALU.mult)
                    k.tt("pool", cx.hmid, cx.hmid[:, ft, ts_], t, t[:, :], gate_bc, gate_bc[:, e, ts_], ALU.mult)

                glu_phase(k, cx, cx.xbf, w1e, f0 * 128, w3e, f0 * 128, 14 * 128, AF.Silu, emit_moe)
                down_phase(k, cx, w2e, f0, 14, z)
        layer_norm(k, cx, z, 3, None)
        for kt in range(8):
            k.dma("sp", oT, oT.t[kt * 128:(kt + 1) * 128, tsl], z, z[:, kt, :])
    return k.finish()


_PROGS = {}


def _prog(name, fn):
    if name not in _PROGS:
        _PROGS[name] = fn()
    return _PROGS[name]


def _run(nc, maps):
    res = run_bass_kernel_spmd(nc, maps, core_ids=list(range(NCORES)))
    return res.results


def kernel(x, s5_w_in, s5_a_re, s5_a_im, s5_log_dt, s5_b_re, s5_b_im, s5_c_re, s5_c_im, s5_d,
           s5_w_glu, attn_w_qkv, attn_w_o, rel_bias, ffn_w1, ffn_w3, ffn_w2, moe_w_router,
           moe_w1, moe_w3, moe_w2, ln_g, ln_b):
    f32 = np.float32
    x = np.asarray(x, f32)
    xT = np.ascontiguousarray(x.reshape(NTOK, D).T)
    cols = [slice(c * TPC, (c + 1) * TPC) for c in range(NCORES)]
    xTs = [np.ascontiguousarray(xT[:, s]) for s in cols]
    lg, lb = ln_layout(np.asarray(ln_g, f32), np.asarray(ln_b, f32))

    w_in = np.ascontiguousarray(np.asarray(s5_w_in, f32)[0])
    r = _run(_prog("A", build_uproj), [{"xT": xTs[c], "w": w_in} for c in range(NCORES)])
    u_full = np.concatenate([r[c]["uT"] for c in range(NCORES)], axis=1)
    maps = []
    for c in range(NCORES):
        m = s5_core_params(c, np.asarray(s5_a_re, f32)[0], np.asarray(s5_a_im, f32)[0], np.asarray(s5_log_dt, f32)[0],
                           np.asarray(s5_b_re, f32)[0], np.asarray(s5_b_im, f32)[0], np.asarray(s5_c_re, f32)[0],
                           np.asarray(s5_c_im, f32)[0], np.asarray(s5_d, f32)[0])
        m["u"] = np.ascontiguousarray(u_full[128 * c:128 * c + 128])
        maps.append(m)
    r = _run(_prog("B", build_s5), maps)
    y_full = np.concatenate([r[c]["y"] for c in range(NCORES)], axis=0)
    wglu = np.ascontiguousarray(np.asarray(s5_w_glu, f32)[0])
    w1 = np.ascontiguousarray(np.asarray(ffn_w1, f32)[0])
    w3 = np.ascontiguousarray(np.asarray(ffn_w3, f32)[0])
    w2 = np.ascontiguousarray(np.asarray(ffn_w2, f32)[0])
    r = _run(_prog("C", build_layer0_rest),
             [{"yT": np.ascontiguousarray(y_full[:, cols[c]]), "xT": xTs[c], "wglu": wglu, "w1": w1, "w3": w3, "w2": w2,
               "lng": lg, "lnb": lb} for c in range(NCORES)])
    x2s = [r[c]["oT"] for c in range(NCORES)]
    wqkv = np.ascontiguousarray(np.asarray(attn_w_qkv, f32)[0])
    r = _run(_prog("D", build_qkv), [{"xT": x2s[c], "w": wqkv} for c in range(NCORES)])
    qkvT = np.concatenate([r[c]["qkvT"] for c in range(NCORES)], axis=1)
    maps = attn_host_layout(qkvT, np.asarray(rel_bias, f32), SEQ, TPC)
    r = _run(_prog("E", build_attn), maps)
    aTs = [r[c]["attnT"] for c in range(NCORES)]
    wo = np.ascontiguousarray(np.asarray(attn_w_o, f32)[0])
    wr = np.ascontiguousarray(np.asarray(moe_w_router, f32)[0].reshape(8, 128, NEXP).transpose(1, 0, 2))
    mw1 = np.ascontiguousarray(np.asarray(moe_w1, f32)[0])
    mw3 = np.ascontiguousarray(np.asarray(moe_w3, f32)[0])
    mw2 = np.ascontiguousarray(np.asarray(moe_w2, f32)[0])
    ident = np.eye(128, dtype=f32)
    r = _run(_prog("F", build_layer1_rest),
             [{"aT": aTs[c], "xT": x2s[c], "wo": wo, "wr": wr, "w1": mw1, "w3": mw3, "w2": mw2, "lng": lg, "lnb": lb,
               "ident": ident} for c in range(NCORES)])
    oT = np.concatenate([r[c]["oT"] for c in range(NCORES)], axis=1)
    return np.ascontiguousarray(oT.T).reshape(BATCH, SEQ, D).astype(f32)
```

```python
import math
import numpy as np
import concourse.bass as bass
import concourse.mybir as mybir
from concourse.bass_utils import run_bass_kernel_spmd
from contextlib import ExitStack

F32 = mybir.dt.float32
BF16 = mybir.dt.bfloat16
I32 = mybir.dt.int32
ALU = mybir.AluOpType
AF = mybir.ActivationFunctionType
AX = mybir.AxisListType

NCORES = 8
D = 1024
SEQ = 16384
BATCH = 2
NTOK = BATCH * SEQ
TPC = NTOK // NCORES
DEPTH = 2
ALPHA = (2 * DEPTH) ** 0.25
LN_EPS = 1e-5
PI = math.pi


class Buf:
    __slots__ = ("name", "t", "lw", "rd", "dsem", "dcnt", "nodep")

    def __init__(self, name, t):
        self.name = name
        self.t = t
        self.lw = None
        self.rd = {}
        self.dsem = None
        self.dcnt = 0
        self.nodep = False

    def __getitem__(self, idx):
        return self.t[idx]


SES = False


class KB:
    def __init__(self, same_engine_sync=None):
        if same_engine_sync is None:
            same_engine_sync = SES
        self.nc = bass.Bass("TRN2", target_bir_lowering=False)
        self.es = ExitStack()
        nc = self.nc
        self.eng = {"pe": nc.tensor, "dve": nc.vector, "act": nc.scalar, "pool": nc.gpsimd, "sp": nc.sync}
        self.sem = {e: self.es.enter_context(nc.semaphore("s_" + e)) for e in self.eng}
        self.cnt = {e: 0 for e in self.eng}
        self.known = {e: {} for e in self.eng}
        self.ses = same_engine_sync
        self.nsem = 0
        self.prog = {e: [] for e in self.eng}
        self.outs = []
        self.uid = 0

    def sb(self, name, shape, dt):
        self.uid += 1
        return Buf(name, self.es.enter_context(self.nc.sbuf_tensor("%s_%d" % (name, self.uid), list(shape), dt)))

    def ps(self, name, shape=(128, 512), dt=F32):
        self.uid += 1
        return Buf(name, self.es.enter_context(self.nc.psum_tensor("%s_%d" % (name, self.uid), list(shape), dt)))

    def dram(self, name, shape, dt, kind):
        b = Buf(name, self.nc.dram_tensor(name, list(shape), dt, kind=kind).ap())
        if kind == "ExternalOutput":
            self.outs.append(b)
            b.nodep = True
        return b

    def _deps(self, e, reads, writes):
        need = {}

        def add(tag):
            if tag is None:
                return
            s, v = tag
            if need.get(s, 0) < v:
                need[s] = v

        for b in reads:
            add(b.lw)
        for b in writes:
            if b.nodep:
                continue
            add(b.lw)
            for s, v in b.rd.items():
                add((s, v))
        k = self.known[e]
        own = self.sem[e]
        for s, v in need.items():
            if s == own and (e == "pe" or not self.ses):
                continue
            if k.get(s, 0) >= v:
                continue
            self.prog[e].append(("w", s, v))
            k[s] = v

    def op(self, e, fn, reads=(), writes=()):
        self._deps(e, reads, writes)
        self.cnt[e] += 1
        self.prog[e].append(("i", fn, self.sem[e], 1))
        tag = (self.sem[e], self.cnt[e])
        for b in writes:
            b.lw = tag
            b.rd = {}
        for b in reads:
            if b not in writes:
                b.rd[tag[0]] = tag[1]

    def dma(self, q, out_buf, out_ap, in_buf, in_ap, **kw):
        self._deps(q, [in_buf], [out_buf])
        if out_buf.dsem is None:
            self.nsem += 1
            out_buf.dsem = self.es.enter_context(self.nc.semaphore("d%d" % self.nsem))
        self.prog[q].append(("i", (lambda e, o=out_ap, i=in_ap, kw=kw: e.dma_start(out=o, in_=i, **kw)),
                             out_buf.dsem, 16))
        out_buf.dcnt += 16
        tag = (out_buf.dsem, out_buf.dcnt)
        out_buf.lw = tag
        out_buf.rd = {}
        in_buf.rd[tag[0]] = tag[1]

    def finish(self):
        for b in self.outs:
            if b.lw is not None:
                self.prog["sp"].append(("w", b.lw[0], b.lw[1]))
        for e in ("pe", "dve", "act", "pool"):
            if self.cnt[e]:
                self.prog["sp"].append(("w", self.sem[e], self.cnt[e]))
        prog = self.prog

        def replay(e):
            def body(eng):
                for it in prog[e]:
                    if it[0] == "w":
                        eng.wait_ge(it[1], it[2])
                    else:
                        it[1](eng).then_inc(it[2], it[3])
            return body

        with self.nc.Block() as block:
            block.sync(replay("sp"))
            block.tensor(replay("pe"))
            block.vector(replay("dve"))
            block.scalar(replay("act"))
            block.gpsimd(replay("pool"))
        self.es.close()
        return self.nc

    def tt(self, e, out_b, out_ap, a_b, a_ap, b_b, b_ap, op):
        self.op(e, lambda g: g.tensor_tensor(out=out_ap, in0=a_ap, in1=b_ap, op=op), [a_b, b_b], [out_b])

    def ts(self, e, out_b, out_ap, a_b, a_ap, s1, op0, s2=None, op1=None, extra_reads=()):
        if op1 is None:
            self.op(e, lambda g: g.tensor_scalar(out=out_ap, in0=a_ap, scalar1=s1, scalar2=None, op0=op0),
                    [a_b] + list(extra_reads), [out_b])
        else:
            self.op(e, lambda g: g.tensor_scalar(out=out_ap, in0=a_ap, scalar1=s1, scalar2=s2, op0=op0, op1=op1),
                    [a_b] + list(extra_reads), [out_b])

    def stt(self, e, out_b, out_ap, a_b, a_ap, scalar, b_b, b_ap, op0, op1, extra_reads=()):
        self.op(e, lambda g: g.scalar_tensor_tensor(out=out_ap, in0=a_ap, scalar=scalar, in1=b_ap, op0=op0, op1=op1),
                [a_b, b_b] + list(extra_reads), [out_b])

    def act(self, out_b, out_ap, in_b, in_ap, func, scale=1.0, bias=0.0, extra_reads=()):
        self.op("act", lambda g: g.activation(out=out_ap, in_=in_ap, func=func, scale=scale, bias=bias),
                [in_b] + list(extra_reads), [out_b])

    def mm(self, out_b, out_ap, l_b, l_ap, r_b, r_ap, start, stop):
        self.op("pe", lambda g: g.matmul(out_ap, l_ap, r_ap, start=start, stop=stop), [l_b, r_b], [out_b])


def build_uproj():
    k = KB()
    xT = k.dram("xT", [D, TPC], F32, "ExternalInput")
    w = k.dram("w", [D, D], F32, "ExternalInput")
    uT = k.dram("uT", [D, TPC], F32, "ExternalOutput")
    wsb = k.sb("w", [128, 8, D], BF16)
    k.dma("pool", wsb, wsb[:, :, :], w, w.t.rearrange("(kt p) m -> p kt m", p=128))
    xs = [k.sb("xs", [128, 8, 512], BF16) for _ in range(2)]
    pbs = [k.ps("pb") for _ in range(4)]
    sts = [k.sb("st", [128, 512], F32) for _ in range(4)]
    i = 0
    for nb in range(TPC // 512):
        xb = xs[nb % 2]
        k.dma("pool", xb, xb[:, :, :], xT, xT.t[:, nb * 512:(nb + 1) * 512].rearrange("(kt p) n -> p kt n", p=128))
        for m in range(8):
            pb = pbs[i % 4]
            st = sts[i % 4]
            for kt in range(8):
                k.mm(pb, pb[:, :], wsb, wsb[:, kt, m * 128:(m + 1) * 128], xb, xb[:, kt, :], kt == 0, kt == 7)
            if i % 2 == 0:
                k.act(st, st[:, :], pb, pb[:, :], AF.Copy)
            else:
                k.op("dve", lambda g, o=st[:, :], a=pb[:, :]: g.tensor_copy(out=o, in_=a), [pb], [st])
            k.dma("sp", uT, uT.t[m * 128:(m + 1) * 128, nb * 512:(nb + 1) * 512], st, st[:, :])
            i += 1
    return k.finish()


S5_BLK = 512


def build_s5(ntok=NTOK, seq=SEQ):
    k = KB()
    u = k.dram("u", [128, ntok], F32, "ExternalInput")
    are = k.dram("are", [128, 4], F32, "ExternalInput")
    aim = k.dram("aim", [128, 4], F32, "ExternalInput")
    ldt = k.dram("ldt", [128, 4], F32, "ExternalInput")
    bre = k.dram("bre", [128, 4, 128], F32, "ExternalInput")
    bim = k.dram("bim", [128, 4, 128], F32, "ExternalInput")
    cre = k.dram("cre", [128, 4, 32], F32, "ExternalInput")
    cim = k.dram("cim", [128, 4, 32], F32, "ExternalInput")
    dsk = k.dram("dsk", [128, 1], F32, "ExternalInput")
    y = k.dram("y", [128, ntok], F32, "ExternalOutput")
    L = S5_BLK

    def small(name, w=4, dt=F32):
        return k.sb(name, [128, w], dt)

    s_are, s_aim, s_ldt = small("are"), small("aim"), small("ldt")
    k.dma("sp", s_are, s_are[:, :], are, are.t[:, :])
    k.dma("sp", s_aim, s_aim[:, :], aim, aim.t[:, :])
    k.dma("sp", s_ldt, s_ldt[:, :], ldt, ldt.t[:, :])
    s_d = small("dsk", 1)
    k.dma("sp", s_d, s_d[:, :], dsk, dsk.t[:, :])
    b_re = k.sb("bre", [128, 4, 128], BF16)
    b_im = k.sb("bim", [128, 4, 128], BF16)
    k.dma("pool", b_re, b_re[:, :, :], bre, bre.t[:, :, :])
    k.dma("pool", b_im, b_im[:, :, :], bim, bim.t[:, :, :])
    c_re = k.sb("cre", [128, 4, 32], F32)
    c_im = k.sb("cim", [128, 4, 32], F32)
    k.dma("sp", c_re, c_re[:, :, :], cre, cre.t[:, :, :])
    k.dma("sp", c_im, c_im[:, :, :], cim, cim.t[:, :, :])

    k.ses = True
    dt_ = small("dt")
    k.act(dt_, dt_[:, :], s_ldt, s_ldt[:, :], AF.Exp)
    lre = small("lre")
    k.ts("dve", lre, lre[:, :], s_are, s_are[:, :], -1e-4, ALU.min)
    lrd = small("lrd")
    k.tt("dve", lrd, lrd[:, :], lre, lre[:, :], dt_, dt_[:, :], ALU.mult)
    rho = small("rho")
    k.act(rho, rho[:, :], lrd, lrd[:, :], AF.Exp)
    th = small("th")
    k.tt("dve", th, th[:, :], s_aim, s_aim[:, :], dt_, dt_[:, :], ALU.mult)

    def sin_of(src, shift, name):
        a = small(name + "a")
        k.ts("dve", a, a[:, :], src, src[:, :], float(shift), ALU.add)
        t = small(name + "t")
        k.ts("dve", t, t[:, :], a, a[:, :], 1.0 / (2 * PI), ALU.mult)
        ti = small(name + "ti", dt=I32)
        k.op("dve", lambda g: g.tensor_copy(out=ti[:, :], in_=t[:, :]), [t], [ti])
        tf = small(name + "tf")
        k.op("dve", lambda g: g.tensor_copy(out=tf[:, :], in_=ti[:, :]), [ti], [tf])
        r = small(name + "r")
        k.stt("dve", r, r[:, :], tf, tf[:, :], -2 * PI, a, a[:, :], ALU.mult, ALU.add)
        m1 = small(name + "m1")
        k.ts("dve", m1, m1[:, :], r, r[:, :], PI, ALU.is_gt)
        r2 = small(name + "r2")
        k.stt("dve", r2, r2[:, :], m1, m1[:, :], -2 * PI, r, r[:, :], ALU.mult, ALU.add)
        m2 = small(name + "m2")
        k.ts("dve", m2, m2[:, :], r2, r2[:, :], -PI, ALU.is_lt)
        r3 = small(name + "r3")
        k.stt("dve", r3, r3[:, :], m2, m2[:, :], 2 * PI, r2, r2[:, :], ALU.mult, ALU.add)
        r4 = small(name + "r4")
        k.ts("dve", r4, r4[:, :], r3, r3[:, :], PI, ALU.min, -PI, ALU.max)
        o = small(name + "o")
        k.act(o, o[:, :], r4, r4[:, :], AF.Sin)
        return o

    sn = sin_of(th, 0.0, "sn")
    cs = sin_of(th, PI / 2, "cs")
    abr, abi = small("abr"), small("abi")
    k.tt("dve", abr, abr[:, :], rho, rho[:, :], cs, cs[:, :], ALU.mult)
    k.tt("dve", abi, abi[:, :], rho, rho[:, :], sn, sn[:, :], ALU.mult)
    den, t0 = small("den"), small("t0")
    k.tt("dve", den, den[:, :], lre, lre[:, :], lre, lre[:, :], ALU.mult)
    k.tt("dve", t0, t0[:, :], s_aim, s_aim[:, :], s_aim, s_aim[:, :], ALU.mult)
    den2 = small("den2")
    k.tt("dve", den2, den2[:, :], den, den[:, :], t0, t0[:, :], ALU.add)
    rden = small("rden")
    k.op("dve", lambda g: g.reciprocal(out=rden[:, :], in_=den2[:, :]), [den2], [rden])
    nr = small("nr")
    k.ts("dve", nr, nr[:, :], abr, abr[:, :], -1.0, ALU.add)
    t1, t2, t3, t4 = small("t1"), small("t2"), small("t3"), small("t4")
    k.tt("dve", t1, t1[:, :], nr, nr[:, :], lre, lre[:, :], ALU.mult)
    k.tt("dve", t2, t2[:, :], abi, abi[:, :], s_aim, s_aim[:, :], ALU.mult)
    k.tt("dve", t3, t3[:, :], abi, abi[:, :], lre, lre[:, :], ALU.mult)
    k.tt("dve", t4, t4[:, :], nr, nr[:, :], s_aim, s_aim[:, :], ALU.mult)
    q1, q2 = small("q1"), small("q2")
    k.tt("dve", q1, q1[:, :], t1, t1[:, :], t2, t2[:, :], ALU.add)
    k.tt("dve", q2, q2[:, :], t3, t3[:, :], t4, t4[:, :], ALU.subtract)
    qre, qim = small("qre"), small("qim")
    k.tt("dve", qre, qre[:, :], q1, q1[:, :], rden, rden[:, :], ALU.mult)
    k.tt("dve", qim, qim[:, :], q2, q2[:, :], rden, rden[:, :], ALU.mult)
    nqim = small("nqim")
    k.ts("dve", nqim, nqim[:, :], qim, qim[:, :], -1.0, ALU.mult)
    nqre = small("nqre")
    k.ts("dve", nqre, nqre[:, :], qre, qre[:, :], -1.0, ALU.mult)

    cp_re = k.sb("cpre", [128, 4, 128], BF16)
    cp_imn = k.sb("cpimn", [128, 4, 128], BF16)
    k.op("pool", lambda g: g.memset(cp_re[:, :, :], 0.0), [], [cp_re])
    k.op("pool", lambda g: g.memset(cp_imn[:, :, :], 0.0), [], [cp_imn])
    ctmp = k.sb("ctmp", [128, 4, 32], F32)
    for qq in range(4):
        k.ts("dve", ctmp, ctmp[:, qq, :], c_im, c_im[:, qq, :], nqim[:, qq:qq + 1], ALU.mult, extra_reads=[nqim])
        k.stt("dve", cp_re, cp_re[:, qq, 32 * qq:32 * qq + 32], c_re, c_re[:, qq, :], qre[:, qq:qq + 1], ctmp, ctmp[:, qq, :],
              ALU.mult, ALU.add, extra_reads=[qre])
    ctmp2 = k.sb("ctmp2", [128, 4, 32], F32)
    for qq in range(4):
        k.ts("dve", ctmp2, ctmp2[:, qq, :], c_im, c_im[:, qq, :], nqre[:, qq:qq + 1], ALU.mult, extra_reads=[nqre])
        k.stt("dve", cp_imn, cp_imn[:, qq, 32 * qq:32 * qq + 32], c_re, c_re[:, qq, :], nqim[:, qq:qq + 1], ctmp2, ctmp2[:, qq, :],
              ALU.mult, ALU.add, extra_reads=[nqim])

    COS = k.sb("COS", [128, 4, L], F32)
    SIN = k.sb("SIN", [128, 4, L], F32)
    RHO = k.sb("RHO", [128, 4, L], F32)
    k.op("pool", lambda g: g.memset(COS[:, :, :], 1.0), [], [COS])
    k.op("pool", lambda g: g.memset(SIN[:, :, :], 0.0), [], [SIN])
    k.op("pool", lambda g: g.memset(RHO[:, :, :], 1.0), [], [RHO])
    for qq in range(4):
        k.ts("dve", RHO, RHO[:, qq, :], RHO, RHO[:, qq, :], rho[:, qq:qq + 1], ALU.mult, extra_reads=[rho])
    pr, pi_ = cs, sn
    ttmp = k.sb("ttmp", [128, 4, L // 2], F32)
    w = 1
    lvl = 0
    while w < L:
        npi = small("npi%d" % lvl)
        k.ts("dve", npi, npi[:, :], pi_, pi_[:, :], -1.0, ALU.mult)
        for qq in range(4):
            k.ts("dve", ttmp, ttmp[:, qq, 0:w], SIN, SIN[:, qq, 0:w], npi[:, qq:qq + 1], ALU.mult, extra_reads=[npi])
            k.stt("dve", COS, COS[:, qq, w:2 * w], COS, COS[:, qq, 0:w], pr[:, qq:qq + 1], ttmp, ttmp[:, qq, 0:w],
                  ALU.mult, ALU.add, extra_reads=[pr])
            k.ts("dve", ttmp, ttmp[:, qq, 0:w], SIN, SIN[:, qq, 0:w], pr[:, qq:qq + 1], ALU.mult, extra_reads=[pr])
            k.stt("dve", SIN, SIN[:, qq, w:2 * w], COS, COS[:, qq, 0:w], pi_[:, qq:qq + 1], ttmp, ttmp[:, qq, 0:w],
                  ALU.mult, ALU.add, extra_reads=[pi_])
        a2, b2, ab_ = small("a2_%d" % lvl), small("b2_%d" % lvl), small("ab_%d" % lvl)
        k.tt("dve", a2, a2[:, :], pr, pr[:, :], pr, pr[:, :], ALU.mult)
        k.tt("dve", b2, b2[:, :], pi_, pi_[:, :], pi_, pi_[:, :], ALU.mult)
        k.tt("dve", ab_, ab_[:, :], pr, pr[:, :], pi_, pi_[:, :], ALU.mult)
        npr, npi2 = small("npr%d" % lvl), small("npi2%d" % lvl)
        k.tt("dve", npr, npr[:, :], a2, a2[:, :], b2, b2[:, :], ALU.subtract)
        k.ts("dve", npi2, npi2[:, :], ab_, ab_[:, :], 2.0, ALU.mult)
        pr, pi_ = npr, npi2
        w *= 2
        lvl += 1
    ELr, ELi = pr, pi_
    nELi = small("nELi")
    k.ts("dve", nELi, nELi[:, :], ELi, ELi[:, :], -1.0, ALU.mult)

    k.ses = SES
    NP = 3
    ub = [k.sb("ub", [128, L], F32) for _ in range(2)]
    ubf = [k.sb("ubf", [128, L], BF16) for _ in range(2)]
    ps_re = [k.ps("psre") for _ in range(NP)]
    ps_im = [k.ps("psim") for _ in range(NP)]
    ps_y = [k.ps("psy") for _ in range(2)]
    T = [[k.sb("T%d" % j, [128, L], F32) for j in range(4)] for _ in range(NP)]
    gin = [[k.sb("gin%d" % j, [128, L], F32) for j in range(2)] for _ in range(NP)]
    gg = [[[k.sb("g%d_%d" % (qq, j), [128, L], F32) for j in range(2)] for qq in range(4)] for _ in range(2)]
    T2 = [[k.sb("U%d" % j, [128, L], F32) for j in range(4)] for _ in range(NP)]
    hh = [[k.sb("h%d" % j, [128, L], BF16) for j in range(2)] for _ in range(NP)]
    ini = [[k.sb("ini%d_%d" % (qq, j), [128, 1], F32) for j in range(2)] for qq in range(4)]
    itmp = [k.sb("itmp%d" % qq, [128, 1], F32) for qq in range(4)]
    yo = [k.sb("yo", [128, L], F32) for _ in range(2)]
    nblk = ntok // L
    bps = seq // L
    it = 0
    for blk in range(nblk):
        sl = slice(blk * L, (blk + 1) * L)
        uB, uF, pY = ub[blk % 2], ubf[blk % 2], ps_y[blk % 2]
        k.dma("sp", uB, uB[:, :], u, u.t[:, sl])
        k.act(uF, uF[:, :], uB, uB[:, :], AF.Copy)
        first = (blk % bps == 0)
        for qq in range(4):
            par = it % NP
            it += 1
            pr_, pi2_ = ps_re[par], ps_im[par]
            k.mm(pr_, pr_[:, :], b_re, b_re[:, qq, :], uF, uF[:, :], True, True)
            k.mm(pi2_, pi2_[:, :], b_im, b_im[:, qq, :], uF, uF[:, :], True, True)
            t = T[par]
            cq, sq, rq = COS[:, qq, :], SIN[:, qq, :], RHO[:, qq, :]
            k.tt("dve", t[0], t[0][:, :], pr_, pr_[:, :], COS, cq, ALU.mult)
            k.tt("dve", t[1], t[1][:, :], pi2_, pi2_[:, :], SIN, sq, ALU.mult)
            k.tt("dve", t[2], t[2][:, :], pi2_, pi2_[:, :], COS, cq, ALU.mult)
            k.tt("dve", t[3], t[3][:, :], pr_, pr_[:, :], SIN, sq, ALU.mult)
            gi = gin[par]
            k.tt("pool", gi[0], gi[0][:, :], t[0], t[0][:, :], t[1], t[1][:, :], ALU.add)
            k.tt("dve", gi[1], gi[1][:, :], t[2], t[2][:, :], t[3], t[3][:, :], ALU.subtract)
            g_prev = gg[(blk + 1) % 2][qq]
            g_cur = gg[blk % 2][qq]
            if first:
                i_re, i_im = 0.0, 0.0
                xr = []
            else:
                k.ts("pool", itmp[qq], itmp[qq][:, :], g_prev[1], g_prev[1][:, L - 1:L], nELi[:, qq:qq + 1], ALU.mult,
                     extra_reads=[nELi])
                k.stt("dve", ini[qq][0], ini[qq][0][:, :], g_prev[0], g_prev[0][:, L - 1:L], ELr[:, qq:qq + 1],
                      itmp[qq], itmp[qq][:, :], ALU.mult, ALU.add, extra_reads=[ELr])
                k.ts("pool", itmp[qq], itmp[qq][:, :], g_prev[1], g_prev[1][:, L - 1:L], ELr[:, qq:qq + 1], ALU.mult,
                     extra_reads=[ELr])
                k.stt("dve", ini[qq][1], ini[qq][1][:, :], g_prev[0], g_prev[0][:, L - 1:L], ELi[:, qq:qq + 1],
                      itmp[qq], itmp[qq][:, :], ALU.mult, ALU.add, extra_reads=[ELi])
                i_re, i_im = ini[qq][0][:, :], ini[qq][1][:, :]
                xr = [ini[qq][0], ini[qq][1]]
            k.op("dve", lambda g, o=g_cur[0][:, :], a=rq, b=gi[0][:, :], i0=i_re: g.tensor_tensor_scan(
                out=o, data0=a, data1=b, initial=i0, op0=ALU.mult, op1=ALU.add), [RHO, gi[0]] + xr[:1], [g_cur[0]])
            k.op("dve", lambda g, o=g_cur[1][:, :], a=rq, b=gi[1][:, :], i0=i_im: g.tensor_tensor_scan(
                out=o, data0=a, data1=b, initial=i0, op0=ALU.mult, op1=ALU.add), [RHO, gi[1]] + xr[1:], [g_cur[1]])
            t2_ = T2[par]
            k.tt("pool", t2_[0], t2_[0][:, :], g_cur[0], g_cur[0][:, :], COS, cq, ALU.mult)
            k.tt("dve", t2_[1], t2_[1][:, :], g_cur[1], g_cur[1][:, :], SIN, sq, ALU.mult)
            k.tt("pool", t2_[2], t2_[2][:, :], g_cur[1], g_cur[1][:, :], COS, cq, ALU.mult)
            k.tt("dve", t2_[3], t2_[3][:, :], g_cur[0], g_cur[0][:, :], SIN, sq, ALU.mult)
            h = hh[par]
            k.tt("dve", h[0], h[0][:, :], t2_[0], t2_[0][:, :], t2_[1], t2_[1][:, :], ALU.subtract)
            k.tt("dve", h[1], h[1][:, :], t2_[2], t2_[2][:, :], t2_[3], t2_[3][:, :], ALU.add)
            k.mm(pY, pY[:, :], cp_re, cp_re[:, qq, :], h[0], h[0][:, :], qq == 0, False)
            k.mm(pY, pY[:, :], cp_imn, cp_imn[:, qq, :], h[1], h[1][:, :], False, qq == 3)
        yB = yo[blk % 2]
        k.stt("dve", yB, yB[:, :], uB, uB[:, :], s_d[:, 0:1], pY, pY[:, :], ALU.mult, ALU.add, extra_reads=[s_d])
        k.dma("sp", y, y.t[:, sl], yB, yB[:, :])
    return k.finish()


def s5_core_params(c, a_re, a_im, log_dt, b_re, b_im, c_re, c_im, d):
    g0 = 8 * c

    def pq(a):
        return np.ascontiguousarray(a[g0:g0 + 8].reshape(4, 2, 64).transpose(1, 2, 0).reshape(128, 4)).astype(np.float32)

    are = pq(a_re)
    aim = pq(a_im)
    ldt = pq(np.broadcast_to(log_dt[:, None], (64, 64)))
    B_re = np.zeros((4, 2, 16, 4, 2, 64), np.float32)
    B_im = np.zeros((4, 2, 16, 4, 2, 64), np.float32)
    C_re = np.zeros((2, 64, 4, 2, 16), np.float32)
    C_im = np.zeros((2, 64, 4, 2, 16), np.float32)
    for qq in range(4):
        for gp in range(2):
            g = g0 + 2 * qq + gp
            B_re[qq, gp, :, qq, gp, :] = b_re[g].T
            B_im[qq, gp, :, qq, gp, :] = b_im[g].T
            C_re[gp, :, qq, gp, :] = c_re[g].T
            C_im[gp, :, qq, gp, :] = c_im[g].T
    return {"are": are, "aim": aim, "ldt": ldt,
            "bre": B_re.reshape(128, 4, 128), "bim": B_im.reshape(128, 4, 128),
            "cre": C_re.reshape(128, 4, 32), "cim": C_im.reshape(128, 4, 32),
            "dsk": np.ascontiguousarray(d[128 * c:128 * c + 128].reshape(128, 1)).astype(np.float32)}


class Ctx:
    def __init__(self, k, NT, nkt_half):
        self.k = k
        self.NT = NT
        self.ps1 = [k.ps("ps1") for _ in range(2)]
        self.ps3 = [k.ps("ps3") for _ in range(2)]
        self.pso = [k.ps("pso") for _ in range(2)]
        self.pss = [k.ps("pss") for _ in range(2)]
        self.wa = [k.sb("wa", [128, 8, 512], BF16) for _ in range(2)]
        self.wb = [k.sb("wb", [128, 8, 512], BF16) for _ in range(2)]
        self.w2h = k.sb("w2h", [128, nkt_half, 1024], BF16)
        self.hmid = k.sb("hmid", [128, max(nkt_half, 8), NT], BF16)
        self.z = k.sb("z", [128, 8, NT], F32)
        self.xbf = k.sb("xbf", [128, 8, NT], BF16)
        self.sact = [k.sb("sact", [128, 512], F32) for _ in range(2)]
        self.tmp = [k.sb("tmp", [128, 512], F32) for _ in range(2)]
        self.ones = k.sb("ones", [128, 128], F32)
        k.op("pool", lambda g: g.memset(self.ones[:, :], 1.0), [], [self.ones])
        self.sq = [k.sb("sq", [128, 512], F32) for _ in range(2)]
        self.mean = k.sb("mean", [128, 512], F32)
        self.ex2 = k.sb("ex2", [128, 512], F32)
        self.rstd = k.sb("rstd", [128, 512], F32)
        self.nmr = k.sb("nmr", [128, 512], F32)
        self.lng = k.sb("lng", [128, 32], F32)
        self.lnb = k.sb("lnb", [128, 32], F32)
        self.wi = 0
        self.pi = 0
        self.oi = 0


def load_wblock(k, buf, wd, k0, nk, c0, cw):
    k.dma("pool", buf, buf[:, 0:nk, 0:cw], wd, wd.t[k0 * 128:(k0 + nk) * 128, c0:c0 + cw].rearrange("(kt p) m -> p kt m", p=128))


def glu_phase(k, cx, x_in, wa_d, a_c0, wb_d, b_c0, ncols, func, emit, hook=None):
    NT = cx.NT
    for c0 in range(0, ncols, 512):
        cw = min(512, ncols - c0)
        wa, wb = cx.wa[cx.wi % 2], cx.wb[cx.wi % 2]
        cx.wi += 1
        load_wblock(k, wa, wa_d, 0, 8, a_c0 + c0, cw)
        load_wblock(k, wb, wb_d, 0, 8, b_c0 + c0, cw)
        if hook is not None and c0 == 512:
            hook()
        for fl in range(cw // 128):
            ft = c0 // 128 + fl
            for tb in range(NT // 512):
                p1, p3 = cx.ps1[cx.pi % 2], cx.ps3[cx.pi % 2]
                sa = cx.sact[cx.pi % 2]
                cx.pi += 1
                ts_ = slice(tb * 512, tb * 512 + 512)
                for kt in range(8):
                    k.mm(p1, p1[:, :], wa, wa[:, kt, fl * 128:fl * 128 + 128], x_in, x_in[:, kt, ts_], kt == 0, kt == 7)
                for kt in range(8):
                    k.mm(p3, p3[:, :], wb, wb[:, kt, fl * 128:fl * 128 + 128], x_in, x_in[:, kt, ts_], kt == 0, kt == 7)
                k.act(sa, sa[:, :], p1, p1[:, :], func)
                emit(ft, tb, sa, p3)


def down_load(k, cx, w2_d, k0, nk):
    k.dma("pool", cx.w2h, cx.w2h[:, 0:nk, :], w2_d, w2_d.t[k0 * 128:(k0 + nk) * 128, :].rearrange("(kt p) m -> p kt m", p=128))


def down_phase(k, cx, w2_d, k0, nk, zacc):
    NT = cx.NT
    for m in range(8):
        for tb in range(NT // 512):
            po = cx.pso[cx.oi % 2]
            cx.oi += 1
            ts_ = slice(tb * 512, tb * 512 + 512)
            for kt in range(nk):
                k.mm(po, po[:, :], cx.w2h, cx.w2h[:, kt, m * 128:m * 128 + 128], cx.hmid, cx.hmid[:, kt, ts_], kt == 0, kt == nk - 1)
            k.tt("dve", zacc, zacc[:, m, ts_], po, po[:, :], zacc, zacc[:, m, ts_], ALU.add)


def layer_norm(k, cx, z, ln_idx, xbf_out):
    NT = cx.NT
    for tb in range(NT // 512):
        ts_ = slice(tb * 512, tb * 512 + 512)
        s1, s2 = cx.pss[0], cx.pss[1]
        for kt in range(8):
            k.mm(s1, s1[:, :], cx.ones, cx.ones[:, :], z, z[:, kt, ts_], kt == 0, kt == 7)
        for kt in range(8):
            sq = cx.sq[kt % 2]
            k.act(sq, sq[:, :], z, z[:, kt, ts_], AF.Square)
            k.mm(s2, s2[:, :], cx.ones, cx.ones[:, :], sq, sq[:, :], kt == 0, kt == 7)
        k.act(cx.mean, cx.mean[:, :], s1, s1[:, :], AF.Copy, scale=1.0 / D)
        k.act(cx.ex2, cx.ex2[:, :], s2, s2[:, :], AF.Copy, scale=1.0 / D)
        k.tt("dve", cx.nmr, cx.nmr[:, :], cx.mean, cx.mean[:, :], cx.mean, cx.mean[:, :], ALU.mult)
        k.tt("dve", cx.ex2, cx.ex2[:, :], cx.ex2, cx.ex2[:, :], cx.nmr, cx.nmr[:, :], ALU.subtract)
        k.ts("dve", cx.ex2, cx.ex2[:, :], cx.ex2, cx.ex2[:, :], LN_EPS, ALU.add)
        k.act(cx.ex2, cx.ex2[:, :], cx.ex2, cx.ex2[:, :], AF.Sqrt)
        k.op("dve", lambda g: g.reciprocal(out=cx.rstd[:, :], in_=cx.ex2[:, :]), [cx.ex2], [cx.rstd])
        k.stt("dve", cx.nmr, cx.nmr[:, :], cx.mean, cx.mean[:, :], -1.0, cx.rstd, cx.rstd[:, :], ALU.mult, ALU.mult)
        for kt in range(8):
            t = cx.tmp[kt % 2]
            k.tt("dve", t, t[:, :], z, z[:, kt, ts_], cx.rstd, cx.rstd[:, :], ALU.mult)
            k.tt("dve", t, t[:, :], t, t[:, :], cx.nmr, cx.nmr[:, :], ALU.add)
            col = ln_idx * 8 + kt
            k.op("act", lambda g, o=z[:, kt, ts_], i=t[:, :], c=col: g.activation(
                out=o, in_=i, func=AF.Identity, scale=cx.lng[:, c:c + 1], bias=cx.lnb[:, c:c + 1]),
                [t, cx.lng, cx.lnb], [z])
            if xbf_out is not None:
                k.act(xbf_out, xbf_out[:, kt, ts_], z, z[:, kt, ts_], AF.Copy)


def ln_layout(ln_g, ln_b):
    g = np.ascontiguousarray(ln_g.reshape(4, 8, 128).transpose(2, 0, 1).reshape(128, 32)).astype(np.float32)
    b = np.ascontiguousarray(ln_b.reshape(4, 8, 128).transpose(2, 0, 1).reshape(128, 32)).astype(np.float32)
    return g, b


DFF = 2816


def build_layer0_rest(tpc=TPC, NT=1024):
    k = KB()
    yT = k.dram("yT", [D, tpc], F32, "ExternalInput")
    xT = k.dram("xT", [D, tpc], F32, "ExternalInput")
    wglu = k.dram("wglu", [D, 2 * D], F32, "ExternalInput")
    w1 = k.dram("w1", [D, DFF], F32, "ExternalInput")
    w3 = k.dram("w3", [D, DFF], F32, "ExternalInput")
    w2 = k.dram("w2", [DFF, D], F32, "ExternalInput")
    lng = k.dram("lng", [128, 32], F32, "ExternalInput")
    lnb = k.dram("lnb", [128, 32], F32, "ExternalInput")
    oT = k.dram("oT", [D, tpc], F32, "ExternalOutput")
    cx = Ctx(k, NT, 11)
    k.dma("sp", cx.lng, cx.lng[:, :], lng, lng.t[:, :])
    k.dma("sp", cx.lnb, cx.lnb[:, :], lnb, lnb.t[:, :])
    ystage = [k.sb("ystage", [128, NT], F32) for _ in range(2)]
    gbf = cx.hmid
    z = cx.z
    for sb_ in range(tpc // NT):
        tsl = slice(sb_ * NT, (sb_ + 1) * NT)
        for kt in range(8):
            ys = ystage[kt % 2]
            k.dma("sp", ys, ys[:, :], yT, yT.t[kt * 128:(kt + 1) * 128, tsl])
            k.act(gbf, gbf[:, kt, :], ys, ys[:, :], AF.Gelu)
            k.dma("sp", z, z[:, kt, :], xT, xT.t[kt * 128:(kt + 1) * 128, tsl])

        def emit_glu(ft, tb, sa, pB):
            t = cx.tmp[(ft + tb) % 2]
            ts_ = slice(tb * 512, tb * 512 + 512)
            k.tt("dve", t, t[:, :], pB, pB[:, :], sa, sa[:, :], ALU.mult)
            k.stt("dve", z, z[:, ft, ts_], z, z[:, ft, ts_], float(ALPHA), t, t[:, :], ALU.mult, ALU.add)

        glu_phase(k, cx, gbf, wglu, D, wglu, 0, D, AF.Sigmoid, emit_glu)
        layer_norm(k, cx, z, 0, cx.xbf)
        for kt in range(8):
            k.ts("dve", z, z[:, kt, :], z, z[:, kt, :], float(ALPHA), ALU.mult)
        for half in range(2):
            f0 = half * 11

            def emit_ffn(ft, tb, sa, pB):
                ts_ = slice(tb * 512, tb * 512 + 512)
                k.tt("dve", cx.hmid, cx.hmid[:, ft, ts_], pB, pB[:, :], sa, sa[:, :], ALU.mult)

            glu_phase(k, cx, cx.xbf, w1, f0 * 128, w3, f0 * 128, 11 * 128, AF.Silu, emit_ffn,
                      hook=lambda f0=f0: down_load(k, cx, w2, f0, 11))
            down_phase(k, cx, w2, f0, 11, z)
        layer_norm(k, cx, z, 1, None)
        for kt in range(8):
            k.dma("sp", oT, oT.t[kt * 128:(kt + 1) * 128, tsl], z, z[:, kt, :])
    return k.finish()


NQKV = 9216
DILS = (1, 4, 16)


def build_qkv(tpc=TPC):
    k = KB()
    xT = k.dram("xT", [D, tpc], F32, "ExternalInput")
    w = k.dram("w", [D, NQKV], F32, "ExternalInput")
    o = k.dram("qkvT", [NQKV, tpc], BF16, "ExternalOutput")
    xb = k.sb("xb", [128, 8, tpc], BF16)
    for kt in range(8):
        k.dma("pool", xb, xb[:, kt, :], xT, xT.t[kt * 128:(kt + 1) * 128, :])
    wbuf = [k.sb("wq", [128, 8, 512], BF16) for _ in range(2)]
    pbs = [k.ps("pb") for _ in range(4)]
    sts = [k.sb("st", [128, 512], BF16) for _ in range(4)]
    i = 0
    for cb in range(NQKV // 512):
        wb = wbuf[cb % 2]
        load_wblock(k, wb, w, 0, 8, cb * 512, 512)
        for fl in range(4):
            for tb in range(tpc // 512):
                pb, st = pbs[i % 4], sts[i % 4]
                ts_ = slice(tb * 512, tb * 512 + 512)
                for kt in range(8):
                    k.mm(pb, pb[:, :], wb, wb[:, kt, fl * 128:fl * 128 + 128], xb, xb[:, kt, ts_], kt == 0, kt == 7)
                if i % 2 == 0:
                    k.act(st, st[:, :], pb, pb[:, :], AF.Copy)
                else:
                    k.op("dve", lambda g, o_=st[:, :], a=pb[:, :]: g.tensor_copy(out=o_, in_=a), [pb], [st])
                r0 = cb * 512 + fl * 128
                k.dma("sp", o, o.t[r0:r0 + 128, ts_], st, st[:, :])
                i += 1
    return k.finish()


ACH = 2048
NEG = -1e30


def build_attn(tpc=TPC, LA=3):
    k = KB()
    nblk = tpc // 128
    qb_d = k.dram("qb", [3, 8, 128, nblk * 128], BF16, "ExternalInput")
    kb_d = k.dram("kb", [3, 8, 128, (nblk + 16) * 128], BF16, "ExternalInput")
    vb_d = k.dram("vb", [3, 8, 128, (nblk + 16) * 128], BF16, "ExternalInput")
    bias_d = k.dram("biasT", [3, 8, 128, 512], F32, "ExternalInput")
    negb_d = k.dram("negb", [128, 1], F32, "ExternalInput")
    o = k.dram("attnT", [D, tpc], F32, "ExternalOutput")
    negb = k.sb("negb", [128, 1], F32)
    k.dma("sp", negb, negb[:, :], negb_d, negb_d.t[:, :])
    ones = k.sb("ones", [128, 64], BF16)
    k.op("pool", lambda g: g.memset(ones[:, :], 1.0), [], [ones])
    Qb = [k.sb("Qb", [128, 16 * 128], BF16) for _ in range(2)]
    Kb = [k.sb("Kb", [128, 32 * 128], BF16) for _ in range(2)]
    Vb = [k.sb("Vb", [128, 32, 128], BF16) for _ in range(2)]
    Bs = [k.sb("Bs", [128, 2, 256], F32) for _ in range(2)]
    num = [k.sb("num", [128, ACH], F32) for _ in range(2)]
    den = [k.sb("den", [128, ACH], F32) for _ in range(2)]
    rec = k.sb("rec", [128, ACH], F32)
    NS = LA + 1
    ps_s = [k.ps("ps_s", (128, 256)) for _ in range(NS)]
    ps_o = [k.ps("ps_o") for _ in range(2)]
    ps_d = [k.ps("ps_d") for _ in range(2)]
    tmp = [k.sb("tmpS", [128, 256], F32) for _ in range(NS + 1)]
    pT = [k.sb("pT", [128, 256], BF16) for _ in range(NS + 1)]
    units = []
    li = 0
    ei = 0
    ci = 0
    for ch in range(tpc // ACH):
        for ft in range(8):
            nm, dn = num[ci % 2], den[ci % 2]
            ci += 1
            for g in range(3):
                d = DILS[g]
                bufs = (Qb[li % 2], Kb[li % 2], Vb[li % 2], Bs[li % 2])
                li += 1
                for B in range(16):
                    for hh in range(2):
                        units.append(dict(ch=ch, ft=ft, g=g, d=d, B=B, hh=hh, bufs=bufs, nm=nm, dn=dn,
                                          po=ps_o[ei % 2], pd=ps_d[ei % 2], load=(B == 0 and hh == 0),
                                          evac=(B % 4 == 3 and hh == 1), last=(g == 2 and B == 15 and hh == 1)))
                    if B % 4 == 3:
                        ei += 1

    def s_part(ui, un):
        qb, kb, vb, bs = un["bufs"]
        ch, ft, g, d, B, hh = un["ch"], un["ft"], un["g"], un["d"], un["B"], un["hh"]
        if un["load"]:
            b0 = ch * 16
            k.dma("sp", qb, qb[:, :], qb_d, qb_d.t[g, ft, :, b0 * 128:(b0 + 16) * 128])
            nkb = 16 + d
            k0 = 16 + b0 - d
            k.dma("sp", kb, kb[:, 0:nkb * 128], kb_d, kb_d.t[g, ft, :, k0 * 128:(k0 + nkb) * 128])
            k.dma("sp", vb, vb[:, 0:nkb, :], vb_d,
                  vb_d.t[g, ft, :, k0 * 128:(k0 + nkb) * 128].rearrange("p (b f) -> p b f", f=128))
            k.dma("sp", bs, bs[:, :, :], bias_d, bias_d.t[g, ft, :, :].rearrange("p (h f) -> p h f", h=2))
        halo = (ch == 0 and B < d)
        rs = slice(64 * hh, 64 * hh + 64)
        pss = ps_s[ui % NS]
        tm, pt = tmp[ui % (NS + 1)], pT[ui % (NS + 1)]
        qs = slice(B * 128, B * 128 + 128)
        kp = slice(B * 128, B * 128 + 128)
        kc = slice((B + d) * 128, (B + d) * 128 + 128)
        k.mm(pss, pss[:, 0:128], kb, kb[rs, kp], qb, qb[rs, qs], True, True)
        k.mm(pss, pss[:, 128:256], kb, kb[rs, kc], qb, qb[rs, qs], True, True)
        k.stt("dve", tm, tm[:, :], pss, pss[:, 0:256], 0.125, bs, bs[:, hh, :], ALU.mult, ALU.add)
        if halo:
            k.op("act", lambda e, o_=pt[:, 0:128], i_=tm[:, 0:128]: e.activation(
                out=o_, in_=i_, func=AF.Exp, bias=negb[:, 0:1], scale=1.0), [tm, negb], [pt])
            k.act(pt, pt[:, 128:256], tm, tm[:, 128:256], AF.Exp)
        else:
            k.act(pt, pt[:, :], tm, tm[:, :], AF.Exp)

    def pv_part(ui, un):
        qb, kb, vb, bs = un["bufs"]
        ch, ft, g, d, B, hh = un["ch"], un["ft"], un["g"], un["d"], un["B"], un["hh"]
        po, pd, nm, dn = un["po"], un["pd"], un["nm"], un["dn"]
        rs = slice(64 * hh, 64 * hh + 64)
        pt = pT[ui % (NS + 1)]
        cs = slice((B % 4) * 128, (B % 4) * 128 + 128)
        k.mm(po, po[rs, cs], vb, vb[:, B, rs], pt, pt[:, 0:128], True, False)
        k.mm(po, po[rs, cs], vb, vb[:, B + d, rs], pt, pt[:, 128:256], False, True)
        k.mm(pd, pd[rs, cs], ones, ones[:, :], pt, pt[:, 0:128], True, False)
        k.mm(pd, pd[rs, cs], ones, ones[:, :], pt, pt[:, 128:256], False, True)
        if un["evac"]:
            B0 = B - 3
            if d == 1:
                ov = lambda t: t[:, B0 * 128:(B0 + 4) * 128]
                iv = lambda p: p[:, :]
            else:
                n_, r0 = B0 // d, B0 % d
                base = n_ * 128 * d
                ov = lambda t, base=base, d=d, r0=r0: t[:, base:base + 128 * d].rearrange(
                    "p (i r) -> p i r", r=d)[:, :, r0:r0 + 4]
                iv = lambda p: p[:, :].rearrange("p (j i) -> p i j", j=4)
            if g == 0:
                k.op("dve", lambda e, o_=ov(nm), i_=iv(po): e.tensor_copy(out=o_, in_=i_), [po], [nm])
                k.act(dn, ov(dn), pd, iv(pd), AF.Copy)
            else:
                k.tt("dve", nm, ov(nm), po, iv(po), nm, ov(nm), ALU.add)
                k.tt("pool", dn, ov(dn), pd, iv(pd), dn, ov(dn), ALU.add) if False else \
                    k.tt("dve", dn, ov(dn), pd, iv(pd), dn, ov(dn), ALU.add)
        if un["last"]:
            k.op("dve", lambda e, o_=rec[:, :], i_=dn[:, :]: e.reciprocal(out=o_, in_=i_), [dn], [rec])
            k.tt("pool", nm, nm[:, :], nm, nm[:, :], rec, rec[:, :], ALU.mult)
            k.dma("sp", o, o.t[ft * 128:(ft + 1) * 128, ch * ACH:(ch + 1) * ACH], nm, nm[:, :])

    n = len(units)
    for i in range(n + LA):
        if i < n:
            s_part(i, units[i])
        if i - LA >= 0:
            pv_part(i - LA, units[i - LA])
    return k.finish()


def t5_bucket(dist):
    n_buckets, max_distance = 32, 2048
    max_exact = n_buckets // 2
    d = np.maximum(dist, 0)
    large = max_exact + (np.log(np.maximum(d, 1) / max_exact) / math.log(max_distance / max_exact)
                         * (n_buckets - max_exact)).astype(np.int64)
    large = np.minimum(large, n_buckets - 1)
    return np.where(d < max_exact, d, large).astype(np.int32)


def attn_bias_table(rel_bias):
    L = 128
    qi = np.arange(L)[:, None]
    ki = np.arange(2 * L)[None, :]
    off = qi + L - ki
    band = (off >= 0) & (off <= L)
    out = np.zeros((3, 8, 128, 2, 2, 128), np.float32)
    for g, d in enumerate(DILS):
        bucket = t5_bucket(np.clip(off, 0, None) * d)
        bias = rel_bias[bucket]
        bias = np.where(band[:, :, None], bias, np.float32(NEG))
        b = bias.reshape(L, 2, L, 8, 2)
        out[g] = b.transpose(3, 2, 4, 1, 0)
    return out.reshape(3, 8, 128, 512)


def attn_host_layout(qkvT, rel_bias, seqlen, tpc):
    ntok = qkvT.shape[1]
    ncore = ntok // tpc
    nblk = tpc // 128
    bias = attn_bias_table(rel_bias)
    perm = {}
    for g, d in enumerate(DILS):
        for j in range(3):
            a = qkvT[(g * 3 + j) * 1024:(g * 3 + j + 1) * 1024]
            a = a.reshape(8, 128, ntok // (128 * d), 128, d).transpose(0, 1, 2, 4, 3)
            perm[g, j] = a.reshape(8, 128, ntok // 128, 128)
    maps = []
    for c in range(ncore):
        first = (c * tpc) % seqlen == 0
        B0 = c * nblk
        qb = np.stack([perm[g, 0][:, :, B0:B0 + nblk].reshape(8, 128, nblk * 128) for g in range(3)])
        kbs, vbs = [], []
        for g in range(3):
            kk = np.zeros((8, 128, nblk + 16, 128), qkvT.dtype)
            vv = np.zeros((8, 128, nblk + 16, 128), qkvT.dtype)
            lo = 0 if first else 16
            kk[:, :, 16 - lo:] = perm[g, 1][:, :, B0 - lo:B0 + nblk]
            vv[:, :, 16 - lo:] = perm[g, 2][:, :, B0 - lo:B0 + nblk]
            kbs.append(kk.reshape(8, 128, -1))
            vbs.append(vv.transpose(0, 3, 2, 1).reshape(8, 128, -1))
        maps.append({"qb": np.ascontiguousarray(qb), "kb": np.ascontiguousarray(np.stack(kbs)),
                     "vb": np.ascontiguousarray(np.stack(vbs)), "biasT": bias,
                     "negb": np.full((128, 1), NEG if first else 0.0, np.float32)})
    return maps


NEXP = 8
DFE = 3584


def build_layer1_rest(tpc=TPC, NT=1024):
    k = KB()
    aT = k.dram("aT", [D, tpc], F32, "ExternalInput")
    xT = k.dram("xT", [D, tpc], F32, "ExternalInput")
    wo = k.dram("wo", [D, D], F32, "ExternalInput")
    wr = k.dram("wr", [128, 8, NEXP], F32, "ExternalInput")
    w1 = k.dram("w1", [NEXP, D, DFE], F32, "ExternalInput")
    w3 = k.dram("w3", [NEXP, D, DFE], F32, "ExternalInput")
    w2 = k.dram("w2", [NEXP, DFE, D], F32, "ExternalInput")
    lng = k.dram("lng", [128, 32], F32, "ExternalInput")
    lnb = k.dram("lnb", [128, 32], F32, "ExternalInput")
    ident_d = k.dram("ident", [128, 128], F32, "ExternalInput")
    oT = k.dram("oT", [D, tpc], F32, "ExternalOutput")
    cx = Ctx(k, NT, 14)
    k.dma("sp", cx.lng, cx.lng[:, :], lng, lng.t[:, :])
    k.dma("sp", cx.lnb, cx.lnb[:, :], lnb, lnb.t[:, :])
    ident = k.sb("ident", [128, 128], F32)
    k.dma("sp", ident, ident[:, :], ident_d, ident_d.t[:, :])
    wr_sb = k.sb("wr", [128, 8, NEXP], F32)
    k.dma("sp", wr_sb, wr_sb[:, :, :], wr, wr.t[:, :, :])
    ones_bf = k.sb("ones_bf", [128, 128], BF16)
    k.op("pool", lambda g: g.memset(ones_bf[:, :], 1.0), [], [ones_bf])
    gate_bc = k.sb("gate_bc", [128, NEXP, NT], BF16)
    ntt = NT // 128
    lg = k.sb("lg", [128, ntt, 8], F32)
    top = k.sb("top", [128, ntt, 8], F32)
    gsm = [k.sb("gsm%d" % i, [128, ntt], F32) for i in range(4)]
    gate = k.sb("gate", [128, ntt, 8], F32)
    gt2 = k.sb("gt2", [128, ntt, 8], F32)
    diag = [k.sb("diag", [128, 128], BF16) for _ in range(2)]
    z = cx.z
    abf = cx.hmid
    for sb_ in range(tpc // NT):
        tsl = slice(sb_ * NT, (sb_ + 1) * NT)
        for kt in range(8):
            k.dma("pool", abf, abf[:, kt, :], aT, aT.t[kt * 128:(kt + 1) * 128, tsl])
            k.dma("sp", z, z[:, kt, :], xT, xT.t[kt * 128:(kt + 1) * 128, tsl])
        for cb in range(2):
            wa = cx.wa[cx.wi % 2]
            cx.wi += 1
            load_wblock(k, wa, wo, 0, 8, cb * 512, 512)
            for fl in range(4):
                m = cb * 4 + fl
                for tb in range(NT // 512):
                    po = cx.pso[cx.oi % 2]
                    cx.oi += 1
                    ts_ = slice(tb * 512, tb * 512 + 512)
                    for kt in range(8):
                        k.mm(po, po[:, :], wa, wa[:, kt, fl * 128:fl * 128 + 128], abf, abf[:, kt, ts_], kt == 0, kt == 7)
                    k.stt("dve", z, z[:, m, ts_], z, z[:, m, ts_], float(ALPHA), po, po[:, :], ALU.mult, ALU.add)
        layer_norm(k, cx, z, 2, cx.xbf)
        k.ses = True
        pl = cx.pss[0]
        for tt in range(ntt):
            for kt in range(8):
                k.mm(pl, pl[:, tt * 8:tt * 8 + 8], z, z[:, kt, tt * 128:(tt + 1) * 128], wr_sb, wr_sb[:, kt, :], kt == 0, kt == 7)
        k.act(lg, lg[:, :, :], pl, pl[:, 0:ntt * 8].rearrange("p (t e) -> p t e", e=8), AF.Copy)
        nv1, e2, g1, g2 = gsm
        for tt in range(ntt):
            k.op("dve", lambda g, o=top[:, tt, :], i=lg[:, tt, :]: g.max(out=o, in_=i), [lg], [top])
        k.ts("dve", nv1, nv1[:, :], top, top[:, :, 0], -1.0, ALU.mult)
        k.tt("dve", e2, e2[:, :], top, top[:, :, 1], nv1, nv1[:, :], ALU.add)
        k.act(e2, e2[:, :], e2, e2[:, :], AF.Exp)
        k.ts("dve", g1, g1[:, :], e2, e2[:, :], 1.0, ALU.add)
        k.op("dve", lambda g: g.reciprocal(out=g1[:, :], in_=g1[:, :]), [g1], [g1])
        k.tt("dve", g2, g2[:, :], e2, e2[:, :], g1, g1[:, :], ALU.mult)
        for tt in range(ntt):
            k.ts("dve", gate, gate[:, tt, :], lg, lg[:, tt, :], top[:, tt, 0:1], ALU.is_equal, g1[:, tt:tt + 1], ALU.mult,
                 extra_reads=[top, g1])
            k.ts("dve", gt2, gt2[:, tt, :], lg, lg[:, tt, :], top[:, tt, 1:2], ALU.is_equal, g2[:, tt:tt + 1], ALU.mult,
                 extra_reads=[top, g2])
        k.tt("dve", gate, gate[:, :, :], gate, gate[:, :, :], gt2, gt2[:, :, :], ALU.add)
        di = 0
        for e in range(NEXP):
            for hb in range(NT // 512):
                pg = cx.pss[1]
                for t4 in range(4):
                    tt = hb * 4 + t4
                    dg = diag[di % 2]
                    di += 1
                    k.ts("dve", dg, dg[:, :], ident, ident[:, :], gate[:, tt, e:e + 1], ALU.mult, extra_reads=[gate])
                    k.mm(pg, pg[:, t4 * 128:(t4 + 1) * 128], ones_bf, ones_bf[:, :], dg, dg[:, :], True, True)
                k.act(gate_bc, gate_bc[:, e, hb * 512:(hb + 1) * 512], pg, pg[:, :], AF.Copy)
        k.ses = SES
        for kt in range(8):
            k.ts("dve", z, z[:, kt, :], z, z[:, kt, :], float(ALPHA), ALU.mult)
        for e in range(NEXP):
            w1e, w3e, w2e = Buf("w1e", w1.t[e]), Buf("w3e", w3.t[e]), Buf("w2e", w2.t[e])
            for half in range(2):
                f0 = half * 14

                def emit_moe(ft, tb, sa, pB, e=e):
                    ts_ = slice(tb * 512, tb * 512 + 512)
                    t = cx.tmp[(ft + tb) % 2]
                    k.tt("dve", t, t[:, :], pB, pB[:, :], sa, sa[:, :], ALU.mult)
                    k.tt("dve", cx.hmid, cx.hmid[:, ft, ts_], t, t[:, :], gate_bc, gate_bc[:, e, ts_], ALU.mult)

                glu_phase(k, cx, cx.xbf, w1e, f0 * 128, w3e, f0 * 128, 14 * 128, AF.Silu, emit_moe,
                          hook=lambda f0=f0, w2e=w2e: down_load(k, cx, w2e, f0, 14))
                down_phase(k, cx, w2e, f0, 14, z)
        layer_norm(k, cx, z, 3, None)
        for kt in range(8):
            k.dma("sp", oT, oT.t[kt * 128:(kt + 1) * 128, tsl], z, z[:, kt, :])
    return k.finish()


_PROGS = {}


def _prog(name, fn):
    if name not in _PROGS:
        _PROGS[name] = fn()
    return _PROGS[name]


def _run(nc, maps):
    res = run_bass_kernel_spmd(nc, maps, core_ids=list(range(NCORES)))
    return res.results


def kernel(x, s5_w_in, s5_a_re, s5_a_im, s5_log_dt, s5_b_re, s5_b_im, s5_c_re, s5_c_im, s5_d,
           s5_w_glu, attn_w_qkv, attn_w_o, rel_bias, ffn_w1, ffn_w3, ffn_w2, moe_w_router,
           moe_w1, moe_w3, moe_w2, ln_g, ln_b):
    f32 = np.float32
    x = np.asarray(x, f32)
    xT = np.ascontiguousarray(x.reshape(NTOK, D).T)
    cols = [slice(c * TPC, (c + 1) * TPC) for c in range(NCORES)]
    xTs = [np.ascontiguousarray(xT[:, s]) for s in cols]
    lg, lb = ln_layout(np.asarray(ln_g, f32), np.asarray(ln_b, f32))

    w_in = np.ascontiguousarray(np.asarray(s5_w_in, f32)[0])
    r = _run(_prog("A", build_uproj), [{"xT": xTs[c], "w": w_in} for c in range(NCORES)])
    u_full = np.concatenate([r[c]["uT"] for c in range(NCORES)], axis=1)
    maps = []
    for c in range(NCORES):
        m = s5_core_params(c, np.asarray(s5_a_re, f32)[0], np.asarray(s5_a_im, f32)[0], np.asarray(s5_log_dt, f32)[0],
                           np.asarray(s5_b_re, f32)[0], np.asarray(s5_b_im, f32)[0], np.asarray(s5_c_re, f32)[0],
                           np.asarray(s5_c_im, f32)[0], np.asarray(s5_d, f32)[0])
        m["u"] = np.ascontiguousarray(u_full[128 * c:128 * c + 128])
        maps.append(m)
    r = _run(_prog("B", build_s5), maps)
    y_full = np.concatenate([r[c]["y"] for c in range(NCORES)], axis=0)
    wglu = np.ascontiguousarray(np.asarray(s5_w_glu, f32)[0])
    w1 = np.ascontiguousarray(np.asarray(ffn_w1, f32)[0])
    w3 = np.ascontiguousarray(np.asarray(ffn_w3, f32)[0])
    w2 = np.ascontiguousarray(np.asarray(ffn_w2, f32)[0])
    r = _run(_prog("C", build_layer0_rest),
             [{"yT": np.ascontiguousarray(y_full[:, cols[c]]), "xT": xTs[c], "wglu": wglu, "w1": w1, "w3": w3, "w2": w2,
               "lng": lg, "lnb": lb} for c in range(NCORES)])
    x2s = [r[c]["oT"] for c in range(NCORES)]
    wqkv = np.ascontiguousarray(np.asarray(attn_w_qkv, f32)[0])
    r = _run(_prog("D", build_qkv), [{"xT": x2s[c], "w": wqkv} for c in range(NCORES)])
    qkvT = np.concatenate([r[c]["qkvT"] for c in range(NCORES)], axis=1)
    maps = attn_host_layout(qkvT, np.asarray(rel_bias, f32), SEQ, TPC)
    r = _run(_prog("E", build_attn), maps)
    aTs = [r[c]["attnT"] for c in range(NCORES)]
    wo = np.ascontiguousarray(np.asarray(attn_w_o, f32)[0])
    wr = np.ascontiguousarray(np.asarray(moe_w_router, f32)[0].reshape(8, 128, NEXP).transpose(1, 0, 2))
    mw1 = np.ascontiguousarray(np.asarray(moe_w1, f32)[0])
    mw3 = np.ascontiguousarray(np.asarray(moe_w3, f32)[0])
    mw2 = np.ascontiguousarray(np.asarray(moe_w2, f32)[0])
    ident = np.eye(128, dtype=f32)
    r = _run(_prog("F", build_layer1_rest),
             [{"aT": aTs[c], "xT": x2s[c], "wo": wo, "wr": wr, "w1": mw1, "w3": mw3, "w2": mw2, "lng": lg, "lnb": lb,
               "ident": ident} for c in range(NCORES)])
    oT = np.concatenate([r[c]["oT"] for c in range(NCORES)], axis=1)
    return np.ascontiguousarray(oT.T).reshape(BATCH, SEQ, D).astype(f32)
```

```python
import math
import numpy as np
import concourse.bass as bass
import concourse.mybir as mybir
from concourse.bass_utils import run_bass_kernel_spmd
from contextlib import ExitStack

F32 = mybir.dt.float32
BF16 = mybir.dt.bfloat16
I32 = mybir.dt.int32
ALU = mybir.AluOpType
AF = mybir.ActivationFunctionType
AX = mybir.AxisListType

NCORES = 8
D = 1024
SEQ = 16384
BATCH = 2
NTOK = BATCH * SEQ
TPC = NTOK // NCORES
DEPTH = 2
ALPHA = (2 * DEPTH) ** 0.25
LN_EPS = 1e-5
PI = math.pi


class Buf:
    __slots__ = ("name", "t", "lw", "rd", "dsem", "dcnt", "nodep")

    def __init__(self, name, t):
        self.name = name
        self.t = t
        self.lw = None
        self.rd = {}
        self.dsem = None
        self.dcnt = 0
        self.nodep = False

    def __getitem__(self, idx):
        return self.t[idx]


SES = False


class KB:
    def __init__(self, same_engine_sync=None):
        if same_engine_sync is None:
            same_engine_sync = SES
        self.nc = bass.Bass("TRN2", target_bir_lowering=False)
        self.es = ExitStack()
        nc = self.nc
        self.eng = {"pe": nc.tensor, "dve": nc.vector, "act": nc.scalar, "pool": nc.gpsimd, "sp": nc.sync}
        self.sem = {e: self.es.enter_context(nc.semaphore("s_" + e)) for e in self.eng}
        self.cnt = {e: 0 for e in self.eng}
        self.known = {e: {} for e in self.eng}
        self.ses = same_engine_sync
        self.nsem = 0
        self.prog = {e: [] for e in self.eng}
        self.outs = []
        self.uid = 0
        self.scopes = []
        self.dbufs = []

    def push_scope(self):
        self.scopes.append(ExitStack())

    def pop_scope(self):
        self.barrier()
        self.scopes.pop().close()

    def _stk(self):
        return self.scopes[-1] if self.scopes else self.es

    def barrier(self):
        tags = [(self.sem[e], self.cnt[e]) for e in self.eng if self.cnt[e]]
        tags += [(b.dsem, b.dcnt) for b in self.dbufs if b.dcnt]
        for e in self.eng:
            kn = self.known[e]
            for s_, v in tags:
                if s_ == self.sem[e]:
                    continue
                if kn.get(s_, 0) >= v:
                    continue
                self.prog[e].append(("w", s_, v))
                kn[s_] = v

    def sb(self, name, shape, dt):
        self.uid += 1
        return Buf(name, self._stk().enter_context(self.nc.sbuf_tensor("%s_%d" % (name, self.uid), list(shape), dt)))

    def ps(self, name, shape=(128, 512), dt=F32):
        self.uid += 1
        return Buf(name, self._stk().enter_context(self.nc.psum_tensor("%s_%d" % (name, self.uid), list(shape), dt)))

    def dram(self, name, shape, dt, kind="Internal"):
        b = Buf(name, self.nc.dram_tensor(name, list(shape), dt, kind=kind).ap())
        if kind == "Internal":
            b.nodep = True
        if kind == "ExternalOutput":
            self.outs.append(b)
            b.nodep = True
        return b

    def _deps(self, e, reads, writes):
        need = {}

        def add(tag):
            if tag is None:
                return
            s, v = tag
            if need.get(s, 0) < v:
                need[s] = v

        for b in reads:
            add(b.lw)
        for b in writes:
            if b.nodep:
                continue
            add(b.lw)
            for s, v in b.rd.items():
                add((s, v))
        k = self.known[e]
        own = self.sem[e]
        for s, v in need.items():
            if s == own and (e == "pe" or not self.ses):
                continue
            if k.get(s, 0) >= v:
                continue
            self.prog[e].append(("w", s, v))
            k[s] = v

    def op(self, e, fn, reads=(), writes=()):
        self._deps(e, reads, writes)
        self.cnt[e] += 1
        self.prog[e].append(("i", fn, self.sem[e], 1))
        tag = (self.sem[e], self.cnt[e])
        for b in writes:
            b.lw = tag
            b.rd = {}
        for b in reads:
            if b not in writes:
                b.rd[tag[0]] = tag[1]

    def dma(self, q, out_buf, out_ap, in_buf, in_ap, **kw):
        self._deps(q, [in_buf], [out_buf])
        if out_buf.dsem is None:
            self.nsem += 1
            out_buf.dsem = self.es.enter_context(self.nc.semaphore("d%d" % self.nsem))
            self.dbufs.append(out_buf)
        self.prog[q].append(("i", (lambda e, o=out_ap, i=in_ap, kw=kw: e.dma_start(out=o, in_=i, **kw)),
                             out_buf.dsem, 16))
        out_buf.dcnt += 16
        tag = (out_buf.dsem, out_buf.dcnt)
        out_buf.lw = tag
        out_buf.rd = {}
        in_buf.rd[tag[0]] = tag[1]

    def idma(self, out_buf, out_ap, in_buf, in_ap, idx_buf, idx_ap, scatter, bound):
        self._deps("pool", [in_buf, idx_buf], [out_buf])
        if out_buf.dsem is None:
            self.nsem += 1
            out_buf.dsem = self.es.enter_context(self.nc.semaphore("d%d" % self.nsem))
            self.dbufs.append(out_buf)
        off = bass.IndirectOffsetOnAxis(idx_ap, 0)
        if scatter:
            fn = lambda e: e.indirect_dma_start(out=out_ap, out_offset=off, in_=in_ap, in_offset=None)
        else:
            fn = lambda e: e.indirect_dma_start(out=out_ap, out_offset=None, in_=in_ap, in_offset=off)
        self.prog["pool"].append(("i", fn, out_buf.dsem, 16))
        out_buf.dcnt += 16
        tag = (out_buf.dsem, out_buf.dcnt)
        out_buf.lw = tag
        out_buf.rd = {}
        in_buf.rd[tag[0]] = tag[1]
        idx_buf.rd[tag[0]] = tag[1]

    def tr(self, out_b, out_ap, in_b, in_ap, id_b, id_ap):
        self.op("pe", lambda g: g.transpose(out_ap, in_ap, id_ap), [in_b, id_b], [out_b])

    def finish(self):
        for b in self.outs:
            if b.lw is not None:
                self.prog["sp"].append(("w", b.lw[0], b.lw[1]))
        for e in ("pe", "dve", "act", "pool"):
            if self.cnt[e]:
                self.prog["sp"].append(("w", self.sem[e], self.cnt[e]))
        prog = self.prog

        def replay(e):
            def body(eng):
                for it in prog[e]:
                    if it[0] == "w":
                        eng.wait_ge(it[1], it[2])
                    else:
                        it[1](eng).then_inc(it[2], it[3])
            return body

        with self.nc.Block() as block:
            block.sync(replay("sp"))
            block.tensor(replay("pe"))
            block.vector(replay("dve"))
            block.scalar(replay("act"))
            block.gpsimd(replay("pool"))
        self.es.close()
        return self.nc

    def tt(self, e, out_b, out_ap, a_b, a_ap, b_b, b_ap, op):
        self.op(e, lambda g: g.tensor_tensor(out=out_ap, in0=a_ap, in1=b_ap, op=op), [a_b, b_b], [out_b])

    def ts(self, e, out_b, out_ap, a_b, a_ap, s1, op0, s2=None, op1=None, extra_reads=()):
        if op1 is None:
            self.op(e, lambda g: g.tensor_scalar(out=out_ap, in0=a_ap, scalar1=s1, scalar2=None, op0=op0),
                    [a_b] + list(extra_reads), [out_b])
        else:
            self.op(e, lambda g: g.tensor_scalar(out=out_ap, in0=a_ap, scalar1=s1, scalar2=s2, op0=op0, op1=op1),
                    [a_b] + list(extra_reads), [out_b])

    def stt(self, e, out_b, out_ap, a_b, a_ap, scalar, b_b, b_ap, op0, op1, extra_reads=()):
        self.op(e, lambda g: g.scalar_tensor_tensor(out=out_ap, in0=a_ap, scalar=scalar, in1=b_ap, op0=op0, op1=op1),
                [a_b, b_b] + list(extra_reads), [out_b])

    def act(self, out_b, out_ap, in_b, in_ap, func, scale=1.0, bias=0.0, extra_reads=()):
        self.op("act", lambda g: g.activation(out=out_ap, in_=in_ap, func=func, scale=scale, bias=bias),
                [in_b] + list(extra_reads), [out_b])

    def mm(self, out_b, out_ap, l_b, l_ap, r_b, r_ap, start, stop):
        self.op("pe", lambda g: g.matmul(out_ap, l_ap, r_ap, start=start, stop=stop), [l_b, r_b], [out_b])


def build_uproj():
    k = KB()
    xT = k.dram("xT", [D, TPC], F32, "ExternalInput")
    w = k.dram("w", [D, D], F32, "ExternalInput")
    uT = k.dram("uT", [D, TPC], F32, "ExternalOutput")
    wsb = k.sb("w", [128, 8, D], BF16)
    k.dma("pool", wsb, wsb[:, :, :], w, w.t.rearrange("(kt p) m -> p kt m", p=128))
    xs = [k.sb("xs", [128, 8, 512], BF16) for _ in range(2)]
    pbs = [k.ps("pb") for _ in range(4)]
    sts = [k.sb("st", [128, 512], F32) for _ in range(4)]
    i = 0
    for nb in range(TPC // 512):
        xb = xs[nb % 2]
        k.dma("pool", xb, xb[:, :, :], xT, xT.t[:, nb * 512:(nb + 1) * 512].rearrange("(kt p) n -> p kt n", p=128))
        for m in range(8):
            pb = pbs[i % 4]
            st = sts[i % 4]
            for kt in range(8):
                k.mm(pb, pb[:, :], wsb, wsb[:, kt, m * 128:(m + 1) * 128], xb, xb[:, kt, :], kt == 0, kt == 7)
            if i % 2 == 0:
                k.act(st, st[:, :], pb, pb[:, :], AF.Copy)
            else:
                k.op("dve", lambda g, o=st[:, :], a=pb[:, :]: g.tensor_copy(out=o, in_=a), [pb], [st])
            k.dma("sp", uT, uT.t[m * 128:(m + 1) * 128, nb * 512:(nb + 1) * 512], st, st[:, :])
            i += 1
    return k.finish()


S5_BLK = 512


def build_s5(ntok=NTOK, seq=SEQ):
    k = KB()
    u = k.dram("u", [128, ntok], F32, "ExternalInput")
    are = k.dram("are", [128, 4], F32, "ExternalInput")
    aim = k.dram("aim", [128, 4], F32, "ExternalInput")
    ldt = k.dram("ldt", [128, 4], F32, "ExternalInput")
    bre = k.dram("bre", [128, 4, 128], F32, "ExternalInput")
    bim = k.dram("bim", [128, 4, 128], F32, "ExternalInput")
    cre = k.dram("cre", [128, 4, 32], F32, "ExternalInput")
    cim = k.dram("cim", [128, 4, 32], F32, "ExternalInput")
    dsk = k.dram("dsk", [128, 1], F32, "ExternalInput")
    y = k.dram("y", [128, ntok], F32, "ExternalOutput")
    L = S5_BLK

    def small(name, w=4, dt=F32):
        return k.sb(name, [128, w], dt)

    s_are, s_aim, s_ldt = small("are"), small("aim"), small("ldt")
    k.dma("sp", s_are, s_are[:, :], are, are.t[:, :])
    k.dma("sp", s_aim, s_aim[:, :], aim, aim.t[:, :])
    k.dma("sp", s_ldt, s_ldt[:, :], ldt, ldt.t[:, :])
    s_d = small("dsk", 1)
    k.dma("sp", s_d, s_d[:, :], dsk, dsk.t[:, :])
    b_re = k.sb("bre", [128, 4, 128], BF16)
    b_im = k.sb("bim", [128, 4, 128], BF16)
    k.dma("pool", b_re, b_re[:, :, :], bre, bre.t[:, :, :])
    k.dma("pool", b_im, b_im[:, :, :], bim, bim.t[:, :, :])
    c_re = k.sb("cre", [128, 4, 32], F32)
    c_im = k.sb("cim", [128, 4, 32], F32)
    k.dma("sp", c_re, c_re[:, :, :], cre, cre.t[:, :, :])
    k.dma("sp", c_im, c_im[:, :, :], cim, cim.t[:, :, :])

    k.ses = True
    dt_ = small("dt")
    k.act(dt_, dt_[:, :], s_ldt, s_ldt[:, :], AF.Exp)
    lre = small("lre")
    k.ts("dve", lre, lre[:, :], s_are, s_are[:, :], -1e-4, ALU.min)
    lrd = small("lrd")
    k.tt("dve", lrd, lrd[:, :], lre, lre[:, :], dt_, dt_[:, :], ALU.mult)
    rho = small("rho")
    k.act(rho, rho[:, :], lrd, lrd[:, :], AF.Exp)
    th = small("th")
    k.tt("dve", th, th[:, :], s_aim, s_aim[:, :], dt_, dt_[:, :], ALU.mult)

    def sin_of(src, shift, name):
        a = small(name + "a")
        k.ts("dve", a, a[:, :], src, src[:, :], float(shift), ALU.add)
        t = small(name + "t")
        k.ts("dve", t, t[:, :], a, a[:, :], 1.0 / (2 * PI), ALU.mult)
        ti = small(name + "ti", dt=I32)
        k.op("dve", lambda g: g.tensor_copy(out=ti[:, :], in_=t[:, :]), [t], [ti])
        tf = small(name + "tf")
        k.op("dve", lambda g: g.tensor_copy(out=tf[:, :], in_=ti[:, :]), [ti], [tf])
        r = small(name + "r")
        k.stt("dve", r, r[:, :], tf, tf[:, :], -2 * PI, a, a[:, :], ALU.mult, ALU.add)
        m1 = small(name + "m1")
        k.ts("dve", m1, m1[:, :], r, r[:, :], PI, ALU.is_gt)
        r2 = small(name + "r2")
        k.stt("dve", r2, r2[:, :], m1, m1[:, :], -2 * PI, r, r[:, :], ALU.mult, ALU.add)
        m2 = small(name + "m2")
        k.ts("dve", m2, m2[:, :], r2, r2[:, :], -PI, ALU.is_lt)
        r3 = small(name + "r3")
        k.stt("dve", r3, r3[:, :], m2, m2[:, :], 2 * PI, r2, r2[:, :], ALU.mult, ALU.add)
        r4 = small(name + "r4")
        k.ts("dve", r4, r4[:, :], r3, r3[:, :], PI, ALU.min, -PI, ALU.max)
        o = small(name + "o")
        k.act(o, o[:, :], r4, r4[:, :], AF.Sin)
        return o

    sn = sin_of(th, 0.0, "sn")
    cs = sin_of(th, PI / 2, "cs")
    abr, abi = small("abr"), small("abi")
    k.tt("dve", abr, abr[:, :], rho, rho[:, :], cs, cs[:, :], ALU.mult)
    k.tt("dve", abi, abi[:, :], rho, rho[:, :], sn, sn[:, :], ALU.mult)
    den, t0 = small("den"), small("t0")
    k.tt("dve", den, den[:, :], lre, lre[:, :], lre, lre[:, :], ALU.mult)
    k.tt("dve", t0, t0[:, :], s_aim, s_aim[:, :], s_aim, s_aim[:, :], ALU.mult)
    den2 = small("den2")
    k.tt("dve", den2, den2[:, :], den, den[:, :], t0, t0[:, :], ALU.add)
    rden = small("rden")
    k.op("dve", lambda g: g.reciprocal(out=rden[:, :], in_=den2[:, :]), [den2], [rden])
    nr = small("nr")
    k.ts("dve", nr, nr[:, :], abr, abr[:, :], -1.0, ALU.add)
    t1, t2, t3, t4 = small("t1"), small("t2"), small("t3"), small("t4")
    k.tt("dve", t1, t1[:, :], nr, nr[:, :], lre, lre[:, :], ALU.mult)
    k.tt("dve", t2, t2[:, :], abi, abi[:, :], s_aim, s_aim[:, :], ALU.mult)
    k.tt("dve", t3, t3[:, :], abi, abi[:, :], lre, lre[:, :], ALU.mult)
    k.tt("dve", t4, t4[:, :], nr, nr[:, :], s_aim, s_aim[:, :], ALU.mult)
    q1, q2 = small("q1"), small("q2")
    k.tt("dve", q1, q1[:, :], t1, t1[:, :], t2, t2[:, :], ALU.add)
    k.tt("dve", q2, q2[:, :], t3, t3[:, :], t4, t4[:, :], ALU.subtract)
    qre, qim = small("qre"), small("qim")
    k.tt("dve", qre, qre[:, :], q1, q1[:, :], rden, rden[:, :], ALU.mult)
    k.tt("dve", qim, qim[:, :], q2, q2[:, :], rden, rden[:, :], ALU.mult)
    nqim = small("nqim")
    k.ts("dve", nqim, nqim[:, :], qim, qim[:, :], -1.0, ALU.mult)
    nqre = small("nqre")
    k.ts("dve", nqre, nqre[:, :], qre, qre[:, :], -1.0, ALU.mult)

    cp_re = k.sb("cpre", [128, 4, 128], BF16)
    cp_imn = k.sb("cpimn", [128, 4, 128], BF16)
    k.op("pool", lambda g: g.memset(cp_re[:, :, :], 0.0), [], [cp_re])
    k.op("pool", lambda g: g.memset(cp_imn[:, :, :], 0.0), [], [cp_imn])
    ctmp = k.sb("ctmp", [128, 4, 32], F32)
    for qq in range(4):
        k.ts("dve", ctmp, ctmp[:, qq, :], c_im, c_im[:, qq, :], nqim[:, qq:qq + 1], ALU.mult, extra_reads=[nqim])
        k.stt("dve", cp_re, cp_re[:, qq, 32 * qq:32 * qq + 32], c_re, c_re[:, qq, :], qre[:, qq:qq + 1], ctmp, ctmp[:, qq, :],
              ALU.mult, ALU.add, extra_reads=[qre])
    ctmp2 = k.sb("ctmp2", [128, 4, 32], F32)
    for qq in range(4):
        k.ts("dve", ctmp2, ctmp2[:, qq, :], c_im, c_im[:, qq, :], nqre[:, qq:qq + 1], ALU.mult, extra_reads=[nqre])
        k.stt("dve", cp_imn, cp_imn[:, qq, 32 * qq:32 * qq + 32], c_re, c_re[:, qq, :], nqim[:, qq:qq + 1], ctmp2, ctmp2[:, qq, :],
              ALU.mult, ALU.add, extra_reads=[nqim])

    COS = k.sb("COS", [128, 4, L], F32)
    SIN = k.sb("SIN", [128, 4, L], F32)
    RHO = k.sb("RHO", [128, 4, L], F32)
    k.op("pool", lambda g: g.memset(COS[:, :, :], 1.0), [], [COS])
    k.op("pool", lambda g: g.memset(SIN[:, :, :], 0.0), [], [SIN])
    k.op("pool", lambda g: g.memset(RHO[:, :, :], 1.0), [], [RHO])
    for qq in range(4):
        k.ts("dve", RHO, RHO[:, qq, :], RHO, RHO[:, qq, :], rho[:, qq:qq + 1], ALU.mult, extra_reads=[rho])
    pr, pi_ = cs, sn
    ttmp = k.sb("ttmp", [128, 4, L // 2], F32)
    w = 1
    lvl = 0
    while w < L:
        npi = small("npi%d" % lvl)
        k.ts("dve", npi, npi[:, :], pi_, pi_[:, :], -1.0, ALU.mult)
        for qq in range(4):
            k.ts("dve", ttmp, ttmp[:, qq, 0:w], SIN, SIN[:, qq, 0:w], npi[:, qq:qq + 1], ALU.mult, extra_reads=[npi])
            k.stt("dve", COS, COS[:, qq, w:2 * w], COS, COS[:, qq, 0:w], pr[:, qq:qq + 1], ttmp, ttmp[:, qq, 0:w],
                  ALU.mult, ALU.add, extra_reads=[pr])
            k.ts("dve", ttmp, ttmp[:, qq, 0:w], SIN, SIN[:, qq, 0:w], pr[:, qq:qq + 1], ALU.mult, extra_reads=[pr])
            k.stt("dve", SIN, SIN[:, qq, w:2 * w], COS, COS[:, qq, 0:w], pi_[:, qq:qq + 1], ttmp, ttmp[:, qq, 0:w],
                  ALU.mult, ALU.add, extra_reads=[pi_])
        a2, b2, ab_ = small("a2_%d" % lvl), small("b2_%d" % lvl), small("ab_%d" % lvl)
        k.tt("dve", a2, a2[:, :], pr, pr[:, :], pr, pr[:, :], ALU.mult)
        k.tt("dve", b2, b2[:, :], pi_, pi_[:, :], pi_, pi_[:, :], ALU.mult)
        k.tt("dve", ab_, ab_[:, :], pr, pr[:, :], pi_, pi_[:, :], ALU.mult)
        npr, npi2 = small("npr%d" % lvl), small("npi2%d" % lvl)
        k.tt("dve", npr, npr[:, :], a2, a2[:, :], b2, b2[:, :], ALU.subtract)
        k.ts("dve", npi2, npi2[:, :], ab_, ab_[:, :], 2.0, ALU.mult)
        pr, pi_ = npr, npi2
        w *= 2
        lvl += 1
    ELr, ELi = pr, pi_
    nELi = small("nELi")
    k.ts("dve", nELi, nELi[:, :], ELi, ELi[:, :], -1.0, ALU.mult)

    k.ses = SES
    NP = 3
    ub = [k.sb("ub", [128, L], F32) for _ in range(2)]
    ubf = [k.sb("ubf", [128, L], BF16) for _ in range(2)]
    ps_re = [k.ps("psre") for _ in range(NP)]
    ps_im = [k.ps("psim") for _ in range(NP)]
    ps_y = [k.ps("psy") for _ in range(2)]
    T = [[k.sb("T%d" % j, [128, L], F32) for j in range(4)] for _ in range(NP)]
    gin = [[k.sb("gin%d" % j, [128, L], F32) for j in range(2)] for _ in range(NP)]
    gg = [[[k.sb("g%d_%d" % (qq, j), [128, L], F32) for j in range(2)] for qq in range(4)] for _ in range(2)]
    T2 = [[k.sb("U%d" % j, [128, L], F32) for j in range(4)] for _ in range(NP)]
    hh = [[k.sb("h%d" % j, [128, L], BF16) for j in range(2)] for _ in range(NP)]
    ini = [[k.sb("ini%d_%d" % (qq, j), [128, 1], F32) for j in range(2)] for qq in range(4)]
    itmp = [k.sb("itmp%d" % qq, [128, 1], F32) for qq in range(4)]
    yo = [k.sb("yo", [128, L], F32) for _ in range(2)]
    nblk = ntok // L
    bps = seq // L
    it = 0
    for blk in range(nblk):
        sl = slice(blk * L, (blk + 1) * L)
        uB, uF, pY = ub[blk % 2], ubf[blk % 2], ps_y[blk % 2]
        k.dma("sp", uB, uB[:, :], u, u.t[:, sl])
        k.act(uF, uF[:, :], uB, uB[:, :], AF.Copy)
        first = (blk % bps == 0)
        for qq in range(4):
            par = it % NP
            it += 1
            pr_, pi2_ = ps_re[par], ps_im[par]
            k.mm(pr_, pr_[:, :], b_re, b_re[:, qq, :], uF, uF[:, :], True, True)
            k.mm(pi2_, pi2_[:, :], b_im, b_im[:, qq, :], uF, uF[:, :], True, True)
            t = T[par]
            cq, sq, rq = COS[:, qq, :], SIN[:, qq, :], RHO[:, qq, :]
            k.tt("dve", t[0], t[0][:, :], pr_, pr_[:, :], COS, cq, ALU.mult)
            k.tt("dve", t[1], t[1][:, :], pi2_, pi2_[:, :], SIN, sq, ALU.mult)
            k.tt("dve", t[2], t[2][:, :], pi2_, pi2_[:, :], COS, cq, ALU.mult)
            k.tt("dve", t[3], t[3][:, :], pr_, pr_[:, :], SIN, sq, ALU.mult)
            gi = gin[par]
            k.tt("pool", gi[0], gi[0][:, :], t[0], t[0][:, :], t[1], t[1][:, :], ALU.add)
            k.tt("dve", gi[1], gi[1][:, :], t[2], t[2][:, :], t[3], t[3][:, :], ALU.subtract)
            g_prev = gg[(blk + 1) % 2][qq]
            g_cur = gg[blk % 2][qq]
            if first:
                i_re, i_im = 0.0, 0.0
                xr = []
            else:
                k.ts("pool", itmp[qq], itmp[qq][:, :], g_prev[1], g_prev[1][:, L - 1:L], nELi[:, qq:qq + 1], ALU.mult,
                     extra_reads=[nELi])
                k.stt("dve", ini[qq][0], ini[qq][0][:, :], g_prev[0], g_prev[0][:, L - 1:L], ELr[:, qq:qq + 1],
                      itmp[qq], itmp[qq][:, :], ALU.mult, ALU.add, extra_reads=[ELr])
                k.ts("pool", itmp[qq], itmp[qq][:, :], g_prev[1], g_prev[1][:, L - 1:L], ELr[:, qq:qq + 1], ALU.mult,
                     extra_reads=[ELr])
                k.stt("dve", ini[qq][1], ini[qq][1][:, :], g_prev[0], g_prev[0][:, L - 1:L], ELi[:, qq:qq + 1],
                      itmp[qq], itmp[qq][:, :], ALU.mult, ALU.add, extra_reads=[ELi])
                i_re, i_im = ini[qq][0][:, :], ini[qq][1][:, :]
                xr = [ini[qq][0], ini[qq][1]]
            k.op("dve", lambda g, o=g_cur[0][:, :], a=rq, b=gi[0][:, :], i0=i_re: g.tensor_tensor_scan(
                out=o, data0=a, data1=b, initial=i0, op0=ALU.mult, op1=ALU.add), [RHO, gi[0]] + xr[:1], [g_cur[0]])
            k.op("dve", lambda g, o=g_cur[1][:, :], a=rq, b=gi[1][:, :], i0=i_im: g.tensor_tensor_scan(
                out=o, data0=a, data1=b, initial=i0, op0=ALU.mult, op1=ALU.add), [RHO, gi[1]] + xr[1:], [g_cur[1]])
            t2_ = T2[par]
            k.tt("pool", t2_[0], t2_[0][:, :], g_cur[0], g_cur[0][:, :], COS, cq, ALU.mult)
            k.tt("dve", t2_[1], t2_[1][:, :], g_cur[1], g_cur[1][:, :], SIN, sq, ALU.mult)
            k.tt("pool", t2_[2], t2_[2][:, :], g_cur[1], g_cur[1][:, :], COS, cq, ALU.mult)
            k.tt("dve", t2_[3], t2_[3][:, :], g_cur[0], g_cur[0][:, :], SIN, sq, ALU.mult)
            h = hh[par]
            k.tt("dve", h[0], h[0][:, :], t2_[0], t2_[0][:, :], t2_[1], t2_[1][:, :], ALU.subtract)
            k.tt("dve", h[1], h[1][:, :], t2_[2], t2_[2][:, :], t2_[3], t2_[3][:, :], ALU.add)
            k.mm(pY, pY[:, :], cp_re, cp_re[:, qq, :], h[0], h[0][:, :], qq == 0, False)
            k.mm(pY, pY[:, :], cp_imn, cp_imn[:, qq, :], h[1], h[1][:, :], False, qq == 3)
        yB = yo[blk % 2]
        k.stt("dve", yB, yB[:, :], uB, uB[:, :], s_d[:, 0:1], pY, pY[:, :], ALU.mult, ALU.add, extra_reads=[s_d])
        k.dma("sp", y, y.t[:, sl], yB, yB[:, :])
    return k.finish()


def s5_core_params(c, a_re, a_im, log_dt, b_re, b_im, c_re, c_im, d):
    g0 = 8 * c

    def pq(a):
        return np.ascontiguousarray(a[g0:g0 + 8].reshape(4, 2, 64).transpose(1, 2, 0).reshape(128, 4)).astype(np.float32)

    are = pq(a_re)
    aim = pq(a_im)
    ldt = pq(np.broadcast_to(log_dt[:, None], (64, 64)))
    B_re = np.zeros((4, 2, 16, 4, 2, 64), np.float32)
    B_im = np.zeros((4, 2, 16, 4, 2, 64), np.float32)
    C_re = np.zeros((2, 64, 4, 2, 16), np.float32)
    C_im = np.zeros((2, 64, 4, 2, 16), np.float32)
    for qq in range(4):
        for gp in range(2):
            g = g0 + 2 * qq + gp
            B_re[qq, gp, :, qq, gp, :] = b_re[g].T
            B_im[qq, gp, :, qq, gp, :] = b_im[g].T
            C_re[gp, :, qq, gp, :] = c_re[g].T
            C_im[gp, :, qq, gp, :] = c_im[g].T
    return {"are": are, "aim": aim, "ldt": ldt,
            "bre": B_re.reshape(128, 4, 128), "bim": B_im.reshape(128, 4, 128),
            "cre": C_re.reshape(128, 4, 32), "cim": C_im.reshape(128, 4, 32),
            "dsk": np.ascontiguousarray(d[128 * c:128 * c + 128].reshape(128, 1)).astype(np.float32)}


class Ctx:
    def __init__(self, k, NT, nkt_half):
        self.k = k
        self.NT = NT
        self.ps1 = [k.ps("ps1") for _ in range(2)]
        self.ps3 = [k.ps("ps3") for _ in range(2)]
        self.pso = [k.ps("pso") for _ in range(2)]
        self.pss = [k.ps("pss") for _ in range(2)]
        self.wa = [k.sb("wa", [128, 8, 512], BF16) for _ in range(2)]
        self.wb = [k.sb("wb", [128, 8, 512], BF16) for _ in range(2)]
        self.w2h = k.sb("w2h", [128, nkt_half, 1024], BF16)
        self.hmid = k.sb("hmid", [128, max(nkt_half, 8), NT], BF16)
        self.z = k.sb("z", [128, 8, NT], F32)
        self.xbf = k.sb("xbf", [128, 8, NT], BF16)
        self.sact = [k.sb("sact", [128, 512], F32) for _ in range(2)]
        self.tmp = [k.sb("tmp", [128, 512], F32) for _ in range(2)]
        self.ones = k.sb("ones", [128, 128], F32)
        k.op("pool", lambda g: g.memset(self.ones[:, :], 1.0), [], [self.ones])
        self.sq = [k.sb("sq", [128, 512], F32) for _ in range(2)]
        self.mean = k.sb("mean", [128, 512], F32)
        self.ex2 = k.sb("ex2", [128, 512], F32)
        self.rstd = k.sb("rstd", [128, 512], F32)
        self.nmr = k.sb("nmr", [128, 512], F32)
        self.lng = k.sb("lng", [128, 32], F32)
        self.lnb = k.sb("lnb", [128, 32], F32)
        self.wi = 0
        self.pi = 0
        self.oi = 0


def load_wblock(k, buf, wd, k0, nk, c0, cw):
    k.dma("pool", buf, buf[:, 0:nk, 0:cw], wd, wd.t[k0 * 128:(k0 + nk) * 128, c0:c0 + cw].rearrange("(kt p) m -> p kt m", p=128))


def glu_phase(k, cx, x_in, wa_d, a_c0, wb_d, b_c0, ncols, func, emit, hook=None, NT=None):
    NT = cx.NT if NT is None else NT
    for c0 in range(0, ncols, 512):
        cw = min(512, ncols - c0)
        wa, wb = cx.wa[cx.wi % 2], cx.wb[cx.wi % 2]
        cx.wi += 1
        load_wblock(k, wa, wa_d, 0, 8, a_c0 + c0, cw)
        load_wblock(k, wb, wb_d, 0, 8, b_c0 + c0, cw)
        if hook is not None and c0 == 512:
            hook()
        for fl in range(cw // 128):
            ft = c0 // 128 + fl
            for t0 in range(0, NT, 512):
                tw = min(512, NT - t0)
                p1, p3 = cx.ps1[cx.pi % 2], cx.ps3[cx.pi % 2]
                sa = cx.sact[cx.pi % 2]
                cx.pi += 1
                ts_ = slice(t0, t0 + tw)
                for kt in range(8):
                    k.mm(p1, p1[:, 0:tw], wa, wa[:, kt, fl * 128:fl * 128 + 128], x_in, x_in[:, kt, ts_], kt == 0, kt == 7)
                for kt in range(8):
                    k.mm(p3, p3[:, 0:tw], wb, wb[:, kt, fl * 128:fl * 128 + 128], x_in, x_in[:, kt, ts_], kt == 0, kt == 7)
                k.act(sa, sa[:, 0:tw], p1, p1[:, 0:tw], func)
                emit(ft, t0, tw, sa, p3)


def down_load(k, cx, w2_d, k0, nk):
    k.dma("pool", cx.w2h, cx.w2h[:, 0:nk, :], w2_d, w2_d.t[k0 * 128:(k0 + nk) * 128, :].rearrange("(kt p) m -> p kt m", p=128))


def down_phase(k, cx, w2_d, k0, nk, zacc):
    NT = cx.NT
    for m in range(8):
        for tb in range(NT // 512):
            po = cx.pso[cx.oi % 2]
            cx.oi += 1
            ts_ = slice(tb * 512, tb * 512 + 512)
            for kt in range(nk):
                k.mm(po, po[:, :], cx.w2h, cx.w2h[:, kt, m * 128:m * 128 + 128], cx.hmid, cx.hmid[:, kt, ts_], kt == 0, kt == nk - 1)
            k.tt("dve", zacc, zacc[:, m, ts_], po, po[:, :], zacc, zacc[:, m, ts_], ALU.add)


def layer_norm(k, cx, z, ln_idx, xbf_out):
    NT = cx.NT
    for tb in range(NT // 512):
        ts_ = slice(tb * 512, tb * 512 + 512)
        s1, s2 = cx.pss[0], cx.pss[1]
        for kt in range(8):
            k.mm(s1, s1[:, :], cx.ones, cx.ones[:, :], z, z[:, kt, ts_], kt == 0, kt == 7)
        for kt in range(8):
            sq = cx.sq[kt % 2]
            k.act(sq, sq[:, :], z, z[:, kt, ts_], AF.Square)
            k.mm(s2, s2[:, :], cx.ones, cx.ones[:, :], sq, sq[:, :], kt == 0, kt == 7)
        k.act(cx.mean, cx.mean[:, :], s1, s1[:, :], AF.Copy, scale=1.0 / D)
        k.act(cx.ex2, cx.ex2[:, :], s2, s2[:, :], AF.Copy, scale=1.0 / D)
        k.tt("dve", cx.nmr, cx.nmr[:, :], cx.mean, cx.mean[:, :], cx.mean, cx.mean[:, :], ALU.mult)
        k.tt("dve", cx.ex2, cx.ex2[:, :], cx.ex2, cx.ex2[:, :], cx.nmr, cx.nmr[:, :], ALU.subtract)
        k.ts("dve", cx.ex2, cx.ex2[:, :], cx.ex2, cx.ex2[:, :], LN_EPS, ALU.add)
        k.act(cx.ex2, cx.ex2[:, :], cx.ex2, cx.ex2[:, :], AF.Sqrt)
        k.op("dve", lambda g, o_=cx.rstd[:, :], i_=cx.ex2[:, :]: g.reciprocal(out=o_, in_=i_), [cx.ex2], [cx.rstd])
        k.stt("dve", cx.nmr, cx.nmr[:, :], cx.mean, cx.mean[:, :], -1.0, cx.rstd, cx.rstd[:, :], ALU.mult, ALU.mult)
        for kt in range(8):
            t = cx.tmp[kt % 2]
            k.tt("dve", t, t[:, :], z, z[:, kt, ts_], cx.rstd, cx.rstd[:, :], ALU.mult)
            k.tt("dve", t, t[:, :], t, t[:, :], cx.nmr, cx.nmr[:, :], ALU.add)
            col = ln_idx * 8 + kt
            k.op("act", lambda g, o=z[:, kt, ts_], i=t[:, :], sc=cx.lng[:, col:col + 1], bi=cx.lnb[:, col:col + 1]: g.activation(
                out=o, in_=i, func=AF.Identity, scale=sc, bias=bi),
                [t, cx.lng, cx.lnb], [z])
            if xbf_out is not None:
                k.act(xbf_out, xbf_out[:, kt, ts_], z, z[:, kt, ts_], AF.Copy)


def ln_layout(ln_g, ln_b):
    g = np.ascontiguousarray(ln_g.reshape(4, 8, 128).transpose(2, 0, 1).reshape(128, 32)).astype(np.float32)
    b = np.ascontiguousarray(ln_b.reshape(4, 8, 128).transpose(2, 0, 1).reshape(128, 32)).astype(np.float32)
    return g, b


DFF = 2816


def build_layer0_rest(tpc=TPC, NT=1024):
    k = KB()
    yT = k.dram("yT", [D, tpc], F32, "ExternalInput")
    xT = k.dram("xT", [D, tpc], F32, "ExternalInput")
    wglu = k.dram("wglu", [D, 2 * D], F32, "ExternalInput")
    w1 = k.dram("w1", [D, DFF], F32, "ExternalInput")
    w3 = k.dram("w3", [D, DFF], F32, "ExternalInput")
    w2 = k.dram("w2", [DFF, D], F32, "ExternalInput")
    lng = k.dram("lng", [128, 32], F32, "ExternalInput")
    lnb = k.dram("lnb", [128, 32], F32, "ExternalInput")
    oT = k.dram("oT", [D, tpc], F32, "ExternalOutput")
    cx = Ctx(k, NT, 11)
    k.dma("sp", cx.lng, cx.lng[:, :], lng, lng.t[:, :])
    k.dma("sp", cx.lnb, cx.lnb[:, :], lnb, lnb.t[:, :])
    ystage = [k.sb("ystage", [128, NT], F32) for _ in range(2)]
    gbf = cx.hmid
    z = cx.z
    for sb_ in range(tpc // NT):
        tsl = slice(sb_ * NT, (sb_ + 1) * NT)
        for kt in range(8):
            ys = ystage[kt % 2]
            k.dma("sp", ys, ys[:, :], yT, yT.t[kt * 128:(kt + 1) * 128, tsl])
            k.act(gbf, gbf[:, kt, :], ys, ys[:, :], AF.Gelu)
            k.dma("sp", z, z[:, kt, :], xT, xT.t[kt * 128:(kt + 1) * 128, tsl])

        def emit_glu(ft, t0, tw, sa, pB):
            t = cx.tmp[(ft + t0 // 512) % 2]
            ts_ = slice(t0, t0 + 512)
            k.tt("dve", t, t[:, :], pB, pB[:, :], sa, sa[:, :], ALU.mult)
            k.stt("dve", z, z[:, ft, ts_], z, z[:, ft, ts_], float(ALPHA), t, t[:, :], ALU.mult, ALU.add)

        glu_phase(k, cx, gbf, wglu, D, wglu, 0, D, AF.Sigmoid, emit_glu)
        layer_norm(k, cx, z, 0, cx.xbf)
        for kt in range(8):
            k.ts("dve", z, z[:, kt, :], z, z[:, kt, :], float(ALPHA), ALU.mult)
        for half in range(2):
            f0 = half * 11

            def emit_ffn(ft, t0, tw, sa, pB):
                ts_ = slice(t0, t0 + 512)
                k.tt("dve", cx.hmid, cx.hmid[:, ft, ts_], pB, pB[:, :], sa, sa[:, :], ALU.mult)

            glu_phase(k, cx, cx.xbf, w1, f0 * 128, w3, f0 * 128, 11 * 128, AF.Silu, emit_ffn,
                      hook=lambda f0=f0: down_load(k, cx, w2, f0, 11))
            down_phase(k, cx, w2, f0, 11, z)
        layer_norm(k, cx, z, 1, None)
        for kt in range(8):
            k.dma("sp", oT, oT.t[kt * 128:(kt + 1) * 128, tsl], z, z[:, kt, :])
    return k.finish()


NQKV = 9216
DILS = (1, 4, 16)


def build_qkv(tpc=TPC):
    k = KB()
    xT = k.dram("xT", [D, tpc], F32, "ExternalInput")
    w = k.dram("w", [D, NQKV], F32, "ExternalInput")
    o = k.dram("qkvT", [NQKV, tpc], BF16, "ExternalOutput")
    xb = k.sb("xb", [128, 8, tpc], BF16)
    for kt in range(8):
        k.dma("pool", xb, xb[:, kt, :], xT, xT.t[kt * 128:(kt + 1) * 128, :])
    wbuf = [k.sb("wq", [128, 8, 512], BF16) for _ in range(2)]
    pbs = [k.ps("pb") for _ in range(4)]
    sts = [k.sb("st", [128, 512], BF16) for _ in range(4)]
    i = 0
    for cb in range(NQKV // 512):
        wb = wbuf[cb % 2]
        load_wblock(k, wb, w, 0, 8, cb * 512, 512)
        for fl in range(4):
            for tb in range(tpc // 512):
                pb, st = pbs[i % 4], sts[i % 4]
                ts_ = slice(tb * 512, tb * 512 + 512)
                for kt in range(8):
                    k.mm(pb, pb[:, :], wb, wb[:, kt, fl * 128:fl * 128 + 128], xb, xb[:, kt, ts_], kt == 0, kt == 7)
                if i % 2 == 0:
                    k.act(st, st[:, :], pb, pb[:, :], AF.Copy)
                else:
                    k.op("dve", lambda g, o_=st[:, :], a=pb[:, :]: g.tensor_copy(out=o_, in_=a), [pb], [st])
                r0 = cb * 512 + fl * 128
                k.dma("sp", o, o.t[r0:r0 + 128, ts_], st, st[:, :])
                i += 1
    return k.finish()


ACH = 2048
NEG = -1e30


def build_attn(tpc=TPC, LA=3):
    k = KB()
    nblk = tpc // 128
    qb_d = k.dram("qb", [3, 8, 128, nblk * 128], BF16, "ExternalInput")
    kb_d = k.dram("kb", [3, 8, 128, (nblk + 16) * 128], BF16, "ExternalInput")
    vb_d = k.dram("vb", [3, 8, 128, (nblk + 16) * 128], BF16, "ExternalInput")
    bias_d = k.dram("biasT", [3, 8, 128, 512], F32, "ExternalInput")
    negb_d = k.dram("negb", [128, 1], F32, "ExternalInput")
    o = k.dram("attnT", [D, tpc], F32, "ExternalOutput")
    negb = k.sb("negb", [128, 1], F32)
    k.dma("sp", negb, negb[:, :], negb_d, negb_d.t[:, :])
    ones = k.sb("ones", [128, 64], BF16)
    k.op("pool", lambda g: g.memset(ones[:, :], 1.0), [], [ones])
    Qb = [k.sb("Qb", [128, 16 * 128], BF16) for _ in range(2)]
    Kb = [k.sb("Kb", [128, 32 * 128], BF16) for _ in range(2)]
    Vb = [k.sb("Vb", [128, 32, 128], BF16) for _ in range(2)]
    Bs = [k.sb("Bs", [128, 2, 256], F32) for _ in range(2)]
    num = [k.sb("num", [128, ACH], F32) for _ in range(2)]
    den = [k.sb("den", [128, ACH], F32) for _ in range(2)]
    rec = k.sb("rec", [128, ACH], F32)
    NS = LA + 1
    ps_s = [k.ps("ps_s", (128, 256)) for _ in range(NS)]
    ps_o = [k.ps("ps_o") for _ in range(2)]
    ps_d = [k.ps("ps_d") for _ in range(2)]
    tmp = [k.sb("tmpS", [128, 256], F32) for _ in range(NS + 1)]
    pT = [k.sb("pT", [128, 256], BF16) for _ in range(NS + 1)]
    units = []
    li = 0
    ei = 0
    ci = 0
    for ch in range(tpc // ACH):
        for ft in range(8):
            nm, dn = num[ci % 2], den[ci % 2]
            ci += 1
            for g in range(3):
                d = DILS[g]
                bufs = (Qb[li % 2], Kb[li % 2], Vb[li % 2], Bs[li % 2])
                li += 1
                for B in range(16):
                    for hh in range(2):
                        units.append(dict(ch=ch, ft=ft, g=g, d=d, B=B, hh=hh, bufs=bufs, nm=nm, dn=dn,
                                          po=ps_o[ei % 2], pd=ps_d[ei % 2], load=(B == 0 and hh == 0),
                                          evac=(B % 4 == 3 and hh == 1), last=(g == 2 and B == 15 and hh == 1)))
                    if B % 4 == 3:
                        ei += 1

    def s_part(ui, un):
        qb, kb, vb, bs = un["bufs"]
        ch, ft, g, d, B, hh = un["ch"], un["ft"], un["g"], un["d"], un["B"], un["hh"]
        if un["load"]:
            b0 = ch * 16
            k.dma("sp", qb, qb[:, :], qb_d, qb_d.t[g, ft, :, b0 * 128:(b0 + 16) * 128])
            nkb = 16 + d
            k0 = 16 + b0 - d
            k.dma("sp", kb, kb[:, 0:nkb * 128], kb_d, kb_d.t[g, ft, :, k0 * 128:(k0 + nkb) * 128])
            k.dma("sp", vb, vb[:, 0:nkb, :], vb_d,
                  vb_d.t[g, ft, :, k0 * 128:(k0 + nkb) * 128].rearrange("p (b f) -> p b f", f=128))
            k.dma("sp", bs, bs[:, :, :], bias_d, bias_d.t[g, ft, :, :].rearrange("p (h f) -> p h f", h=2))
        halo = (ch == 0 and B < d)
        rs = slice(64 * hh, 64 * hh + 64)
        pss = ps_s[ui % NS]
        tm, pt = tmp[ui % (NS + 1)], pT[ui % (NS + 1)]
        qs = slice(B * 128, B * 128 + 128)
        kp = slice(B * 128, B * 128 + 128)
        kc = slice((B + d) * 128, (B + d) * 128 + 128)
        k.mm(pss, pss[:, 0:128], kb, kb[rs, kp], qb, qb[rs, qs], True, True)
        k.mm(pss, pss[:, 128:256], kb, kb[rs, kc], qb, qb[rs, qs], True, True)
        k.stt("dve", tm, tm[:, :], pss, pss[:, 0:256], 0.125, bs, bs[:, hh, :], ALU.mult, ALU.add)
        if halo:
            k.op("act", lambda e, o_=pt[:, 0:128], i_=tm[:, 0:128]: e.activation(
                out=o_, in_=i_, func=AF.Exp, bias=negb[:, 0:1], scale=1.0), [tm, negb], [pt])
            k.act(pt, pt[:, 128:256], tm, tm[:, 128:256], AF.Exp)
        else:
            k.act(pt, pt[:, :], tm, tm[:, :], AF.Exp)

    def pv_part(ui, un):
        qb, kb, vb, bs = un["bufs"]
        ch, ft, g, d, B, hh = un["ch"], un["ft"], un["g"], un["d"], un["B"], un["hh"]
        po, pd, nm, dn = un["po"], un["pd"], un["nm"], un["dn"]
        rs = slice(64 * hh, 64 * hh + 64)
        pt = pT[ui % (NS + 1)]
        cs = slice((B % 4) * 128, (B % 4) * 128 + 128)
        k.mm(po, po[rs, cs], vb, vb[:, B, rs], pt, pt[:, 0:128], True, False)
        k.mm(po, po[rs, cs], vb, vb[:, B + d, rs], pt, pt[:, 128:256], False, True)
        k.mm(pd, pd[rs, cs], ones, ones[:, :], pt, pt[:, 0:128], True, False)
        k.mm(pd, pd[rs, cs], ones, ones[:, :], pt, pt[:, 128:256], False, True)
        if un["evac"]:
            B0 = B - 3
            if d == 1:
                ov = lambda t: t[:, B0 * 128:(B0 + 4) * 128]
                iv = lambda p: p[:, :]
            else:
                n_, r0 = B0 // d, B0 % d
                base = n_ * 128 * d
                ov = lambda t, base=base, d=d, r0=r0: t[:, base:base + 128 * d].rearrange(
                    "p (i r) -> p i r", r=d)[:, :, r0:r0 + 4]
                iv = lambda p: p[:, :].rearrange("p (j i) -> p i j", j=4)
            if g == 0:
                k.op("dve", lambda e, o_=ov(nm), i_=iv(po): e.tensor_copy(out=o_, in_=i_), [po], [nm])
                k.act(dn, ov(dn), pd, iv(pd), AF.Copy)
            else:
                k.tt("dve", nm, ov(nm), po, iv(po), nm, ov(nm), ALU.add)
                k.tt("pool", dn, ov(dn), pd, iv(pd), dn, ov(dn), ALU.add) if False else \
                    k.tt("dve", dn, ov(dn), pd, iv(pd), dn, ov(dn), ALU.add)
        if un["last"]:
            k.op("dve", lambda e, o_=rec[:, :], i_=dn[:, :]: e.reciprocal(out=o_, in_=i_), [dn], [rec])
            k.tt("pool", nm, nm[:, :], nm, nm[:, :], rec, rec[:, :], ALU.mult)
            k.dma("sp", o, o.t[ft * 128:(ft + 1) * 128, ch * ACH:(ch + 1) * ACH], nm, nm[:, :])

    n = len(units)
    for i in range(n + LA):
        if i < n:
            s_part(i, units[i])
        if i - LA >= 0:
            pv_part(i - LA, units[i - LA])
    return k.finish()


def t5_bucket(dist):
    n_buckets, max_distance = 32, 2048
    max_exact = n_buckets // 2
    d = np.maximum(dist, 0)
    large = max_exact + (np.log(np.maximum(d, 1) / max_exact) / math.log(max_distance / max_exact)
                         * (n_buckets - max_exact)).astype(np.int64)
    large = np.minimum(large, n_buckets - 1)
    return np.where(d < max_exact, d, large).astype(np.int32)


def attn_bias_table(rel_bias):
    L = 128
    qi = np.arange(L)[:, None]
    ki = np.arange(2 * L)[None, :]
    off = qi + L - ki
    band = (off >= 0) & (off <= L)
    out = np.zeros((3, 8, 128, 2, 2, 128), np.float32)
    for g, d in enumerate(DILS):
        bucket = t5_bucket(np.clip(off, 0, None) * d)
        bias = rel_bias[bucket]
        bias = np.where(band[:, :, None], bias, np.float32(NEG))
        b = bias.reshape(L, 2, L, 8, 2)
        out[g] = b.transpose(3, 2, 4, 1, 0)
    return out.reshape(3, 8, 128, 512)


def attn_host_layout(qkvT, rel_bias, seqlen, tpc):
    ntok = qkvT.shape[1]
    ncore = ntok // tpc
    nblk = tpc // 128
    bias = attn_bias_table(rel_bias)
    perm = {}
    for g, d in enumerate(DILS):
        for j in range(3):
            a = qkvT[(g * 3 + j) * 1024:(g * 3 + j + 1) * 1024]
            a = a.reshape(8, 128, ntok // (128 * d), 128, d).transpose(0, 1, 2, 4, 3)
            perm[g, j] = a.reshape(8, 128, ntok // 128, 128)
    maps = []
    for c in range(ncore):
        first = (c * tpc) % seqlen == 0
        B0 = c * nblk
        qb = np.stack([perm[g, 0][:, :, B0:B0 + nblk].reshape(8, 128, nblk * 128) for g in range(3)])
        kbs, vbs = [], []
        for g in range(3):
            kk = np.zeros((8, 128, nblk + 16, 128), qkvT.dtype)
            vv = np.zeros((8, 128, nblk + 16, 128), qkvT.dtype)
            lo = 0 if first else 16
            kk[:, :, 16 - lo:] = perm[g, 1][:, :, B0 - lo:B0 + nblk]
            vv[:, :, 16 - lo:] = perm[g, 2][:, :, B0 - lo:B0 + nblk]
            kbs.append(kk.reshape(8, 128, -1))
            vbs.append(vv.transpose(0, 3, 2, 1).reshape(8, 128, -1))
        maps.append({"qb": np.ascontiguousarray(qb), "kb": np.ascontiguousarray(np.stack(kbs)),
                     "vb": np.ascontiguousarray(np.stack(vbs)), "biasT": bias,
                     "negb": np.full((128, 1), NEG if first else 0.0, np.float32)})
    return maps


NEXP = 8
DFE = 3584


def build_layer1_rest(tpc=TPC, NT=1024):
    k = KB()
    aT = k.dram("aT", [D, tpc], F32, "ExternalInput")
    xT = k.dram("xT", [D, tpc], F32, "ExternalInput")
    wo = k.dram("wo", [D, D], F32, "ExternalInput")
    wr = k.dram("wr", [128, 8, NEXP], F32, "ExternalInput")
    w1 = k.dram("w1", [NEXP, D, DFE], F32, "ExternalInput")
    w3 = k.dram("w3", [NEXP, D, DFE], F32, "ExternalInput")
    w2 = k.dram("w2", [NEXP, DFE, D], F32, "ExternalInput")
    lng = k.dram("lng", [128, 32], F32, "ExternalInput")
    lnb = k.dram("lnb", [128, 32], F32, "ExternalInput")
    ident_d = k.dram("ident", [128, 128], F32, "ExternalInput")
    oT = k.dram("oT", [D, tpc], F32, "ExternalOutput")
    cx = Ctx(k, NT, 14)
    k.dma("sp", cx.lng, cx.lng[:, :], lng, lng.t[:, :])
    k.dma("sp", cx.lnb, cx.lnb[:, :], lnb, lnb.t[:, :])
    ident = k.sb("ident", [128, 128], F32)
    k.dma("sp", ident, ident[:, :], ident_d, ident_d.t[:, :])
    wr_sb = k.sb("wr", [128, 8, NEXP], F32)
    k.dma("sp", wr_sb, wr_sb[:, :, :], wr, wr.t[:, :, :])
    ones_bf = k.sb("ones_bf", [128, 128], BF16)
    k.op("pool", lambda g: g.memset(ones_bf[:, :], 1.0), [], [ones_bf])
    gate_bc = k.sb("gate_bc", [128, NEXP, NT], BF16)
    ntt = NT // 128
    lg = k.sb("lg", [128, ntt, 8], F32)
    top = k.sb("top", [128, ntt, 8], F32)
    gsm = [k.sb("gsm%d" % i, [128, ntt], F32) for i in range(4)]
    gate = k.sb("gate", [128, ntt, 8], F32)
    gt2 = k.sb("gt2", [128, ntt, 8], F32)
    diag = [k.sb("diag", [128, 128], BF16) for _ in range(2)]
    z = cx.z
    abf = cx.hmid
    for sb_ in range(tpc // NT):
        tsl = slice(sb_ * NT, (sb_ + 1) * NT)
        for kt in range(8):
            k.dma("pool", abf, abf[:, kt, :], aT, aT.t[kt * 128:(kt + 1) * 128, tsl])
            k.dma("sp", z, z[:, kt, :], xT, xT.t[kt * 128:(kt + 1) * 128, tsl])
        for cb in range(2):
            wa = cx.wa[cx.wi % 2]
            cx.wi += 1
            load_wblock(k, wa, wo, 0, 8, cb * 512, 512)
            for fl in range(4):
                m = cb * 4 + fl
                for tb in range(NT // 512):
                    po = cx.pso[cx.oi % 2]
                    cx.oi += 1
                    ts_ = slice(tb * 512, tb * 512 + 512)
                    for kt in range(8):
                        k.mm(po, po[:, :], wa, wa[:, kt, fl * 128:fl * 128 + 128], abf, abf[:, kt, ts_], kt == 0, kt == 7)
                    k.stt("dve", z, z[:, m, ts_], z, z[:, m, ts_], float(ALPHA), po, po[:, :], ALU.mult, ALU.add)
        layer_norm(k, cx, z, 2, cx.xbf)
        k.ses = True
        pl = cx.pss[0]
        for tt in range(ntt):
            for kt in range(8):
                k.mm(pl, pl[:, tt * 8:tt * 8 + 8], z, z[:, kt, tt * 128:(tt + 1) * 128], wr_sb, wr_sb[:, kt, :], kt == 0, kt == 7)
        k.act(lg, lg[:, :, :], pl, pl[:, 0:ntt * 8].rearrange("p (t e) -> p t e", e=8), AF.Copy)
        nv1, e2, g1, g2 = gsm
        for tt in range(ntt):
            k.op("dve", lambda g, o=top[:, tt, :], i=lg[:, tt, :]: g.max(out=o, in_=i), [lg], [top])
        k.ts("dve", nv1, nv1[:, :], top, top[:, :, 0], -1.0, ALU.mult)
        k.tt("dve", e2, e2[:, :], top, top[:, :, 1], nv1, nv1[:, :], ALU.add)
        k.act(e2, e2[:, :], e2, e2[:, :], AF.Exp)
        k.ts("dve", g1, g1[:, :], e2, e2[:, :], 1.0, ALU.add)
        k.op("dve", lambda g: g.reciprocal(out=g1[:, :], in_=g1[:, :]), [g1], [g1])
        k.tt("dve", g2, g2[:, :], e2, e2[:, :], g1, g1[:, :], ALU.mult)
        for tt in range(ntt):
            k.ts("dve", gate, gate[:, tt, :], lg, lg[:, tt, :], top[:, tt, 0:1], ALU.is_equal, g1[:, tt:tt + 1], ALU.mult,
                 extra_reads=[top, g1])
            k.ts("dve", gt2, gt2[:, tt, :], lg, lg[:, tt, :], top[:, tt, 1:2], ALU.is_equal, g2[:, tt:tt + 1], ALU.mult,
                 extra_reads=[top, g2])
        k.tt("dve", gate, gate[:, :, :], gate, gate[:, :, :], gt2, gt2[:, :, :], ALU.add)
        di = 0
        for e in range(NEXP):
            for hb in range(NT // 512):
                pg = cx.pss[1]
                for t4 in range(4):
                    tt = hb * 4 + t4
                    dg = diag[di % 2]
                    di += 1
                    k.ts("dve", dg, dg[:, :], ident, ident[:, :], gate[:, tt, e:e + 1], ALU.mult, extra_reads=[gate])
                    k.mm(pg, pg[:, t4 * 128:(t4 + 1) * 128], ones_bf, ones_bf[:, :], dg, dg[:, :], True, True)
                k.act(gate_bc, gate_bc[:, e, hb * 512:(hb + 1) * 512], pg, pg[:, :], AF.Copy)
        k.ses = SES
        for kt in range(8):
            k.ts("dve", z, z[:, kt, :], z, z[:, kt, :], float(ALPHA), ALU.mult)
        for e in range(NEXP):
            w1e, w3e, w2e = Buf("w1e", w1.t[e]), Buf("w3e", w3.t[e]), Buf("w2e", w2.t[e])
            for half in range(2):
                f0 = half * 14

                def emit_moe(ft, t0, tw, sa, pB, e=e):
                    ts_ = slice(t0, t0 + 512)
                    t = cx.tmp[(ft + t0 // 512) % 2]
                    k.tt("dve", t, t[:, :], pB, pB[:, :], sa, sa[:, :], ALU.mult)
                    k.tt("dve", cx.hmid, cx.hmid[:, ft, ts_], t, t[:, :], gate_bc, gate_bc[:, e, ts_], ALU.mult)

                glu_phase(k, cx, cx.xbf, w1e, f0 * 128, w3e, f0 * 128, 14 * 128, AF.Silu, emit_moe,
                          hook=lambda f0=f0, w2e=w2e: down_load(k, cx, w2e, f0, 14))
                down_phase(k, cx, w2e, f0, 14, z)
        layer_norm(k, cx, z, 3, None)
        for kt in range(8):
            k.dma("sp", oT, oT.t[kt * 128:(kt + 1) * 128, tsl], z, z[:, kt, :])
    return k.finish()


_PROGS = {}


def _prog(name, fn):
    if name not in _PROGS:
        _PROGS[name] = fn()
    return _PROGS[name]


def _run(nc, maps):
    res = run_bass_kernel_spmd(nc, maps, core_ids=list(range(NCORES)))
    return res.results


def kernel(x, s5_w_in, s5_a_re, s5_a_im, s5_log_dt, s5_b_re, s5_b_im, s5_c_re, s5_c_im, s5_d,
           s5_w_glu, attn_w_qkv, attn_w_o, rel_bias, ffn_w1, ffn_w3, ffn_w2, moe_w_router,
           moe_w1, moe_w3, moe_w2, ln_g, ln_b):
    f32 = np.float32
    x = np.asarray(x, f32)
    xT = np.ascontiguousarray(x.reshape(NTOK, D).T)
    cols = [slice(c * TPC, (c + 1) * TPC) for c in range(NCORES)]
    xTs = [np.ascontiguousarray(xT[:, s]) for s in cols]
    lg, lb = ln_layout(np.asarray(ln_g, f32), np.asarray(ln_b, f32))

    w_in = np.ascontiguousarray(np.asarray(s5_w_in, f32)[0])
    r = _run(_prog("A", build_uproj), [{"xT": xTs[c], "w": w_in} for c in range(NCORES)])
    u_full = np.concatenate([r[c]["uT"] for c in range(NCORES)], axis=1)
    maps = []
    for c in range(NCORES):
        m = s5_core_params(c, np.asarray(s5_a_re, f32)[0], np.asarray(s5_a_im, f32)[0], np.asarray(s5_log_dt, f32)[0],
                           np.asarray(s5_b_re, f32)[0], np.asarray(s5_b_im, f32)[0], np.asarray(s5_c_re, f32)[0],
                           np.asarray(s5_c_im, f32)[0], np.asarray(s5_d, f32)[0])
        m["u"] = np.ascontiguousarray(u_full[128 * c:128 * c + 128])
        maps.append(m)
    r = _run(_prog("B", build_s5), maps)
    y_full = np.concatenate([r[c]["y"] for c in range(NCORES)], axis=0)
    wglu = np.ascontiguousarray(np.asarray(s5_w_glu, f32)[0])
    w1 = np.ascontiguousarray(np.asarray(ffn_w1, f32)[0])
    w3 = np.ascontiguousarray(np.asarray(ffn_w3, f32)[0])
    w2 = np.ascontiguousarray(np.asarray(ffn_w2, f32)[0])
    r = _run(_prog("C", build_layer0_rest),
             [{"yT": np.ascontiguousarray(y_full[:, cols[c]]), "xT": xTs[c], "wglu": wglu, "w1": w1, "w3": w3, "w2": w2,
               "lng": lg, "lnb": lb} for c in range(NCORES)])
    x2s = [r[c]["oT"] for c in range(NCORES)]
    wqkv = np.ascontiguousarray(np.asarray(attn_w_qkv, f32)[0])
    r = _run(_prog("D", build_qkv), [{"xT": x2s[c], "w": wqkv} for c in range(NCORES)])
    qkvT = np.concatenate([r[c]["qkvT"] for c in range(NCORES)], axis=1)
    maps = attn_host_layout(qkvT, np.asarray(rel_bias, f32), SEQ, TPC)
    r = _run(_prog("E", build_attn), maps)
    aTs = [r[c]["attnT"] for c in range(NCORES)]
    wo = np.ascontiguousarray(np.asarray(attn_w_o, f32)[0])
    wr = np.ascontiguousarray(np.asarray(moe_w_router, f32)[0].reshape(8, 128, NEXP).transpose(1, 0, 2))
    mw1 = np.ascontiguousarray(np.asarray(moe_w1, f32)[0])
    mw3 = np.ascontiguousarray(np.asarray(moe_w3, f32)[0])
    mw2 = np.ascontiguousarray(np.asarray(moe_w2, f32)[0])
    ident = np.eye(128, dtype=f32)
    ltri = np.triu(np.ones((128, 128), f32), 1)
    ln4 = np.ascontiguousarray(np.stack([np.asarray(ln_g, f32)[1, 1], np.asarray(ln_b, f32)[1, 1]]))
    r = _run(_prog("F2", build_layer1_fast),
             [{"aT": aTs[c], "xT": x2s[c], "wo": wo, "wr": wr, "w1": mw1, "w3": mw3, "w2": mw2, "lng": lg, "lnb": lb,
               "ln4": ln4, "ident": ident, "ltri": ltri} for c in range(NCORES)])
    counts = np.stack([r[c]["cnt"][0] for c in range(NCORES)])
    if counts.max() <= CAP:
        out = np.concatenate([r[c]["o"] for c in range(NCORES)], axis=0)
        return np.ascontiguousarray(out).reshape(BATCH, SEQ, D).astype(f32)
    r = _run(_prog("F", build_layer1_rest),
             [{"aT": aTs[c], "xT": x2s[c], "wo": wo, "wr": wr, "w1": mw1, "w3": mw3, "w2": mw2, "lng": lg, "lnb": lb,
               "ident": ident} for c in range(NCORES)])
    oT = np.concatenate([r[c]["oT"] for c in range(NCORES)], axis=1)
    return np.ascontiguousarray(oT.T).reshape(BATCH, SEQ, D).astype(f32)


CAP = 1280
U32 = mybir.dt.uint32
OOR = 1.0e6


class NS:
    pass


def build_layer1_fast(tpc=TPC, NT=1024, cap=CAP):
    k = KB()
    aT = k.dram("aT", [D, tpc], F32, "ExternalInput")
    xT = k.dram("xT", [D, tpc], F32, "ExternalInput")
    wo = k.dram("wo", [D, D], F32, "ExternalInput")
    wr = k.dram("wr", [128, 8, NEXP], F32, "ExternalInput")
    w1 = k.dram("w1", [NEXP, D, DFE], F32, "ExternalInput")
    w3 = k.dram("w3", [NEXP, D, DFE], F32, "ExternalInput")
    w2 = k.dram("w2", [NEXP, DFE, D], F32, "ExternalInput")
    lng = k.dram("lng", [128, 32], F32, "ExternalInput")
    lnb = k.dram("lnb", [128, 32], F32, "ExternalInput")
    ln4 = k.dram("ln4", [2, D], F32, "ExternalInput")
    ident_d = k.dram("ident", [128, 128], F32, "ExternalInput")
    ltri_d = k.dram("ltri", [128, 128], F32, "ExternalInput")
    o = k.dram("o", [tpc, D], F32, "ExternalOutput")
    cnt_o = k.dram("cnt", [128, NEXP], F32, "ExternalOutput")
    xg = k.dram("xg", [NEXP * cap + 1, D], BF16)
    ye = k.dram("ye", [NEXP * cap + 1, D], F32)
    x3tm = k.dram("x3tm", [tpc, D], F32)
    ntile = tpc // 128
    ntt = NT // 128
    nst = cap // 128

    ident = k.sb("ident", [128, 128], F32)
    k.dma("sp", ident, ident[:, :], ident_d, ident_d.t[:, :])
    ident_bf = k.sb("ident_bf", [128, 128], BF16)
    k.dma("pool", ident_bf, ident_bf[:, :], ident_d, ident_d.t[:, :])
    ltri = k.sb("ltri", [128, 128], BF16)
    k.dma("pool", ltri, ltri[:, :], ltri_d, ltri_d.t[:, :])
    ones_bf = k.sb("ones_bf", [128, 128], BF16)
    k.op("pool", lambda g, a=ones_bf[:, :]: g.memset(a, 1.0), [], [ones_bf])
    IDX = [k.sb("idx%d" % j, [128, ntile], U32) for j in range(2)]
    G = [k.sb("G%d" % j, [128, ntile], F32) for j in range(2)]
    base = k.sb("base", [128, NEXP], F32)
    k.op("pool", lambda g, a=base[:, :]: g.memset(a, 0.0), [], [base])
    iota_e = k.sb("iota_e", [128, ntt, NEXP], F32)
    for e in range(NEXP):
        k.op("pool", lambda g, e=e, a=iota_e[:, :, e:e + 1]: g.memset(a, float(e)), [], [iota_e])

    k.push_scope()
    cx = NS()
    cx.k, cx.NT = k, NT
    cx.pso = [k.ps("pso") for _ in range(2)]
    cx.pss = [k.ps("pss") for _ in range(2)]
    ptb = [k.ps("ptb", (128, 1024), BF16) for _ in range(2)]
    ptf = [k.ps("ptf") for _ in range(2)]
    cx.wa = [k.sb("wa", [128, 8, 512], BF16) for _ in range(2)]
    cx.z = k.sb("z", [128, 8, NT], F32)
    cx.xbf = k.sb("xbf", [128, 8, NT], BF16)
    abf = k.sb("abf", [128, 8, NT], BF16)
    cx.tmp = [k.sb("tmp", [128, 512], F32) for _ in range(2)]
    cx.ones = k.sb("ones", [128, 128], F32)
    k.op("pool", lambda g, a=cx.ones[:, :]: g.memset(a, 1.0), [], [cx.ones])
    cx.sq = [k.sb("sq", [128, 512], F32) for _ in range(2)]
    cx.mean = k.sb("mean", [128, 512], F32)
    cx.ex2 = k.sb("ex2", [128, 512], F32)
    cx.rstd = k.sb("rstd", [128, 512], F32)
    cx.nmr = k.sb("nmr", [128, 512], F32)
    cx.lng = k.sb("lng", [128, 32], F32)
    cx.lnb = k.sb("lnb", [128, 32], F32)
    cx.wi = cx.pi = cx.oi = 0
    k.dma("sp", cx.lng, cx.lng[:, :], lng, lng.t[:, :])
    k.dma("sp", cx.lnb, cx.lnb[:, :], lnb, lnb.t[:, :])
    wr_sb = k.sb("wr", [128, 8, NEXP], F32)
    k.dma("sp", wr_sb, wr_sb[:, :, :], wr, wr.t[:, :, :])
    lg = k.sb("lg", [128, ntt, 8], F32)
    top = k.sb("top", [128, ntt, 8], F32)
    nv1, e2, g1, g2 = [k.sb("gsm%d" % i, [128, ntt], F32) for i in range(4)]
    m = [k.sb("m%d" % j, [128, ntt, 8], F32) for j in range(2)]
    msel = k.sb("msel", [128, ntt, 8], BF16)
    Pw = k.sb("Pw", [128, ntt, 8], F32)
    cn = k.sb("cn", [128, ntt, 8], F32)
    offs = k.sb("offs", [128, ntt + 1, 8], F32)
    pos = k.sb("pos", [128, ntt, 8], F32)
    prod = k.sb("prod", [128, ntt, 8], F32)
    psel = [k.sb("psel%d" % j, [128, ntt], F32) for j in range(2)]
    esel = [k.sb("esel%d" % j, [128, ntt], F32) for j in range(2)]
    ovf = k.sb("ovf", [128, ntt], F32)
    flat = k.sb("flat", [128, ntt], F32)
    xtm = [k.sb("xtm", [128, D], BF16) for _ in range(2)]
    x3s = [k.sb("x3s", [128, D], F32) for _ in range(2)]
    z = cx.z
    for sb_ in range(tpc // NT):
        tsl = slice(sb_ * NT, (sb_ + 1) * NT)
        k.ses = SES
        for kt in range(8):
            k.dma("pool", abf, abf[:, kt, :], aT, aT.t[kt * 128:(kt + 1) * 128, tsl])
            k.dma("sp", z, z[:, kt, :], xT, xT.t[kt * 128:(kt + 1) * 128, tsl])
        for cb in range(2):
            wa = cx.wa[cx.wi % 2]
            cx.wi += 1
            load_wblock(k, wa, wo, 0, 8, cb * 512, 512)
            for fl in range(4):
                mt = cb * 4 + fl
                for tb in range(NT // 512):
                    po = cx.pso[cx.oi % 2]
                    cx.oi += 1
                    ts_ = slice(tb * 512, tb * 512 + 512)
                    for kt in range(8):
                        k.mm(po, po[:, :], wa, wa[:, kt, fl * 128:fl * 128 + 128], abf, abf[:, kt, ts_], kt == 0, kt == 7)
                    k.stt("dve", z, z[:, mt, ts_], z, z[:, mt, ts_], float(ALPHA), po, po[:, :], ALU.mult, ALU.add)
        layer_norm(k, cx, z, 2, cx.xbf)
        k.ses = True
        pl = cx.pss[0]
        for tt in range(ntt):
            for kt in range(8):
                k.mm(pl, pl[:, tt * 8:tt * 8 + 8], z, z[:, kt, tt * 128:(tt + 1) * 128], wr_sb, wr_sb[:, kt, :], kt == 0, kt == 7)
        k.act(lg, lg[:, :, :], pl, pl[:, 0:ntt * 8].rearrange("p (t e) -> p t e", e=8), AF.Copy)
        for tt in range(ntt):
            k.op("dve", lambda g, o_=top[:, tt, :], i=lg[:, tt, :]: g.max(out=o_, in_=i), [lg], [top])
        k.ts("dve", nv1, nv1[:, :], top, top[:, :, 0], -1.0, ALU.mult)
        k.tt("dve", e2, e2[:, :], top, top[:, :, 1], nv1, nv1[:, :], ALU.add)
        k.act(e2, e2[:, :], e2, e2[:, :], AF.Exp)
        k.ts("dve", g1, g1[:, :], e2, e2[:, :], 1.0, ALU.add)
        k.op("dve", lambda g, a=g1[:, :]: g.reciprocal(out=a, in_=a), [g1], [g1])
        k.tt("dve", g2, g2[:, :], e2, e2[:, :], g1, g1[:, :], ALU.mult)
        t0_ = sb_ * ntt
        k.op("dve", lambda g, o_=G[0][:, t0_:t0_ + ntt], i_=g1[:, :]: g.tensor_copy(out=o_, in_=i_), [g1], [G[0]])
        k.op("dve", lambda g, o_=G[1][:, t0_:t0_ + ntt], i_=g2[:, :]: g.tensor_copy(out=o_, in_=i_), [g2], [G[1]])
        for tt in range(ntt):
            k.ts("dve", m[0], m[0][:, tt, :], lg, lg[:, tt, :], top[:, tt, 0:1], ALU.is_equal, extra_reads=[top])
            k.ts("dve", m[1], m[1][:, tt, :], lg, lg[:, tt, :], top[:, tt, 1:2], ALU.is_equal, extra_reads=[top])
        k.tt("dve", msel, msel[:, :, :], m[0], m[0][:, :, :], m[1], m[1][:, :, :], ALU.add)
        pp = cx.pss[1]
        mflat = msel[:, :, :].rearrange("p t e -> p (t e)")
        k.mm(pp, pp[:, 0:ntt * 8], ltri, ltri[:, :], msel, mflat, True, True)
        k.mm(pp, pp[:, 256:256 + ntt * 8], ones_bf, ones_bf[:, :], msel, mflat, True, True)
        k.act(Pw, Pw[:, :, :], pp, pp[:, 0:ntt * 8].rearrange("p (t e) -> p t e", e=8), AF.Copy)
        k.act(cn, cn[:, :, :], pp, pp[:, 256:256 + ntt * 8].rearrange("p (t e) -> p t e", e=8), AF.Copy)
        k.op("dve", lambda g, o_=offs[:, 0, :], i_=base[:, :]: g.tensor_copy(out=o_, in_=i_), [base], [offs])
        for tt in range(ntt):
            k.tt("dve", offs, offs[:, tt + 1, :], offs, offs[:, tt, :], cn, cn[:, tt, :], ALU.add)
        k.op("dve", lambda g, o_=base[:, :], i_=offs[:, ntt, :]: g.tensor_copy(out=o_, in_=i_), [offs], [base])
        k.tt("dve", pos, pos[:, :, :], Pw, Pw[:, :, :], offs, offs[:, 0:ntt, :], ALU.add)
        for j in range(2):
            k.tt("dve", prod, prod[:, :, :], m[j], m[j][:, :, :], pos, pos[:, :, :], ALU.mult)
            k.op("dve", lambda g, o_=psel[j][:, :], i_=prod[:, :, :]: g.reduce_sum(out=o_, in_=i_, axis=AX.X), [prod], [psel[j]])
            k.tt("dve", prod, prod[:, :, :], m[j], m[j][:, :, :], iota_e, iota_e[:, :, :], ALU.mult)
            k.op("dve", lambda g, o_=esel[j][:, :], i_=prod[:, :, :]: g.reduce_sum(out=o_, in_=i_, axis=AX.X), [prod], [esel[j]])
            k.stt("dve", flat, flat[:, :], esel[j], esel[j][:, :], float(cap), psel[j], psel[j][:, :], ALU.mult, ALU.add)
            k.ts("dve", ovf, ovf[:, :], psel[j], psel[j][:, :], float(cap) - 0.5, ALU.is_gt)
            k.stt("dve", flat, flat[:, :], ovf, ovf[:, :], OOR, flat, flat[:, :], ALU.mult, ALU.add)
            k.ts("dve", flat, flat[:, :], flat, flat[:, :], float(NEXP * cap), ALU.min)
            k.op("dve", lambda g, o_=IDX[j][:, t0_:t0_ + ntt], i_=flat[:, :]: g.tensor_copy(out=o_, in_=i_), [flat], [IDX[j]])
        k.ses = SES
        for kt in range(8):
            k.ts("dve", z, z[:, kt, :], z, z[:, kt, :], float(ALPHA), ALU.mult)
        for tt in range(ntt):
            gt = t0_ + tt
            pb_, xt_ = ptb[tt % 2], xtm[tt % 2]
            for kt in range(8):
                k.tr(pb_, pb_[:, kt * 128:(kt + 1) * 128], cx.xbf, cx.xbf[:, kt, tt * 128:(tt + 1) * 128], ident_bf, ident_bf[:, :])
            k.act(xt_, xt_[:, :], pb_, pb_[:, :], AF.Copy)
            for j in range(2):
                k.idma(xg, xg.t[:, :], xt_, xt_[:, :], IDX[j], IDX[j][:, gt:gt + 1], True, NEXP * cap - 1)
            x3_ = x3s[tt % 2]
            for hf in range(2):
                pf_ = ptf[hf]
                for kq in range(4):
                    kt = hf * 4 + kq
                    k.tr(pf_, pf_[:, kq * 128:(kq + 1) * 128], z, z[:, kt, tt * 128:(tt + 1) * 128], ident, ident[:, :])
                k.op("dve", lambda g, o_=x3_[:, hf * 512:(hf + 1) * 512], i_=pf_[:, :]: g.tensor_copy(out=o_, in_=i_), [pf_], [x3_])
            k.dma("sp", x3tm, x3tm.t[gt * 128:(gt + 1) * 128, :], x3_, x3_[:, :])
    k.dma("sp", cnt_o, cnt_o.t[:, :], base, base[:, :])
    k.pop_scope()

    k.push_scope()
    cx = NS()
    cx.k, cx.NT = k, cap
    cx.ps1 = [k.ps("ps1") for _ in range(2)]
    cx.ps3 = [k.ps("ps3") for _ in range(2)]
    cx.pso = [k.ps("pso") for _ in range(2)]
    ptr = [k.ps("ptr", (128, 512), BF16) for _ in range(2)]
    cx.wa = [k.sb("wa", [128, 8, 512], BF16) for _ in range(2)]
    cx.wb = [k.sb("wb", [128, 8, 512], BF16) for _ in range(2)]
    cx.w2h = k.sb("w2h", [128, 14, 1024], BF16)
    cx.hmid = k.sb("hmid", [128, 14, cap], BF16)
    cx.sact = [k.sb("sact", [128, 512], F32) for _ in range(2)]
    cx.wi = cx.pi = cx.oi = 0
    xe = k.sb("xge", [128, 8, cap], BF16)
    xs = [k.sb("xs", [128, 4, D], BF16) for _ in range(2)]
    yacc = k.sb("yacc", [128, nst, D], F32)
    xi = 0
    ti = 0
    for e in range(NEXP):
        for sg in range((nst + 3) // 4):
            n_ = min(4, nst - 4 * sg)
            xs_ = xs[xi % 2]
            xi += 1
            r0 = e * cap + sg * 512
            k.dma("sp", xs_, xs_[:, 0:n_, :], xg, xg.t[r0:r0 + n_ * 128, :].rearrange("(t p) f -> p t f", p=128))
            for kt in range(8):
                pt_ = ptr[ti % 2]
                for t in range(n_):
                    k.tr(pt_, pt_[:, t * 128:(t + 1) * 128], xs_, xs_[:, t, kt * 128:(kt + 1) * 128], ident_bf, ident_bf[:, :])
                if ti % 2 == 0:
                    k.act(xe, xe[:, kt, sg * 512:sg * 512 + n_ * 128], pt_, pt_[:, 0:n_ * 128], AF.Copy)
                else:
                    k.op("dve", lambda g, o_=xe[:, kt, sg * 512:sg * 512 + n_ * 128], i_=pt_[:, 0:n_ * 128]:
                         g.tensor_copy(out=o_, in_=i_), [pt_], [xe])
                ti += 1
        w1e, w3e, w2e = Buf("w1e", w1.t[e]), Buf("w3e", w3.t[e]), Buf("w2e", w2.t[e])
        for half in range(2):
            f0 = half * 14

            def emit_h(ft, t0, tw, sa, pB, cx=cx):
                k.tt("dve", cx.hmid, cx.hmid[:, ft, t0:t0 + tw], pB, pB[:, 0:tw], sa, sa[:, 0:tw], ALU.mult)

            glu_phase(k, cx, xe, w1e, f0 * 128, w3e, f0 * 128, 14 * 128, AF.Silu, emit_h,
                      hook=lambda f0=f0, w2e=w2e, cx=cx: down_load(k, cx, w2e, f0, 14), NT=cap)
            for st in range(nst):
                for fh in range(2):
                    po = cx.pso[cx.oi % 2]
                    cx.oi += 1
                    for kt in range(14):
                        k.mm(po, po[:, :], cx.hmid, cx.hmid[:, kt, st * 128:(st + 1) * 128], cx.w2h, cx.w2h[:, kt, fh * 512:(fh + 1) * 512],
                             kt == 0, kt == 13)
                    ysl = yacc[:, st, fh * 512:(fh + 1) * 512]
                    if half == 0:
                        k.act(yacc, ysl, po, po[:, :], AF.Copy)
                    else:
                        k.tt("dve", yacc, ysl, po, po[:, :], yacc, ysl, ALU.add)
        k.dma("sp", ye, ye.t[e * cap:(e + 1) * cap, :].rearrange("(t p) f -> p t f", p=128), yacc, yacc[:, :, :])
    k.pop_scope()

    k.push_scope()
    gB = k.sb("gB", [128, D], F32)
    bB = k.sb("bB", [128, D], F32)
    k.dma("sp", gB, gB[:, :], ln4, ln4.t[0:1, :].to_broadcast([128, D]))
    k.dma("sp", bB, bB[:, :], ln4, ln4.t[1:2, :].to_broadcast([128, D]))
    r1 = [k.sb("r1", [128, D], F32) for _ in range(2)]
    r2 = [k.sb("r2", [128, D], F32) for _ in range(2)]
    xa = [k.sb("xa", [128, D], F32) for _ in range(2)]
    zt = [k.sb("zt", [128, D], F32) for _ in range(2)]
    st6 = k.sb("st6", [128, 2, 6], F32)
    mv = k.sb("mv", [128, 2], F32)
    sd = k.sb("sd", [128, 1], F32)
    rs_ = k.sb("rs", [128, 1], F32)
    nm_ = k.sb("nm", [128, 1], F32)
    k.ses = True
    for t in range(ntile):
        a_, b_, x_, z_ = r1[t % 2], r2[t % 2], xa[t % 2], zt[t % 2]
        k.idma(a_, a_[:, :], ye, ye.t[:, :], IDX[0], IDX[0][:, t:t + 1], False, NEXP * cap - 1)
        k.idma(b_, b_[:, :], ye, ye.t[:, :], IDX[1], IDX[1][:, t:t + 1], False, NEXP * cap - 1)
        k.dma("sp", x_, x_[:, :], x3tm, x3tm.t[t * 128:(t + 1) * 128, :])
        k.stt("dve", z_, z_[:, :], a_, a_[:, :], G[0][:, t:t + 1], x_, x_[:, :], ALU.mult, ALU.add, extra_reads=[G[0]])
        k.stt("dve", z_, z_[:, :], b_, b_[:, :], G[1][:, t:t + 1], z_, z_[:, :], ALU.mult, ALU.add, extra_reads=[G[1]])
        for hf in range(2):
            k.op("dve", lambda g, o_=st6[:, hf, :], i_=z_[:, hf * 512:(hf + 1) * 512]: g.bn_stats(out=o_, in_=i_), [z_], [st6])
        k.op("dve", lambda g, o_=mv[:, :], i_=st6[:, :, :].rearrange("p a b -> p (a b)"): g.bn_aggr(out=o_, in_=i_), [st6], [mv])
        k.act(sd, sd[:, :], mv, mv[:, 1:2], AF.Sqrt, bias=LN_EPS)
        k.op("dve", lambda g, o_=rs_[:, :], i_=sd[:, :]: g.reciprocal(out=o_, in_=i_), [sd], [rs_])
        k.stt("dve", nm_, nm_[:, :], mv, mv[:, 0:1], -1.0, rs_, rs_[:, :], ALU.mult, ALU.mult)
        k.ts("dve", z_, z_[:, :], z_, z_[:, :], rs_[:, 0:1], ALU.mult, nm_[:, 0:1], ALU.add, extra_reads=[rs_, nm_])
        k.tt("dve", z_, z_[:, :], z_, z_[:, :], gB, gB[:, :], ALU.mult)
        k.tt("pool", z_, z_[:, :], z_, z_[:, :], bB, bB[:, :], ALU.add)
        k.dma("sp", o, o.t[t * 128:(t + 1) * 128, :], z_, z_[:, :])
    k.pop_scope()
    return k.finish()
```

```python
import math
import numpy as np
import concourse.bass as bass
import concourse.mybir as mybir
from concourse.bass_utils import run_bass_kernel_spmd
from contextlib import ExitStack

F32 = mybir.dt.float32
BF16 = mybir.dt.bfloat16
I32 = mybir.dt.int32
ALU = mybir.AluOpType
AF = mybir.ActivationFunctionType
AX = mybir.AxisListType

NCORES = 8
D = 1024
SEQ = 16384
BATCH = 2
NTOK = BATCH * SEQ
TPC = NTOK // NCORES
DEPTH = 2
ALPHA = (2 * DEPTH) ** 0.25
LN_EPS = 1e-5
PI = math.pi


class Buf:
    __slots__ = ("name", "t", "lw", "rd", "dsem", "dcnt", "nodep")

    def __init__(self, name, t):
        self.name = name
        self.t = t
        self.lw = None
        self.rd = {}
        self.dsem = None
        self.dcnt = 0
        self.nodep = False

    def __getitem__(self, idx):
        return self.t[idx]


SES = False


class KB:
    def __init__(self, same_engine_sync=None):
        if same_engine_sync is None:
            same_engine_sync = SES
        self.nc = bass.Bass("TRN2", target_bir_lowering=False)
        self.es = ExitStack()
        nc = self.nc
        self.eng = {"pe": nc.tensor, "dve": nc.vector, "act": nc.scalar, "pool": nc.gpsimd, "sp": nc.sync}
        self.sem = {e: self.es.enter_context(nc.semaphore("s_" + e)) for e in self.eng}
        self.cnt = {e: 0 for e in self.eng}
        self.known = {e: {} for e in self.eng}
        self.ses = same_engine_sync
        self.nsem = 0
        self.prog = {e: [] for e in self.eng}
        self.outs = []
        self.uid = 0
        self.scopes = []
        self.dbufs = []

    def push_scope(self):
        self.scopes.append(ExitStack())

    def pop_scope(self):
        self.barrier()
        self.scopes.pop().close()

    def _stk(self):
        return self.scopes[-1] if self.scopes else self.es

    def barrier(self):
        tags = [(self.sem[e], self.cnt[e]) for e in self.eng if self.cnt[e]]
        tags += [(b.dsem, b.dcnt) for b in self.dbufs if b.dcnt]
        for e in self.eng:
            kn = self.known[e]
            for s_, v in tags:
                if s_ == self.sem[e]:
                    continue
                if kn.get(s_, 0) >= v:
                    continue
                self.prog[e].append(("w", s_, v))
                kn[s_] = v

    def sb(self, name, shape, dt):
        self.uid += 1
        return Buf(name, self._stk().enter_context(self.nc.sbuf_tensor("%s_%d" % (name, self.uid), list(shape), dt)))

    def ps(self, name, shape=(128, 512), dt=F32):
        self.uid += 1
        return Buf(name, self._stk().enter_context(self.nc.psum_tensor("%s_%d" % (name, self.uid), list(shape), dt)))

    def dram(self, name, shape, dt, kind="Internal"):
        b = Buf(name, self.nc.dram_tensor(name, list(shape), dt, kind=kind).ap())
        if kind == "Internal":
            b.nodep = True
        if kind == "ExternalOutput":
            self.outs.append(b)
            b.nodep = True
        return b

    def _deps(self, e, reads, writes):
        need = {}

        def add(tag):
            if tag is None:
                return
            s, v = tag
            if need.get(s, 0) < v:
                need[s] = v

        for b in reads:
            add(b.lw)
        for b in writes:
            if b.nodep:
                continue
            add(b.lw)
            for s, v in b.rd.items():
                add((s, v))
        k = self.known[e]
        own = self.sem[e]
        for s, v in need.items():
            if s == own and (e == "pe" or not self.ses):
                continue
            if k.get(s, 0) >= v:
                continue
            self.prog[e].append(("w", s, v))
            k[s] = v

    def op(self, e, fn, reads=(), writes=()):
        self._deps(e, reads, writes)
        self.cnt[e] += 1
        self.prog[e].append(("i", fn, self.sem[e], 1))
        tag = (self.sem[e], self.cnt[e])
        for b in writes:
            b.lw = tag
            b.rd = {}
        for b in reads:
            if b not in writes:
                b.rd[tag[0]] = tag[1]

    def dma(self, q, out_buf, out_ap, in_buf, in_ap, **kw):
        self._deps(q, [in_buf], [out_buf])
        if out_buf.dsem is None:
            self.nsem += 1
            out_buf.dsem = self.es.enter_context(self.nc.semaphore("d%d" % self.nsem))
            self.dbufs.append(out_buf)
        self.prog[q].append(("i", (lambda e, o=out_ap, i=in_ap, kw=kw: e.dma_start(out=o, in_=i, **kw)),
                             out_buf.dsem, 16))
        out_buf.dcnt += 16
        tag = (out_buf.dsem, out_buf.dcnt)
        out_buf.lw = tag
        out_buf.rd = {}
        in_buf.rd[tag[0]] = tag[1]

    def idma(self, out_buf, out_ap, in_buf, in_ap, idx_buf, idx_ap, scatter, bound):
        self._deps("pool", [in_buf, idx_buf], [out_buf])
        if out_buf.dsem is None:
            self.nsem += 1
            out_buf.dsem = self.es.enter_context(self.nc.semaphore("d%d" % self.nsem))
            self.dbufs.append(out_buf)
        off = bass.IndirectOffsetOnAxis(idx_ap, 0)
        if scatter:
            fn = lambda e: e.indirect_dma_start(out=out_ap, out_offset=off, in_=in_ap, in_offset=None)
        else:
            fn = lambda e: e.indirect_dma_start(out=out_ap, out_offset=None, in_=in_ap, in_offset=off)
        self.prog["pool"].append(("i", fn, out_buf.dsem, 16))
        out_buf.dcnt += 16
        tag = (out_buf.dsem, out_buf.dcnt)
        out_buf.lw = tag
        out_buf.rd = {}
        in_buf.rd[tag[0]] = tag[1]
        idx_buf.rd[tag[0]] = tag[1]

    def tr(self, out_b, out_ap, in_b, in_ap, id_b, id_ap):
        self.op("pe", lambda g: g.transpose(out_ap, in_ap, id_ap), [in_b, id_b], [out_b])

    def finish(self):
        for b in self.outs:
            if b.lw is not None:
                self.prog["sp"].append(("w", b.lw[0], b.lw[1]))
        for e in ("pe", "dve", "act", "pool"):
            if self.cnt[e]:
                self.prog["sp"].append(("w", self.sem[e], self.cnt[e]))
        prog = self.prog

        def replay(e):
            def body(eng):
                for it in prog[e]:
                    if it[0] == "w":
                        eng.wait_ge(it[1], it[2])
                    else:
                        it[1](eng).then_inc(it[2], it[3])
            return body

        with self.nc.Block() as block:
            block.sync(replay("sp"))
            block.tensor(replay("pe"))
            block.vector(replay("dve"))
            block.scalar(replay("act"))
            block.gpsimd(replay("pool"))
        self.es.close()
        return self.nc

    def tt(self, e, out_b, out_ap, a_b, a_ap, b_b, b_ap, op):
        self.op(e, lambda g: g.tensor_tensor(out=out_ap, in0=a_ap, in1=b_ap, op=op), [a_b, b_b], [out_b])

    def ts(self, e, out_b, out_ap, a_b, a_ap, s1, op0, s2=None, op1=None, extra_reads=()):
        if op1 is None:
            self.op(e, lambda g: g.tensor_scalar(out=out_ap, in0=a_ap, scalar1=s1, scalar2=None, op0=op0),
                    [a_b] + list(extra_reads), [out_b])
        else:
            self.op(e, lambda g: g.tensor_scalar(out=out_ap, in0=a_ap, scalar1=s1, scalar2=s2, op0=op0, op1=op1),
                    [a_b] + list(extra_reads), [out_b])

    def stt(self, e, out_b, out_ap, a_b, a_ap, scalar, b_b, b_ap, op0, op1, extra_reads=()):
        self.op(e, lambda g: g.scalar_tensor_tensor(out=out_ap, in0=a_ap, scalar=scalar, in1=b_ap, op0=op0, op1=op1),
                [a_b, b_b] + list(extra_reads), [out_b])

    def act(self, out_b, out_ap, in_b, in_ap, func, scale=1.0, bias=0.0, extra_reads=()):
        self.op("act", lambda g: g.activation(out=out_ap, in_=in_ap, func=func, scale=scale, bias=bias),
                [in_b] + list(extra_reads), [out_b])

    def mm(self, out_b, out_ap, l_b, l_ap, r_b, r_ap, start, stop):
        self.op("pe", lambda g: g.matmul(out_ap, l_ap, r_ap, start=start, stop=stop), [l_b, r_b], [out_b])


def build_uproj():
    k = KB()
    xT = k.dram("xT", [D, TPC], F32, "ExternalInput")
    w = k.dram("w", [D, D], F32, "ExternalInput")
    uT = k.dram("uT", [D, TPC], F32, "ExternalOutput")
    wsb = k.sb("w", [128, 8, D], BF16)
    k.dma("pool", wsb, wsb[:, :, :], w, w.t.rearrange("(kt p) m -> p kt m", p=128))
    xs = [k.sb("xs", [128, 8, 512], BF16) for _ in range(2)]
    pbs = [k.ps("pb") for _ in range(4)]
    sts = [k.sb("st", [128, 512], F32) for _ in range(4)]
    i = 0
    for nb in range(TPC // 512):
        xb = xs[nb % 2]
        k.dma("pool", xb, xb[:, :, :], xT, xT.t[:, nb * 512:(nb + 1) * 512].rearrange("(kt p) n -> p kt n", p=128))
        for m in range(8):
            pb = pbs[i % 4]
            st = sts[i % 4]
            for kt in range(8):
                k.mm(pb, pb[:, :], wsb, wsb[:, kt, m * 128:(m + 1) * 128], xb, xb[:, kt, :], kt == 0, kt == 7)
            if i % 2 == 0:
                k.act(st, st[:, :], pb, pb[:, :], AF.Copy)
            else:
                k.op("dve", lambda g, o=st[:, :], a=pb[:, :]: g.tensor_copy(out=o, in_=a), [pb], [st])
            k.dma("sp", uT, uT.t[m * 128:(m + 1) * 128, nb * 512:(nb + 1) * 512], st, st[:, :])
            i += 1
    return k.finish()


S5_BLK = 512


def build_s5(ntok=NTOK, seq=SEQ):
    k = KB()
    u = k.dram("u", [128, ntok], F32, "ExternalInput")
    are = k.dram("are", [128, 4], F32, "ExternalInput")
    aim = k.dram("aim", [128, 4], F32, "ExternalInput")
    ldt = k.dram("ldt", [128, 4], F32, "ExternalInput")
    bre = k.dram("bre", [128, 4, 128], F32, "ExternalInput")
    bim = k.dram("bim", [128, 4, 128], F32, "ExternalInput")
    cre = k.dram("cre", [128, 4, 32], F32, "ExternalInput")
    cim = k.dram("cim", [128, 4, 32], F32, "ExternalInput")
    dsk = k.dram("dsk", [128, 1], F32, "ExternalInput")
    y = k.dram("y", [128, ntok], F32, "ExternalOutput")
    L = S5_BLK

    def small(name, w=4, dt=F32):
        return k.sb(name, [128, w], dt)

    s_are, s_aim, s_ldt = small("are"), small("aim"), small("ldt")
    k.dma("sp", s_are, s_are[:, :], are, are.t[:, :])
    k.dma("sp", s_aim, s_aim[:, :], aim, aim.t[:, :])
    k.dma("sp", s_ldt, s_ldt[:, :], ldt, ldt.t[:, :])
    s_d = small("dsk", 1)
    k.dma("sp", s_d, s_d[:, :], dsk, dsk.t[:, :])
    b_re = k.sb("bre", [128, 4, 128], BF16)
    b_im = k.sb("bim", [128, 4, 128], BF16)
    k.dma("pool", b_re, b_re[:, :, :], bre, bre.t[:, :, :])
    k.dma("pool", b_im, b_im[:, :, :], bim, bim.t[:, :, :])
    c_re = k.sb("cre", [128, 4, 32], F32)
    c_im = k.sb("cim", [128, 4, 32], F32)
    k.dma("sp", c_re, c_re[:, :, :], cre, cre.t[:, :, :])
    k.dma("sp", c_im, c_im[:, :, :], cim, cim.t[:, :, :])

    k.ses = True
    dt_ = small("dt")
    k.act(dt_, dt_[:, :], s_ldt, s_ldt[:, :], AF.Exp)
    lre = small("lre")
    k.ts("dve", lre, lre[:, :], s_are, s_are[:, :], -1e-4, ALU.min)
    lrd = small("lrd")
    k.tt("dve", lrd, lrd[:, :], lre, lre[:, :], dt_, dt_[:, :], ALU.mult)
    rho = small("rho")
    k.act(rho, rho[:, :], lrd, lrd[:, :], AF.Exp)
    th = small("th")
    k.tt("dve", th, th[:, :], s_aim, s_aim[:, :], dt_, dt_[:, :], ALU.mult)

    def sin_of(src, shift, name):
        a = small(name + "a")
        k.ts("dve", a, a[:, :], src, src[:, :], float(shift), ALU.add)
        t = small(name + "t")
        k.ts("dve", t, t[:, :], a, a[:, :], 1.0 / (2 * PI), ALU.mult)
        ti = small(name + "ti", dt=I32)
        k.op("dve", lambda g: g.tensor_copy(out=ti[:, :], in_=t[:, :]), [t], [ti])
        tf = small(name + "tf")
        k.op("dve", lambda g: g.tensor_copy(out=tf[:, :], in_=ti[:, :]), [ti], [tf])
        r = small(name + "r")
        k.stt("dve", r, r[:, :], tf, tf[:, :], -2 * PI, a, a[:, :], ALU.mult, ALU.add)
        m1 = small(name + "m1")
        k.ts("dve", m1, m1[:, :], r, r[:, :], PI, ALU.is_gt)
        r2 = small(name + "r2")
        k.stt("dve", r2, r2[:, :], m1, m1[:, :], -2 * PI, r, r[:, :], ALU.mult, ALU.add)
        m2 = small(name + "m2")
        k.ts("dve", m2, m2[:, :], r2, r2[:, :], -PI, ALU.is_lt)
        r3 = small(name + "r3")
        k.stt("dve", r3, r3[:, :], m2, m2[:, :], 2 * PI, r2, r2[:, :], ALU.mult, ALU.add)
        r4 = small(name + "r4")
        k.ts("dve", r4, r4[:, :], r3, r3[:, :], PI, ALU.min, -PI, ALU.max)
        o = small(name + "o")
        k.act(o, o[:, :], r4, r4[:, :], AF.Sin)
        return o

    sn = sin_of(th, 0.0, "sn")
    cs = sin_of(th, PI / 2, "cs")
    abr, abi = small("abr"), small("abi")
    k.tt("dve", abr, abr[:, :], rho, rho[:, :], cs, cs[:, :], ALU.mult)
    k.tt("dve", abi, abi[:, :], rho, rho[:, :], sn, sn[:, :], ALU.mult)
    den, t0 = small("den"), small("t0")
    k.tt("dve", den, den[:, :], lre, lre[:, :], lre, lre[:, :], ALU.mult)
    k.tt("dve", t0, t0[:, :], s_aim, s_aim[:, :], s_aim, s_aim[:, :], ALU.mult)
    den2 = small("den2")
    k.tt("dve", den2, den2[:, :], den, den[:, :], t0, t0[:, :], ALU.add)
    rden = small("rden")
    k.op("dve", lambda g: g.reciprocal(out=rden[:, :], in_=den2[:, :]), [den2], [rden])
    nr = small("nr")
    k.ts("dve", nr, nr[:, :], abr, abr[:, :], -1.0, ALU.add)
    t1, t2, t3, t4 = small("t1"), small("t2"), small("t3"), small("t4")
    k.tt("dve", t1, t1[:, :], nr, nr[:, :], lre, lre[:, :], ALU.mult)
    k.tt("dve", t2, t2[:, :], abi, abi[:, :], s_aim, s_aim[:, :], ALU.mult)
    k.tt("dve", t3, t3[:, :], abi, abi[:, :], lre, lre[:, :], ALU.mult)
    k.tt("dve", t4, t4[:, :], nr, nr[:, :], s_aim, s_aim[:, :], ALU.mult)
    q1, q2 = small("q1"), small("q2")
    k.tt("dve", q1, q1[:, :], t1, t1[:, :], t2, t2[:, :], ALU.add)
    k.tt("dve", q2, q2[:, :], t3, t3[:, :], t4, t4[:, :], ALU.subtract)
    qre, qim = small("qre"), small("qim")
    k.tt("dve", qre, qre[:, :], q1, q1[:, :], rden, rden[:, :], ALU.mult)
    k.tt("dve", qim, qim[:, :], q2, q2[:, :], rden, rden[:, :], ALU.mult)
    nqim = small("nqim")
    k.ts("dve", nqim, nqim[:, :], qim, qim[:, :], -1.0, ALU.mult)
    nqre = small("nqre")
    k.ts("dve", nqre, nqre[:, :], qre, qre[:, :], -1.0, ALU.mult)

    cp_re = k.sb("cpre", [128, 4, 128], BF16)
    cp_imn = k.sb("cpimn", [128, 4, 128], BF16)
    k.op("pool", lambda g: g.memset(cp_re[:, :, :], 0.0), [], [cp_re])
    k.op("pool", lambda g: g.memset(cp_imn[:, :, :], 0.0), [], [cp_imn])
    ctmp = k.sb("ctmp", [128, 4, 32], F32)
    for qq in range(4):
        k.ts("dve", ctmp, ctmp[:, qq, :], c_im, c_im[:, qq, :], nqim[:, qq:qq + 1], ALU.mult, extra_reads=[nqim])
        k.stt("dve", cp_re, cp_re[:, qq, 32 * qq:32 * qq + 32], c_re, c_re[:, qq, :], qre[:, qq:qq + 1], ctmp, ctmp[:, qq, :],
              ALU.mult, ALU.add, extra_reads=[qre])
    ctmp2 = k.sb("ctmp2", [128, 4, 32], F32)
    for qq in range(4):
        k.ts("dve", ctmp2, ctmp2[:, qq, :], c_im, c_im[:, qq, :], nqre[:, qq:qq + 1], ALU.mult, extra_reads=[nqre])
        k.stt("dve", cp_imn, cp_imn[:, qq, 32 * qq:32 * qq + 32], c_re, c_re[:, qq, :], nqim[:, qq:qq + 1], ctmp2, ctmp2[:, qq, :],
              ALU.mult, ALU.add, extra_reads=[nqim])

    COS = k.sb("COS", [128, 4, L], F32)
    SIN = k.sb("SIN", [128, 4, L], F32)
    RHO = k.sb("RHO", [128, 4, L], F32)
    k.op("pool", lambda g: g.memset(COS[:, :, :], 1.0), [], [COS])
    k.op("pool", lambda g: g.memset(SIN[:, :, :], 0.0), [], [SIN])
    k.op("pool", lambda g: g.memset(RHO[:, :, :], 1.0), [], [RHO])
    for qq in range(4):
        k.ts("dve", RHO, RHO[:, qq, :], RHO, RHO[:, qq, :], rho[:, qq:qq + 1], ALU.mult, extra_reads=[rho])
    pr, pi_ = cs, sn
    ttmp = k.sb("ttmp", [128, 4, L // 2], F32)
    w = 1
    lvl = 0
    while w < L:
        npi = small("npi%d" % lvl)
        k.ts("dve", npi, npi[:, :], pi_, pi_[:, :], -1.0, ALU.mult)
        for qq in range(4):
            k.ts("dve", ttmp, ttmp[:, qq, 0:w], SIN, SIN[:, qq, 0:w], npi[:, qq:qq + 1], ALU.mult, extra_reads=[npi])
            k.stt("dve", COS, COS[:, qq, w:2 * w], COS, COS[:, qq, 0:w], pr[:, qq:qq + 1], ttmp, ttmp[:, qq, 0:w],
                  ALU.mult, ALU.add, extra_reads=[pr])
            k.ts("dve", ttmp, ttmp[:, qq, 0:w], SIN, SIN[:, qq, 0:w], pr[:, qq:qq + 1], ALU.mult, extra_reads=[pr])
            k.stt("dve", SIN, SIN[:, qq, w:2 * w], COS, COS[:, qq, 0:w], pi_[:, qq:qq + 1], ttmp, ttmp[:, qq, 0:w],
                  ALU.mult, ALU.add, extra_reads=[pi_])
        a2, b2, ab_ = small("a2_%d" % lvl), small("b2_%d" % lvl), small("ab_%d" % lvl)
        k.tt("dve", a2, a2[:, :], pr, pr[:, :], pr, pr[:, :], ALU.mult)
        k.tt("dve", b2, b2[:, :], pi_, pi_[:, :], pi_, pi_[:, :], ALU.mult)
        k.tt("dve", ab_, ab_[:, :], pr, pr[:, :], pi_, pi_[:, :], ALU.mult)
        npr, npi2 = small("npr%d" % lvl), small("npi2%d" % lvl)
        k.tt("dve", npr, npr[:, :], a2, a2[:, :], b2, b2[:, :], ALU.subtract)
        k.ts("dve", npi2, npi2[:, :], ab_, ab_[:, :], 2.0, ALU.mult)
        pr, pi_ = npr, npi2
        w *= 2
        lvl += 1
    ELr, ELi = pr, pi_
    nELi = small("nELi")
    k.ts("dve", nELi, nELi[:, :], ELi, ELi[:, :], -1.0, ALU.mult)

    k.ses = SES
    NP = 3
    ub = [k.sb("ub", [128, L], F32) for _ in range(2)]
    ubf = [k.sb("ubf", [128, L], BF16) for _ in range(2)]
    ps_re = [k.ps("psre") for _ in range(NP)]
    ps_im = [k.ps("psim") for _ in range(NP)]
    ps_y = [k.ps("psy") for _ in range(2)]
    T = [[k.sb("T%d" % j, [128, L], F32) for j in range(4)] for _ in range(NP)]
    gin = [[k.sb("gin%d" % j, [128, L], F32) for j in range(2)] for _ in range(NP)]
    gg = [[[k.sb("g%d_%d" % (qq, j), [128, L], F32) for j in range(2)] for qq in range(4)] for _ in range(2)]
    T2 = [[k.sb("U%d" % j, [128, L], F32) for j in range(4)] for _ in range(NP)]
    hh = [[k.sb("h%d" % j, [128, L], BF16) for j in range(2)] for _ in range(NP)]
    ini = [[k.sb("ini%d_%d" % (qq, j), [128, 1], F32) for j in range(2)] for qq in range(4)]
    itmp = [k.sb("itmp%d" % qq, [128, 1], F32) for qq in range(4)]
    yo = [k.sb("yo", [128, L], F32) for _ in range(2)]
    nblk = ntok // L
    bps = seq // L
    it = 0
    for blk in range(nblk):
        sl = slice(blk * L, (blk + 1) * L)
        uB, uF, pY = ub[blk % 2], ubf[blk % 2], ps_y[blk % 2]
        k.dma("sp", uB, uB[:, :], u, u.t[:, sl])
        k.act(uF, uF[:, :], uB, uB[:, :], AF.Copy)
        first = (blk % bps == 0)
        for qq in range(4):
            par = it % NP
            it += 1
            pr_, pi2_ = ps_re[par], ps_im[par]
            k.mm(pr_, pr_[:, :], b_re, b_re[:, qq, :], uF, uF[:, :], True, True)
            k.mm(pi2_, pi2_[:, :], b_im, b_im[:, qq, :], uF, uF[:, :], True, True)
            t = T[par]
            cq, sq, rq = COS[:, qq, :], SIN[:, qq, :], RHO[:, qq, :]
            k.tt("dve", t[0], t[0][:, :], pr_, pr_[:, :], COS, cq, ALU.mult)
            k.tt("dve", t[1], t[1][:, :], pi2_, pi2_[:, :], SIN, sq, ALU.mult)
            k.tt("dve", t[2], t[2][:, :], pi2_, pi2_[:, :], COS, cq, ALU.mult)
            k.tt("dve", t[3], t[3][:, :], pr_, pr_[:, :], SIN, sq, ALU.mult)
            gi = gin[par]
            k.tt("pool", gi[0], gi[0][:, :], t[0], t[0][:, :], t[1], t[1][:, :], ALU.add)
            k.tt("dve", gi[1], gi[1][:, :], t[2], t[2][:, :], t[3], t[3][:, :], ALU.subtract)
            g_prev = gg[(blk + 1) % 2][qq]
            g_cur = gg[blk % 2][qq]
            if first:
                i_re, i_im = 0.0, 0.0
                xr = []
            else:
                k.ts("pool", itmp[qq], itmp[qq][:, :], g_prev[1], g_prev[1][:, L - 1:L], nELi[:, qq:qq + 1], ALU.mult,
                     extra_reads=[nELi])
                k.stt("dve", ini[qq][0], ini[qq][0][:, :], g_prev[0], g_prev[0][:, L - 1:L], ELr[:, qq:qq + 1],
                      itmp[qq], itmp[qq][:, :], ALU.mult, ALU.add, extra_reads=[ELr])
                k.ts("pool", itmp[qq], itmp[qq][:, :], g_prev[1], g_prev[1][:, L - 1:L], ELr[:, qq:qq + 1], ALU.mult,
                     extra_reads=[ELr])
                k.stt("dve", ini[qq][1], ini[qq][1][:, :], g_prev[0], g_prev[0][:, L - 1:L], ELi[:, qq:qq + 1],
                      itmp[qq], itmp[qq][:, :], ALU.mult, ALU.add, extra_reads=[ELi])
                i_re, i_im = ini[qq][0][:, :], ini[qq][1][:, :]
                xr = [ini[qq][0], ini[qq][1]]
            k.op("dve", lambda g, o=g_cur[0][:, :], a=rq, b=gi[0][:, :], i0=i_re: g.tensor_tensor_scan(
                out=o, data0=a, data1=b, initial=i0, op0=ALU.mult, op1=ALU.add), [RHO, gi[0]] + xr[:1], [g_cur[0]])
            k.op("dve", lambda g, o=g_cur[1][:, :], a=rq, b=gi[1][:, :], i0=i_im: g.tensor_tensor_scan(
                out=o, data0=a, data1=b, initial=i0, op0=ALU.mult, op1=ALU.add), [RHO, gi[1]] + xr[1:], [g_cur[1]])
            t2_ = T2[par]
            k.tt("pool", t2_[0], t2_[0][:, :], g_cur[0], g_cur[0][:, :], COS, cq, ALU.mult)
            k.tt("dve", t2_[1], t2_[1][:, :], g_cur[1], g_cur[1][:, :], SIN, sq, ALU.mult)
            k.tt("pool", t2_[2], t2_[2][:, :], g_cur[1], g_cur[1][:, :], COS, cq, ALU.mult)
            k.tt("dve", t2_[3], t2_[3][:, :], g_cur[0], g_cur[0][:, :], SIN, sq, ALU.mult)
            h = hh[par]
            k.tt("dve", h[0], h[0][:, :], t2_[0], t2_[0][:, :], t2_[1], t2_[1][:, :], ALU.subtract)
            k.tt("dve", h[1], h[1][:, :], t2_[2], t2_[2][:, :], t2_[3], t2_[3][:, :], ALU.add)
            k.mm(pY, pY[:, :], cp_re, cp_re[:, qq, :], h[0], h[0][:, :], qq == 0, False)
            k.mm(pY, pY[:, :], cp_imn, cp_imn[:, qq, :], h[1], h[1][:, :], False, qq == 3)
        yB = yo[blk % 2]
        k.stt("dve", yB, yB[:, :], uB, uB[:, :], s_d[:, 0:1], pY, pY[:, :], ALU.mult, ALU.add, extra_reads=[s_d])
        k.dma("sp", y, y.t[:, sl], yB, yB[:, :])
    return k.finish()


def s5_core_params(c, a_re, a_im, log_dt, b_re, b_im, c_re, c_im, d):
    g0 = 8 * c

    def pq(a):
        return np.ascontiguousarray(a[g0:g0 + 8].reshape(4, 2, 64).transpose(1, 2, 0).reshape(128, 4)).astype(np.float32)

    are = pq(a_re)
    aim = pq(a_im)
    ldt = pq(np.broadcast_to(log_dt[:, None], (64, 64)))
    B_re = np.zeros((4, 2, 16, 4, 2, 64), np.float32)
    B_im = np.zeros((4, 2, 16, 4, 2, 64), np.float32)
    C_re = np.zeros((2, 64, 4, 2, 16), np.float32)
    C_im = np.zeros((2, 64, 4, 2, 16), np.float32)
    for qq in range(4):
        for gp in range(2):
            g = g0 + 2 * qq + gp
            B_re[qq, gp, :, qq, gp, :] = b_re[g].T
            B_im[qq, gp, :, qq, gp, :] = b_im[g].T
            C_re[gp, :, qq, gp, :] = c_re[g].T
            C_im[gp, :, qq, gp, :] = c_im[g].T
    return {"are": are, "aim": aim, "ldt": ldt,
            "bre": B_re.reshape(128, 4, 128), "bim": B_im.reshape(128, 4, 128),
            "cre": C_re.reshape(128, 4, 32), "cim": C_im.reshape(128, 4, 32),
            "dsk": np.ascontiguousarray(d[128 * c:128 * c + 128].reshape(128, 1)).astype(np.float32)}


class Ctx:
    def __init__(self, k, NT, nkt_half):
        self.k = k
        self.NT = NT
        self.ps1 = [k.ps("ps1") for _ in range(2)]
        self.ps3 = [k.ps("ps3") for _ in range(2)]
        self.pso = [k.ps("pso") for _ in range(2)]
        self.pss = [k.ps("pss") for _ in range(2)]
        self.wa = [k.sb("wa", [128, 8, 512], BF16) for _ in range(2)]
        self.wb = [k.sb("wb", [128, 8, 512], BF16) for _ in range(2)]
        self.w2h = k.sb("w2h", [128, nkt_half, 1024], BF16)
        self.hmid = k.sb("hmid", [128, max(nkt_half, 8), NT], BF16)
        self.z = k.sb("z", [128, 8, NT], F32)
        self.xbf = k.sb("xbf", [128, 8, NT], BF16)
        self.sact = [k.sb("sact", [128, 512], F32) for _ in range(2)]
        self.tmp = [k.sb("tmp", [128, 512], F32) for _ in range(2)]
        self.ones = k.sb("ones", [128, 128], F32)
        k.op("pool", lambda g: g.memset(self.ones[:, :], 1.0), [], [self.ones])
        self.sq = [k.sb("sq", [128, 512], F32) for _ in range(2)]
        self.mean = k.sb("mean", [128, 512], F32)
        self.ex2 = k.sb("ex2", [128, 512], F32)
        self.rstd = k.sb("rstd", [128, 512], F32)
        self.nmr = k.sb("nmr", [128, 512], F32)
        self.lng = k.sb("lng", [128, 32], F32)
        self.lnb = k.sb("lnb", [128, 32], F32)
        self.wi = 0
        self.pi = 0
        self.oi = 0


def load_wblock(k, buf, wd, k0, nk, c0, cw):
    k.dma("pool", buf, buf[:, 0:nk, 0:cw], wd, wd.t[k0 * 128:(k0 + nk) * 128, c0:c0 + cw].rearrange("(kt p) m -> p kt m", p=128))


def glu_phase(k, cx, x_in, wa_d, a_c0, wb_d, b_c0, ncols, func, emit, hook=None, NT=None):
    NT = cx.NT if NT is None else NT
    for c0 in range(0, ncols, 512):
        cw = min(512, ncols - c0)
        wa, wb = cx.wa[cx.wi % 2], cx.wb[cx.wi % 2]
        cx.wi += 1
        load_wblock(k, wa, wa_d, 0, 8, a_c0 + c0, cw)
        load_wblock(k, wb, wb_d, 0, 8, b_c0 + c0, cw)
        if hook is not None and c0 == 512:
            hook()
        for fl in range(cw // 128):
            ft = c0 // 128 + fl
            for t0 in range(0, NT, 512):
                tw = min(512, NT - t0)
                p1, p3 = cx.ps1[cx.pi % 2], cx.ps3[cx.pi % 2]
                sa = cx.sact[cx.pi % 2]
                cx.pi += 1
                ts_ = slice(t0, t0 + tw)
                for kt in range(8):
                    k.mm(p1, p1[:, 0:tw], wa, wa[:, kt, fl * 128:fl * 128 + 128], x_in, x_in[:, kt, ts_], kt == 0, kt == 7)
                for kt in range(8):
                    k.mm(p3, p3[:, 0:tw], wb, wb[:, kt, fl * 128:fl * 128 + 128], x_in, x_in[:, kt, ts_], kt == 0, kt == 7)
                k.act(sa, sa[:, 0:tw], p1, p1[:, 0:tw], func)
                emit(ft, t0, tw, sa, p3)


def down_load(k, cx, w2_d, k0, nk):
    k.dma("pool", cx.w2h, cx.w2h[:, 0:nk, :], w2_d, w2_d.t[k0 * 128:(k0 + nk) * 128, :].rearrange("(kt p) m -> p kt m", p=128))


def down_phase(k, cx, w2_d, k0, nk, zacc):
    NT = cx.NT
    for m in range(8):
        for tb in range(NT // 512):
            po = cx.pso[cx.oi % 2]
            cx.oi += 1
            ts_ = slice(tb * 512, tb * 512 + 512)
            for kt in range(nk):
                k.mm(po, po[:, :], cx.w2h, cx.w2h[:, kt, m * 128:m * 128 + 128], cx.hmid, cx.hmid[:, kt, ts_], kt == 0, kt == nk - 1)
            k.tt("dve", zacc, zacc[:, m, ts_], po, po[:, :], zacc, zacc[:, m, ts_], ALU.add)


def layer_norm(k, cx, z, ln_idx, xbf_out):
    NT = cx.NT
    for tb in range(NT // 512):
        ts_ = slice(tb * 512, tb * 512 + 512)
        s1, s2 = cx.pss[0], cx.pss[1]
        for kt in range(8):
            k.mm(s1, s1[:, :], cx.ones, cx.ones[:, :], z, z[:, kt, ts_], kt == 0, kt == 7)
        for kt in range(8):
            sq = cx.sq[kt % 2]
            k.act(sq, sq[:, :], z, z[:, kt, ts_], AF.Square)
            k.mm(s2, s2[:, :], cx.ones, cx.ones[:, :], sq, sq[:, :], kt == 0, kt == 7)
        k.act(cx.mean, cx.mean[:, :], s1, s1[:, :], AF.Copy, scale=1.0 / D)
        k.act(cx.ex2, cx.ex2[:, :], s2, s2[:, :], AF.Copy, scale=1.0 / D)
        k.tt("dve", cx.nmr, cx.nmr[:, :], cx.mean, cx.mean[:, :], cx.mean, cx.mean[:, :], ALU.mult)
        k.tt("dve", cx.ex2, cx.ex2[:, :], cx.ex2, cx.ex2[:, :], cx.nmr, cx.nmr[:, :], ALU.subtract)
        k.ts("dve", cx.ex2, cx.ex2[:, :], cx.ex2, cx.ex2[:, :], LN_EPS, ALU.add)
        k.act(cx.ex2, cx.ex2[:, :], cx.ex2, cx.ex2[:, :], AF.Sqrt)
        k.op("dve", lambda g, o_=cx.rstd[:, :], i_=cx.ex2[:, :]: g.reciprocal(out=o_, in_=i_), [cx.ex2], [cx.rstd])
        k.stt("dve", cx.nmr, cx.nmr[:, :], cx.mean, cx.mean[:, :], -1.0, cx.rstd, cx.rstd[:, :], ALU.mult, ALU.mult)
        for kt in range(8):
            t = cx.tmp[kt % 2]
            k.tt("dve", t, t[:, :], z, z[:, kt, ts_], cx.rstd, cx.rstd[:, :], ALU.mult)
            k.tt("dve", t, t[:, :], t, t[:, :], cx.nmr, cx.nmr[:, :], ALU.add)
            col = ln_idx * 8 + kt
            k.op("act", lambda g, o=z[:, kt, ts_], i=t[:, :], sc=cx.lng[:, col:col + 1], bi=cx.lnb[:, col:col + 1]: g.activation(
                out=o, in_=i, func=AF.Identity, scale=sc, bias=bi),
                [t, cx.lng, cx.lnb], [z])
            if xbf_out is not None:
                k.act(xbf_out, xbf_out[:, kt, ts_], z, z[:, kt, ts_], AF.Copy)


def ln_layout(ln_g, ln_b):
    g = np.ascontiguousarray(ln_g.reshape(4, 8, 128).transpose(2, 0, 1).reshape(128, 32)).astype(np.float32)
    b = np.ascontiguousarray(ln_b.reshape(4, 8, 128).transpose(2, 0, 1).reshape(128, 32)).astype(np.float32)
    return g, b


DFF = 2816


def build_layer0_rest(tpc=TPC, NT=1024):
    k = KB()
    yT = k.dram("yT", [D, tpc], F32, "ExternalInput")
    xT = k.dram("xT", [D, tpc], F32, "ExternalInput")
    wglu = k.dram("wglu", [D, 2 * D], F32, "ExternalInput")
    w1 = k.dram("w1", [D, DFF], F32, "ExternalInput")
    w3 = k.dram("w3", [D, DFF], F32, "ExternalInput")
    w2 = k.dram("w2", [DFF, D], F32, "ExternalInput")
    lng = k.dram("lng", [128, 32], F32, "ExternalInput")
    lnb = k.dram("lnb", [128, 32], F32, "ExternalInput")
    oT = k.dram("oT", [D, tpc], F32, "ExternalOutput")
    cx = Ctx(k, NT, 11)
    k.dma("sp", cx.lng, cx.lng[:, :], lng, lng.t[:, :])
    k.dma("sp", cx.lnb, cx.lnb[:, :], lnb, lnb.t[:, :])
    ystage = [k.sb("ystage", [128, NT], F32) for _ in range(2)]
    gbf = cx.hmid
    z = cx.z
    for sb_ in range(tpc // NT):
        tsl = slice(sb_ * NT, (sb_ + 1) * NT)
        for kt in range(8):
            ys = ystage[kt % 2]
            k.dma("sp", ys, ys[:, :], yT, yT.t[kt * 128:(kt + 1) * 128, tsl])
            k.act(gbf, gbf[:, kt, :], ys, ys[:, :], AF.Gelu)
            k.dma("sp", z, z[:, kt, :], xT, xT.t[kt * 128:(kt + 1) * 128, tsl])

        def emit_glu(ft, t0, tw, sa, pB):
            t = cx.tmp[(ft + t0 // 512) % 2]
            ts_ = slice(t0, t0 + 512)
            k.tt("dve", t, t[:, :], pB, pB[:, :], sa, sa[:, :], ALU.mult)
            k.stt("dve", z, z[:, ft, ts_], z, z[:, ft, ts_], float(ALPHA), t, t[:, :], ALU.mult, ALU.add)

        glu_phase(k, cx, gbf, wglu, D, wglu, 0, D, AF.Sigmoid, emit_glu)
        layer_norm(k, cx, z, 0, cx.xbf)
        for kt in range(8):
            k.ts("dve", z, z[:, kt, :], z, z[:, kt, :], float(ALPHA), ALU.mult)
        for half in range(2):
            f0 = half * 11

            def emit_ffn(ft, t0, tw, sa, pB):
                ts_ = slice(t0, t0 + 512)
                k.tt("dve", cx.hmid, cx.hmid[:, ft, ts_], pB, pB[:, :], sa, sa[:, :], ALU.mult)

            glu_phase(k, cx, cx.xbf, w1, f0 * 128, w3, f0 * 128, 11 * 128, AF.Silu, emit_ffn,
                      hook=lambda f0=f0: down_load(k, cx, w2, f0, 11))
            down_phase(k, cx, w2, f0, 11, z)
        layer_norm(k, cx, z, 1, None)
        for kt in range(8):
            k.dma("sp", oT, oT.t[kt * 128:(kt + 1) * 128, tsl], z, z[:, kt, :])
    return k.finish()


NQKV = 9216
DILS = (1, 4, 16)


def build_qkv(tpc=TPC):
    k = KB()
    xT = k.dram("xT", [D, tpc], F32, "ExternalInput")
    w = k.dram("w", [D, NQKV], F32, "ExternalInput")
    o = k.dram("qkvT", [NQKV, tpc], BF16, "ExternalOutput")
    xb = k.sb("xb", [128, 8, tpc], BF16)
    for kt in range(8):
        k.dma("pool", xb, xb[:, kt, :], xT, xT.t[kt * 128:(kt + 1) * 128, :])
    wbuf = [k.sb("wq", [128, 8, 512], BF16) for _ in range(2)]
    pbs = [k.ps("pb") for _ in range(4)]
    sts = [k.sb("st", [128, 512], BF16) for _ in range(4)]
    i = 0
    for cb in range(NQKV // 512):
        wb = wbuf[cb % 2]
        load_wblock(k, wb, w, 0, 8, cb * 512, 512)
        for fl in range(4):
            for tb in range(tpc // 512):
                pb, st = pbs[i % 4], sts[i % 4]
                ts_ = slice(tb * 512, tb * 512 + 512)
                for kt in range(8):
                    k.mm(pb, pb[:, :], wb, wb[:, kt, fl * 128:fl * 128 + 128], xb, xb[:, kt, ts_], kt == 0, kt == 7)
                if i % 2 == 0:
                    k.act(st, st[:, :], pb, pb[:, :], AF.Copy)
                else:
                    k.op("dve", lambda g, o_=st[:, :], a=pb[:, :]: g.tensor_copy(out=o_, in_=a), [pb], [st])
                r0 = cb * 512 + fl * 128
                k.dma("sp", o, o.t[r0:r0 + 128, ts_], st, st[:, :])
                i += 1
    return k.finish()


ACH = 2048
NEG = -1e30


def build_attn(tpc=TPC, LA=3):
    k = KB()
    nblk = tpc // 128
    qb_d = k.dram("qb", [3, 8, 128, nblk * 128], BF16, "ExternalInput")
    kb_d = k.dram("kb", [3, 8, 128, (nblk + 16) * 128], BF16, "ExternalInput")
    vb_d = k.dram("vb", [3, 8, 128, (nblk + 16) * 128], BF16, "ExternalInput")
    bias_d = k.dram("biasT", [3, 8, 128, 512], F32, "ExternalInput")
    negb_d = k.dram("negb", [128, 1], F32, "ExternalInput")
    o = k.dram("attnT", [D, tpc], F32, "ExternalOutput")
    negb = k.sb("negb", [128, 1], F32)
    k.dma("sp", negb, negb[:, :], negb_d, negb_d.t[:, :])
    ones = k.sb("ones", [128, 64], BF16)
    k.op("pool", lambda g: g.memset(ones[:, :], 1.0), [], [ones])
    Qb = [k.sb("Qb", [128, 16 * 128], BF16) for _ in range(2)]
    Kb = [k.sb("Kb", [128, 32 * 128], BF16) for _ in range(2)]
    Vb = [k.sb("Vb", [128, 32, 128], BF16) for _ in range(2)]
    Bs = [k.sb("Bs", [128, 2, 256], F32) for _ in range(2)]
    num = [k.sb("num", [128, ACH], F32) for _ in range(2)]
    den = [k.sb("den", [128, ACH], F32) for _ in range(2)]
    rec = k.sb("rec", [128, ACH], F32)
    NS = LA + 1
    ps_s = [k.ps("ps_s", (128, 256)) for _ in range(NS)]
    ps_o = [k.ps("ps_o") for _ in range(2)]
    ps_d = [k.ps("ps_d") for _ in range(2)]
    tmp = [k.sb("tmpS", [128, 256], F32) for _ in range(NS + 1)]
    pT = [k.sb("pT", [128, 256], BF16) for _ in range(NS + 1)]
    units = []
    li = 0
    ei = 0
    ci = 0
    for ch in range(tpc // ACH):
        for ft in range(8):
            nm, dn = num[ci % 2], den[ci % 2]
            ci += 1
            for g in range(3):
                d = DILS[g]
                bufs = (Qb[li % 2], Kb[li % 2], Vb[li % 2], Bs[li % 2])
                li += 1
                for B in range(16):
                    for hh in range(2):
                        units.append(dict(ch=ch, ft=ft, g=g, d=d, B=B, hh=hh, bufs=bufs, nm=nm, dn=dn,
                                          po=ps_o[ei % 2], pd=ps_d[ei % 2], load=(B == 0 and hh == 0),
                                          evac=(B % 4 == 3 and hh == 1), last=(g == 2 and B == 15 and hh == 1)))
                    if B % 4 == 3:
                        ei += 1

    def s_part(ui, un):
        qb, kb, vb, bs = un["bufs"]
        ch, ft, g, d, B, hh = un["ch"], un["ft"], un["g"], un["d"], un["B"], un["hh"]
        if un["load"]:
            b0 = ch * 16
            k.dma("sp", qb, qb[:, :], qb_d, qb_d.t[g, ft, :, b0 * 128:(b0 + 16) * 128])
            nkb = 16 + d
            k0 = 16 + b0 - d
            k.dma("sp", kb, kb[:, 0:nkb * 128], kb_d, kb_d.t[g, ft, :, k0 * 128:(k0 + nkb) * 128])
            k.dma("sp", vb, vb[:, 0:nkb, :], vb_d,
                  vb_d.t[g, ft, :, k0 * 128:(k0 + nkb) * 128].rearrange("p (b f) -> p b f", f=128))
            k.dma("sp", bs, bs[:, :, :], bias_d, bias_d.t[g, ft, :, :].rearrange("p (h f) -> p h f", h=2))
        halo = (ch == 0 and B < d)
        rs = slice(64 * hh, 64 * hh + 64)
        pss = ps_s[ui % NS]
        tm, pt = tmp[ui % (NS + 1)], pT[ui % (NS + 1)]
        qs = slice(B * 128, B * 128 + 128)
        kp = slice(B * 128, B * 128 + 128)
        kc = slice((B + d) * 128, (B + d) * 128 + 128)
        k.mm(pss, pss[:, 0:128], kb, kb[rs, kp], qb, qb[rs, qs], True, True)
        k.mm(pss, pss[:, 128:256], kb, kb[rs, kc], qb, qb[rs, qs], True, True)
        k.stt("dve", tm, tm[:, :], pss, pss[:, 0:256], 0.125, bs, bs[:, hh, :], ALU.mult, ALU.add)
        if halo:
            k.op("act", lambda e, o_=pt[:, 0:128], i_=tm[:, 0:128]: e.activation(
                out=o_, in_=i_, func=AF.Exp, bias=negb[:, 0:1], scale=1.0), [tm, negb], [pt])
            k.act(pt, pt[:, 128:256], tm, tm[:, 128:256], AF.Exp)
        else:
            k.act(pt, pt[:, :], tm, tm[:, :], AF.Exp)

    def pv_part(ui, un):
        qb, kb, vb, bs = un["bufs"]
        ch, ft, g, d, B, hh = un["ch"], un["ft"], un["g"], un["d"], un["B"], un["hh"]
        po, pd, nm, dn = un["po"], un["pd"], un["nm"], un["dn"]
        rs = slice(64 * hh, 64 * hh + 64)
        pt = pT[ui % (NS + 1)]
        cs = slice((B % 4) * 128, (B % 4) * 128 + 128)
        k.mm(po, po[rs, cs], vb, vb[:, B, rs], pt, pt[:, 0:128], True, False)
        k.mm(po, po[rs, cs], vb, vb[:, B + d, rs], pt, pt[:, 128:256], False, True)
        k.mm(pd, pd[rs, cs], ones, ones[:, :], pt, pt[:, 0:128], True, False)
        k.mm(pd, pd[rs, cs], ones, ones[:, :], pt, pt[:, 128:256], False, True)
        if un["evac"]:
            B0 = B - 3
            if d == 1:
                ov = lambda t: t[:, B0 * 128:(B0 + 4) * 128]
                iv = lambda p: p[:, :]
            else:
                n_, r0 = B0 // d, B0 % d
                base = n_ * 128 * d
                ov = lambda t, base=base, d=d, r0=r0: t[:, base:base + 128 * d].rearrange(
                    "p (i r) -> p i r", r=d)[:, :, r0:r0 + 4]
                iv = lambda p: p[:, :].rearrange("p (j i) -> p i j", j=4)
            if g == 0:
                k.op("dve", lambda e, o_=ov(nm), i_=iv(po): e.tensor_copy(out=o_, in_=i_), [po], [nm])
                k.act(dn, ov(dn), pd, iv(pd), AF.Copy)
            else:
                k.tt("dve", nm, ov(nm), po, iv(po), nm, ov(nm), ALU.add)
                k.tt("pool", dn, ov(dn), pd, iv(pd), dn, ov(dn), ALU.add) if False else \
                    k.tt("dve", dn, ov(dn), pd, iv(pd), dn, ov(dn), ALU.add)
        if un["last"]:
            k.op("dve", lambda e, o_=rec[:, :], i_=dn[:, :]: e.reciprocal(out=o_, in_=i_), [dn], [rec])
            k.tt("pool", nm, nm[:, :], nm, nm[:, :], rec, rec[:, :], ALU.mult)
            k.dma("sp", o, o.t[ft * 128:(ft + 1) * 128, ch * ACH:(ch + 1) * ACH], nm, nm[:, :])

    n = len(units)
    for i in range(n + LA):
        if i < n:
            s_part(i, units[i])
        if i - LA >= 0:
            pv_part(i - LA, units[i - LA])
    return k.finish()


def t5_bucket(dist):
    n_buckets, max_distance = 32, 2048
    max_exact = n_buckets // 2
    d = np.maximum(dist, 0)
    large = max_exact + (np.log(np.maximum(d, 1) / max_exact) / math.log(max_distance / max_exact)
                         * (n_buckets - max_exact)).astype(np.int64)
    large = np.minimum(large, n_buckets - 1)
    return np.where(d < max_exact, d, large).astype(np.int32)


def attn_bias_table(rel_bias):
    L = 128
    qi = np.arange(L)[:, None]
    ki = np.arange(2 * L)[None, :]
    off = qi + L - ki
    band = (off >= 0) & (off <= L)
    out = np.zeros((3, 8, 128, 2, 2, 128), np.float32)
    for g, d in enumerate(DILS):
        bucket = t5_bucket(np.clip(off, 0, None) * d)
        bias = rel_bias[bucket]
        bias = np.where(band[:, :, None], bias, np.float32(NEG))
        b = bias.reshape(L, 2, L, 8, 2)
        out[g] = b.transpose(3, 2, 4, 1, 0)
    return out.reshape(3, 8, 128, 512)


def attn_host_layout(qkvT, rel_bias, seqlen, tpc):
    ntok = qkvT.shape[1]
    ncore = ntok // tpc
    nblk = tpc // 128
    bias = attn_bias_table(rel_bias)
    perm = {}
    for g, d in enumerate(DILS):
        for j in range(3):
            a = qkvT[(g * 3 + j) * 1024:(g * 3 + j + 1) * 1024]
            a = a.reshape(8, 128, ntok // (128 * d), 128, d).transpose(0, 1, 2, 4, 3)
            perm[g, j] = a.reshape(8, 128, ntok // 128, 128)
    maps = []
    for c in range(ncore):
        first = (c * tpc) % seqlen == 0
        B0 = c * nblk
        qb = np.stack([perm[g, 0][:, :, B0:B0 + nblk].reshape(8, 128, nblk * 128) for g in range(3)])
        kbs, vbs = [], []
        for g in range(3):
            kk = np.zeros((8, 128, nblk + 16, 128), qkvT.dtype)
            vv = np.zeros((8, 128, nblk + 16, 128), qkvT.dtype)
            lo = 0 if first else 16
            kk[:, :, 16 - lo:] = perm[g, 1][:, :, B0 - lo:B0 + nblk]
            vv[:, :, 16 - lo:] = perm[g, 2][:, :, B0 - lo:B0 + nblk]
            kbs.append(kk.reshape(8, 128, -1))
            vbs.append(vv.transpose(0, 3, 2, 1).reshape(8, 128, -1))
        maps.append({"qb": np.ascontiguousarray(qb), "kb": np.ascontiguousarray(np.stack(kbs)),
                     "vb": np.ascontiguousarray(np.stack(vbs)), "biasT": bias,
                     "negb": np.full((128, 1), NEG if first else 0.0, np.float32)})
    return maps


NEXP = 8
DFE = 3584


def build_layer1_rest(tpc=TPC, NT=1024):
    k = KB()
    aT = k.dram("aT", [D, tpc], F32, "ExternalInput")
    xT = k.dram("xT", [D, tpc], F32, "ExternalInput")
    wo = k.dram("wo", [D, D], F32, "ExternalInput")
    wr = k.dram("wr", [128, 8, NEXP], F32, "ExternalInput")
    w1 = k.dram("w1", [NEXP, D, DFE], F32, "ExternalInput")
    w3 = k.dram("w3", [NEXP, D, DFE], F32, "ExternalInput")
    w2 = k.dram("w2", [NEXP, DFE, D], F32, "ExternalInput")
    lng = k.dram("lng", [128, 32], F32, "ExternalInput")
    lnb = k.dram("lnb", [128, 32], F32, "ExternalInput")
    ident_d = k.dram("ident", [128, 128], F32, "ExternalInput")
    oT = k.dram("oT", [D, tpc], F32, "ExternalOutput")
    cx = Ctx(k, NT, 14)
    k.dma("sp", cx.lng, cx.lng[:, :], lng, lng.t[:, :])
    k.dma("sp", cx.lnb, cx.lnb[:, :], lnb, lnb.t[:, :])
    ident = k.sb("ident", [128, 128], F32)
    k.dma("sp", ident, ident[:, :], ident_d, ident_d.t[:, :])
    wr_sb = k.sb("wr", [128, 8, NEXP], F32)
    k.dma("sp", wr_sb, wr_sb[:, :, :], wr, wr.t[:, :, :])
    ones_bf = k.sb("ones_bf", [128, 128], BF16)
    k.op("pool", lambda g: g.memset(ones_bf[:, :], 1.0), [], [ones_bf])
    gate_bc = k.sb("gate_bc", [128, NEXP, NT], BF16)
    ntt = NT // 128
    lg = k.sb("lg", [128, ntt, 8], F32)
    top = k.sb("top", [128, ntt, 8], F32)
    gsm = [k.sb("gsm%d" % i, [128, ntt], F32) for i in range(4)]
    gate = k.sb("gate", [128, ntt, 8], F32)
    gt2 = k.sb("gt2", [128, ntt, 8], F32)
    diag = [k.sb("diag", [128, 128], BF16) for _ in range(2)]
    z = cx.z
    abf = cx.hmid
    for sb_ in range(tpc // NT):
        tsl = slice(sb_ * NT, (sb_ + 1) * NT)
        for kt in range(8):
            k.dma("pool", abf, abf[:, kt, :], aT, aT.t[kt * 128:(kt + 1) * 128, tsl])
            k.dma("sp", z, z[:, kt, :], xT, xT.t[kt * 128:(kt + 1) * 128, tsl])
        for cb in range(2):
            wa = cx.wa[cx.wi % 2]
            cx.wi += 1
            load_wblock(k, wa, wo, 0, 8, cb * 512, 512)
            for fl in range(4):
                m = cb * 4 + fl
                for tb in range(NT // 512):
                    po = cx.pso[cx.oi % 2]
                    cx.oi += 1
                    ts_ = slice(tb * 512, tb * 512 + 512)
                    for kt in range(8):
                        k.mm(po, po[:, :], wa, wa[:, kt, fl * 128:fl * 128 + 128], abf, abf[:, kt, ts_], kt == 0, kt == 7)
                    k.stt("dve", z, z[:, m, ts_], z, z[:, m, ts_], float(ALPHA), po, po[:, :], ALU.mult, ALU.add)
        layer_norm(k, cx, z, 2, cx.xbf)
        k.ses = True
        pl = cx.pss[0]
        for tt in range(ntt):
            for kt in range(8):
                k.mm(pl, pl[:, tt * 8:tt * 8 + 8], z, z[:, kt, tt * 128:(tt + 1) * 128], wr_sb, wr_sb[:, kt, :], kt == 0, kt == 7)
        k.act(lg, lg[:, :, :], pl, pl[:, 0:ntt * 8].rearrange("p (t e) -> p t e", e=8), AF.Copy)
        nv1, e2, g1, g2 = gsm
        for tt in range(ntt):
            k.op("dve", lambda g, o=top[:, tt, :], i=lg[:, tt, :]: g.max(out=o, in_=i), [lg], [top])
        k.ts("dve", nv1, nv1[:, :], top, top[:, :, 0], -1.0, ALU.mult)
        k.tt("dve", e2, e2[:, :], top, top[:, :, 1], nv1, nv1[:, :], ALU.add)
        k.act(e2, e2[:, :], e2, e2[:, :], AF.Exp)
        k.ts("dve", g1, g1[:, :], e2, e2[:, :], 1.0, ALU.add)
        k.op("dve", lambda g: g.reciprocal(out=g1[:, :], in_=g1[:, :]), [g1], [g1])
        k.tt("dve", g2, g2[:, :], e2, e2[:, :], g1, g1[:, :], ALU.mult)
        for tt in range(ntt):
            k.ts("dve", gate, gate[:, tt, :], lg, lg[:, tt, :], top[:, tt, 0:1], ALU.is_equal, g1[:, tt:tt + 1], ALU.mult,
                 extra_reads=[top, g1])
            k.ts("dve", gt2, gt2[:, tt, :], lg, lg[:, tt, :], top[:, tt, 1:2], ALU.is_equal, g2[:, tt:tt + 1], ALU.mult,
                 extra_reads=[top, g2])
        k.tt("dve", gate, gate[:, :, :], gate, gate[:, :, :], gt2, gt2[:, :, :], ALU.add)
        di = 0
        for e in range(NEXP):
            for hb in range(NT // 512):
                pg = cx.pss[1]
                for t4 in range(4):
                    tt = hb * 4 + t4
                    dg = diag[di % 2]
                    di += 1
                    k.ts("dve", dg, dg[:, :], ident, ident[:, :], gate[:, tt, e:e + 1], ALU.mult, extra_reads=[gate])
                    k.mm(pg, pg[:, t4 * 128:(t4 + 1) * 128], ones_bf, ones_bf[:, :], dg, dg[:, :], True, True)
                k.act(gate_bc, gate_bc[:, e, hb * 512:(hb + 1) * 512], pg, pg[:, :], AF.Copy)
        k.ses = SES
        for kt in range(8):
            k.ts("dve", z, z[:, kt, :], z, z[:, kt, :], float(ALPHA), ALU.mult)
        for e in range(NEXP):
            w1e, w3e, w2e = Buf("w1e", w1.t[e]), Buf("w3e", w3.t[e]), Buf("w2e", w2.t[e])
            for half in range(2):
                f0 = half * 14

                def emit_moe(ft, t0, tw, sa, pB, e=e):
                    ts_ = slice(t0, t0 + 512)
                    t = cx.tmp[(ft + t0 // 512) % 2]
                    k.tt("dve", t, t[:, :], pB, pB[:, :], sa, sa[:, :], ALU.mult)
                    k.tt("dve", cx.hmid, cx.hmid[:, ft, ts_], t, t[:, :], gate_bc, gate_bc[:, e, ts_], ALU.mult)

                glu_phase(k, cx, cx.xbf, w1e, f0 * 128, w3e, f0 * 128, 14 * 128, AF.Silu, emit_moe,
                          hook=lambda f0=f0, w2e=w2e: down_load(k, cx, w2e, f0, 14))
                down_phase(k, cx, w2e, f0, 14, z)
        layer_norm(k, cx, z, 3, None)
        for kt in range(8):
            k.dma("sp", oT, oT.t[kt * 128:(kt + 1) * 128, tsl], z, z[:, kt, :])
    return k.finish()


_PROGS = {}


def _prog(name, fn):
    if name not in _PROGS:
        _PROGS[name] = fn()
    return _PROGS[name]


def _run(nc, maps):
    res = run_bass_kernel_spmd(nc, maps, core_ids=list(range(NCORES)))
    return res.results


def kernel(x, s5_w_in, s5_a_re, s5_a_im, s5_log_dt, s5_b_re, s5_b_im, s5_c_re, s5_c_im, s5_d,
           s5_w_glu, attn_w_qkv, attn_w_o, rel_bias, ffn_w1, ffn_w3, ffn_w2, moe_w_router,
           moe_w1, moe_w3, moe_w2, ln_g, ln_b):
    f32 = np.float32
    x = np.asarray(x, f32)
    xT = np.ascontiguousarray(x.reshape(NTOK, D).T)
    cols = [slice(c * TPC, (c + 1) * TPC) for c in range(NCORES)]
    xTs = [np.ascontiguousarray(xT[:, s]) for s in cols]
    lg, lb = ln_layout(np.asarray(ln_g, f32), np.asarray(ln_b, f32))

    w_in = np.ascontiguousarray(np.asarray(s5_w_in, f32)[0])
    r = _run(_prog("A", build_uproj), [{"xT": xTs[c], "w": w_in} for c in range(NCORES)])
    u_full = np.concatenate([r[c]["uT"] for c in range(NCORES)], axis=1)
    maps = []
    for c in range(NCORES):
        m = s5_core_params(c, np.asarray(s5_a_re, f32)[0], np.asarray(s5_a_im, f32)[0], np.asarray(s5_log_dt, f32)[0],
                           np.asarray(s5_b_re, f32)[0], np.asarray(s5_b_im, f32)[0], np.asarray(s5_c_re, f32)[0],
                           np.asarray(s5_c_im, f32)[0], np.asarray(s5_d, f32)[0])
        m["u"] = np.ascontiguousarray(u_full[128 * c:128 * c + 128])
        maps.append(m)
    r = _run(_prog("B", build_s5), maps)
    y_full = np.concatenate([r[c]["y"] for c in range(NCORES)], axis=0)
    wglu = np.ascontiguousarray(np.asarray(s5_w_glu, f32)[0])
    w1 = np.ascontiguousarray(np.asarray(ffn_w1, f32)[0])
    w3 = np.ascontiguousarray(np.asarray(ffn_w3, f32)[0])
    w2 = np.ascontiguousarray(np.asarray(ffn_w2, f32)[0])
    r = _run(_prog("C", build_layer0_rest),
             [{"yT": np.ascontiguousarray(y_full[:, cols[c]]), "xT": xTs[c], "wglu": wglu, "w1": w1, "w3": w3, "w2": w2,
               "lng": lg, "lnb": lb} for c in range(NCORES)])
    x2s = [r[c]["oT"] for c in range(NCORES)]
    wqkv = np.ascontiguousarray(np.asarray(attn_w_qkv, f32)[0])
    r = _run(_prog("D", build_qkv), [{"xT": x2s[c], "w": wqkv} for c in range(NCORES)])
    qkvT = np.concatenate([r[c]["qkvT"] for c in range(NCORES)], axis=1)
    maps = attn_host_layout(qkvT, np.asarray(rel_bias, f32), SEQ, TPC)
    r = _run(_prog("E", build_attn), maps)
    aTs = [r[c]["attnT"] for c in range(NCORES)]
    wo = np.ascontiguousarray(np.asarray(attn_w_o, f32)[0])
    wr = np.ascontiguousarray(np.asarray(moe_w_router, f32)[0].reshape(8, 128, NEXP).transpose(1, 0, 2))
    mw1 = np.ascontiguousarray(np.asarray(moe_w1, f32)[0])
    mw3 = np.ascontiguousarray(np.asarray(moe_w3, f32)[0])
    mw2 = np.ascontiguousarray(np.asarray(moe_w2, f32)[0])
    ident = np.eye(128, dtype=f32)
    ltri = np.triu(np.ones((128, 128), f32), 1)
    ln4 = np.ascontiguousarray(np.stack([np.asarray(ln_g, f32)[1, 1], np.asarray(ln_b, f32)[1, 1]]))
    r = _run(_prog("F2", build_layer1_fast),
             [{"aT": aTs[c], "xT": x2s[c], "wo": wo, "wr": wr, "w1": mw1, "w3": mw3, "w2": mw2, "lng": lg, "lnb": lb,
               "ln4": ln4, "ident": ident, "ltri": ltri} for c in range(NCORES)])
    counts = np.stack([r[c]["cnt"][0] for c in range(NCORES)])
    if counts.max() <= CAP:
        out = np.concatenate([r[c]["o"] for c in range(NCORES)], axis=0)
        return np.ascontiguousarray(out).reshape(BATCH, SEQ, D).astype(f32)
    r = _run(_prog("F", build_layer1_rest),
             [{"aT": aTs[c], "xT": x2s[c], "wo": wo, "wr": wr, "w1": mw1, "w3": mw3, "w2": mw2, "lng": lg, "lnb": lb,
               "ident": ident} for c in range(NCORES)])
    oT = np.concatenate([r[c]["oT"] for c in range(NCORES)], axis=1)
    return np.ascontiguousarray(oT.T).reshape(BATCH, SEQ, D).astype(f32)


CAP = 1280
U32 = mybir.dt.uint32
OOR = 1.0e6


class NS:
    pass


def build_layer1_fast(tpc=TPC, NT=1024, cap=CAP):
    k = KB()
    aT = k.dram("aT", [D, tpc], F32, "ExternalInput")
    xT = k.dram("xT", [D, tpc], F32, "ExternalInput")
    wo = k.dram("wo", [D, D], F32, "ExternalInput")
    wr = k.dram("wr", [128, 8, NEXP], F32, "ExternalInput")
    w1 = k.dram("w1", [NEXP, D, DFE], F32, "ExternalInput")
    w3 = k.dram("w3", [NEXP, D, DFE], F32, "ExternalInput")
    w2 = k.dram("w2", [NEXP, DFE, D], F32, "ExternalInput")
    lng = k.dram("lng", [128, 32], F32, "ExternalInput")
    lnb = k.dram("lnb", [128, 32], F32, "ExternalInput")
    ln4 = k.dram("ln4", [2, D], F32, "ExternalInput")
    ident_d = k.dram("ident", [128, 128], F32, "ExternalInput")
    ltri_d = k.dram("ltri", [128, 128], F32, "ExternalInput")
    o = k.dram("o", [tpc, D], F32, "ExternalOutput")
    cnt_o = k.dram("cnt", [128, NEXP], F32, "ExternalOutput")
    xg = k.dram("xg", [NEXP * cap + 1, D], BF16)
    ye = k.dram("ye", [NEXP * cap + 1, D], F32)
    x3tm = k.dram("x3tm", [tpc, D], F32)
    ntile = tpc // 128
    ntt = NT // 128
    nst = cap // 128

    ident = k.sb("ident", [128, 128], F32)
    k.dma("sp", ident, ident[:, :], ident_d, ident_d.t[:, :])
    ident_bf = k.sb("ident_bf", [128, 128], BF16)
    k.dma("pool", ident_bf, ident_bf[:, :], ident_d, ident_d.t[:, :])
    ltri = k.sb("ltri", [128, 128], BF16)
    k.dma("pool", ltri, ltri[:, :], ltri_d, ltri_d.t[:, :])
    ones_bf = k.sb("ones_bf", [128, 128], BF16)
    k.op("pool", lambda g, a=ones_bf[:, :]: g.memset(a, 1.0), [], [ones_bf])
    IDX = [k.sb("idx%d" % j, [128, ntile], U32) for j in range(2)]
    G = [k.sb("G%d" % j, [128, ntile], F32) for j in range(2)]
    base = k.sb("base", [128, NEXP], F32)
    k.op("pool", lambda g, a=base[:, :]: g.memset(a, 0.0), [], [base])
    iota_e = k.sb("iota_e", [128, ntt, NEXP], F32)
    for e in range(NEXP):
        k.op("pool", lambda g, e=e, a=iota_e[:, :, e:e + 1]: g.memset(a, float(e)), [], [iota_e])

    k.push_scope()
    cx = NS()
    cx.k, cx.NT = k, NT
    cx.pso = [k.ps("pso") for _ in range(2)]
    cx.pss = [k.ps("pss") for _ in range(2)]
    ptb = [k.ps("ptb", (128, 1024), BF16) for _ in range(2)]
    ptf = [k.ps("ptf") for _ in range(2)]
    cx.wa = [k.sb("wa", [128, 8, 512], BF16) for _ in range(2)]
    cx.z = k.sb("z", [128, 8, NT], F32)
    cx.xbf = k.sb("xbf", [128, 8, NT], BF16)
    abf = k.sb("abf", [128, 8, NT], BF16)
    cx.tmp = [k.sb("tmp", [128, 512], F32) for _ in range(2)]
    cx.ones = k.sb("ones", [128, 128], F32)
    k.op("pool", lambda g, a=cx.ones[:, :]: g.memset(a, 1.0), [], [cx.ones])
    cx.sq = [k.sb("sq", [128, 512], F32) for _ in range(2)]
    cx.mean = k.sb("mean", [128, 512], F32)
    cx.ex2 = k.sb("ex2", [128, 512], F32)
    cx.rstd = k.sb("rstd", [128, 512], F32)
    cx.nmr = k.sb("nmr", [128, 512], F32)
    cx.lng = k.sb("lng", [128, 32], F32)
    cx.lnb = k.sb("lnb", [128, 32], F32)
    cx.wi = cx.pi = cx.oi = 0
    k.dma("sp", cx.lng, cx.lng[:, :], lng, lng.t[:, :])
    k.dma("sp", cx.lnb, cx.lnb[:, :], lnb, lnb.t[:, :])
    wr_sb = k.sb("wr", [128, 8, NEXP], F32)
    k.dma("sp", wr_sb, wr_sb[:, :, :], wr, wr.t[:, :, :])
    lg = k.sb("lg", [128, ntt, 8], F32)
    top = k.sb("top", [128, ntt, 8], F32)
    nv1, e2, g1, g2 = [k.sb("gsm%d" % i, [128, ntt], F32) for i in range(4)]
    m = [k.sb("m%d" % j, [128, ntt, 8], F32) for j in range(2)]
    msel = k.sb("msel", [128, ntt, 8], BF16)
    Pw = k.sb("Pw", [128, ntt, 8], F32)
    cn = k.sb("cn", [128, ntt, 8], F32)
    offs = k.sb("offs", [128, ntt + 1, 8], F32)
    pos = k.sb("pos", [128, ntt, 8], F32)
    prod = k.sb("prod", [128, ntt, 8], F32)
    psel = [k.sb("psel%d" % j, [128, ntt], F32) for j in range(2)]
    esel = [k.sb("esel%d" % j, [128, ntt], F32) for j in range(2)]
    ovf = k.sb("ovf", [128, ntt], F32)
    flat = k.sb("flat", [128, ntt], F32)
    xtm = [k.sb("xtm", [128, D], BF16) for _ in range(2)]
    x3s = [k.sb("x3s", [128, D], F32) for _ in range(2)]
    z = cx.z
    for sb_ in range(tpc // NT):
        tsl = slice(sb_ * NT, (sb_ + 1) * NT)
        k.ses = SES
        for kt in range(8):
            k.dma("pool", abf, abf[:, kt, :], aT, aT.t[kt * 128:(kt + 1) * 128, tsl])
            k.dma("sp", z, z[:, kt, :], xT, xT.t[kt * 128:(kt + 1) * 128, tsl])
        for cb in range(2):
            wa = cx.wa[cx.wi % 2]
            cx.wi += 1
            load_wblock(k, wa, wo, 0, 8, cb * 512, 512)
            for fl in range(4):
                mt = cb * 4 + fl
                for tb in range(NT // 512):
                    po = cx.pso[cx.oi % 2]
                    cx.oi += 1
                    ts_ = slice(tb * 512, tb * 512 + 512)
                    for kt in range(8):
                        k.mm(po, po[:, :], wa, wa[:, kt, fl * 128:fl * 128 + 128], abf, abf[:, kt, ts_], kt == 0, kt == 7)
                    k.stt("dve", z, z[:, mt, ts_], z, z[:, mt, ts_], float(ALPHA), po, po[:, :], ALU.mult, ALU.add)
        layer_norm(k, cx, z, 2, cx.xbf)
        k.ses = True
        pl = cx.pss[0]
        for tt in range(ntt):
            for kt in range(8):
                k.mm(pl, pl[:, tt * 8:tt * 8 + 8], z, z[:, kt, tt * 128:(tt + 1) * 128], wr_sb, wr_sb[:, kt, :], kt == 0, kt == 7)
        k.act(lg, lg[:, :, :], pl, pl[:, 0:ntt * 8].rearrange("p (t e) -> p t e", e=8), AF.Copy)
        for tt in range(ntt):
            k.op("dve", lambda g, o_=top[:, tt, :], i=lg[:, tt, :]: g.max(out=o_, in_=i), [lg], [top])
        k.ts("dve", nv1, nv1[:, :], top, top[:, :, 0], -1.0, ALU.mult)
        k.tt("dve", e2, e2[:, :], top, top[:, :, 1], nv1, nv1[:, :], ALU.add)
        k.act(e2, e2[:, :], e2, e2[:, :], AF.Exp)
        k.ts("dve", g1, g1[:, :], e2, e2[:, :], 1.0, ALU.add)
        k.op("dve", lambda g, a=g1[:, :]: g.reciprocal(out=a, in_=a), [g1], [g1])
        k.tt("dve", g2, g2[:, :], e2, e2[:, :], g1, g1[:, :], ALU.mult)
        t0_ = sb_ * ntt
        k.op("dve", lambda g, o_=G[0][:, t0_:t0_ + ntt], i_=g1[:, :]: g.tensor_copy(out=o_, in_=i_), [g1], [G[0]])
        k.op("dve", lambda g, o_=G[1][:, t0_:t0_ + ntt], i_=g2[:, :]: g.tensor_copy(out=o_, in_=i_), [g2], [G[1]])
        for tt in range(ntt):
            k.ts("dve", m[0], m[0][:, tt, :], lg, lg[:, tt, :], top[:, tt, 0:1], ALU.is_equal, extra_reads=[top])
            k.ts("dve", m[1], m[1][:, tt, :], lg, lg[:, tt, :], top[:, tt, 1:2], ALU.is_equal, extra_reads=[top])
        k.tt("dve", msel, msel[:, :, :], m[0], m[0][:, :, :], m[1], m[1][:, :, :], ALU.add)
        pp = cx.pss[1]
        mflat = msel[:, :, :].rearrange("p t e -> p (t e)")
        k.mm(pp, pp[:, 0:ntt * 8], ltri, ltri[:, :], msel, mflat, True, True)
        k.mm(pp, pp[:, 256:256 + ntt * 8], ones_bf, ones_bf[:, :], msel, mflat, True, True)
        k.act(Pw, Pw[:, :, :], pp, pp[:, 0:ntt * 8].rearrange("p (t e) -> p t e", e=8), AF.Copy)
        k.act(cn, cn[:, :, :], pp, pp[:, 256:256 + ntt * 8].rearrange("p (t e) -> p t e", e=8), AF.Copy)
        k.op("dve", lambda g, o_=offs[:, 0, :], i_=base[:, :]: g.tensor_copy(out=o_, in_=i_), [base], [offs])
        for tt in range(ntt):
            k.tt("dve", offs, offs[:, tt + 1, :], offs, offs[:, tt, :], cn, cn[:, tt, :], ALU.add)
        k.op("dve", lambda g, o_=base[:, :], i_=offs[:, ntt, :]: g.tensor_copy(out=o_, in_=i_), [offs], [base])
        k.tt("dve", pos, pos[:, :, :], Pw, Pw[:, :, :], offs, offs[:, 0:ntt, :], ALU.add)
        for j in range(2):
            k.tt("dve", prod, prod[:, :, :], m[j], m[j][:, :, :], pos, pos[:, :, :], ALU.mult)
            k.op("dve", lambda g, o_=psel[j][:, :], i_=prod[:, :, :]: g.reduce_sum(out=o_, in_=i_, axis=AX.X), [prod], [psel[j]])
            k.tt("dve", prod, prod[:, :, :], m[j], m[j][:, :, :], iota_e, iota_e[:, :, :], ALU.mult)
            k.op("dve", lambda g, o_=esel[j][:, :], i_=prod[:, :, :]: g.reduce_sum(out=o_, in_=i_, axis=AX.X), [prod], [esel[j]])
            k.stt("dve", flat, flat[:, :], esel[j], esel[j][:, :], float(cap), psel[j], psel[j][:, :], ALU.mult, ALU.add)
            k.ts("dve", ovf, ovf[:, :], psel[j], psel[j][:, :], float(cap) - 0.5, ALU.is_gt)
            k.stt("dve", flat, flat[:, :], ovf, ovf[:, :], OOR, flat, flat[:, :], ALU.mult, ALU.add)
            k.ts("dve", flat, flat[:, :], flat, flat[:, :], float(NEXP * cap), ALU.min)
            k.op("dve", lambda g, o_=IDX[j][:, t0_:t0_ + ntt], i_=flat[:, :]: g.tensor_copy(out=o_, in_=i_), [flat], [IDX[j]])
        k.ses = SES
        for kt in range(8):
            k.ts("dve", z, z[:, kt, :], z, z[:, kt, :], float(ALPHA), ALU.mult)
        for tt in range(ntt):
            gt = t0_ + tt
            pb_, xt_ = ptb[tt % 2], xtm[tt % 2]
            for kt in range(8):
                k.tr(pb_, pb_[:, kt * 128:(kt + 1) * 128], cx.xbf, cx.xbf[:, kt, tt * 128:(tt + 1) * 128], ident_bf, ident_bf[:, :])
            k.act(xt_, xt_[:, :], pb_, pb_[:, :], AF.Copy)
            for j in range(2):
                k.idma(xg, xg.t[:, :], xt_, xt_[:, :], IDX[j], IDX[j][:, gt:gt + 1], True, NEXP * cap - 1)
            x3_ = x3s[tt % 2]
            for hf in range(2):
                pf_ = ptf[hf]
                for kq in range(4):
                    kt = hf * 4 + kq
                    k.tr(pf_, pf_[:, kq * 128:(kq + 1) * 128], z, z[:, kt, tt * 128:(tt + 1) * 128], ident, ident[:, :])
                k.op("dve", lambda g, o_=x3_[:, hf * 512:(hf + 1) * 512], i_=pf_[:, :]: g.tensor_copy(out=o_, in_=i_), [pf_], [x3_])
            k.dma("sp", x3tm, x3tm.t[gt * 128:(gt + 1) * 128, :], x3_, x3_[:, :])
    k.dma("sp", cnt_o, cnt_o.t[:, :], base, base[:, :])
    k.pop_scope()

    k.push_scope()
    cx = NS()
    cx.k, cx.NT = k, cap
    cx.ps1 = [k.ps("ps1") for _ in range(2)]
    cx.ps3 = [k.ps("ps3") for _ in range(2)]
    cx.pso = [k.ps("pso") for _ in range(2)]
    ptr = [k.ps("ptr", (128, 512), BF16) for _ in range(2)]
    cx.wa = [k.sb("wa", [128, 8, 512], BF16) for _ in range(2)]
    cx.wb = [k.sb("wb", [128, 8, 512], BF16) for _ in range(2)]
    cx.w2h = k.sb("w2h", [128, 14, 1024], BF16)
    cx.hmid = k.sb("hmid", [128, 14, cap], BF16)
    cx.sact = [k.sb("sact", [128, 512], F32) for _ in range(2)]
    cx.wi = cx.pi = cx.oi = 0
    xe = k.sb("xge", [128, 8, cap], BF16)
    xs = [k.sb("xs", [128, 4, D], BF16) for _ in range(2)]
    yacc = k.sb("yacc", [128, nst, D], F32)
    xi = 0
    ti = 0
    for e in range(NEXP):
        for sg in range((nst + 3) // 4):
            n_ = min(4, nst - 4 * sg)
            xs_ = xs[xi % 2]
            xi += 1
            r0 = e * cap + sg * 512
            k.dma("sp", xs_, xs_[:, 0:n_, :], xg, xg.t[r0:r0 + n_ * 128, :].rearrange("(t p) f -> p t f", p=128))
            for kt in range(8):
                pt_ = ptr[ti % 2]
                for t in range(n_):
                    k.tr(pt_, pt_[:, t * 128:(t + 1) * 128], xs_, xs_[:, t, kt * 128:(kt + 1) * 128], ident_bf, ident_bf[:, :])
                if ti % 2 == 0:
                    k.act(xe, xe[:, kt, sg * 512:sg * 512 + n_ * 128], pt_, pt_[:, 0:n_ * 128], AF.Copy)
                else:
                    k.op("dve", lambda g, o_=xe[:, kt, sg * 512:sg * 512 + n_ * 128], i_=pt_[:, 0:n_ * 128]:
                         g.tensor_copy(out=o_, in_=i_), [pt_], [xe])
                ti += 1
        w1e, w3e, w2e = Buf("w1e", w1.t[e]), Buf("w3e", w3.t[e]), Buf("w2e", w2.t[e])
        for half in range(2):
            f0 = half * 14

            def emit_h(ft, t0, tw, sa, pB, cx=cx):
                k.tt("dve", cx.hmid, cx.hmid[:, ft, t0:t0 + tw], pB, pB[:, 0:tw], sa, sa[:, 0:tw], ALU.mult)

            glu_phase(k, cx, xe, w1e, f0 * 128, w3e, f0 * 128, 14 * 128, AF.Silu, emit_h,
                      hook=lambda f0=f0, w2e=w2e, cx=cx: down_load(k, cx, w2e, f0, 14), NT=cap)
            for st in range(nst):
                for fh in range(2):
                    po = cx.pso[cx.oi % 2]
                    cx.oi += 1
                    for kt in range(14):
                        k.mm(po, po[:, :], cx.hmid, cx.hmid[:, kt, st * 128:(st + 1) * 128], cx.w2h, cx.w2h[:, kt, fh * 512:(fh + 1) * 512],
                             kt == 0, kt == 13)
                    ysl = yacc[:, st, fh * 512:(fh + 1) * 512]
                    if half == 0:
                        k.act(yacc, ysl, po, po[:, :], AF.Copy)
                    else:
                        k.tt("dve", yacc, ysl, po, po[:, :], yacc, ysl, ALU.add)
        k.dma("sp", ye, ye.t[e * cap:(e + 1) * cap, :].rearrange("(t p) f -> p t f", p=128), yacc, yacc[:, :, :])
    k.pop_scope()

    k.push_scope()
    gB = k.sb("gB", [128, D], F32)
    bB = k.sb("bB", [128, D], F32)
    k.dma("sp", gB, gB[:, :], ln4, ln4.t[0:1, :].to_broadcast([128, D]))
    k.dma("sp", bB, bB[:, :], ln4, ln4.t[1:2, :].to_broadcast([128, D]))
    NB3 = 3
    r1 = [k.sb("r1", [128, D], F32) for _ in range(NB3)]
    r2 = [k.sb("r2", [128, D], F32) for _ in range(NB3)]
    xa = [k.sb("xa", [128, D], F32) for _ in range(NB3)]
    zt = [k.sb("zt", [128, D], F32) for _ in range(NB3)]
    st6 = [k.sb("st6", [128, 2, 6], F32) for _ in range(NB3)]
    mv = [k.sb("mv", [128, 2], F32) for _ in range(NB3)]
    sd = [k.sb("sd", [128, 1], F32) for _ in range(NB3)]
    rs_l = [k.sb("rs", [128, 1], F32) for _ in range(NB3)]
    nm_l = [k.sb("nm", [128, 1], F32) for _ in range(NB3)]
    for t in range(ntile):
        i3 = t % NB3
        a_, b_, x_, z_ = r1[i3], r2[i3], xa[i3], zt[i3]
        s6, mv_, sd_, rs_, nm_ = st6[i3], mv[i3], sd[i3], rs_l[i3], nm_l[i3]
        k.ses = SES
        k.idma(a_, a_[:, :], ye, ye.t[:, :], IDX[0], IDX[0][:, t:t + 1], False, NEXP * cap - 1)
        k.idma(b_, b_[:, :], ye, ye.t[:, :], IDX[1], IDX[1][:, t:t + 1], False, NEXP * cap - 1)
        k.dma("sp", x_, x_[:, :], x3tm, x3tm.t[t * 128:(t + 1) * 128, :])
        k.stt("dve", z_, z_[:, :], a_, a_[:, :], G[0][:, t:t + 1], x_, x_[:, :], ALU.mult, ALU.add, extra_reads=[G[0]])
        k.stt("dve", z_, z_[:, :], b_, b_[:, :], G[1][:, t:t + 1], z_, z_[:, :], ALU.mult, ALU.add, extra_reads=[G[1]])
        k.ses = True
        for hf in range(2):
            k.op("dve", lambda g, o_=s6[:, hf, :], i_=z_[:, hf * 512:(hf + 1) * 512]: g.bn_stats(out=o_, in_=i_), [z_], [s6])
        k.op("dve", lambda g, o_=mv_[:, :], i_=s6[:, :, :].rearrange("p a b -> p (a b)"): g.bn_aggr(out=o_, in_=i_), [s6], [mv_])
        k.act(sd_, sd_[:, :], mv_, mv_[:, 1:2], AF.Sqrt, bias=LN_EPS)
        k.op("dve", lambda g, o_=rs_[:, :], i_=sd_[:, :]: g.reciprocal(out=o_, in_=i_), [sd_], [rs_])
        k.stt("dve", nm_, nm_[:, :], mv_, mv_[:, 0:1], -1.0, rs_, rs_[:, :], ALU.mult, ALU.mult)
        k.op("act", lambda g, a=z_[:, :], sc=rs_[:, 0:1], bi=nm_[:, 0:1]: g.activation(
            out=a, in_=a, func=AF.Identity, scale=sc, bias=bi), [z_, rs_, nm_], [z_])
        k.ses = SES
        k.tt("dve", z_, z_[:, :], z_, z_[:, :], gB, gB[:, :], ALU.mult)
        k.tt("pool", z_, z_[:, :], z_, z_[:, :], bB, bB[:, :], ALU.add)
        k.dma("sp", o, o.t[t * 128:(t + 1) * 128, :], z_, z_[:, :])
    k.pop_scope()
    return k.finish()
```

```python
import math
import numpy as np
import concourse.bass as bass
import concourse.mybir as mybir
from concourse.bass_utils import run_bass_kernel_spmd
from contextlib import ExitStack

F32 = mybir.dt.float32
BF16 = mybir.dt.bfloat16
I32 = mybir.dt.int32
ALU = mybir.AluOpType
AF = mybir.ActivationFunctionType
AX = mybir.AxisListType

NCORES = 8
D = 1024
SEQ = 16384
BATCH = 2
NTOK = BATCH * SEQ
TPC = NTOK // NCORES
DEPTH = 2
ALPHA = (2 * DEPTH) ** 0.25
LN_EPS = 1e-5
PI = math.pi


class Buf:
    __slots__ = ("name", "t", "lw", "rd", "dsem", "dcnt", "nodep")

    def __init__(self, name, t):
        self.name = name
        self.t = t
        self.lw = None
        self.rd = {}
        self.dsem = None
        self.dcnt = 0
        self.nodep = False

    def __getitem__(self, idx):
        return self.t[idx]


SES = False


class KB:
    def __init__(self, same_engine_sync=None):
        if same_engine_sync is None:
            same_engine_sync = SES
        self.nc = bass.Bass("TRN2", target_bir_lowering=False)
        self.es = ExitStack()
        nc = self.nc
        self.eng = {"pe": nc.tensor, "dve": nc.vector, "act": nc.scalar, "pool": nc.gpsimd, "sp": nc.sync}
        self.sem = {e: self.es.enter_context(nc.semaphore("s_" + e)) for e in self.eng}
        self.cnt = {e: 0 for e in self.eng}
        self.known = {e: {} for e in self.eng}
        self.ses = same_engine_sync
        self.nsem = 0
        self.prog = {e: [] for e in self.eng}
        self.outs = []
        self.uid = 0
        self.scopes = []
        self.dbufs = []

    def push_scope(self):
        self.scopes.append(ExitStack())

    def pop_scope(self):
        self.barrier()
        self.scopes.pop().close()

    def _stk(self):
        return self.scopes[-1] if self.scopes else self.es

    def barrier(self):
        tags = [(self.sem[e], self.cnt[e]) for e in self.eng if self.cnt[e]]
        tags += [(b.dsem, b.dcnt) for b in self.dbufs if b.dcnt]
        for e in self.eng:
            kn = self.known[e]
            for s_, v in tags:
                if s_ == self.sem[e]:
                    continue
                if kn.get(s_, 0) >= v:
                    continue
                self.prog[e].append(("w", s_, v))
                kn[s_] = v

    def sb(self, name, shape, dt):
        self.uid += 1
        return Buf(name, self._stk().enter_context(self.nc.sbuf_tensor("%s_%d" % (name, self.uid), list(shape), dt)))

    def ps(self, name, shape=(128, 512), dt=F32):
        self.uid += 1
        return Buf(name, self._stk().enter_context(self.nc.psum_tensor("%s_%d" % (name, self.uid), list(shape), dt)))

    def dram(self, name, shape, dt, kind="Internal"):
        b = Buf(name, self.nc.dram_tensor(name, list(shape), dt, kind=kind).ap())
        if kind == "Internal":
            b.nodep = True
        if kind == "ExternalOutput":
            self.outs.append(b)
            b.nodep = True
        return b

    def _deps(self, e, reads, writes):
        need = {}

        def add(tag):
            if tag is None:
                return
            s, v = tag
            if need.get(s, 0) < v:
                need[s] = v

        for b in reads:
            add(b.lw)
        for b in writes:
            if b.nodep:
                continue
            add(b.lw)
            for s, v in b.rd.items():
                add((s, v))
        k = self.known[e]
        own = self.sem[e]
        for s, v in need.items():
            if s == own and (e == "pe" or not self.ses):
                continue
            if k.get(s, 0) >= v:
                continue
            self.prog[e].append(("w", s, v))
            k[s] = v

    def op(self, e, fn, reads=(), writes=()):
        self._deps(e, reads, writes)
        self.cnt[e] += 1
        self.prog[e].append(("i", fn, self.sem[e], 1))
        tag = (self.sem[e], self.cnt[e])
        for b in writes:
            b.lw = tag
            b.rd = {}
        for b in reads:
            if b not in writes:
                b.rd[tag[0]] = tag[1]

    def dma(self, q, out_buf, out_ap, in_buf, in_ap, **kw):
        self._deps(q, [in_buf], [out_buf])
        if out_buf.dsem is None:
            self.nsem += 1
            out_buf.dsem = self.es.enter_context(self.nc.semaphore("d%d" % self.nsem))
            self.dbufs.append(out_buf)
        self.prog[q].append(("i", (lambda e, o=out_ap, i=in_ap, kw=kw: e.dma_start(out=o, in_=i, **kw)),
                             out_buf.dsem, 16))
        out_buf.dcnt += 16
        tag = (out_buf.dsem, out_buf.dcnt)
        out_buf.lw = tag
        out_buf.rd = {}
        in_buf.rd[tag[0]] = tag[1]

    def idma(self, out_buf, out_ap, in_buf, in_ap, idx_buf, idx_ap, scatter, bound):
        self._deps("pool", [in_buf, idx_buf], [out_buf])
        if out_buf.dsem is None:
            self.nsem += 1
            out_buf.dsem = self.es.enter_context(self.nc.semaphore("d%d" % self.nsem))
            self.dbufs.append(out_buf)
        off = bass.IndirectOffsetOnAxis(idx_ap, 0)
        if scatter:
            fn = lambda e: e.indirect_dma_start(out=out_ap, out_offset=off, in_=in_ap, in_offset=None)
        else:
            fn = lambda e: e.indirect_dma_start(out=out_ap, out_offset=None, in_=in_ap, in_offset=off)
        self.prog["pool"].append(("i", fn, out_buf.dsem, 16))
        out_buf.dcnt += 16
        tag = (out_buf.dsem, out_buf.dcnt)
        out_buf.lw = tag
        out_buf.rd = {}
        in_buf.rd[tag[0]] = tag[1]
        idx_buf.rd[tag[0]] = tag[1]

    def tr(self, out_b, out_ap, in_b, in_ap, id_b, id_ap):
        self.op("pe", lambda g: g.transpose(out_ap, in_ap, id_ap), [in_b, id_b], [out_b])

    def finish(self):
        for b in self.outs:
            if b.lw is not None:
                self.prog["sp"].append(("w", b.lw[0], b.lw[1]))
        for e in ("pe", "dve", "act", "pool"):
            if self.cnt[e]:
                self.prog["sp"].append(("w", self.sem[e], self.cnt[e]))
        prog = self.prog

        def replay(e):
            def body(eng):
                for it in prog[e]:
                    if it[0] == "w":
                        eng.wait_ge(it[1], it[2])
                    else:
                        it[1](eng).then_inc(it[2], it[3])
            return body

        with self.nc.Block() as block:
            block.sync(replay("sp"))
            block.tensor(replay("pe"))
            block.vector(replay("dve"))
            block.scalar(replay("act"))
            block.gpsimd(replay("pool"))
        self.es.close()
        return self.nc

    def tt(self, e, out_b, out_ap, a_b, a_ap, b_b, b_ap, op):
        self.op(e, lambda g: g.tensor_tensor(out=out_ap, in0=a_ap, in1=b_ap, op=op), [a_b, b_b], [out_b])

    def ts(self, e, out_b, out_ap, a_b, a_ap, s1, op0, s2=None, op1=None, extra_reads=()):
        if op1 is None:
            self.op(e, lambda g: g.tensor_scalar(out=out_ap, in0=a_ap, scalar1=s1, scalar2=None, op0=op0),
                    [a_b] + list(extra_reads), [out_b])
        else:
            self.op(e, lambda g: g.tensor_scalar(out=out_ap, in0=a_ap, scalar1=s1, scalar2=s2, op0=op0, op1=op1),
                    [a_b] + list(extra_reads), [out_b])

    def stt(self, e, out_b, out_ap, a_b, a_ap, scalar, b_b, b_ap, op0, op1, extra_reads=()):
        self.op(e, lambda g: g.scalar_tensor_tensor(out=out_ap, in0=a_ap, scalar=scalar, in1=b_ap, op0=op0, op1=op1),
                [a_b, b_b] + list(extra_reads), [out_b])

    def act(self, out_b, out_ap, in_b, in_ap, func, scale=1.0, bias=0.0, extra_reads=()):
        self.op("act", lambda g: g.activation(out=out_ap, in_=in_ap, func=func, scale=scale, bias=bias),
                [in_b] + list(extra_reads), [out_b])

    def mm(self, out_b, out_ap, l_b, l_ap, r_b, r_ap, start, stop):
        self.op("pe", lambda g: g.matmul(out_ap, l_ap, r_ap, start=start, stop=stop), [l_b, r_b], [out_b])


def build_uproj():
    k = KB()
    xT = k.dram("xT", [D, TPC], F32, "ExternalInput")
    w = k.dram("w", [D, D], F32, "ExternalInput")
    uT = k.dram("uT", [D, TPC], F32, "ExternalOutput")
    wsb = k.sb("w", [128, 8, D], BF16)
    k.dma("pool", wsb, wsb[:, :, :], w, w.t.rearrange("(kt p) m -> p kt m", p=128))
    xs = [k.sb("xs", [128, 8, 512], BF16) for _ in range(2)]
    pbs = [k.ps("pb") for _ in range(4)]
    sts = [k.sb("st", [128, 512], F32) for _ in range(4)]
    i = 0
    for nb in range(TPC // 512):
        xb = xs[nb % 2]
        k.dma("pool", xb, xb[:, :, :], xT, xT.t[:, nb * 512:(nb + 1) * 512].rearrange("(kt p) n -> p kt n", p=128))
        for m in range(8):
            pb = pbs[i % 4]
            st = sts[i % 4]
            for kt in range(8):
                k.mm(pb, pb[:, :], wsb, wsb[:, kt, m * 128:(m + 1) * 128], xb, xb[:, kt, :], kt == 0, kt == 7)
            if i % 2 == 0:
                k.act(st, st[:, :], pb, pb[:, :], AF.Copy)
            else:
                k.op("dve", lambda g, o=st[:, :], a=pb[:, :]: g.tensor_copy(out=o, in_=a), [pb], [st])
            k.dma("sp", uT, uT.t[m * 128:(m + 1) * 128, nb * 512:(nb + 1) * 512], st, st[:, :])
            i += 1
    return k.finish()


S5_BLK = 512


def build_s5(ntok=NTOK, seq=SEQ):
    k = KB()
    u = k.dram("u", [128, ntok], F32, "ExternalInput")
    are = k.dram("are", [128, 4], F32, "ExternalInput")
    aim = k.dram("aim", [128, 4], F32, "ExternalInput")
    ldt = k.dram("ldt", [128, 4], F32, "ExternalInput")
    bre = k.dram("bre", [128, 4, 128], F32, "ExternalInput")
    bim = k.dram("bim", [128, 4, 128], F32, "ExternalInput")
    cre = k.dram("cre", [128, 4, 32], F32, "ExternalInput")
    cim = k.dram("cim", [128, 4, 32], F32, "ExternalInput")
    dsk = k.dram("dsk", [128, 1], F32, "ExternalInput")
    y = k.dram("y", [128, ntok], F32, "ExternalOutput")
    L = S5_BLK

    def small(name, w=4, dt=F32):
        return k.sb(name, [128, w], dt)

    s_are, s_aim, s_ldt = small("are"), small("aim"), small("ldt")
    k.dma("sp", s_are, s_are[:, :], are, are.t[:, :])
    k.dma("sp", s_aim, s_aim[:, :], aim, aim.t[:, :])
    k.dma("sp", s_ldt, s_ldt[:, :], ldt, ldt.t[:, :])
    s_d = small("dsk", 1)
    k.dma("sp", s_d, s_d[:, :], dsk, dsk.t[:, :])
    b_re = k.sb("bre", [128, 4, 128], BF16)
    b_im = k.sb("bim", [128, 4, 128], BF16)
    k.dma("pool", b_re, b_re[:, :, :], bre, bre.t[:, :, :])
    k.dma("pool", b_im, b_im[:, :, :], bim, bim.t[:, :, :])
    c_re = k.sb("cre", [128, 4, 32], F32)
    c_im = k.sb("cim", [128, 4, 32], F32)
    k.dma("sp", c_re, c_re[:, :, :], cre, cre.t[:, :, :])
    k.dma("sp", c_im, c_im[:, :, :], cim, cim.t[:, :, :])

    k.ses = True
    dt_ = small("dt")
    k.act(dt_, dt_[:, :], s_ldt, s_ldt[:, :], AF.Exp)
    lre = small("lre")
    k.ts("dve", lre, lre[:, :], s_are, s_are[:, :], -1e-4, ALU.min)
    lrd = small("lrd")
    k.tt("dve", lrd, lrd[:, :], lre, lre[:, :], dt_, dt_[:, :], ALU.mult)
    rho = small("rho")
    k.act(rho, rho[:, :], lrd, lrd[:, :], AF.Exp)
    th = small("th")
    k.tt("dve", th, th[:, :], s_aim, s_aim[:, :], dt_, dt_[:, :], ALU.mult)

    def sin_of(src, shift, name):
        a = small(name + "a")
        k.ts("dve", a, a[:, :], src, src[:, :], float(shift), ALU.add)
        t = small(name + "t")
        k.ts("dve", t, t[:, :], a, a[:, :], 1.0 / (2 * PI), ALU.mult)
        ti = small(name + "ti", dt=I32)
        k.op("dve", lambda g: g.tensor_copy(out=ti[:, :], in_=t[:, :]), [t], [ti])
        tf = small(name + "tf")
        k.op("dve", lambda g: g.tensor_copy(out=tf[:, :], in_=ti[:, :]), [ti], [tf])
        r = small(name + "r")
        k.stt("dve", r, r[:, :], tf, tf[:, :], -2 * PI, a, a[:, :], ALU.mult, ALU.add)
        m1 = small(name + "m1")
        k.ts("dve", m1, m1[:, :], r, r[:, :], PI, ALU.is_gt)
        r2 = small(name + "r2")
        k.stt("dve", r2, r2[:, :], m1, m1[:, :], -2 * PI, r, r[:, :], ALU.mult, ALU.add)
        m2 = small(name + "m2")
        k.ts("dve", m2, m2[:, :], r2, r2[:, :], -PI, ALU.is_lt)
        r3 = small(name + "r3")
        k.stt("dve", r3, r3[:, :], m2, m2[:, :], 2 * PI, r2, r2[:, :], ALU.mult, ALU.add)
        r4 = small(name + "r4")
        k.ts("dve", r4, r4[:, :], r3, r3[:, :], PI, ALU.min, -PI, ALU.max)
        o = small(name + "o")
        k.act(o, o[:, :], r4, r4[:, :], AF.Sin)
        return o

    sn = sin_of(th, 0.0, "sn")
    cs = sin_of(th, PI / 2, "cs")
    abr, abi = small("abr"), small("abi")
    k.tt("dve", abr, abr[:, :], rho, rho[:, :], cs, cs[:, :], ALU.mult)
    k.tt("dve", abi, abi[:, :], rho, rho[:, :], sn, sn[:, :], ALU.mult)
    den, t0 = small("den"), small("t0")
    k.tt("dve", den, den[:, :], lre, lre[:, :], lre, lre[:, :], ALU.mult)
    k.tt("dve", t0, t0[:, :], s_aim, s_aim[:, :], s_aim, s_aim[:, :], ALU.mult)
    den2 = small("den2")
    k.tt("dve", den2, den2[:, :], den, den[:, :], t0, t0[:, :], ALU.add)
    rden = small("rden")
    k.op("dve", lambda g: g.reciprocal(out=rden[:, :], in_=den2[:, :]), [den2], [rden])
    nr = small("nr")
    k.ts("dve", nr, nr[:, :], abr, abr[:, :], -1.0, ALU.add)
    t1, t2, t3, t4 = small("t1"), small("t2"), small("t3"), small("t4")
    k.tt("dve", t1, t1[:, :], nr, nr[:, :], lre, lre[:, :], ALU.mult)
    k.tt("dve", t2, t2[:, :], abi, abi[:, :], s_aim, s_aim[:, :], ALU.mult)
    k.tt("dve", t3, t3[:, :], abi, abi[:, :], lre, lre[:, :], ALU.mult)
    k.tt("dve", t4, t4[:, :], nr, nr[:, :], s_aim, s_aim[:, :], ALU.mult)
    q1, q2 = small("q1"), small("q2")
    k.tt("dve", q1, q1[:, :], t1, t1[:, :], t2, t2[:, :], ALU.add)
    k.tt("dve", q2, q2[:, :], t3, t3[:, :], t4, t4[:, :], ALU.subtract)
    qre, qim = small("qre"), small("qim")
    k.tt("dve", qre, qre[:, :], q1, q1[:, :], rden, rden[:, :], ALU.mult)
    k.tt("dve", qim, qim[:, :], q2, q2[:, :], rden, rden[:, :], ALU.mult)
    nqim = small("nqim")
    k.ts("dve", nqim, nqim[:, :], qim, qim[:, :], -1.0, ALU.mult)
    nqre = small("nqre")
    k.ts("dve", nqre, nqre[:, :], qre, qre[:, :], -1.0, ALU.mult)

    cp_re = k.sb("cpre", [128, 4, 128], BF16)
    cp_imn = k.sb("cpimn", [128, 4, 128], BF16)
    k.op("pool", lambda g: g.memset(cp_re[:, :, :], 0.0), [], [cp_re])
    k.op("pool", lambda g: g.memset(cp_imn[:, :, :], 0.0), [], [cp_imn])
    ctmp = k.sb("ctmp", [128, 4, 32], F32)
    for qq in range(4):
        k.ts("dve", ctmp, ctmp[:, qq, :], c_im, c_im[:, qq, :], nqim[:, qq:qq + 1], ALU.mult, extra_reads=[nqim])
        k.stt("dve", cp_re, cp_re[:, qq, 32 * qq:32 * qq + 32], c_re, c_re[:, qq, :], qre[:, qq:qq + 1], ctmp, ctmp[:, qq, :],
              ALU.mult, ALU.add, extra_reads=[qre])
    ctmp2 = k.sb("ctmp2", [128, 4, 32], F32)
    for qq in range(4):
        k.ts("dve", ctmp2, ctmp2[:, qq, :], c_im, c_im[:, qq, :], nqre[:, qq:qq + 1], ALU.mult, extra_reads=[nqre])
        k.stt("dve", cp_imn, cp_imn[:, qq, 32 * qq:32 * qq + 32], c_re, c_re[:, qq, :], nqim[:, qq:qq + 1], ctmp2, ctmp2[:, qq, :],
              ALU.mult, ALU.add, extra_reads=[nqim])

    COS = k.sb("COS", [128, 4, L], F32)
    SIN = k.sb("SIN", [128, 4, L], F32)
    RHO = k.sb("RHO", [128, 4, L], F32)
    k.op("pool", lambda g: g.memset(COS[:, :, :], 1.0), [], [COS])
    k.op("pool", lambda g: g.memset(SIN[:, :, :], 0.0), [], [SIN])
    k.op("pool", lambda g: g.memset(RHO[:, :, :], 1.0), [], [RHO])
    for qq in range(4):
        k.ts("dve", RHO, RHO[:, qq, :], RHO, RHO[:, qq, :], rho[:, qq:qq + 1], ALU.mult, extra_reads=[rho])
    pr, pi_ = cs, sn
    ttmp = k.sb("ttmp", [128, 4, L // 2], F32)
    w = 1
    lvl = 0
    while w < L:
        npi = small("npi%d" % lvl)
        k.ts("dve", npi, npi[:, :], pi_, pi_[:, :], -1.0, ALU.mult)
        for qq in range(4):
            k.ts("dve", ttmp, ttmp[:, qq, 0:w], SIN, SIN[:, qq, 0:w], npi[:, qq:qq + 1], ALU.mult, extra_reads=[npi])
            k.stt("dve", COS, COS[:, qq, w:2 * w], COS, COS[:, qq, 0:w], pr[:, qq:qq + 1], ttmp, ttmp[:, qq, 0:w],
                  ALU.mult, ALU.add, extra_reads=[pr])
            k.ts("dve", ttmp, ttmp[:, qq, 0:w], SIN, SIN[:, qq, 0:w], pr[:, qq:qq + 1], ALU.mult, extra_reads=[pr])
            k.stt("dve", SIN, SIN[:, qq, w:2 * w], COS, COS[:, qq, 0:w], pi_[:, qq:qq + 1], ttmp, ttmp[:, qq, 0:w],
                  ALU.mult, ALU.add, extra_reads=[pi_])
        a2, b2, ab_ = small("a2_%d" % lvl), small("b2_%d" % lvl), small("ab_%d" % lvl)
        k.tt("dve", a2, a2[:, :], pr, pr[:, :], pr, pr[:, :], ALU.mult)
        k.tt("dve", b2, b2[:, :], pi_, pi_[:, :], pi_, pi_[:, :], ALU.mult)
        k.tt("dve", ab_, ab_[:, :], pr, pr[:, :], pi_, pi_[:, :], ALU.mult)
        npr, npi2 = small("npr%d" % lvl), small("npi2%d" % lvl)
        k.tt("dve", npr, npr[:, :], a2, a2[:, :], b2, b2[:, :], ALU.subtract)
        k.ts("dve", npi2, npi2[:, :], ab_, ab_[:, :], 2.0, ALU.mult)
        pr, pi_ = npr, npi2
        w *= 2
        lvl += 1
    ELr, ELi = pr, pi_
    nELi = small("nELi")
    k.ts("dve", nELi, nELi[:, :], ELi, ELi[:, :], -1.0, ALU.mult)

    k.ses = SES
    NP = 3
    ub = [k.sb("ub", [128, L], F32) for _ in range(2)]
    ubf = [k.sb("ubf", [128, L], BF16) for _ in range(2)]
    ps_re = [k.ps("psre") for _ in range(NP)]
    ps_im = [k.ps("psim") for _ in range(NP)]
    ps_y = [k.ps("psy") for _ in range(2)]
    T = [[k.sb("T%d" % j, [128, L], F32) for j in range(4)] for _ in range(NP)]
    gin = [[k.sb("gin%d" % j, [128, L], F32) for j in range(2)] for _ in range(NP)]
    gg = [[[k.sb("g%d_%d" % (qq, j), [128, L], F32) for j in range(2)] for qq in range(4)] for _ in range(2)]
    T2 = [[k.sb("U%d" % j, [128, L], F32) for j in range(4)] for _ in range(NP)]
    hh = [[k.sb("h%d" % j, [128, L], BF16) for j in range(2)] for _ in range(NP)]
    ini = [[k.sb("ini%d_%d" % (qq, j), [128, 1], F32) for j in range(2)] for qq in range(4)]
    itmp = [k.sb("itmp%d" % qq, [128, 1], F32) for qq in range(4)]
    yo = [k.sb("yo", [128, L], F32) for _ in range(2)]
    nblk = ntok // L
    bps = seq // L
    it = 0
    for blk in range(nblk):
        sl = slice(blk * L, (blk + 1) * L)
        uB, uF, pY = ub[blk % 2], ubf[blk % 2], ps_y[blk % 2]
        k.dma("sp", uB, uB[:, :], u, u.t[:, sl])
        k.act(uF, uF[:, :], uB, uB[:, :], AF.Copy)
        first = (blk % bps == 0)
        for qq in range(4):
            par = it % NP
            it += 1
            pr_, pi2_ = ps_re[par], ps_im[par]
            k.mm(pr_, pr_[:, :], b_re, b_re[:, qq, :], uF, uF[:, :], True, True)
            k.mm(pi2_, pi2_[:, :], b_im, b_im[:, qq, :], uF, uF[:, :], True, True)
            t = T[par]
            cq, sq, rq = COS[:, qq, :], SIN[:, qq, :], RHO[:, qq, :]
            k.tt("dve", t[0], t[0][:, :], pr_, pr_[:, :], COS, cq, ALU.mult)
            k.tt("dve", t[1], t[1][:, :], pi2_, pi2_[:, :], SIN, sq, ALU.mult)
            k.tt("dve", t[2], t[2][:, :], pi2_, pi2_[:, :], COS, cq, ALU.mult)
            k.tt("dve", t[3], t[3][:, :], pr_, pr_[:, :], SIN, sq, ALU.mult)
            gi = gin[par]
            k.tt("pool", gi[0], gi[0][:, :], t[0], t[0][:, :], t[1], t[1][:, :], ALU.add)
            k.tt("dve", gi[1], gi[1][:, :], t[2], t[2][:, :], t[3], t[3][:, :], ALU.subtract)
            g_prev = gg[(blk + 1) % 2][qq]
            g_cur = gg[blk % 2][qq]
            if first:
                i_re, i_im = 0.0, 0.0
                xr = []
            else:
                k.ts("pool", itmp[qq], itmp[qq][:, :], g_prev[1], g_prev[1][:, L - 1:L], nELi[:, qq:qq + 1], ALU.mult,
                     extra_reads=[nELi])
                k.stt("dve", ini[qq][0], ini[qq][0][:, :], g_prev[0], g_prev[0][:, L - 1:L], ELr[:, qq:qq + 1],
                      itmp[qq], itmp[qq][:, :], ALU.mult, ALU.add, extra_reads=[ELr])
                k.ts("pool", itmp[qq], itmp[qq][:, :], g_prev[1], g_prev[1][:, L - 1:L], ELr[:, qq:qq + 1], ALU.mult,
                     extra_reads=[ELr])
                k.stt("dve", ini[qq][1], ini[qq][1][:, :], g_prev[0], g_prev[0][:, L - 1:L], ELi[:, qq:qq + 1],
                      itmp[qq], itmp[qq][:, :], ALU.mult, ALU.add, extra_reads=[ELi])
                i_re, i_im = ini[qq][0][:, :], ini[qq][1][:, :]
                xr = [ini[qq][0], ini[qq][1]]
            k.op("dve", lambda g, o=g_cur[0][:, :], a=rq, b=gi[0][:, :], i0=i_re: g.tensor_tensor_scan(
                out=o, data0=a, data1=b, initial=i0, op0=ALU.mult, op1=ALU.add), [RHO, gi[0]] + xr[:1], [g_cur[0]])
            k.op("dve", lambda g, o=g_cur[1][:, :], a=rq, b=gi[1][:, :], i0=i_im: g.tensor_tensor_scan(
                out=o, data0=a, data1=b, initial=i0, op0=ALU.mult, op1=ALU.add), [RHO, gi[1]] + xr[1:], [g_cur[1]])
            t2_ = T2[par]
            k.tt("pool", t2_[0], t2_[0][:, :], g_cur[0], g_cur[0][:, :], COS, cq, ALU.mult)
            k.tt("dve", t2_[1], t2_[1][:, :], g_cur[1], g_cur[1][:, :], SIN, sq, ALU.mult)
            k.tt("pool", t2_[2], t2_[2][:, :], g_cur[1], g_cur[1][:, :], COS, cq, ALU.mult)
            k.tt("dve", t2_[3], t2_[3][:, :], g_cur[0], g_cur[0][:, :], SIN, sq, ALU.mult)
            h = hh[par]
            k.tt("dve", h[0], h[0][:, :], t2_[0], t2_[0][:, :], t2_[1], t2_[1][:, :], ALU.subtract)
            k.tt("dve", h[1], h[1][:, :], t2_[2], t2_[2][:, :], t2_[3], t2_[3][:, :], ALU.add)
            k.mm(pY, pY[:, :], cp_re, cp_re[:, qq, :], h[0], h[0][:, :], qq == 0, False)
            k.mm(pY, pY[:, :], cp_imn, cp_imn[:, qq, :], h[1], h[1][:, :], False, qq == 3)
        yB = yo[blk % 2]
        k.stt("dve", yB, yB[:, :], uB, uB[:, :], s_d[:, 0:1], pY, pY[:, :], ALU.mult, ALU.add, extra_reads=[s_d])
        k.dma("sp", y, y.t[:, sl], yB, yB[:, :])
    return k.finish()


def s5_core_params(c, a_re, a_im, log_dt, b_re, b_im, c_re, c_im, d):
    g0 = 8 * c

    def pq(a):
        return np.ascontiguousarray(a[g0:g0 + 8].reshape(4, 2, 64).transpose(1, 2, 0).reshape(128, 4)).astype(np.float32)

    are = pq(a_re)
    aim = pq(a_im)
    ldt = pq(np.broadcast_to(log_dt[:, None], (64, 64)))
    B_re = np.zeros((4, 2, 16, 4, 2, 64), np.float32)
    B_im = np.zeros((4, 2, 16, 4, 2, 64), np.float32)
    C_re = np.zeros((2, 64, 4, 2, 16), np.float32)
    C_im = np.zeros((2, 64, 4, 2, 16), np.float32)
    for qq in range(4):
        for gp in range(2):
            g = g0 + 2 * qq + gp
            B_re[qq, gp, :, qq, gp, :] = b_re[g].T
            B_im[qq, gp, :, qq, gp, :] = b_im[g].T
            C_re[gp, :, qq, gp, :] = c_re[g].T
            C_im[gp, :, qq, gp, :] = c_im[g].T
    return {"are": are, "aim": aim, "ldt": ldt,
            "bre": B_re.reshape(128, 4, 128), "bim": B_im.reshape(128, 4, 128),
            "cre": C_re.reshape(128, 4, 32), "cim": C_im.reshape(128, 4, 32),
            "dsk": np.ascontiguousarray(d[128 * c:128 * c + 128].reshape(128, 1)).astype(np.float32)}


class Ctx:
    def __init__(self, k, NT, nkt_half):
        self.k = k
        self.NT = NT
        self.ps1 = [k.ps("ps1") for _ in range(2)]
        self.ps3 = [k.ps("ps3") for _ in range(2)]
        self.pso = [k.ps("pso") for _ in range(2)]
        self.pss = [k.ps("pss") for _ in range(2)]
        self.wa = [k.sb("wa", [128, 8, 512], BF16) for _ in range(2)]
        self.wb = [k.sb("wb", [128, 8, 512], BF16) for _ in range(2)]
        self.w2h = k.sb("w2h", [128, nkt_half, 1024], BF16)
        self.hmid = k.sb("hmid", [128, max(nkt_half, 8), NT], BF16)
        self.z = k.sb("z", [128, 8, NT], F32)
        self.xbf = k.sb("xbf", [128, 8, NT], BF16)
        self.sact = [k.sb("sact", [128, 512], F32) for _ in range(2)]
        self.tmp = [k.sb("tmp", [128, 512], F32) for _ in range(2)]
        self.ones = k.sb("ones", [128, 128], F32)
        k.op("pool", lambda g: g.memset(self.ones[:, :], 1.0), [], [self.ones])
        self.sq = [k.sb("sq", [128, 512], F32) for _ in range(2)]
        self.mean = k.sb("mean", [128, 512], F32)
        self.ex2 = k.sb("ex2", [128, 512], F32)
        self.rstd = k.sb("rstd", [128, 512], F32)
        self.nmr = k.sb("nmr", [128, 512], F32)
        self.lng = k.sb("lng", [128, 32], F32)
        self.lnb = k.sb("lnb", [128, 32], F32)
        self.wi = 0
        self.pi = 0
        self.oi = 0


def load_wblock(k, buf, wd, k0, nk, c0, cw):
    k.dma("pool", buf, buf[:, 0:nk, 0:cw], wd, wd.t[k0 * 128:(k0 + nk) * 128, c0:c0 + cw].rearrange("(kt p) m -> p kt m", p=128))


def glu_phase(k, cx, x_in, wa_d, a_c0, wb_d, b_c0, ncols, func, emit, hook=None, NT=None):
    NT = cx.NT if NT is None else NT
    for c0 in range(0, ncols, 512):
        cw = min(512, ncols - c0)
        wa, wb = cx.wa[cx.wi % 2], cx.wb[cx.wi % 2]
        cx.wi += 1
        load_wblock(k, wa, wa_d, 0, 8, a_c0 + c0, cw)
        load_wblock(k, wb, wb_d, 0, 8, b_c0 + c0, cw)
        if hook is not None and c0 == 512:
            hook()
        for fl in range(cw // 128):
            ft = c0 // 128 + fl
            for t0 in range(0, NT, 512):
                tw = min(512, NT - t0)
                p1, p3 = cx.ps1[cx.pi % 2], cx.ps3[cx.pi % 2]
                sa = cx.sact[cx.pi % 2]
                cx.pi += 1
                ts_ = slice(t0, t0 + tw)
                for kt in range(8):
                    k.mm(p1, p1[:, 0:tw], wa, wa[:, kt, fl * 128:fl * 128 + 128], x_in, x_in[:, kt, ts_], kt == 0, kt == 7)
                for kt in range(8):
                    k.mm(p3, p3[:, 0:tw], wb, wb[:, kt, fl * 128:fl * 128 + 128], x_in, x_in[:, kt, ts_], kt == 0, kt == 7)
                k.act(sa, sa[:, 0:tw], p1, p1[:, 0:tw], func)
                emit(ft, t0, tw, sa, p3)


def down_load(k, cx, w2_d, k0, nk):
    k.dma("pool", cx.w2h, cx.w2h[:, 0:nk, :], w2_d, w2_d.t[k0 * 128:(k0 + nk) * 128, :].rearrange("(kt p) m -> p kt m", p=128))


def down_phase(k, cx, w2_d, k0, nk, zacc):
    NT = cx.NT
    for m in range(8):
        for tb in range(NT // 512):
            po = cx.pso[cx.oi % 2]
            cx.oi += 1
            ts_ = slice(tb * 512, tb * 512 + 512)
            for kt in range(nk):
                k.mm(po, po[:, :], cx.w2h, cx.w2h[:, kt, m * 128:m * 128 + 128], cx.hmid, cx.hmid[:, kt, ts_], kt == 0, kt == nk - 1)
            k.tt("dve", zacc, zacc[:, m, ts_], po, po[:, :], zacc, zacc[:, m, ts_], ALU.add)


def layer_norm(k, cx, z, ln_idx, xbf_out):
    NT = cx.NT
    for tb in range(NT // 512):
        ts_ = slice(tb * 512, tb * 512 + 512)
        s1, s2 = cx.pss[0], cx.pss[1]
        if not hasattr(cx, "ones_b"):
            cx.ones_b = k.sb("ones_b", [128, 128], BF16)
            k.op("dve", lambda g, a=cx.ones_b[:, :]: g.memset(a, 1.0), [], [cx.ones_b])
            cx.zb = [k.sb("zb", [128, 512], BF16) for _ in range(2)]
            cx.sqb = [k.sb("sqb", [128, 512], BF16) for _ in range(2)]
        for kt in range(8):
            zb, sq = cx.zb[kt % 2], cx.sqb[kt % 2]
            k.act(zb, zb[:, :], z, z[:, kt, ts_], AF.Copy)
            k.act(sq, sq[:, :], z, z[:, kt, ts_], AF.Square)
            k.mm(s1, s1[:, :], cx.ones_b, cx.ones_b[:, :], zb, zb[:, :], kt == 0, kt == 7)
            k.mm(s2, s2[:, :], cx.ones_b, cx.ones_b[:, :], sq, sq[:, :], kt == 0, kt == 7)
        k.act(cx.mean, cx.mean[:, :], s1, s1[:, :], AF.Copy, scale=1.0 / D)
        k.act(cx.ex2, cx.ex2[:, :], s2, s2[:, :], AF.Copy, scale=1.0 / D)
        k.tt("dve", cx.nmr, cx.nmr[:, :], cx.mean, cx.mean[:, :], cx.mean, cx.mean[:, :], ALU.mult)
        k.tt("dve", cx.ex2, cx.ex2[:, :], cx.ex2, cx.ex2[:, :], cx.nmr, cx.nmr[:, :], ALU.subtract)
        k.ts("dve", cx.ex2, cx.ex2[:, :], cx.ex2, cx.ex2[:, :], LN_EPS, ALU.add)
        k.act(cx.ex2, cx.ex2[:, :], cx.ex2, cx.ex2[:, :], AF.Sqrt)
        k.op("dve", lambda g, o_=cx.rstd[:, :], i_=cx.ex2[:, :]: g.reciprocal(out=o_, in_=i_), [cx.ex2], [cx.rstd])
        k.stt("dve", cx.nmr, cx.nmr[:, :], cx.mean, cx.mean[:, :], -1.0, cx.rstd, cx.rstd[:, :], ALU.mult, ALU.mult)
        for kt in range(8):
            t = cx.tmp[kt % 2]
            k.tt("dve", t, t[:, :], z, z[:, kt, ts_], cx.rstd, cx.rstd[:, :], ALU.mult)
            k.tt("dve", t, t[:, :], t, t[:, :], cx.nmr, cx.nmr[:, :], ALU.add)
            col = ln_idx * 8 + kt
            k.op("act", lambda g, o=z[:, kt, ts_], i=t[:, :], sc=cx.lng[:, col:col + 1], bi=cx.lnb[:, col:col + 1]: g.activation(
                out=o, in_=i, func=AF.Identity, scale=sc, bias=bi),
                [t, cx.lng, cx.lnb], [z])
            if xbf_out is not None:
                k.act(xbf_out, xbf_out[:, kt, ts_], z, z[:, kt, ts_], AF.Copy)


def ln_layout(ln_g, ln_b):
    g = np.ascontiguousarray(ln_g.reshape(4, 8, 128).transpose(2, 0, 1).reshape(128, 32)).astype(np.float32)
    b = np.ascontiguousarray(ln_b.reshape(4, 8, 128).transpose(2, 0, 1).reshape(128, 32)).astype(np.float32)
    return g, b


DFF = 2816


def build_layer0_rest(tpc=TPC, NT=1024):
    k = KB()
    yT = k.dram("yT", [D, tpc], F32, "ExternalInput")
    xT = k.dram("xT", [D, tpc], F32, "ExternalInput")
    wglu = k.dram("wglu", [D, 2 * D], F32, "ExternalInput")
    w1 = k.dram("w1", [D, DFF], F32, "ExternalInput")
    w3 = k.dram("w3", [D, DFF], F32, "ExternalInput")
    w2 = k.dram("w2", [DFF, D], F32, "ExternalInput")
    lng = k.dram("lng", [128, 32], F32, "ExternalInput")
    lnb = k.dram("lnb", [128, 32], F32, "ExternalInput")
    oT = k.dram("oT", [D, tpc], F32, "ExternalOutput")
    cx = Ctx(k, NT, 11)
    k.dma("sp", cx.lng, cx.lng[:, :], lng, lng.t[:, :])
    k.dma("sp", cx.lnb, cx.lnb[:, :], lnb, lnb.t[:, :])
    ystage = [k.sb("ystage", [128, NT], F32) for _ in range(2)]
    gbf = cx.hmid
    z = cx.z
    for sb_ in range(tpc // NT):
        tsl = slice(sb_ * NT, (sb_ + 1) * NT)
        for kt in range(8):
            ys = ystage[kt % 2]
            k.dma("sp", ys, ys[:, :], yT, yT.t[kt * 128:(kt + 1) * 128, tsl])
            k.act(gbf, gbf[:, kt, :], ys, ys[:, :], AF.Gelu)
            k.dma("sp", z, z[:, kt, :], xT, xT.t[kt * 128:(kt + 1) * 128, tsl])

        def emit_glu(ft, t0, tw, sa, pB):
            t = cx.tmp[(ft + t0 // 512) % 2]
            ts_ = slice(t0, t0 + 512)
            k.tt("dve", t, t[:, :], pB, pB[:, :], sa, sa[:, :], ALU.mult)
            k.stt("dve", z, z[:, ft, ts_], z, z[:, ft, ts_], float(ALPHA), t, t[:, :], ALU.mult, ALU.add)

        glu_phase(k, cx, gbf, wglu, D, wglu, 0, D, AF.Sigmoid, emit_glu)
        layer_norm(k, cx, z, 0, cx.xbf)
        for kt in range(8):
            k.ts("dve", z, z[:, kt, :], z, z[:, kt, :], float(ALPHA), ALU.mult)
        for half in range(2):
            f0 = half * 11

            def emit_ffn(ft, t0, tw, sa, pB):
                ts_ = slice(t0, t0 + 512)
                k.tt("dve", cx.hmid, cx.hmid[:, ft, ts_], pB, pB[:, :], sa, sa[:, :], ALU.mult)

            glu_phase(k, cx, cx.xbf, w1, f0 * 128, w3, f0 * 128, 11 * 128, AF.Silu, emit_ffn,
                      hook=lambda f0=f0: down_load(k, cx, w2, f0, 11))
            down_phase(k, cx, w2, f0, 11, z)
        layer_norm(k, cx, z, 1, None)
        for kt in range(8):
            k.dma("sp", oT, oT.t[kt * 128:(kt + 1) * 128, tsl], z, z[:, kt, :])
    return k.finish()


NQKV = 9216
DILS = (1, 4, 16)


def build_qkv(tpc=TPC):
    k = KB()
    xT = k.dram("xT", [D, tpc], F32, "ExternalInput")
    w = k.dram("w", [D, NQKV], F32, "ExternalInput")
    o = k.dram("qkvT", [NQKV, tpc], BF16, "ExternalOutput")
    xb = k.sb("xb", [128, 8, tpc], BF16)
    for kt in range(8):
        k.dma("pool", xb, xb[:, kt, :], xT, xT.t[kt * 128:(kt + 1) * 128, :])
    wbuf = [k.sb("wq", [128, 8, 512], BF16) for _ in range(2)]
    pbs = [k.ps("pb") for _ in range(4)]
    sts = [k.sb("st", [128, 512], BF16) for _ in range(4)]
    i = 0
    for cb in range(NQKV // 512):
        wb = wbuf[cb % 2]
        load_wblock(k, wb, w, 0, 8, cb * 512, 512)
        for fl in range(4):
            for tb in range(tpc // 512):
                pb, st = pbs[i % 4], sts[i % 4]
                ts_ = slice(tb * 512, tb * 512 + 512)
                for kt in range(8):
                    k.mm(pb, pb[:, :], wb, wb[:, kt, fl * 128:fl * 128 + 128], xb, xb[:, kt, ts_], kt == 0, kt == 7)
                if i % 2 == 0:
                    k.act(st, st[:, :], pb, pb[:, :], AF.Copy)
                else:
                    k.op("dve", lambda g, o_=st[:, :], a=pb[:, :]: g.tensor_copy(out=o_, in_=a), [pb], [st])
                r0 = cb * 512 + fl * 128
                k.dma("sp", o, o.t[r0:r0 + 128, ts_], st, st[:, :])
                i += 1
    return k.finish()


ACH = 2048
NEG = -1e30


def build_attn(tpc=TPC, LA=3):
    k = KB()
    nblk = tpc // 128
    qb_d = k.dram("qb", [3, 8, 128, nblk * 128], BF16, "ExternalInput")
    kb_d = k.dram("kb", [3, 8, 128, (nblk + 16) * 128], BF16, "ExternalInput")
    vb_d = k.dram("vb", [3, 8, 128, (nblk + 16) * 128], BF16, "ExternalInput")
    bias_d = k.dram("biasT", [3, 8, 128, 512], F32, "ExternalInput")
    negb_d = k.dram("negb", [128, 1], F32, "ExternalInput")
    o = k.dram("attnT", [D, tpc], F32, "ExternalOutput")
    negb = k.sb("negb", [128, 1], F32)
    k.dma("sp", negb, negb[:, :], negb_d, negb_d.t[:, :])
    ones = k.sb("ones", [128, 64], BF16)
    k.op("pool", lambda g: g.memset(ones[:, :], 1.0), [], [ones])
    Qb = [k.sb("Qb", [128, 16 * 128], BF16) for _ in range(2)]
    Kb = [k.sb("Kb", [128, 32 * 128], BF16) for _ in range(2)]
    Vb = [k.sb("Vb", [128, 32, 128], BF16) for _ in range(2)]
    Bs = [k.sb("Bs", [128, 2, 256], F32) for _ in range(2)]
    num = [k.sb("num", [128, ACH], F32) for _ in range(2)]
    den = [k.sb("den", [128, ACH], F32) for _ in range(2)]
    rec = k.sb("rec", [128, ACH], F32)
    NS = LA + 1
    ps_s = [k.ps("ps_s", (128, 256)) for _ in range(NS)]
    ps_o = [k.ps("ps_o") for _ in range(2)]
    ps_d = [k.ps("ps_d") for _ in range(2)]
    tmp = [k.sb("tmpS", [128, 256], F32) for _ in range(NS + 1)]
    pT = [k.sb("pT", [128, 256], BF16) for _ in range(NS + 1)]
    units = []
    li = 0
    ei = 0
    ci = 0
    for ch in range(tpc // ACH):
        for ft in range(8):
            nm, dn = num[ci % 2], den[ci % 2]
            ci += 1
            for g in range(3):
                d = DILS[g]
                bufs = (Qb[li % 2], Kb[li % 2], Vb[li % 2], Bs[li % 2])
                li += 1
                for B in range(16):
                    for hh in range(2):
                        units.append(dict(ch=ch, ft=ft, g=g, d=d, B=B, hh=hh, bufs=bufs, nm=nm, dn=dn,
                                          po=ps_o[ei % 2], pd=ps_d[ei % 2], load=(B == 0 and hh == 0),
                                          evac=(B % 4 == 3 and hh == 1), last=(g == 2 and B == 15 and hh == 1)))
                    if B % 4 == 3:
                        ei += 1

    def s_part(ui, un):
        qb, kb, vb, bs = un["bufs"]
        ch, ft, g, d, B, hh = un["ch"], un["ft"], un["g"], un["d"], un["B"], un["hh"]
        if un["load"]:
            b0 = ch * 16
            k.dma("sp", qb, qb[:, :], qb_d, qb_d.t[g, ft, :, b0 * 128:(b0 + 16) * 128])
            nkb = 16 + d
            k0 = 16 + b0 - d
            k.dma("sp", kb, kb[:, 0:nkb * 128], kb_d, kb_d.t[g, ft, :, k0 * 128:(k0 + nkb) * 128])
            k.dma("sp", vb, vb[:, 0:nkb, :], vb_d,
                  vb_d.t[g, ft, :, k0 * 128:(k0 + nkb) * 128].rearrange("p (b f) -> p b f", f=128))
            k.dma("sp", bs, bs[:, :, :], bias_d, bias_d.t[g, ft, :, :].rearrange("p (h f) -> p h f", h=2))
        halo = (ch == 0 and B < d)
        rs = slice(64 * hh, 64 * hh + 64)
        pss = ps_s[ui % NS]
        tm, pt = tmp[ui % (NS + 1)], pT[ui % (NS + 1)]
        qs = slice(B * 128, B * 128 + 128)
        kp = slice(B * 128, B * 128 + 128)
        kc = slice((B + d) * 128, (B + d) * 128 + 128)
        k.mm(pss, pss[:, 0:128], kb, kb[rs, kp], qb, qb[rs, qs], True, True)
        k.mm(pss, pss[:, 128:256], kb, kb[rs, kc], qb, qb[rs, qs], True, True)
        k.stt("dve", tm, tm[:, :], pss, pss[:, 0:256], 0.125, bs, bs[:, hh, :], ALU.mult, ALU.add)
        if halo:
            k.op("act", lambda e, o_=pt[:, 0:128], i_=tm[:, 0:128]: e.activation(
                out=o_, in_=i_, func=AF.Exp, bias=negb[:, 0:1], scale=1.0), [tm, negb], [pt])
            k.act(pt, pt[:, 128:256], tm, tm[:, 128:256], AF.Exp)
        else:
            k.act(pt, pt[:, :], tm, tm[:, :], AF.Exp)

    def pv_part(ui, un):
        qb, kb, vb, bs = un["bufs"]
        ch, ft, g, d, B, hh = un["ch"], un["ft"], un["g"], un["d"], un["B"], un["hh"]
        po, pd, nm, dn = un["po"], un["pd"], un["nm"], un["dn"]
        rs = slice(64 * hh, 64 * hh + 64)
        pt = pT[ui % (NS + 1)]
        cs = slice((B % 4) * 128, (B % 4) * 128 + 128)
        k.mm(po, po[rs, cs], vb, vb[:, B, rs], pt, pt[:, 0:128], True, False)
        k.mm(po, po[rs, cs], vb, vb[:, B + d, rs], pt, pt[:, 128:256], False, True)
        k.mm(pd, pd[rs, cs], ones, ones[:, :], pt, pt[:, 0:128], True, False)
        k.mm(pd, pd[rs, cs], ones, ones[:, :], pt, pt[:, 128:256], False, True)
        if un["evac"]:
            B0 = B - 3
            if d == 1:
                ov = lambda t: t[:, B0 * 128:(B0 + 4) * 128]
                iv = lambda p: p[:, :]
            else:
                n_, r0 = B0 // d, B0 % d
                base = n_ * 128 * d
                ov = lambda t, base=base, d=d, r0=r0: t[:, base:base + 128 * d].rearrange(
                    "p (i r) -> p i r", r=d)[:, :, r0:r0 + 4]
                iv = lambda p: p[:, :].rearrange("p (j i) -> p i j", j=4)
            if g == 0:
                k.op("dve", lambda e, o_=ov(nm), i_=iv(po): e.tensor_copy(out=o_, in_=i_), [po], [nm])
                k.act(dn, ov(dn), pd, iv(pd), AF.Copy)
            else:
                k.tt("dve", nm, ov(nm), po, iv(po), nm, ov(nm), ALU.add)
                k.tt("pool", dn, ov(dn), pd, iv(pd), dn, ov(dn), ALU.add) if False else \
                    k.tt("dve", dn, ov(dn), pd, iv(pd), dn, ov(dn), ALU.add)
        if un["last"]:
            k.op("dve", lambda e, o_=rec[:, :], i_=dn[:, :]: e.reciprocal(out=o_, in_=i_), [dn], [rec])
            k.tt("pool", nm, nm[:, :], nm, nm[:, :], rec, rec[:, :], ALU.mult)
            k.dma("sp", o, o.t[ft * 128:(ft + 1) * 128, ch * ACH:(ch + 1) * ACH], nm, nm[:, :])

    n = len(units)
    for i in range(n + LA):
        if i < n:
            s_part(i, units[i])
        if i - LA >= 0:
            pv_part(i - LA, units[i - LA])
    return k.finish()


def t5_bucket(dist):
    n_buckets, max_distance = 32, 2048
    max_exact = n_buckets // 2
    d = np.maximum(dist, 0)
    large = max_exact + (np.log(np.maximum(d, 1) / max_exact) / math.log(max_distance / max_exact)
                         * (n_buckets - max_exact)).astype(np.int64)
    large = np.minimum(large, n_buckets - 1)
    return np.where(d < max_exact, d, large).astype(np.int32)


def attn_bias_table(rel_bias):
    L = 128
    qi = np.arange(L)[:, None]
    ki = np.arange(2 * L)[None, :]
    off = qi + L - ki
    band = (off >= 0) & (off <= L)
    out = np.zeros((3, 8, 128, 2, 2, 128), np.float32)
    for g, d in enumerate(DILS):
        bucket = t5_bucket(np.clip(off, 0, None) * d)
        bias = rel_bias[bucket]
        bias = np.where(band[:, :, None], bias, np.float32(NEG))
        b = bias.reshape(L, 2, L, 8, 2)
        out[g] = b.transpose(3, 2, 4, 1, 0)
    return out.reshape(3, 8, 128, 512)


def attn_host_layout(qkvT, rel_bias, seqlen, tpc):
    ntok = qkvT.shape[1]
    ncore = ntok // tpc
    nblk = tpc // 128
    bias = attn_bias_table(rel_bias)
    perm = {}
    for g, d in enumerate(DILS):
        for j in range(3):
            a = qkvT[(g * 3 + j) * 1024:(g * 3 + j + 1) * 1024]
            a = a.reshape(8, 128, ntok // (128 * d), 128, d).transpose(0, 1, 2, 4, 3)
            perm[g, j] = a.reshape(8, 128, ntok // 128, 128)
    maps = []
    for c in range(ncore):
        first = (c * tpc) % seqlen == 0
        B0 = c * nblk
        qb = np.stack([perm[g, 0][:, :, B0:B0 + nblk].reshape(8, 128, nblk * 128) for g in range(3)])
        kbs, vbs = [], []
        for g in range(3):
            kk = np.zeros((8, 128, nblk + 16, 128), qkvT.dtype)
            vv = np.zeros((8, 128, nblk + 16, 128), qkvT.dtype)
            lo = 0 if first else 16
            kk[:, :, 16 - lo:] = perm[g, 1][:, :, B0 - lo:B0 + nblk]
            vv[:, :, 16 - lo:] = perm[g, 2][:, :, B0 - lo:B0 + nblk]
            kbs.append(kk.reshape(8, 128, -1))
            vbs.append(vv.transpose(0, 3, 2, 1).reshape(8, 128, -1))
        maps.append({"qb": np.ascontiguousarray(qb), "kb": np.ascontiguousarray(np.stack(kbs)),
                     "vb": np.ascontiguousarray(np.stack(vbs)), "biasT": bias,
                     "negb": np.full((128, 1), NEG if first else 0.0, np.float32)})
    return maps


NEXP = 8
DFE = 3584


def build_layer1_rest(tpc=TPC, NT=1024):
    k = KB()
    aT = k.dram("aT", [D, tpc], F32, "ExternalInput")
    xT = k.dram("xT", [D, tpc], F32, "ExternalInput")
    wo = k.dram("wo", [D, D], F32, "ExternalInput")
    wr = k.dram("wr", [128, 8, NEXP], F32, "ExternalInput")
    w1 = k.dram("w1", [NEXP, D, DFE], F32, "ExternalInput")
    w3 = k.dram("w3", [NEXP, D, DFE], F32, "ExternalInput")
    w2 = k.dram("w2", [NEXP, DFE, D], F32, "ExternalInput")
    lng = k.dram("lng", [128, 32], F32, "ExternalInput")
    lnb = k.dram("lnb", [128, 32], F32, "ExternalInput")
    ident_d = k.dram("ident", [128, 128], F32, "ExternalInput")
    oT = k.dram("oT", [D, tpc], F32, "ExternalOutput")
    cx = Ctx(k, NT, 14)
    k.dma("sp", cx.lng, cx.lng[:, :], lng, lng.t[:, :])
    k.dma("sp", cx.lnb, cx.lnb[:, :], lnb, lnb.t[:, :])
    ident = k.sb("ident", [128, 128], F32)
    k.dma("sp", ident, ident[:, :], ident_d, ident_d.t[:, :])
    wr_sb = k.sb("wr", [128, 8, NEXP], F32)
    k.dma("sp", wr_sb, wr_sb[:, :, :], wr, wr.t[:, :, :])
    ones_bf = k.sb("ones_bf", [128, 128], BF16)
    k.op("pool", lambda g: g.memset(ones_bf[:, :], 1.0), [], [ones_bf])
    gate_bc = k.sb("gate_bc", [128, NEXP, NT], BF16)
    ntt = NT // 128
    lg = k.sb("lg", [128, ntt, 8], F32)
    top = k.sb("top", [128, ntt, 8], F32)
    gsm = [k.sb("gsm%d" % i, [128, ntt], F32) for i in range(4)]
    gate = k.sb("gate", [128, ntt, 8], F32)
    gt2 = k.sb("gt2", [128, ntt, 8], F32)
    diag = [k.sb("diag", [128, 128], BF16) for _ in range(2)]
    z = cx.z
    abf = cx.hmid
    for sb_ in range(tpc // NT):
        tsl = slice(sb_ * NT, (sb_ + 1) * NT)
        for kt in range(8):
            k.dma("pool", abf, abf[:, kt, :], aT, aT.t[kt * 128:(kt + 1) * 128, tsl])
            k.dma("sp", z, z[:, kt, :], xT, xT.t[kt * 128:(kt + 1) * 128, tsl])
        for cb in range(2):
            wa = cx.wa[cx.wi % 2]
            cx.wi += 1
            load_wblock(k, wa, wo, 0, 8, cb * 512, 512)
            for fl in range(4):
                m = cb * 4 + fl
                for tb in range(NT // 512):
                    po = cx.pso[cx.oi % 2]
                    cx.oi += 1
                    ts_ = slice(tb * 512, tb * 512 + 512)
                    for kt in range(8):
                        k.mm(po, po[:, :], wa, wa[:, kt, fl * 128:fl * 128 + 128], abf, abf[:, kt, ts_], kt == 0, kt == 7)
                    k.stt("dve", z, z[:, m, ts_], z, z[:, m, ts_], float(ALPHA), po, po[:, :], ALU.mult, ALU.add)
        layer_norm(k, cx, z, 2, cx.xbf)
        k.ses = True
        pl = cx.pss[0]
        for tt in range(ntt):
            for kt in range(8):
                k.mm(pl, pl[:, tt * 8:tt * 8 + 8], z, z[:, kt, tt * 128:(tt + 1) * 128], wr_sb, wr_sb[:, kt, :], kt == 0, kt == 7)
        k.act(lg, lg[:, :, :], pl, pl[:, 0:ntt * 8].rearrange("p (t e) -> p t e", e=8), AF.Copy)
        nv1, e2, g1, g2 = gsm
        for tt in range(ntt):
            k.op("dve", lambda g, o=top[:, tt, :], i=lg[:, tt, :]: g.max(out=o, in_=i), [lg], [top])
        k.ts("dve", nv1, nv1[:, :], top, top[:, :, 0], -1.0, ALU.mult)
        k.tt("dve", e2, e2[:, :], top, top[:, :, 1], nv1, nv1[:, :], ALU.add)
        k.act(e2, e2[:, :], e2, e2[:, :], AF.Exp)
        k.ts("dve", g1, g1[:, :], e2, e2[:, :], 1.0, ALU.add)
        k.op("dve", lambda g: g.reciprocal(out=g1[:, :], in_=g1[:, :]), [g1], [g1])
        k.tt("dve", g2, g2[:, :], e2, e2[:, :], g1, g1[:, :], ALU.mult)
        for tt in range(ntt):
            k.ts("dve", gate, gate[:, tt, :], lg, lg[:, tt, :], top[:, tt, 0:1], ALU.is_equal, g1[:, tt:tt + 1], ALU.mult,
                 extra_reads=[top, g1])
            k.ts("dve", gt2, gt2[:, tt, :], lg, lg[:, tt, :], top[:, tt, 1:2], ALU.is_equal, g2[:, tt:tt + 1], ALU.mult,
                 extra_reads=[top, g2])
        k.tt("dve", gate, gate[:, :, :], gate, gate[:, :, :], gt2, gt2[:, :, :], ALU.add)
        di = 0
        for e in range(NEXP):
            for hb in range(NT // 512):
                pg = cx.pss[1]
                for t4 in range(4):
                    tt = hb * 4 + t4
                    dg = diag[di % 2]
                    di += 1
                    k.ts("dve", dg, dg[:, :], ident, ident[:, :], gate[:, tt, e:e + 1], ALU.mult, extra_reads=[gate])
                    k.mm(pg, pg[:, t4 * 128:(t4 + 1) * 128], ones_bf, ones_bf[:, :], dg, dg[:, :], True, True)
                k.act(gate_bc, gate_bc[:, e, hb * 512:(hb + 1) * 512], pg, pg[:, :], AF.Copy)
        k.ses = SES
        for kt in range(8):
            k.ts("dve", z, z[:, kt, :], z, z[:, kt, :], float(ALPHA), ALU.mult)
        for e in range(NEXP):
            w1e, w3e, w2e = Buf("w1e", w1.t[e]), Buf("w3e", w3.t[e]), Buf("w2e", w2.t[e])
            for half in range(2):
                f0 = half * 14

                def emit_moe(ft, t0, tw, sa, pB, e=e):
                    ts_ = slice(t0, t0 + 512)
                    t = cx.tmp[(ft + t0 // 512) % 2]
                    k.tt("dve", t, t[:, :], pB, pB[:, :], sa, sa[:, :], ALU.mult)
                    k.tt("dve", cx.hmid, cx.hmid[:, ft, ts_], t, t[:, :], gate_bc, gate_bc[:, e, ts_], ALU.mult)

                glu_phase(k, cx, cx.xbf, w1e, f0 * 128, w3e, f0 * 128, 14 * 128, AF.Silu, emit_moe,
                          hook=lambda f0=f0, w2e=w2e: down_load(k, cx, w2e, f0, 14))
                down_phase(k, cx, w2e, f0, 14, z)
        layer_norm(k, cx, z, 3, None)
        for kt in range(8):
            k.dma("sp", oT, oT.t[kt * 128:(kt + 1) * 128, tsl], z, z[:, kt, :])
    return k.finish()


_PROGS = {}


def _prog(name, fn):
    if name not in _PROGS:
        _PROGS[name] = fn()
    return _PROGS[name]


def _run(nc, maps):
    res = run_bass_kernel_spmd(nc, maps, core_ids=list(range(NCORES)))
    return res.results


def kernel(x, s5_w_in, s5_a_re, s5_a_im, s5_log_dt, s5_b_re, s5_b_im, s5_c_re, s5_c_im, s5_d,
           s5_w_glu, attn_w_qkv, attn_w_o, rel_bias, ffn_w1, ffn_w3, ffn_w2, moe_w_router,
           moe_w1, moe_w3, moe_w2, ln_g, ln_b):
    f32 = np.float32
    x = np.asarray(x, f32)
    xT = np.ascontiguousarray(x.reshape(NTOK, D).T)
    cols = [slice(c * TPC, (c + 1) * TPC) for c in range(NCORES)]
    xTs = [np.ascontiguousarray(xT[:, s]) for s in cols]
    lg, lb = ln_layout(np.asarray(ln_g, f32), np.asarray(ln_b, f32))

    w_in = np.ascontiguousarray(np.asarray(s5_w_in, f32)[0])
    r = _run(_prog("A", build_uproj), [{"xT": xTs[c], "w": w_in} for c in range(NCORES)])
    u_full = np.concatenate([r[c]["uT"] for c in range(NCORES)], axis=1)
    maps = []
    for c in range(NCORES):
        m = s5_core_params(c, np.asarray(s5_a_re, f32)[0], np.asarray(s5_a_im, f32)[0], np.asarray(s5_log_dt, f32)[0],
                           np.asarray(s5_b_re, f32)[0], np.asarray(s5_b_im, f32)[0], np.asarray(s5_c_re, f32)[0],
                           np.asarray(s5_c_im, f32)[0], np.asarray(s5_d, f32)[0])
        m["u"] = np.ascontiguousarray(u_full[128 * c:128 * c + 128])
        maps.append(m)
    r = _run(_prog("B", build_s5), maps)
    y_full = np.concatenate([r[c]["y"] for c in range(NCORES)], axis=0)
    wglu = np.ascontiguousarray(np.asarray(s5_w_glu, f32)[0])
    w1 = np.ascontiguousarray(np.asarray(ffn_w1, f32)[0])
    w3 = np.ascontiguousarray(np.asarray(ffn_w3, f32)[0])
    w2 = np.ascontiguousarray(np.asarray(ffn_w2, f32)[0])
    r = _run(_prog("C", build_layer0_rest),
             [{"yT": np.ascontiguousarray(y_full[:, cols[c]]), "xT": xTs[c], "wglu": wglu, "w1": w1, "w3": w3, "w2": w2,
               "lng": lg, "lnb": lb} for c in range(NCORES)])
    x2s = [r[c]["oT"] for c in range(NCORES)]
    wqkv = np.ascontiguousarray(np.asarray(attn_w_qkv, f32)[0])
    r = _run(_prog("D", build_qkv), [{"xT": x2s[c], "w": wqkv} for c in range(NCORES)])
    qkvT = np.concatenate([r[c]["qkvT"] for c in range(NCORES)], axis=1)
    maps = attn_host_layout(qkvT, np.asarray(rel_bias, f32), SEQ, TPC)
    r = _run(_prog("E", build_attn), maps)
    aTs = [r[c]["attnT"] for c in range(NCORES)]
    wo = np.ascontiguousarray(np.asarray(attn_w_o, f32)[0])
    wr = np.ascontiguousarray(np.asarray(moe_w_router, f32)[0].reshape(8, 128, NEXP).transpose(1, 0, 2))
    mw1 = np.ascontiguousarray(np.asarray(moe_w1, f32)[0])
    mw3 = np.ascontiguousarray(np.asarray(moe_w3, f32)[0])
    mw2 = np.ascontiguousarray(np.asarray(moe_w2, f32)[0])
    ident = np.eye(128, dtype=f32)
    ltri = np.triu(np.ones((128, 128), f32), 1)
    ln4 = np.ascontiguousarray(np.stack([np.asarray(ln_g, f32)[1, 1], np.asarray(ln_b, f32)[1, 1]]))
    r = _run(_prog("F2", build_layer1_fast),
             [{"aT": aTs[c], "xT": x2s[c], "wo": wo, "wr": wr, "w1": mw1, "w3": mw3, "w2": mw2, "lng": lg, "lnb": lb,
               "ln4": ln4, "ident": ident, "ltri": ltri} for c in range(NCORES)])
    counts = np.stack([r[c]["cnt"][0] for c in range(NCORES)])
    if counts.max() <= CAP:
        out = np.concatenate([r[c]["o"] for c in range(NCORES)], axis=0)
        return np.ascontiguousarray(out).reshape(BATCH, SEQ, D).astype(f32)
    r = _run(_prog("F", build_layer1_rest),
             [{"aT": aTs[c], "xT": x2s[c], "wo": wo, "wr": wr, "w1": mw1, "w3": mw3, "w2": mw2, "lng": lg, "lnb": lb,
               "ident": ident} for c in range(NCORES)])
    oT = np.concatenate([r[c]["oT"] for c in range(NCORES)], axis=1)
    return np.ascontiguousarray(oT.T).reshape(BATCH, SEQ, D).astype(f32)


CAP = 1280
U32 = mybir.dt.uint32
OOR = 1.0e6


class NS:
    pass


def build_layer1_fast(tpc=TPC, NT=1024, cap=CAP):
    k = KB()
    aT = k.dram("aT", [D, tpc], F32, "ExternalInput")
    xT = k.dram("xT", [D, tpc], F32, "ExternalInput")
    wo = k.dram("wo", [D, D], F32, "ExternalInput")
    wr = k.dram("wr", [128, 8, NEXP], F32, "ExternalInput")
    w1 = k.dram("w1", [NEXP, D, DFE], F32, "ExternalInput")
    w3 = k.dram("w3", [NEXP, D, DFE], F32, "ExternalInput")
    w2 = k.dram("w2", [NEXP, DFE, D], F32, "ExternalInput")
    lng = k.dram("lng", [128, 32], F32, "ExternalInput")
    lnb = k.dram("lnb", [128, 32], F32, "ExternalInput")
    ln4 = k.dram("ln4", [2, D], F32, "ExternalInput")
    ident_d = k.dram("ident", [128, 128], F32, "ExternalInput")
    ltri_d = k.dram("ltri", [128, 128], F32, "ExternalInput")
    o = k.dram("o", [tpc, D], F32, "ExternalOutput")
    cnt_o = k.dram("cnt", [128, NEXP], F32, "ExternalOutput")
    xg = k.dram("xg", [NEXP * cap + 1, D], BF16)
    ye = k.dram("ye", [NEXP * cap + 1, D], F32)
    x3tm = k.dram("x3tm", [tpc, D], F32)
    ntile = tpc // 128
    ntt = NT // 128
    nst = cap // 128

    ident = k.sb("ident", [128, 128], F32)
    k.dma("sp", ident, ident[:, :], ident_d, ident_d.t[:, :])
    ident_bf = k.sb("ident_bf", [128, 128], BF16)
    k.dma("pool", ident_bf, ident_bf[:, :], ident_d, ident_d.t[:, :])
    ltri = k.sb("ltri", [128, 128], BF16)
    k.dma("pool", ltri, ltri[:, :], ltri_d, ltri_d.t[:, :])
    ones_bf = k.sb("ones_bf", [128, 128], BF16)
    k.op("pool", lambda g, a=ones_bf[:, :]: g.memset(a, 1.0), [], [ones_bf])
    IDX = [k.sb("idx%d" % j, [128, ntile], U32) for j in range(2)]
    G = [k.sb("G%d" % j, [128, ntile], F32) for j in range(2)]
    base = k.sb("base", [128, NEXP], F32)
    k.op("pool", lambda g, a=base[:, :]: g.memset(a, 0.0), [], [base])
    iota_e = k.sb("iota_e", [128, ntt, NEXP], F32)
    for e in range(NEXP):
        k.op("pool", lambda g, e=e, a=iota_e[:, :, e:e + 1]: g.memset(a, float(e)), [], [iota_e])

    k.push_scope()
    cx = NS()
    cx.k, cx.NT = k, NT
    cx.pso = [k.ps("pso") for _ in range(2)]
    cx.pss = [k.ps("pss") for _ in range(2)]
    ptb = [k.ps("ptb", (128, 1024), BF16) for _ in range(2)]
    ptf = [k.ps("ptf") for _ in range(2)]
    cx.wa = [k.sb("wa", [128, 8, 512], BF16) for _ in range(2)]
    cx.z = k.sb("z", [128, 8, NT], F32)
    cx.xbf = k.sb("xbf", [128, 8, NT], BF16)
    abf = k.sb("abf", [128, 8, NT], BF16)
    cx.tmp = [k.sb("tmp", [128, 512], F32) for _ in range(2)]
    cx.ones = k.sb("ones", [128, 128], F32)
    k.op("pool", lambda g, a=cx.ones[:, :]: g.memset(a, 1.0), [], [cx.ones])
    cx.sq = [k.sb("sq", [128, 512], F32) for _ in range(2)]
    cx.mean = k.sb("mean", [128, 512], F32)
    cx.ex2 = k.sb("ex2", [128, 512], F32)
    cx.rstd = k.sb("rstd", [128, 512], F32)
    cx.nmr = k.sb("nmr", [128, 512], F32)
    cx.lng = k.sb("lng", [128, 32], F32)
    cx.lnb = k.sb("lnb", [128, 32], F32)
    cx.wi = cx.pi = cx.oi = 0
    k.dma("sp", cx.lng, cx.lng[:, :], lng, lng.t[:, :])
    k.dma("sp", cx.lnb, cx.lnb[:, :], lnb, lnb.t[:, :])
    wr_sb = k.sb("wr", [128, 8, NEXP], F32)
    k.dma("sp", wr_sb, wr_sb[:, :, :], wr, wr.t[:, :, :])
    lg = k.sb("lg", [128, ntt, 8], F32)
    top = k.sb("top", [128, ntt, 8], F32)
    nv1, e2, g1, g2 = [k.sb("gsm%d" % i, [128, ntt], F32) for i in range(4)]
    m = [k.sb("m%d" % j, [128, ntt, 8], F32) for j in range(2)]
    msel = k.sb("msel", [128, ntt, 8], BF16)
    Pw = k.sb("Pw", [128, ntt, 8], F32)
    cn = k.sb("cn", [128, ntt, 8], F32)
    offs = k.sb("offs", [128, ntt + 1, 8], F32)
    pos = k.sb("pos", [128, ntt, 8], F32)
    prod = k.sb("prod", [128, ntt, 8], F32)
    psel = [k.sb("psel%d" % j, [128, ntt], F32) for j in range(2)]
    esel = [k.sb("esel%d" % j, [128, ntt], F32) for j in range(2)]
    ovf = k.sb("ovf", [128, ntt], F32)
    flat = k.sb("flat", [128, ntt], F32)
    xtm = [k.sb("xtm", [128, D], BF16) for _ in range(2)]
    x3s = [k.sb("x3s", [128, D], F32) for _ in range(2)]
    z = cx.z
    for sb_ in range(tpc // NT):
        tsl = slice(sb_ * NT, (sb_ + 1) * NT)
        k.ses = SES
        for kt in range(8):
            k.dma("pool", abf, abf[:, kt, :], aT, aT.t[kt * 128:(kt + 1) * 128, tsl])
            k.dma("sp", z, z[:, kt, :], xT, xT.t[kt * 128:(kt + 1) * 128, tsl])
        for cb in range(2):
            wa = cx.wa[cx.wi % 2]
            cx.wi += 1
            load_wblock(k, wa, wo, 0, 8, cb * 512, 512)
            for fl in range(4):
                mt = cb * 4 + fl
                for tb in range(NT // 512):
                    po = cx.pso[cx.oi % 2]
                    cx.oi += 1
                    ts_ = slice(tb * 512, tb * 512 + 512)
                    for kt in range(8):
                        k.mm(po, po[:, :], wa, wa[:, kt, fl * 128:fl * 128 + 128], abf, abf[:, kt, ts_], kt == 0, kt == 7)
                    k.stt("dve", z, z[:, mt, ts_], z, z[:, mt, ts_], float(ALPHA), po, po[:, :], ALU.mult, ALU.add)
        layer_norm(k, cx, z, 2, cx.xbf)
        k.ses = True
        pl = cx.pss[0]
        for tt in range(ntt):
            for kt in range(8):
                k.mm(pl, pl[:, tt * 8:tt * 8 + 8], z, z[:, kt, tt * 128:(tt + 1) * 128], wr_sb, wr_sb[:, kt, :], kt == 0, kt == 7)
        k.act(lg, lg[:, :, :], pl, pl[:, 0:ntt * 8].rearrange("p (t e) -> p t e", e=8), AF.Copy)
        for tt in range(ntt):
            k.op("dve", lambda g, o_=top[:, tt, :], i=lg[:, tt, :]: g.max(out=o_, in_=i), [lg], [top])
        k.ts("dve", nv1, nv1[:, :], top, top[:, :, 0], -1.0, ALU.mult)
        k.tt("dve", e2, e2[:, :], top, top[:, :, 1], nv1, nv1[:, :], ALU.add)
        k.act(e2, e2[:, :], e2, e2[:, :], AF.Exp)
        k.ts("dve", g1, g1[:, :], e2, e2[:, :], 1.0, ALU.add)
        k.op("dve", lambda g, a=g1[:, :]: g.reciprocal(out=a, in_=a), [g1], [g1])
        k.tt("dve", g2, g2[:, :], e2, e2[:, :], g1, g1[:, :], ALU.mult)
        t0_ = sb_ * ntt
        k.op("dve", lambda g, o_=G[0][:, t0_:t0_ + ntt], i_=g1[:, :]: g.tensor_copy(out=o_, in_=i_), [g1], [G[0]])
        k.op("dve", lambda g, o_=G[1][:, t0_:t0_ + ntt], i_=g2[:, :]: g.tensor_copy(out=o_, in_=i_), [g2], [G[1]])
        for tt in range(ntt):
            k.ts("dve", m[0], m[0][:, tt, :], lg, lg[:, tt, :], top[:, tt, 0:1], ALU.is_equal, extra_reads=[top])
            k.ts("dve", m[1], m[1][:, tt, :], lg, lg[:, tt, :], top[:, tt, 1:2], ALU.is_equal, extra_reads=[top])
        k.tt("dve", msel, msel[:, :, :], m[0], m[0][:, :, :], m[1], m[1][:, :, :], ALU.add)
        pp = cx.pss[1]
        mflat = msel[:, :, :].rearrange("p t e -> p (t e)")
        k.mm(pp, pp[:, 0:ntt * 8], ltri, ltri[:, :], msel, mflat, True, True)
        k.mm(pp, pp[:, 256:256 + ntt * 8], ones_bf, ones_bf[:, :], msel, mflat, True, True)
        k.act(Pw, Pw[:, :, :], pp, pp[:, 0:ntt * 8].rearrange("p (t e) -> p t e", e=8), AF.Copy)
        k.act(cn, cn[:, :, :], pp, pp[:, 256:256 + ntt * 8].rearrange("p (t e) -> p t e", e=8), AF.Copy)
        k.op("dve", lambda g, o_=offs[:, 0, :], i_=base[:, :]: g.tensor_copy(out=o_, in_=i_), [base], [offs])
        for tt in range(ntt):
            k.tt("dve", offs, offs[:, tt + 1, :], offs, offs[:, tt, :], cn, cn[:, tt, :], ALU.add)
        k.op("dve", lambda g, o_=base[:, :], i_=offs[:, ntt, :]: g.tensor_copy(out=o_, in_=i_), [offs], [base])
        k.tt("dve", pos, pos[:, :, :], Pw, Pw[:, :, :], offs, offs[:, 0:ntt, :], ALU.add)
        for j in range(2):
            k.tt("dve", prod, prod[:, :, :], m[j], m[j][:, :, :], pos, pos[:, :, :], ALU.mult)
            k.op("dve", lambda g, o_=psel[j][:, :], i_=prod[:, :, :]: g.reduce_sum(out=o_, in_=i_, axis=AX.X), [prod], [psel[j]])
            k.tt("dve", prod, prod[:, :, :], m[j], m[j][:, :, :], iota_e, iota_e[:, :, :], ALU.mult)
            k.op("dve", lambda g, o_=esel[j][:, :], i_=prod[:, :, :]: g.reduce_sum(out=o_, in_=i_, axis=AX.X), [prod], [esel[j]])
            k.stt("dve", flat, flat[:, :], esel[j], esel[j][:, :], float(cap), psel[j], psel[j][:, :], ALU.mult, ALU.add)
            k.ts("dve", ovf, ovf[:, :], psel[j], psel[j][:, :], float(cap) - 0.5, ALU.is_gt)
            k.stt("dve", flat, flat[:, :], ovf, ovf[:, :], OOR, flat, flat[:, :], ALU.mult, ALU.add)
            k.ts("dve", flat, flat[:, :], flat, flat[:, :], float(NEXP * cap), ALU.min)
            k.op("dve", lambda g, o_=IDX[j][:, t0_:t0_ + ntt], i_=flat[:, :]: g.tensor_copy(out=o_, in_=i_), [flat], [IDX[j]])
        k.ses = SES
        for kt in range(8):
            k.ts("dve", z, z[:, kt, :], z, z[:, kt, :], float(ALPHA), ALU.mult)
        for tt in range(ntt):
            gt = t0_ + tt
            pb_, xt_ = ptb[tt % 2], xtm[tt % 2]
            for kt in range(8):
                k.tr(pb_, pb_[:, kt * 128:(kt + 1) * 128], cx.xbf, cx.xbf[:, kt, tt * 128:(tt + 1) * 128], ident_bf, ident_bf[:, :])
            k.act(xt_, xt_[:, :], pb_, pb_[:, :], AF.Copy)
            for j in range(2):
                k.idma(xg, xg.t[:, :], xt_, xt_[:, :], IDX[j], IDX[j][:, gt:gt + 1], True, NEXP * cap - 1)
            x3_ = x3s[tt % 2]
            for hf in range(2):
                pf_ = ptf[hf]
                for kq in range(4):
                    kt = hf * 4 + kq
                    k.tr(pf_, pf_[:, kq * 128:(kq + 1) * 128], z, z[:, kt, tt * 128:(tt + 1) * 128], ident, ident[:, :])
                k.op("dve", lambda g, o_=x3_[:, hf * 512:(hf + 1) * 512], i_=pf_[:, :]: g.tensor_copy(out=o_, in_=i_), [pf_], [x3_])
            k.dma("sp", x3tm, x3tm.t[gt * 128:(gt + 1) * 128, :], x3_, x3_[:, :])
    k.dma("sp", cnt_o, cnt_o.t[:, :], base, base[:, :])
    k.pop_scope()

    k.push_scope()
    cx = NS()
    cx.k, cx.NT = k, cap
    cx.ps1 = [k.ps("ps1") for _ in range(2)]
    cx.ps3 = [k.ps("ps3") for _ in range(2)]
    cx.pso = [k.ps("pso") for _ in range(2)]
    ptr = [k.ps("ptr", (128, 512), BF16) for _ in range(2)]
    cx.wa = [k.sb("wa", [128, 8, 512], BF16) for _ in range(2)]
    cx.wb = [k.sb("wb", [128, 8, 512], BF16) for _ in range(2)]
    cx.w2h = k.sb("w2h", [128, 14, 1024], BF16)
    cx.hmid = k.sb("hmid", [128, 14, cap], BF16)
    cx.sact = [k.sb("sact", [128, 512], F32) for _ in range(2)]
    cx.wi = cx.pi = cx.oi = 0
    xe = k.sb("xge", [128, 8, cap], BF16)
    xs = [k.sb("xs", [128, 4, D], BF16) for _ in range(2)]
    yacc = k.sb("yacc", [128, nst, D], F32)
    xi = 0
    ti = 0
    for e in range(NEXP):
        for sg in range((nst + 3) // 4):
            n_ = min(4, nst - 4 * sg)
            xs_ = xs[xi % 2]
            xi += 1
            r0 = e * cap + sg * 512
            k.dma("sp", xs_, xs_[:, 0:n_, :], xg, xg.t[r0:r0 + n_ * 128, :].rearrange("(t p) f -> p t f", p=128))
            for kt in range(8):
                pt_ = ptr[ti % 2]
                for t in range(n_):
                    k.tr(pt_, pt_[:, t * 128:(t + 1) * 128], xs_, xs_[:, t, kt * 128:(kt + 1) * 128], ident_bf, ident_bf[:, :])
                if ti % 2 == 0:
                    k.act(xe, xe[:, kt, sg * 512:sg * 512 + n_ * 128], pt_, pt_[:, 0:n_ * 128], AF.Copy)
                else:
                    k.op("dve", lambda g, o_=xe[:, kt, sg * 512:sg * 512 + n_ * 128], i_=pt_[:, 0:n_ * 128]:
                         g.tensor_copy(out=o_, in_=i_), [pt_], [xe])
                ti += 1
        w1e, w3e, w2e = Buf("w1e", w1.t[e]), Buf("w3e", w3.t[e]), Buf("w2e", w2.t[e])
        for half in range(2):
            f0 = half * 14

            def emit_h(ft, t0, tw, sa, pB, cx=cx):
                k.tt("dve", cx.hmid, cx.hmid[:, ft, t0:t0 + tw], pB, pB[:, 0:tw], sa, sa[:, 0:tw], ALU.mult)

            glu_phase(k, cx, xe, w1e, f0 * 128, w3e, f0 * 128, 14 * 128, AF.Silu, emit_h,
                      hook=lambda f0=f0, w2e=w2e, cx=cx: down_load(k, cx, w2e, f0, 14), NT=cap)
            for st in range(nst):
                for fh in range(2):
                    po = cx.pso[cx.oi % 2]
                    cx.oi += 1
                    for kt in range(14):
                        k.mm(po, po[:, :], cx.hmid, cx.hmid[:, kt, st * 128:(st + 1) * 128], cx.w2h, cx.w2h[:, kt, fh * 512:(fh + 1) * 512],
                             kt == 0, kt == 13)
                    ysl = yacc[:, st, fh * 512:(fh + 1) * 512]
                    if half == 0:
                        k.act(yacc, ysl, po, po[:, :], AF.Copy)
                    else:
                        k.tt("dve", yacc, ysl, po, po[:, :], yacc, ysl, ALU.add)
        k.dma("sp", ye, ye.t[e * cap:(e + 1) * cap, :].rearrange("(t p) f -> p t f", p=128), yacc, yacc[:, :, :])
    k.pop_scope()

    k.push_scope()
    gB = k.sb("gB", [128, D], F32)
    bB = k.sb("bB", [128, D], F32)
    k.dma("sp", gB, gB[:, :], ln4, ln4.t[0:1, :].to_broadcast([128, D]))
    k.dma("sp", bB, bB[:, :], ln4, ln4.t[1:2, :].to_broadcast([128, D]))
    NB3 = 3
    r1 = [k.sb("r1", [128, D], F32) for _ in range(NB3)]
    r2 = [k.sb("r2", [128, D], F32) for _ in range(NB3)]
    xa = [k.sb("xa", [128, D], F32) for _ in range(NB3)]
    zt = [k.sb("zt", [128, D], F32) for _ in range(NB3)]
    st6 = [k.sb("st6", [128, 2, 6], F32) for _ in range(NB3)]
    mv = [k.sb("mv", [128, 2], F32) for _ in range(NB3)]
    sd = [k.sb("sd", [128, 1], F32) for _ in range(NB3)]
    rs_l = [k.sb("rs", [128, 1], F32) for _ in range(NB3)]
    nm_l = [k.sb("nm", [128, 1], F32) for _ in range(NB3)]
    for t in range(ntile):
        i3 = t % NB3
        a_, b_, x_, z_ = r1[i3], r2[i3], xa[i3], zt[i3]
        s6, mv_, sd_, rs_, nm_ = st6[i3], mv[i3], sd[i3], rs_l[i3], nm_l[i3]
        k.ses = SES
        k.idma(a_, a_[:, :], ye, ye.t[:, :], IDX[0], IDX[0][:, t:t + 1], False, NEXP * cap - 1)
        k.idma(b_, b_[:, :], ye, ye.t[:, :], IDX[1], IDX[1][:, t:t + 1], False, NEXP * cap - 1)
        k.dma("sp", x_, x_[:, :], x3tm, x3tm.t[t * 128:(t + 1) * 128, :])
        k.stt("dve", z_, z_[:, :], a_, a_[:, :], G[0][:, t:t + 1], x_, x_[:, :], ALU.mult, ALU.add, extra_reads=[G[0]])
        k.stt("dve", z_, z_[:, :], b_, b_[:, :], G[1][:, t:t + 1], z_, z_[:, :], ALU.mult, ALU.add, extra_reads=[G[1]])
        k.ses = True
        for hf in range(2):
            k.op("dve", lambda g, o_=s6[:, hf, :], i_=z_[:, hf * 512:(hf + 1) * 512]: g.bn_stats(out=o_, in_=i_), [z_], [s6])
        k.op("dve", lambda g, o_=mv_[:, :], i_=s6[:, :, :].rearrange("p a b -> p (a b)"): g.bn_aggr(out=o_, in_=i_), [s6], [mv_])
        k.act(sd_, sd_[:, :], mv_, mv_[:, 1:2], AF.Sqrt, bias=LN_EPS)
        k.op("dve", lambda g, o_=rs_[:, :], i_=sd_[:, :]: g.reciprocal(out=o_, in_=i_), [sd_], [rs_])
        k.stt("dve", nm_, nm_[:, :], mv_, mv_[:, 0:1], -1.0, rs_, rs_[:, :], ALU.mult, ALU.mult)
        k.op("act", lambda g, a=z_[:, :], sc=rs_[:, 0:1], bi=nm_[:, 0:1]: g.activation(
            out=a, in_=a, func=AF.Identity, scale=sc, bias=bi), [z_, rs_, nm_], [z_])
        k.ses = SES
        k.tt("dve", z_, z_[:, :], z_, z_[:, :], gB, gB[:, :], ALU.mult)
        k.tt("pool", z_, z_[:, :], z_, z_[:, :], bB, bB[:, :], ALU.add)
        k.dma("sp", o, o.t[t * 128:(t + 1) * 128, :], z_, z_[:, :])
    k.pop_scope()
    return k.finish()
```

```python
import math
import numpy as np
import concourse.bass as bass
import concourse.mybir as mybir
from concourse.bass_utils import run_bass_kernel_spmd
from contextlib import ExitStack

F32 = mybir.dt.float32
BF16 = mybir.dt.bfloat16
I32 = mybir.dt.int32
ALU = mybir.AluOpType
AF = mybir.ActivationFunctionType
AX = mybir.AxisListType

NCORES = 8
D = 1024
SEQ = 16384
BATCH = 2
NTOK = BATCH * SEQ
TPC = NTOK // NCORES
DEPTH = 2
ALPHA = (2 * DEPTH) ** 0.25
LN_EPS = 1e-5
PI = math.pi


class Buf:
    __slots__ = ("name", "t", "lw", "rd", "dsem", "dcnt", "nodep", "lwd", "is_dram")

    def __init__(self, name, t):
        self.name = name
        self.t = t
        self.lw = None
        self.rd = {}
        self.dsem = None
        self.dcnt = 0
        self.nodep = False
        self.lwd = {}
        self.is_dram = False

    def __getitem__(self, idx):
        return self.t[idx]


SES = False


class KB:
    def __init__(self, same_engine_sync=None):
        if same_engine_sync is None:
            same_engine_sync = SES
        self.nc = bass.Bass("TRN2", target_bir_lowering=False)
        self.es = ExitStack()
        nc = self.nc
        self.eng = {"pe": nc.tensor, "dve": nc.vector, "act": nc.scalar, "pool": nc.gpsimd, "sp": nc.sync}
        self.sem = {e: self.es.enter_context(nc.semaphore("s_" + e)) for e in self.eng}
        self.cnt = {e: 0 for e in self.eng}
        self.known = {e: {} for e in self.eng}
        self.ses = same_engine_sync
        self.nsem = 0
        self.prog = {e: [] for e in self.eng}
        self.outs = []
        self.uid = 0
        self.scopes = []
        self.dbufs = []

    def push_scope(self):
        self.scopes.append(ExitStack())

    def pop_scope(self):
        self.barrier()
        self.scopes.pop().close()

    def _stk(self):
        return self.scopes[-1] if self.scopes else self.es

    def barrier(self):
        tags = [(self.sem[e], self.cnt[e]) for e in self.eng if self.cnt[e]]
        tags += [(b.dsem, b.dcnt) for b in self.dbufs if b.dcnt]
        for e in self.eng:
            kn = self.known[e]
            for s_, v in tags:
                if s_ == self.sem[e]:
                    continue
                if kn.get(s_, 0) >= v:
                    continue
                self.prog[e].append(("w", s_, v))
                kn[s_] = v

    def sb(self, name, shape, dt):
        self.uid += 1
        return Buf(name, self._stk().enter_context(self.nc.sbuf_tensor("%s_%d" % (name, self.uid), list(shape), dt)))

    def ps(self, name, shape=(128, 512), dt=F32):
        self.uid += 1
        return Buf(name, self._stk().enter_context(self.nc.psum_tensor("%s_%d" % (name, self.uid), list(shape), dt)))

    def dram(self, name, shape, dt, kind="Internal"):
        b = Buf(name, self.nc.dram_tensor(name, list(shape), dt, kind=kind).ap())
        b.is_dram = True
        if kind == "Internal":
            b.nodep = True
        if kind == "ExternalOutput":
            self.outs.append(b)
            b.nodep = True
        return b

    def _deps(self, e, reads, writes):
        need = {}

        def add(tag):
            if tag is None:
                return
            s, v = tag
            if need.get(s, 0) < v:
                need[s] = v

        for b in reads:
            add(b.lw)
            for s_, v_ in b.lwd.items():
                add((s_, v_))
        for b in writes:
            if b.nodep:
                continue
            add(b.lw)
            for s, v in b.rd.items():
                add((s, v))
        k = self.known[e]
        own = self.sem[e]
        for s, v in need.items():
            if s == own and (e == "pe" or not self.ses):
                continue
            if k.get(s, 0) >= v:
                continue
            self.prog[e].append(("w", s, v))
            k[s] = v

    def op(self, e, fn, reads=(), writes=()):
        self._deps(e, reads, writes)
        self.cnt[e] += 1
        self.prog[e].append(("i", fn, self.sem[e], 1))
        tag = (self.sem[e], self.cnt[e])
        for b in writes:
            b.lw = tag
            b.rd = {}
        for b in reads:
            if b not in writes:
                b.rd[tag[0]] = tag[1]

    def _dsem(self, b):
        if b.dsem is None:
            self.nsem += 1
            b.dsem = self.es.enter_context(self.nc.semaphore("d%d" % self.nsem))
            self.dbufs.append(b)
        return b.dsem

    def _dma_done(self, out_buf, in_buf, extra_reads=()):
        owner = in_buf if out_buf.is_dram else out_buf
        sem = self._dsem(owner)
        owner.dcnt += 16
        tag = (sem, owner.dcnt)
        if out_buf.is_dram:
            out_buf.lwd[sem] = owner.dcnt
        else:
            out_buf.lw = tag
            out_buf.rd = {}
        in_buf.rd[tag[0]] = tag[1]
        for b in extra_reads:
            b.rd[tag[0]] = tag[1]
        return sem

    def dma(self, q, out_buf, out_ap, in_buf, in_ap, **kw):
        self._deps(q, [in_buf], [out_buf])
        owner = in_buf if out_buf.is_dram else out_buf
        sem = self._dsem(owner)
        self.prog[q].append(("i", (lambda e, o=out_ap, i=in_ap, kw=kw: e.dma_start(out=o, in_=i, **kw)), sem, 16))
        self._dma_done(out_buf, in_buf)

    def idma(self, out_buf, out_ap, in_buf, in_ap, idx_buf, idx_ap, scatter, bound):
        self._deps("pool", [in_buf, idx_buf], [out_buf])
        owner = in_buf if out_buf.is_dram else out_buf
        sem = self._dsem(owner)
        off = bass.IndirectOffsetOnAxis(idx_ap, 0)
        if scatter:
            fn = lambda e: e.indirect_dma_start(out=out_ap, out_offset=off, in_=in_ap, in_offset=None)
        else:
            fn = lambda e: e.indirect_dma_start(out=out_ap, out_offset=None, in_=in_ap, in_offset=off)
        self.prog["pool"].append(("i", fn, sem, 16))
        self._dma_done(out_buf, in_buf, extra_reads=[idx_buf])

    def tr(self, out_b, out_ap, in_b, in_ap, id_b, id_ap):
        self.op("pe", lambda g: g.transpose(out_ap, in_ap, id_ap), [in_b, id_b], [out_b])

    def finish(self):
        for b in self.outs:
            for s_, v_ in b.lwd.items():
                self.prog["sp"].append(("w", s_, v_))
        for e in ("pe", "dve", "act", "pool"):
            if self.cnt[e]:
                self.prog["sp"].append(("w", self.sem[e], self.cnt[e]))
        prog = self.prog

        def replay(e):
            def body(eng):
                for it in prog[e]:
                    if it[0] == "w":
                        eng.wait_ge(it[1], it[2])
                    else:
                        it[1](eng).then_inc(it[2], it[3])
            return body

        with self.nc.Block() as block:
            block.sync(replay("sp"))
            block.tensor(replay("pe"))
            block.vector(replay("dve"))
            block.scalar(replay("act"))
            block.gpsimd(replay("pool"))
        self.es.close()
        return self.nc

    def tt(self, e, out_b, out_ap, a_b, a_ap, b_b, b_ap, op):
        self.op(e, lambda g: g.tensor_tensor(out=out_ap, in0=a_ap, in1=b_ap, op=op), [a_b, b_b], [out_b])

    def ts(self, e, out_b, out_ap, a_b, a_ap, s1, op0, s2=None, op1=None, extra_reads=()):
        if op1 is None:
            self.op(e, lambda g: g.tensor_scalar(out=out_ap, in0=a_ap, scalar1=s1, scalar2=None, op0=op0),
                    [a_b] + list(extra_reads), [out_b])
        else:
            self.op(e, lambda g: g.tensor_scalar(out=out_ap, in0=a_ap, scalar1=s1, scalar2=s2, op0=op0, op1=op1),
                    [a_b] + list(extra_reads), [out_b])

    def stt(self, e, out_b, out_ap, a_b, a_ap, scalar, b_b, b_ap, op0, op1, extra_reads=()):
        self.op(e, lambda g: g.scalar_tensor_tensor(out=out_ap, in0=a_ap, scalar=scalar, in1=b_ap, op0=op0, op1=op1),
                [a_b, b_b] + list(extra_reads), [out_b])

    def act(self, out_b, out_ap, in_b, in_ap, func, scale=1.0, bias=0.0, extra_reads=()):
        self.op("act", lambda g: g.activation(out=out_ap, in_=in_ap, func=func, scale=scale, bias=bias),
                [in_b] + list(extra_reads), [out_b])

    def mm(self, out_b, out_ap, l_b, l_ap, r_b, r_ap, start, stop):
        self.op("pe", lambda g: g.matmul(out_ap, l_ap, r_ap, start=start, stop=stop), [l_b, r_b], [out_b])


def build_uproj():
    k = KB()
    xT = k.dram("xT", [D, TPC], F32, "ExternalInput")
    w = k.dram("w", [D, D], F32, "ExternalInput")
    uT = k.dram("uT", [D, TPC], F32, "ExternalOutput")
    wsb = k.sb("w", [128, 8, D], BF16)
    k.dma("pool", wsb, wsb[:, :, :], w, w.t.rearrange("(kt p) m -> p kt m", p=128))
    xs = [k.sb("xs", [128, 8, 512], BF16) for _ in range(2)]
    pbs = [k.ps("pb") for _ in range(4)]
    sts = [k.sb("st", [128, 512], F32) for _ in range(4)]
    i = 0
    for nb in range(TPC // 512):
        xb = xs[nb % 2]
        k.dma("pool", xb, xb[:, :, :], xT, xT.t[:, nb * 512:(nb + 1) * 512].rearrange("(kt p) n -> p kt n", p=128))
        for m in range(8):
            pb = pbs[i % 4]
            st = sts[i % 4]
            for kt in range(8):
                k.mm(pb, pb[:, :], wsb, wsb[:, kt, m * 128:(m + 1) * 128], xb, xb[:, kt, :], kt == 0, kt == 7)
            if i % 2 == 0:
                k.act(st, st[:, :], pb, pb[:, :], AF.Copy)
            else:
                k.op("dve", lambda g, o=st[:, :], a=pb[:, :]: g.tensor_copy(out=o, in_=a), [pb], [st])
            k.dma("sp", uT, uT.t[m * 128:(m + 1) * 128, nb * 512:(nb + 1) * 512], st, st[:, :])
            i += 1
    return k.finish()


S5_BLK = 512


def build_s5(ntok=NTOK, seq=SEQ):
    k = KB()
    u = k.dram("u", [128, ntok], F32, "ExternalInput")
    are = k.dram("are", [128, 4], F32, "ExternalInput")
    aim = k.dram("aim", [128, 4], F32, "ExternalInput")
    ldt = k.dram("ldt", [128, 4], F32, "ExternalInput")
    bre = k.dram("bre", [128, 4, 128], F32, "ExternalInput")
    bim = k.dram("bim", [128, 4, 128], F32, "ExternalInput")
    cre = k.dram("cre", [128, 4, 32], F32, "ExternalInput")
    cim = k.dram("cim", [128, 4, 32], F32, "ExternalInput")
    dsk = k.dram("dsk", [128, 1], F32, "ExternalInput")
    y = k.dram("y", [128, ntok], F32, "ExternalOutput")
    L = S5_BLK

    def small(name, w=4, dt=F32):
        return k.sb(name, [128, w], dt)

    s_are, s_aim, s_ldt = small("are"), small("aim"), small("ldt")
    k.dma("sp", s_are, s_are[:, :], are, are.t[:, :])
    k.dma("sp", s_aim, s_aim[:, :], aim, aim.t[:, :])
    k.dma("sp", s_ldt, s_ldt[:, :], ldt, ldt.t[:, :])
    s_d = small("dsk", 1)
    k.dma("sp", s_d, s_d[:, :], dsk, dsk.t[:, :])
    b_re = k.sb("bre", [128, 4, 128], BF16)
    b_im = k.sb("bim", [128, 4, 128], BF16)
    k.dma("pool", b_re, b_re[:, :, :], bre, bre.t[:, :, :])
    k.dma("pool", b_im, b_im[:, :, :], bim, bim.t[:, :, :])
    c_re = k.sb("cre", [128, 4, 32], F32)
    c_im = k.sb("cim", [128, 4, 32], F32)
    k.dma("sp", c_re, c_re[:, :, :], cre, cre.t[:, :, :])
    k.dma("sp", c_im, c_im[:, :, :], cim, cim.t[:, :, :])

    k.ses = True
    dt_ = small("dt")
    k.act(dt_, dt_[:, :], s_ldt, s_ldt[:, :], AF.Exp)
    lre = small("lre")
    k.ts("dve", lre, lre[:, :], s_are, s_are[:, :], -1e-4, ALU.min)
    lrd = small("lrd")
    k.tt("dve", lrd, lrd[:, :], lre, lre[:, :], dt_, dt_[:, :], ALU.mult)
    rho = small("rho")
    k.act(rho, rho[:, :], lrd, lrd[:, :], AF.Exp)
    th = small("th")
    k.tt("dve", th, th[:, :], s_aim, s_aim[:, :], dt_, dt_[:, :], ALU.mult)

    def sin_of(src, shift, name):
        a = small(name + "a")
        k.ts("dve", a, a[:, :], src, src[:, :], float(shift), ALU.add)
        t = small(name + "t")
        k.ts("dve", t, t[:, :], a, a[:, :], 1.0 / (2 * PI), ALU.mult)
        ti = small(name + "ti", dt=I32)
        k.op("dve", lambda g: g.tensor_copy(out=ti[:, :], in_=t[:, :]), [t], [ti])
        tf = small(name + "tf")
        k.op("dve", lambda g: g.tensor_copy(out=tf[:, :], in_=ti[:, :]), [ti], [tf])
        r = small(name + "r")
        k.stt("dve", r, r[:, :], tf, tf[:, :], -2 * PI, a, a[:, :], ALU.mult, ALU.add)
        m1 = small(name + "m1")
        k.ts("dve", m1, m1[:, :], r, r[:, :], PI, ALU.is_gt)
        r2 = small(name + "r2")
        k.stt("dve", r2, r2[:, :], m1, m1[:, :], -2 * PI, r, r[:, :], ALU.mult, ALU.add)
        m2 = small(name + "m2")
        k.ts("dve", m2, m2[:, :], r2, r2[:, :], -PI, ALU.is_lt)
        r3 = small(name + "r3")
        k.stt("dve", r3, r3[:, :], m2, m2[:, :], 2 * PI, r2, r2[:, :], ALU.mult, ALU.add)
        r4 = small(name + "r4")
        k.ts("dve", r4, r4[:, :], r3, r3[:, :], PI, ALU.min, -PI, ALU.max)
        o = small(name + "o")
        k.act(o, o[:, :], r4, r4[:, :], AF.Sin)
        return o

    sn = sin_of(th, 0.0, "sn")
    cs = sin_of(th, PI / 2, "cs")
    abr, abi = small("abr"), small("abi")
    k.tt("dve", abr, abr[:, :], rho, rho[:, :], cs, cs[:, :], ALU.mult)
    k.tt("dve", abi, abi[:, :], rho, rho[:, :], sn, sn[:, :], ALU.mult)
    den, t0 = small("den"), small("t0")
    k.tt("dve", den, den[:, :], lre, lre[:, :], lre, lre[:, :], ALU.mult)
    k.tt("dve", t0, t0[:, :], s_aim, s_aim[:, :], s_aim, s_aim[:, :], ALU.mult)
    den2 = small("den2")
    k.tt("dve", den2, den2[:, :], den, den[:, :], t0, t0[:, :], ALU.add)
    rden = small("rden")
    k.op("dve", lambda g: g.reciprocal(out=rden[:, :], in_=den2[:, :]), [den2], [rden])
    nr = small("nr")
    k.ts("dve", nr, nr[:, :], abr, abr[:, :], -1.0, ALU.add)
    t1, t2, t3, t4 = small("t1"), small("t2"), small("t3"), small("t4")
    k.tt("dve", t1, t1[:, :], nr, nr[:, :], lre, lre[:, :], ALU.mult)
    k.tt("dve", t2, t2[:, :], abi, abi[:, :], s_aim, s_aim[:, :], ALU.mult)
    k.tt("dve", t3, t3[:, :], abi, abi[:, :], lre, lre[:, :], ALU.mult)
    k.tt("dve", t4, t4[:, :], nr, nr[:, :], s_aim, s_aim[:, :], ALU.mult)
    q1, q2 = small("q1"), small("q2")
    k.tt("dve", q1, q1[:, :], t1, t1[:, :], t2, t2[:, :], ALU.add)
    k.tt("dve", q2, q2[:, :], t3, t3[:, :], t4, t4[:, :], ALU.subtract)
    qre, qim = small("qre"), small("qim")
    k.tt("dve", qre, qre[:, :], q1, q1[:, :], rden, rden[:, :], ALU.mult)
    k.tt("dve", qim, qim[:, :], q2, q2[:, :], rden, rden[:, :], ALU.mult)
    nqim = small("nqim")
    k.ts("dve", nqim, nqim[:, :], qim, qim[:, :], -1.0, ALU.mult)
    nqre = small("nqre")
    k.ts("dve", nqre, nqre[:, :], qre, qre[:, :], -1.0, ALU.mult)

    cp_re = k.sb("cpre", [128, 4, 128], BF16)
    cp_imn = k.sb("cpimn", [128, 4, 128], BF16)
    k.op("pool", lambda g: g.memset(cp_re[:, :, :], 0.0), [], [cp_re])
    k.op("pool", lambda g: g.memset(cp_imn[:, :, :], 0.0), [], [cp_imn])
    ctmp = k.sb("ctmp", [128, 4, 32], F32)
    for qq in range(4):
        k.ts("dve", ctmp, ctmp[:, qq, :], c_im, c_im[:, qq, :], nqim[:, qq:qq + 1], ALU.mult, extra_reads=[nqim])
        k.stt("dve", cp_re, cp_re[:, qq, 32 * qq:32 * qq + 32], c_re, c_re[:, qq, :], qre[:, qq:qq + 1], ctmp, ctmp[:, qq, :],
              ALU.mult, ALU.add, extra_reads=[qre])
    ctmp2 = k.sb("ctmp2", [128, 4, 32], F32)
    for qq in range(4):
        k.ts("dve", ctmp2, ctmp2[:, qq, :], c_im, c_im[:, qq, :], nqre[:, qq:qq + 1], ALU.mult, extra_reads=[nqre])
        k.stt("dve", cp_imn, cp_imn[:, qq, 32 * qq:32 * qq + 32], c_re, c_re[:, qq, :], nqim[:, qq:qq + 1], ctmp2, ctmp2[:, qq, :],
              ALU.mult, ALU.add, extra_reads=[nqim])

    COS = k.sb("COS", [128, 4, L], F32)
    SIN = k.sb("SIN", [128, 4, L], F32)
    RHO = k.sb("RHO", [128, 4, L], F32)
    k.op("pool", lambda g: g.memset(COS[:, :, :], 1.0), [], [COS])
    k.op("pool", lambda g: g.memset(SIN[:, :, :], 0.0), [], [SIN])
    k.op("pool", lambda g: g.memset(RHO[:, :, :], 1.0), [], [RHO])
    for qq in range(4):
        k.ts("dve", RHO, RHO[:, qq, :], RHO, RHO[:, qq, :], rho[:, qq:qq + 1], ALU.mult, extra_reads=[rho])
    pr, pi_ = cs, sn
    ttmp = k.sb("ttmp", [128, 4, L // 2], F32)
    w = 1
    lvl = 0
    while w < L:
        npi = small("npi%d" % lvl)
        k.ts("dve", npi, npi[:, :], pi_, pi_[:, :], -1.0, ALU.mult)
        for qq in range(4):
            k.ts("dve", ttmp, ttmp[:, qq, 0:w], SIN, SIN[:, qq, 0:w], npi[:, qq:qq + 1], ALU.mult, extra_reads=[npi])
            k.stt("dve", COS, COS[:, qq, w:2 * w], COS, COS[:, qq, 0:w], pr[:, qq:qq + 1], ttmp, ttmp[:, qq, 0:w],
                  ALU.mult, ALU.add, extra_reads=[pr])
            k.ts("dve", ttmp, ttmp[:, qq, 0:w], SIN, SIN[:, qq, 0:w], pr[:, qq:qq + 1], ALU.mult, extra_reads=[pr])
            k.stt("dve", SIN, SIN[:, qq, w:2 * w], COS, COS[:, qq, 0:w], pi_[:, qq:qq + 1], ttmp, ttmp[:, qq, 0:w],
                  ALU.mult, ALU.add, extra_reads=[pi_])
        a2, b2, ab_ = small("a2_%d" % lvl), small("b2_%d" % lvl), small("ab_%d" % lvl)
        k.tt("dve", a2, a2[:, :], pr, pr[:, :], pr, pr[:, :], ALU.mult)
        k.tt("dve", b2, b2[:, :], pi_, pi_[:, :], pi_, pi_[:, :], ALU.mult)
        k.tt("dve", ab_, ab_[:, :], pr, pr[:, :], pi_, pi_[:, :], ALU.mult)
        npr, npi2 = small("npr%d" % lvl), small("npi2%d" % lvl)
        k.tt("dve", npr, npr[:, :], a2, a2[:, :], b2, b2[:, :], ALU.subtract)
        k.ts("dve", npi2, npi2[:, :], ab_, ab_[:, :], 2.0, ALU.mult)
        pr, pi_ = npr, npi2
        w *= 2
        lvl += 1
    ELr, ELi = pr, pi_
    nELi = small("nELi")
    k.ts("dve", nELi, nELi[:, :], ELi, ELi[:, :], -1.0, ALU.mult)

    k.ses = SES
    NP = 3
    ub = [k.sb("ub", [128, L], F32) for _ in range(2)]
    ubf = [k.sb("ubf", [128, L], BF16) for _ in range(2)]
    ps_re = [k.ps("psre") for _ in range(NP)]
    ps_im = [k.ps("psim") for _ in range(NP)]
    ps_y = [k.ps("psy") for _ in range(2)]
    T = [[k.sb("T%d" % j, [128, L], F32) for j in range(4)] for _ in range(NP)]
    gin = [[k.sb("gin%d" % j, [128, L], F32) for j in range(2)] for _ in range(NP)]
    gg = [[[k.sb("g%d_%d" % (qq, j), [128, L], F32) for j in range(2)] for qq in range(4)] for _ in range(2)]
    T2 = [[k.sb("U%d" % j, [128, L], F32) for j in range(4)] for _ in range(NP)]
    hh = [[k.sb("h%d" % j, [128, L], BF16) for j in range(2)] for _ in range(NP)]
    ini = [[k.sb("ini%d_%d" % (qq, j), [128, 1], F32) for j in range(2)] for qq in range(4)]
    itmp = [k.sb("itmp%d" % qq, [128, 1], F32) for qq in range(4)]
    yo = [k.sb("yo", [128, L], F32) for _ in range(2)]
    nblk = ntok // L
    bps = seq // L
    it = 0
    for blk in range(nblk):
        sl = slice(blk * L, (blk + 1) * L)
        uB, uF, pY = ub[blk % 2], ubf[blk % 2], ps_y[blk % 2]
        k.dma("sp", uB, uB[:, :], u, u.t[:, sl])
        k.act(uF, uF[:, :], uB, uB[:, :], AF.Copy)
        first = (blk % bps == 0)
        for qq in range(4):
            par = it % NP
            it += 1
            pr_, pi2_ = ps_re[par], ps_im[par]
            k.mm(pr_, pr_[:, :], b_re, b_re[:, qq, :], uF, uF[:, :], True, True)
            k.mm(pi2_, pi2_[:, :], b_im, b_im[:, qq, :], uF, uF[:, :], True, True)
            t = T[par]
            cq, sq, rq = COS[:, qq, :], SIN[:, qq, :], RHO[:, qq, :]
            k.tt("dve", t[0], t[0][:, :], pr_, pr_[:, :], COS, cq, ALU.mult)
            k.tt("dve", t[1], t[1][:, :], pi2_, pi2_[:, :], SIN, sq, ALU.mult)
            k.tt("dve", t[2], t[2][:, :], pi2_, pi2_[:, :], COS, cq, ALU.mult)
            k.tt("dve", t[3], t[3][:, :], pr_, pr_[:, :], SIN, sq, ALU.mult)
            gi = gin[par]
            k.tt("pool", gi[0], gi[0][:, :], t[0], t[0][:, :], t[1], t[1][:, :], ALU.add)
            k.tt("dve", gi[1], gi[1][:, :], t[2], t[2][:, :], t[3], t[3][:, :], ALU.subtract)
            g_prev = gg[(blk + 1) % 2][qq]
            g_cur = gg[blk % 2][qq]
            if first:
                i_re, i_im = 0.0, 0.0
                xr = []
            else:
                k.ts("pool", itmp[qq], itmp[qq][:, :], g_prev[1], g_prev[1][:, L - 1:L], nELi[:, qq:qq + 1], ALU.mult,
                     extra_reads=[nELi])
                k.stt("dve", ini[qq][0], ini[qq][0][:, :], g_prev[0], g_prev[0][:, L - 1:L], ELr[:, qq:qq + 1],
                      itmp[qq], itmp[qq][:, :], ALU.mult, ALU.add, extra_reads=[ELr])
                k.ts("pool", itmp[qq], itmp[qq][:, :], g_prev[1], g_prev[1][:, L - 1:L], ELr[:, qq:qq + 1], ALU.mult,
                     extra_reads=[ELr])
                k.stt("dve", ini[qq][1], ini[qq][1][:, :], g_prev[0], g_prev[0][:, L - 1:L], ELi[:, qq:qq + 1],
                      itmp[qq], itmp[qq][:, :], ALU.mult, ALU.add, extra_reads=[ELi])
                i_re, i_im = ini[qq][0][:, :], ini[qq][1][:, :]
                xr = [ini[qq][0], ini[qq][1]]
            k.op("dve", lambda g, o=g_cur[0][:, :], a=rq, b=gi[0][:, :], i0=i_re: g.tensor_tensor_scan(
                out=o, data0=a, data1=b, initial=i0, op0=ALU.mult, op1=ALU.add), [RHO, gi[0]] + xr[:1], [g_cur[0]])
            k.op("dve", lambda g, o=g_cur[1][:, :], a=rq, b=gi[1][:, :], i0=i_im: g.tensor_tensor_scan(
                out=o, data0=a, data1=b, initial=i0, op0=ALU.mult, op1=ALU.add), [RHO, gi[1]] + xr[1:], [g_cur[1]])
            t2_ = T2[par]
            k.tt("pool", t2_[0], t2_[0][:, :], g_cur[0], g_cur[0][:, :], COS, cq, ALU.mult)
            k.tt("dve", t2_[1], t2_[1][:, :], g_cur[1], g_cur[1][:, :], SIN, sq, ALU.mult)
            k.tt("pool", t2_[2], t2_[2][:, :], g_cur[1], g_cur[1][:, :], COS, cq, ALU.mult)
            k.tt("dve", t2_[3], t2_[3][:, :], g_cur[0], g_cur[0][:, :], SIN, sq, ALU.mult)
            h = hh[par]
            k.tt("dve", h[0], h[0][:, :], t2_[0], t2_[0][:, :], t2_[1], t2_[1][:, :], ALU.subtract)
            k.tt("dve", h[1], h[1][:, :], t2_[2], t2_[2][:, :], t2_[3], t2_[3][:, :], ALU.add)
            k.mm(pY, pY[:, :], cp_re, cp_re[:, qq, :], h[0], h[0][:, :], qq == 0, False)
            k.mm(pY, pY[:, :], cp_imn, cp_imn[:, qq, :], h[1], h[1][:, :], False, qq == 3)
        yB = yo[blk % 2]
        k.stt("dve", yB, yB[:, :], uB, uB[:, :], s_d[:, 0:1], pY, pY[:, :], ALU.mult, ALU.add, extra_reads=[s_d])
        k.dma("sp", y, y.t[:, sl], yB, yB[:, :])
    return k.finish()


def s5_core_params(c, a_re, a_im, log_dt, b_re, b_im, c_re, c_im, d):
    g0 = 8 * c

    def pq(a):
        return np.ascontiguousarray(a[g0:g0 + 8].reshape(4, 2, 64).transpose(1, 2, 0).reshape(128, 4)).astype(np.float32)

    are = pq(a_re)
    aim = pq(a_im)
    ldt = pq(np.broadcast_to(log_dt[:, None], (64, 64)))
    B_re = np.zeros((4, 2, 16, 4, 2, 64), np.float32)
    B_im = np.zeros((4, 2, 16, 4, 2, 64), np.float32)
    C_re = np.zeros((2, 64, 4, 2, 16), np.float32)
    C_im = np.zeros((2, 64, 4, 2, 16), np.float32)
    for qq in range(4):
        for gp in range(2):
            g = g0 + 2 * qq + gp
            B_re[qq, gp, :, qq, gp, :] = b_re[g].T
            B_im[qq, gp, :, qq, gp, :] = b_im[g].T
            C_re[gp, :, qq, gp, :] = c_re[g].T
            C_im[gp, :, qq, gp, :] = c_im[g].T
    return {"are": are, "aim": aim, "ldt": ldt,
            "bre": B_re.reshape(128, 4, 128), "bim": B_im.reshape(128, 4, 128),
            "cre": C_re.reshape(128, 4, 32), "cim": C_im.reshape(128, 4, 32),
            "dsk": np.ascontiguousarray(d[128 * c:128 * c + 128].reshape(128, 1)).astype(np.float32)}


class Ctx:
    def __init__(self, k, NT, nkt_half):
        self.k = k
        self.NT = NT
        self.ps1 = [k.ps("ps1") for _ in range(2)]
        self.ps3 = [k.ps("ps3") for _ in range(2)]
        self.pso = [k.ps("pso") for _ in range(2)]
        self.pss = [k.ps("pss") for _ in range(2)]
        self.wa = [k.sb("wa", [128, 8, 512], BF16) for _ in range(2)]
        self.wb = [k.sb("wb", [128, 8, 512], BF16) for _ in range(2)]
        self.w2h = k.sb("w2h", [128, nkt_half, 1024], BF16)
        self.hmid = k.sb("hmid", [128, max(nkt_half, 8), NT], BF16)
        self.z = k.sb("z", [128, 8, NT], F32)
        self.xbf = k.sb("xbf", [128, 8, NT], BF16)
        self.sact = [k.sb("sact", [128, 512], F32) for _ in range(2)]
        self.tmp = [k.sb("tmp", [128, 512], F32) for _ in range(2)]
        self.ones = k.sb("ones", [128, 128], F32)
        k.op("pool", lambda g: g.memset(self.ones[:, :], 1.0), [], [self.ones])
        self.sq = [k.sb("sq", [128, 512], F32) for _ in range(2)]
        self.mean = k.sb("mean", [128, 512], F32)
        self.ex2 = k.sb("ex2", [128, 512], F32)
        self.rstd = k.sb("rstd", [128, 512], F32)
        self.nmr = k.sb("nmr", [128, 512], F32)
        self.lng = k.sb("lng", [128, 32], F32)
        self.lnb = k.sb("lnb", [128, 32], F32)
        self.wi = 0
        self.pi = 0
        self.oi = 0


def load_wblock(k, buf, wd, k0, nk, c0, cw):
    k.dma("pool", buf, buf[:, 0:nk, 0:cw], wd, wd.t[k0 * 128:(k0 + nk) * 128, c0:c0 + cw].rearrange("(kt p) m -> p kt m", p=128))


def glu_phase(k, cx, x_in, wa_d, a_c0, wb_d, b_c0, ncols, func, emit, hook=None, NT=None):
    NT = cx.NT if NT is None else NT
    for c0 in range(0, ncols, 512):
        cw = min(512, ncols - c0)
        wa, wb = cx.wa[cx.wi % 2], cx.wb[cx.wi % 2]
        cx.wi += 1
        load_wblock(k, wa, wa_d, 0, 8, a_c0 + c0, cw)
        load_wblock(k, wb, wb_d, 0, 8, b_c0 + c0, cw)
        if hook is not None and c0 == 512:
            hook()
        for fl in range(cw // 128):
            ft = c0 // 128 + fl
            for t0 in range(0, NT, 512):
                tw = min(512, NT - t0)
                p1, p3 = cx.ps1[cx.pi % 2], cx.ps3[cx.pi % 2]
                sa = cx.sact[cx.pi % 2]
                cx.pi += 1
                ts_ = slice(t0, t0 + tw)
                for kt in range(8):
                    k.mm(p1, p1[:, 0:tw], wa, wa[:, kt, fl * 128:fl * 128 + 128], x_in, x_in[:, kt, ts_], kt == 0, kt == 7)
                for kt in range(8):
                    k.mm(p3, p3[:, 0:tw], wb, wb[:, kt, fl * 128:fl * 128 + 128], x_in, x_in[:, kt, ts_], kt == 0, kt == 7)
                k.act(sa, sa[:, 0:tw], p1, p1[:, 0:tw], func)
                emit(ft, t0, tw, sa, p3)


def down_load(k, cx, w2_d, k0, nk):
    k.dma("pool", cx.w2h, cx.w2h[:, 0:nk, :], w2_d, w2_d.t[k0 * 128:(k0 + nk) * 128, :].rearrange("(kt p) m -> p kt m", p=128))


def down_phase(k, cx, w2_d, k0, nk, zacc):
    NT = cx.NT
    for m in range(8):
        for tb in range(NT // 512):
            po = cx.pso[cx.oi % 2]
            cx.oi += 1
            ts_ = slice(tb * 512, tb * 512 + 512)
            for kt in range(nk):
                k.mm(po, po[:, :], cx.w2h, cx.w2h[:, kt, m * 128:m * 128 + 128], cx.hmid, cx.hmid[:, kt, ts_], kt == 0, kt == nk - 1)
            k.tt("dve", zacc, zacc[:, m, ts_], po, po[:, :], zacc, zacc[:, m, ts_], ALU.add)


def layer_norm(k, cx, z, ln_idx, xbf_out):
    NT = cx.NT
    for tb in range(NT // 512):
        ts_ = slice(tb * 512, tb * 512 + 512)
        s1, s2 = cx.pss[0], cx.pss[1]
        if not hasattr(cx, "ones_b"):
            cx.ones_b = k.sb("ones_b", [128, 128], BF16)
            k.op("dve", lambda g, a=cx.ones_b[:, :]: g.memset(a, 1.0), [], [cx.ones_b])
            cx.zb = [k.sb("zb", [128, 512], BF16) for _ in range(2)]
            cx.sqb = [k.sb("sqb", [128, 512], BF16) for _ in range(2)]
        for kt in range(8):
            zb, sq = cx.zb[kt % 2], cx.sqb[kt % 2]
            k.act(zb, zb[:, :], z, z[:, kt, ts_], AF.Copy)
            k.act(sq, sq[:, :], z, z[:, kt, ts_], AF.Square)
            k.mm(s1, s1[:, :], cx.ones_b, cx.ones_b[:, :], zb, zb[:, :], kt == 0, kt == 7)
            k.mm(s2, s2[:, :], cx.ones_b, cx.ones_b[:, :], sq, sq[:, :], kt == 0, kt == 7)
        k.act(cx.mean, cx.mean[:, :], s1, s1[:, :], AF.Copy, scale=1.0 / D)
        k.act(cx.ex2, cx.ex2[:, :], s2, s2[:, :], AF.Copy, scale=1.0 / D)
        k.tt("dve", cx.nmr, cx.nmr[:, :], cx.mean, cx.mean[:, :], cx.mean, cx.mean[:, :], ALU.mult)
        k.tt("dve", cx.ex2, cx.ex2[:, :], cx.ex2, cx.ex2[:, :], cx.nmr, cx.nmr[:, :], ALU.subtract)
        k.ts("dve", cx.ex2, cx.ex2[:, :], cx.ex2, cx.ex2[:, :], LN_EPS, ALU.add)
        k.act(cx.ex2, cx.ex2[:, :], cx.ex2, cx.ex2[:, :], AF.Sqrt)
        k.op("dve", lambda g, o_=cx.rstd[:, :], i_=cx.ex2[:, :]: g.reciprocal(out=o_, in_=i_), [cx.ex2], [cx.rstd])
        k.stt("dve", cx.nmr, cx.nmr[:, :], cx.mean, cx.mean[:, :], -1.0, cx.rstd, cx.rstd[:, :], ALU.mult, ALU.mult)
        for kt in range(8):
            t = cx.tmp[kt % 2]
            k.tt("dve", t, t[:, :], z, z[:, kt, ts_], cx.rstd, cx.rstd[:, :], ALU.mult)
            k.tt("dve", t, t[:, :], t, t[:, :], cx.nmr, cx.nmr[:, :], ALU.add)
            col = ln_idx * 8 + kt
            k.op("act", lambda g, o=z[:, kt, ts_], i=t[:, :], sc=cx.lng[:, col:col + 1], bi=cx.lnb[:, col:col + 1]: g.activation(
                out=o, in_=i, func=AF.Identity, scale=sc, bias=bi),
                [t, cx.lng, cx.lnb], [z])
            if xbf_out is not None:
                k.act(xbf_out, xbf_out[:, kt, ts_], z, z[:, kt, ts_], AF.Copy)


def ln_layout(ln_g, ln_b):
    g = np.ascontiguousarray(ln_g.reshape(4, 8, 128).transpose(2, 0, 1).reshape(128, 32)).astype(np.float32)
    b = np.ascontiguousarray(ln_b.reshape(4, 8, 128).transpose(2, 0, 1).reshape(128, 32)).astype(np.float32)
    return g, b


DFF = 2816


def build_layer0_rest(tpc=TPC, NT=1024):
    k = KB()
    yT = k.dram("yT", [D, tpc], F32, "ExternalInput")
    xT = k.dram("xT", [D, tpc], F32, "ExternalInput")
    wglu = k.dram("wglu", [D, 2 * D], F32, "ExternalInput")
    w1 = k.dram("w1", [D, DFF], F32, "ExternalInput")
    w3 = k.dram("w3", [D, DFF], F32, "ExternalInput")
    w2 = k.dram("w2", [DFF, D], F32, "ExternalInput")
    lng = k.dram("lng", [128, 32], F32, "ExternalInput")
    lnb = k.dram("lnb", [128, 32], F32, "ExternalInput")
    oT = k.dram("oT", [D, tpc], F32, "ExternalOutput")
    cx = Ctx(k, NT, 11)
    k.dma("sp", cx.lng, cx.lng[:, :], lng, lng.t[:, :])
    k.dma("sp", cx.lnb, cx.lnb[:, :], lnb, lnb.t[:, :])
    ystage = [k.sb("ystage", [128, NT], F32) for _ in range(2)]
    gbf = cx.hmid
    z = cx.z
    for sb_ in range(tpc // NT):
        tsl = slice(sb_ * NT, (sb_ + 1) * NT)
        for kt in range(8):
            ys = ystage[kt % 2]
            k.dma("sp", ys, ys[:, :], yT, yT.t[kt * 128:(kt + 1) * 128, tsl])
            k.act(gbf, gbf[:, kt, :], ys, ys[:, :], AF.Gelu)
            k.dma("sp", z, z[:, kt, :], xT, xT.t[kt * 128:(kt + 1) * 128, tsl])

        def emit_glu(ft, t0, tw, sa, pB):
            t = cx.tmp[(ft + t0 // 512) % 2]
            ts_ = slice(t0, t0 + 512)
            k.tt("dve", t, t[:, :], pB, pB[:, :], sa, sa[:, :], ALU.mult)
            k.stt("dve", z, z[:, ft, ts_], z, z[:, ft, ts_], float(ALPHA), t, t[:, :], ALU.mult, ALU.add)

        glu_phase(k, cx, gbf, wglu, D, wglu, 0, D, AF.Sigmoid, emit_glu)
        layer_norm(k, cx, z, 0, cx.xbf)
        for kt in range(8):
            k.ts("dve", z, z[:, kt, :], z, z[:, kt, :], float(ALPHA), ALU.mult)
        for half in range(2):
            f0 = half * 11

            def emit_ffn(ft, t0, tw, sa, pB):
                ts_ = slice(t0, t0 + 512)
                k.tt("dve", cx.hmid, cx.hmid[:, ft, ts_], pB, pB[:, :], sa, sa[:, :], ALU.mult)

            glu_phase(k, cx, cx.xbf, w1, f0 * 128, w3, f0 * 128, 11 * 128, AF.Silu, emit_ffn,
                      hook=lambda f0=f0: down_load(k, cx, w2, f0, 11))
            down_phase(k, cx, w2, f0, 11, z)
        layer_norm(k, cx, z, 1, None)
        for kt in range(8):
            k.dma("sp", oT, oT.t[kt * 128:(kt + 1) * 128, tsl], z, z[:, kt, :])
    return k.finish()


NQKV = 9216
DILS = (1, 4, 16)


def build_qkv(tpc=TPC):
    k = KB()
    xT = k.dram("xT", [D, tpc], F32, "ExternalInput")
    w = k.dram("w", [D, NQKV], F32, "ExternalInput")
    o = k.dram("qkvT", [NQKV, tpc], BF16, "ExternalOutput")
    xb = k.sb("xb", [128, 8, tpc], BF16)
    for kt in range(8):
        k.dma("pool", xb, xb[:, kt, :], xT, xT.t[kt * 128:(kt + 1) * 128, :])
    wbuf = [k.sb("wq", [128, 8, 512], BF16) for _ in range(2)]
    pbs = [k.ps("pb") for _ in range(4)]
    sts = [k.sb("st", [128, 512], BF16) for _ in range(4)]
    i = 0
    for cb in range(NQKV // 512):
        wb = wbuf[cb % 2]
        load_wblock(k, wb, w, 0, 8, cb * 512, 512)
        for fl in range(4):
            for tb in range(tpc // 512):
                pb, st = pbs[i % 4], sts[i % 4]
                ts_ = slice(tb * 512, tb * 512 + 512)
                for kt in range(8):
                    k.mm(pb, pb[:, :], wb, wb[:, kt, fl * 128:fl * 128 + 128], xb, xb[:, kt, ts_], kt == 0, kt == 7)
                if i % 2 == 0:
                    k.act(st, st[:, :], pb, pb[:, :], AF.Copy)
                else:
                    k.op("dve", lambda g, o_=st[:, :], a=pb[:, :]: g.tensor_copy(out=o_, in_=a), [pb], [st])
                r0 = cb * 512 + fl * 128
                k.dma("sp", o, o.t[r0:r0 + 128, ts_], st, st[:, :])
                i += 1
    return k.finish()


ACH = 2048
NEG = -1e30


def build_attn(tpc=TPC, LA=3):
    k = KB()
    nblk = tpc // 128
    qb_d = k.dram("qb", [3, 8, 128, nblk * 128], BF16, "ExternalInput")
    kb_d = k.dram("kb", [3, 8, 128, (nblk + 16) * 128], BF16, "ExternalInput")
    vb_d = k.dram("vb", [3, 8, 128, (nblk + 16) * 128], BF16, "ExternalInput")
    bias_d = k.dram("biasT", [3, 8, 128, 512], F32, "ExternalInput")
    negb_d = k.dram("negb", [128, 1], F32, "ExternalInput")
    o = k.dram("attnT", [D, tpc], F32, "ExternalOutput")
    negb = k.sb("negb", [128, 1], F32)
    k.dma("sp", negb, negb[:, :], negb_d, negb_d.t[:, :])
    ones = k.sb("ones", [128, 64], BF16)
    k.op("pool", lambda g: g.memset(ones[:, :], 1.0), [], [ones])
    Qb = [k.sb("Qb", [128, 16 * 128], BF16) for _ in range(2)]
    Kb = [k.sb("Kb", [128, 32 * 128], BF16) for _ in range(2)]
    Vb = [k.sb("Vb", [128, 32, 128], BF16) for _ in range(2)]
    Bs = [k.sb("Bs", [128, 2, 256], F32) for _ in range(2)]
    num = [k.sb("num", [128, ACH], F32) for _ in range(2)]
    den = [k.sb("den", [128, ACH], F32) for _ in range(2)]
    rec = k.sb("rec", [128, ACH], F32)
    NS = LA + 1
    ps_s = [k.ps("ps_s", (128, 256)) for _ in range(NS)]
    ps_o = [k.ps("ps_o") for _ in range(2)]
    ps_d = [k.ps("ps_d") for _ in range(2)]
    tmp = [k.sb("tmpS", [128, 256], F32) for _ in range(NS + 1)]
    pT = [k.sb("pT", [128, 256], BF16) for _ in range(NS + 1)]
    units = []
    li = 0
    ei = 0
    ci = 0
    for ch in range(tpc // ACH):
        for ft in range(8):
            nm, dn = num[ci % 2], den[ci % 2]
            ci += 1
            for g in range(3):
                d = DILS[g]
                bufs = (Qb[li % 2], Kb[li % 2], Vb[li % 2], Bs[li % 2])
                li += 1
                for B in range(16):
                    for hh in range(2):
                        units.append(dict(ch=ch, ft=ft, g=g, d=d, B=B, hh=hh, bufs=bufs, nm=nm, dn=dn,
                                          po=ps_o[ei % 2], pd=ps_d[ei % 2], load=(B == 0 and hh == 0),
                                          evac=(B % 4 == 3 and hh == 1), last=(g == 2 and B == 15 and hh == 1)))
                    if B % 4 == 3:
                        ei += 1

    def s_part(ui, un):
        qb, kb, vb, bs = un["bufs"]
        ch, ft, g, d, B, hh = un["ch"], un["ft"], un["g"], un["d"], un["B"], un["hh"]
        if un["load"]:
            b0 = ch * 16
            k.dma("sp", qb, qb[:, :], qb_d, qb_d.t[g, ft, :, b0 * 128:(b0 + 16) * 128])
            nkb = 16 + d
            k0 = 16 + b0 - d
            k.dma("sp", kb, kb[:, 0:nkb * 128], kb_d, kb_d.t[g, ft, :, k0 * 128:(k0 + nkb) * 128])
            k.dma("sp", vb, vb[:, 0:nkb, :], vb_d,
                  vb_d.t[g, ft, :, k0 * 128:(k0 + nkb) * 128].rearrange("p (b f) -> p b f", f=128))
            k.dma("sp", bs, bs[:, :, :], bias_d, bias_d.t[g, ft, :, :].rearrange("p (h f) -> p h f", h=2))
        halo = (ch == 0 and B < d)
        rs = slice(64 * hh, 64 * hh + 64)
        pss = ps_s[ui % NS]
        tm, pt = tmp[ui % (NS + 1)], pT[ui % (NS + 1)]
        qs = slice(B * 128, B * 128 + 128)
        kp = slice(B * 128, B * 128 + 128)
        kc = slice((B + d) * 128, (B + d) * 128 + 128)
        k.mm(pss, pss[:, 0:128], kb, kb[rs, kp], qb, qb[rs, qs], True, True)
        k.mm(pss, pss[:, 128:256], kb, kb[rs, kc], qb, qb[rs, qs], True, True)
        k.stt("dve", tm, tm[:, :], pss, pss[:, 0:256], 0.125, bs, bs[:, hh, :], ALU.mult, ALU.add)
        if halo:
            k.op("act", lambda e, o_=pt[:, 0:128], i_=tm[:, 0:128]: e.activation(
                out=o_, in_=i_, func=AF.Exp, bias=negb[:, 0:1], scale=1.0), [tm, negb], [pt])
            k.act(pt, pt[:, 128:256], tm, tm[:, 128:256], AF.Exp)
        else:
            k.act(pt, pt[:, :], tm, tm[:, :], AF.Exp)

    def pv_part(ui, un):
        qb, kb, vb, bs = un["bufs"]
        ch, ft, g, d, B, hh = un["ch"], un["ft"], un["g"], un["d"], un["B"], un["hh"]
        po, pd, nm, dn = un["po"], un["pd"], un["nm"], un["dn"]
        rs = slice(64 * hh, 64 * hh + 64)
        pt = pT[ui % (NS + 1)]
        cs = slice((B % 4) * 128, (B % 4) * 128 + 128)
        k.mm(po, po[rs, cs], vb, vb[:, B, rs], pt, pt[:, 0:128], True, False)
        k.mm(po, po[rs, cs], vb, vb[:, B + d, rs], pt, pt[:, 128:256], False, True)
        k.mm(pd, pd[rs, cs], ones, ones[:, :], pt, pt[:, 0:128], True, False)
        k.mm(pd, pd[rs, cs], ones, ones[:, :], pt, pt[:, 128:256], False, True)
        if un["evac"]:
            B0 = B - 3
            if d == 1:
                ov = lambda t: t[:, B0 * 128:(B0 + 4) * 128]
                iv = lambda p: p[:, :]
            else:
                n_, r0 = B0 // d, B0 % d
                base = n_ * 128 * d
                ov = lambda t, base=base, d=d, r0=r0: t[:, base:base + 128 * d].rearrange(
                    "p (i r) -> p i r", r=d)[:, :, r0:r0 + 4]
                iv = lambda p: p[:, :].rearrange("p (j i) -> p i j", j=4)
            if g == 0:
                k.op("dve", lambda e, o_=ov(nm), i_=iv(po): e.tensor_copy(out=o_, in_=i_), [po], [nm])
                k.act(dn, ov(dn), pd, iv(pd), AF.Copy)
            else:
                k.tt("dve", nm, ov(nm), po, iv(po), nm, ov(nm), ALU.add)
                k.tt("pool", dn, ov(dn), pd, iv(pd), dn, ov(dn), ALU.add) if False else \
                    k.tt("dve", dn, ov(dn), pd, iv(pd), dn, ov(dn), ALU.add)
        if un["last"]:
            k.op("dve", lambda e, o_=rec[:, :], i_=dn[:, :]: e.reciprocal(out=o_, in_=i_), [dn], [rec])
            k.tt("pool", nm, nm[:, :], nm, nm[:, :], rec, rec[:, :], ALU.mult)
            k.dma("sp", o, o.t[ft * 128:(ft + 1) * 128, ch * ACH:(ch + 1) * ACH], nm, nm[:, :])

    n = len(units)
    for i in range(n + LA):
        if i < n:
            s_part(i, units[i])
        if i - LA >= 0:
            pv_part(i - LA, units[i - LA])
    return k.finish()


def t5_bucket(dist):
    n_buckets, max_distance = 32, 2048
    max_exact = n_buckets // 2
    d = np.maximum(dist, 0)
    large = max_exact + (np.log(np.maximum(d, 1) / max_exact) / math.log(max_distance / max_exact)
                         * (n_buckets - max_exact)).astype(np.int64)
    large = np.minimum(large, n_buckets - 1)
    return np.where(d < max_exact, d, large).astype(np.int32)


def attn_bias_table(rel_bias):
    L = 128
    qi = np.arange(L)[:, None]
    ki = np.arange(2 * L)[None, :]
    off = qi + L - ki
    band = (off >= 0) & (off <= L)
    out = np.zeros((3, 8, 128, 2, 2, 128), np.float32)
    for g, d in enumerate(DILS):
        bucket = t5_bucket(np.clip(off, 0, None) * d)
        bias = rel_bias[bucket]
        bias = np.where(band[:, :, None], bias, np.float32(NEG))
        b = bias.reshape(L, 2, L, 8, 2)
        out[g] = b.transpose(3, 2, 4, 1, 0)
    return out.reshape(3, 8, 128, 512)


def attn_host_layout(qkvT, rel_bias, seqlen, tpc):
    ntok = qkvT.shape[1]
    ncore = ntok // tpc
    nblk = tpc // 128
    bias = attn_bias_table(rel_bias)
    perm = {}
    for g, d in enumerate(DILS):
        for j in range(3):
            a = qkvT[(g * 3 + j) * 1024:(g * 3 + j + 1) * 1024]
            a = a.reshape(8, 128, ntok // (128 * d), 128, d).transpose(0, 1, 2, 4, 3)
            perm[g, j] = a.reshape(8, 128, ntok // 128, 128)
    maps = []
    for c in range(ncore):
        first = (c * tpc) % seqlen == 0
        B0 = c * nblk
        qb = np.stack([perm[g, 0][:, :, B0:B0 + nblk].reshape(8, 128, nblk * 128) for g in range(3)])
        kbs, vbs = [], []
        for g in range(3):
            kk = np.zeros((8, 128, nblk + 16, 128), qkvT.dtype)
            vv = np.zeros((8, 128, nblk + 16, 128), qkvT.dtype)
            lo = 0 if first else 16
            kk[:, :, 16 - lo:] = perm[g, 1][:, :, B0 - lo:B0 + nblk]
            vv[:, :, 16 - lo:] = perm[g, 2][:, :, B0 - lo:B0 + nblk]
            kbs.append(kk.reshape(8, 128, -1))
            vbs.append(vv.transpose(0, 3, 2, 1).reshape(8, 128, -1))
        maps.append({"qb": np.ascontiguousarray(qb), "kb": np.ascontiguousarray(np.stack(kbs)),
                     "vb": np.ascontiguousarray(np.stack(vbs)), "biasT": bias,
                     "negb": np.full((128, 1), NEG if first else 0.0, np.float32)})
    return maps


NEXP = 8
DFE = 3584


def build_layer1_rest(tpc=TPC, NT=1024):
    k = KB()
    aT = k.dram("aT", [D, tpc], F32, "ExternalInput")
    xT = k.dram("xT", [D, tpc], F32, "ExternalInput")
    wo = k.dram("wo", [D, D], F32, "ExternalInput")
    wr = k.dram("wr", [128, 8, NEXP], F32, "ExternalInput")
    w1 = k.dram("w1", [NEXP, D, DFE], F32, "ExternalInput")
    w3 = k.dram("w3", [NEXP, D, DFE], F32, "ExternalInput")
    w2 = k.dram("w2", [NEXP, DFE, D], F32, "ExternalInput")
    lng = k.dram("lng", [128, 32], F32, "ExternalInput")
    lnb = k.dram("lnb", [128, 32], F32, "ExternalInput")
    ident_d = k.dram("ident", [128, 128], F32, "ExternalInput")
    oT = k.dram("oT", [D, tpc], F32, "ExternalOutput")
    cx = Ctx(k, NT, 14)
    k.dma("sp", cx.lng, cx.lng[:, :], lng, lng.t[:, :])
    k.dma("sp", cx.lnb, cx.lnb[:, :], lnb, lnb.t[:, :])
    ident = k.sb("ident", [128, 128], F32)
    k.dma("sp", ident, ident[:, :], ident_d, ident_d.t[:, :])
    wr_sb = k.sb("wr", [128, 8, NEXP], F32)
    k.dma("sp", wr_sb, wr_sb[:, :, :], wr, wr.t[:, :, :])
    ones_bf = k.sb("ones_bf", [128, 128], BF16)
    k.op("pool", lambda g: g.memset(ones_bf[:, :], 1.0), [], [ones_bf])
    gate_bc = k.sb("gate_bc", [128, NEXP, NT], BF16)
    ntt = NT // 128
    lg = k.sb("lg", [128, ntt, 8], F32)
    top = k.sb("top", [128, ntt, 8], F32)
    gsm = [k.sb("gsm%d" % i, [128, ntt], F32) for i in range(4)]
    gate = k.sb("gate", [128, ntt, 8], F32)
    gt2 = k.sb("gt2", [128, ntt, 8], F32)
    diag = [k.sb("diag", [128, 128], BF16) for _ in range(2)]
    z = cx.z
    abf = cx.hmid
    for sb_ in range(tpc // NT):
        tsl = slice(sb_ * NT, (sb_ + 1) * NT)
        for kt in range(8):
            k.dma("pool", abf, abf[:, kt, :], aT, aT.t[kt * 128:(kt + 1) * 128, tsl])
            k.dma("sp", z, z[:, kt, :], xT, xT.t[kt * 128:(kt + 1) * 128, tsl])
        for cb in range(2):
            wa = cx.wa[cx.wi % 2]
            cx.wi += 1
            load_wblock(k, wa, wo, 0, 8, cb * 512, 512)
            for fl in range(4):
                m = cb * 4 + fl
                for tb in range(NT // 512):
                    po = cx.pso[cx.oi % 2]
                    cx.oi += 1
                    ts_ = slice(tb * 512, tb * 512 + 512)
                    for kt in range(8):
                        k.mm(po, po[:, :], wa, wa[:, kt, fl * 128:fl * 128 + 128], abf, abf[:, kt, ts_], kt == 0, kt == 7)
                    k.stt("dve", z, z[:, m, ts_], z, z[:, m, ts_], float(ALPHA), po, po[:, :], ALU.mult, ALU.add)
        layer_norm(k, cx, z, 2, cx.xbf)
        k.ses = True
        pl = cx.pss[0]
        for tt in range(ntt):
            for kt in range(8):
                k.mm(pl, pl[:, tt * 8:tt * 8 + 8], z, z[:, kt, tt * 128:(tt + 1) * 128], wr_sb, wr_sb[:, kt, :], kt == 0, kt == 7)
        k.act(lg, lg[:, :, :], pl, pl[:, 0:ntt * 8].rearrange("p (t e) -> p t e", e=8), AF.Copy)
        nv1, e2, g1, g2 = gsm
        for tt in range(ntt):
            k.op("dve", lambda g, o=top[:, tt, :], i=lg[:, tt, :]: g.max(out=o, in_=i), [lg], [top])
        k.ts("dve", nv1, nv1[:, :], top, top[:, :, 0], -1.0, ALU.mult)
        k.tt("dve", e2, e2[:, :], top, top[:, :, 1], nv1, nv1[:, :], ALU.add)
        k.act(e2, e2[:, :], e2, e2[:, :], AF.Exp)
        k.ts("dve", g1, g1[:, :], e2, e2[:, :], 1.0, ALU.add)
        k.op("dve", lambda g: g.reciprocal(out=g1[:, :], in_=g1[:, :]), [g1], [g1])
        k.tt("dve", g2, g2[:, :], e2, e2[:, :], g1, g1[:, :], ALU.mult)
        for tt in range(ntt):
            k.ts("dve", gate, gate[:, tt, :], lg, lg[:, tt, :], top[:, tt, 0:1], ALU.is_equal, g1[:, tt:tt + 1], ALU.mult,
                 extra_reads=[top, g1])
            k.ts("dve", gt2, gt2[:, tt, :], lg, lg[:, tt, :], top[:, tt, 1:2], ALU.is_equal, g2[:, tt:tt + 1], ALU.mult,
                 extra_reads=[top, g2])
        k.tt("dve", gate, gate[:, :, :], gate, gate[:, :, :], gt2, gt2[:, :, :], ALU.add)
        di = 0
        for e in range(NEXP):
            for hb in range(NT // 512):
                pg = cx.pss[1]
                for t4 in range(4):
                    tt = hb * 4 + t4
                    dg = diag[di % 2]
                    di += 1
                    k.ts("dve", dg, dg[:, :], ident, ident[:, :], gate[:, tt, e:e + 1], ALU.mult, extra_reads=[gate])
                    k.mm(pg, pg[:, t4 * 128:(t4 + 1) * 128], ones_bf, ones_bf[:, :], dg, dg[:, :], True, True)
                k.act(gate_bc, gate_bc[:, e, hb * 512:(hb + 1) * 512], pg, pg[:, :], AF.Copy)
        k.ses = SES
        for kt in range(8):
            k.ts("dve", z, z[:, kt, :], z, z[:, kt, :], float(ALPHA), ALU.mult)
        for e in range(NEXP):
            w1e, w3e, w2e = Buf("w1e", w1.t[e]), Buf("w3e", w3.t[e]), Buf("w2e", w2.t[e])
            for half in range(2):
                f0 = half * 14

                def emit_moe(ft, t0, tw, sa, pB, e=e):
                    ts_ = slice(t0, t0 + 512)
                    t = cx.tmp[(ft + t0 // 512) % 2]
                    k.tt("dve", t, t[:, :], pB, pB[:, :], sa, sa[:, :], ALU.mult)
                    k.tt("dve", cx.hmid, cx.hmid[:, ft, ts_], t, t[:, :], gate_bc, gate_bc[:, e, ts_], ALU.mult)

                glu_phase(k, cx, cx.xbf, w1e, f0 * 128, w3e, f0 * 128, 14 * 128, AF.Silu, emit_moe,
                          hook=lambda f0=f0, w2e=w2e: down_load(k, cx, w2e, f0, 14))
                down_phase(k, cx, w2e, f0, 14, z)
        layer_norm(k, cx, z, 3, None)
        for kt in range(8):
            k.dma("sp", oT, oT.t[kt * 128:(kt + 1) * 128, tsl], z, z[:, kt, :])
    return k.finish()


_PROGS = {}


def _prog(name, fn):
    if name not in _PROGS:
        _PROGS[name] = fn()
    return _PROGS[name]


def _run(nc, maps):
    res = run_bass_kernel_spmd(nc, maps, core_ids=list(range(NCORES)))
    return res.results


def kernel(x, s5_w_in, s5_a_re, s5_a_im, s5_log_dt, s5_b_re, s5_b_im, s5_c_re, s5_c_im, s5_d,
           s5_w_glu, attn_w_qkv, attn_w_o, rel_bias, ffn_w1, ffn_w3, ffn_w2, moe_w_router,
           moe_w1, moe_w3, moe_w2, ln_g, ln_b):
    f32 = np.float32
    x = np.asarray(x, f32)
    xT = np.ascontiguousarray(x.reshape(NTOK, D).T)
    cols = [slice(c * TPC, (c + 1) * TPC) for c in range(NCORES)]
    xTs = [np.ascontiguousarray(xT[:, s]) for s in cols]
    lg, lb = ln_layout(np.asarray(ln_g, f32), np.asarray(ln_b, f32))

    w_in = np.ascontiguousarray(np.asarray(s5_w_in, f32)[0])
    r = _run(_prog("A", build_uproj), [{"xT": xTs[c], "w": w_in} for c in range(NCORES)])
    u_full = np.concatenate([r[c]["uT"] for c in range(NCORES)], axis=1)
    maps = []
    for c in range(NCORES):
        m = s5_core_params(c, np.asarray(s5_a_re, f32)[0], np.asarray(s5_a_im, f32)[0], np.asarray(s5_log_dt, f32)[0],
                           np.asarray(s5_b_re, f32)[0], np.asarray(s5_b_im, f32)[0], np.asarray(s5_c_re, f32)[0],
                           np.asarray(s5_c_im, f32)[0], np.asarray(s5_d, f32)[0])
        m["u"] = np.ascontiguousarray(u_full[128 * c:128 * c + 128])
        maps.append(m)
    r = _run(_prog("B", build_s5), maps)
    y_full = np.concatenate([r[c]["y"] for c in range(NCORES)], axis=0)
    wglu = np.ascontiguousarray(np.asarray(s5_w_glu, f32)[0])
    w1 = np.ascontiguousarray(np.asarray(ffn_w1, f32)[0])
    w3 = np.ascontiguousarray(np.asarray(ffn_w3, f32)[0])
    w2 = np.ascontiguousarray(np.asarray(ffn_w2, f32)[0])
    r = _run(_prog("C", build_layer0_rest),
             [{"yT": np.ascontiguousarray(y_full[:, cols[c]]), "xT": xTs[c], "wglu": wglu, "w1": w1, "w3": w3, "w2": w2,
               "lng": lg, "lnb": lb} for c in range(NCORES)])
    x2s = [r[c]["oT"] for c in range(NCORES)]
    wqkv = np.ascontiguousarray(np.asarray(attn_w_qkv, f32)[0])
    r = _run(_prog("D", build_qkv), [{"xT": x2s[c], "w": wqkv} for c in range(NCORES)])
    qkvT = np.concatenate([r[c]["qkvT"] for c in range(NCORES)], axis=1)
    maps = attn_host_layout(qkvT, np.asarray(rel_bias, f32), SEQ, TPC)
    r = _run(_prog("E", build_attn), maps)
    aTs = [r[c]["attnT"] for c in range(NCORES)]
    wo = np.ascontiguousarray(np.asarray(attn_w_o, f32)[0])
    wr = np.ascontiguousarray(np.asarray(moe_w_router, f32)[0].reshape(8, 128, NEXP).transpose(1, 0, 2))
    mw1 = np.ascontiguousarray(np.asarray(moe_w1, f32)[0])
    mw3 = np.ascontiguousarray(np.asarray(moe_w3, f32)[0])
    mw2 = np.ascontiguousarray(np.asarray(moe_w2, f32)[0])
    ident = np.eye(128, dtype=f32)
    ltri = np.triu(np.ones((128, 128), f32), 1)
    ln4 = np.ascontiguousarray(np.stack([np.asarray(ln_g, f32)[1, 1], np.asarray(ln_b, f32)[1, 1]]))
    r = _run(_prog("F2", build_layer1_fast),
             [{"aT": aTs[c], "xT": x2s[c], "wo": wo, "wr": wr, "w1": mw1, "w3": mw3, "w2": mw2, "lng": lg, "lnb": lb,
               "ln4": ln4, "ident": ident, "ltri": ltri} for c in range(NCORES)])
    counts = np.stack([r[c]["cnt"][0] for c in range(NCORES)])
    if counts.max() <= CAP:
        out = np.concatenate([r[c]["o"] for c in range(NCORES)], axis=0)
        return np.ascontiguousarray(out).reshape(BATCH, SEQ, D).astype(f32)
    r = _run(_prog("F", build_layer1_rest),
             [{"aT": aTs[c], "xT": x2s[c], "wo": wo, "wr": wr, "w1": mw1, "w3": mw3, "w2": mw2, "lng": lg, "lnb": lb,
               "ident": ident} for c in range(NCORES)])
    oT = np.concatenate([r[c]["oT"] for c in range(NCORES)], axis=1)
    return np.ascontiguousarray(oT.T).reshape(BATCH, SEQ, D).astype(f32)


CAP = 1280
U32 = mybir.dt.uint32
OOR = 1.0e6


class NS:
    pass


def build_layer1_fast(tpc=TPC, NT=1024, cap=CAP):
    k = KB()
    aT = k.dram("aT", [D, tpc], F32, "ExternalInput")
    xT = k.dram("xT", [D, tpc], F32, "ExternalInput")
    wo = k.dram("wo", [D, D], F32, "ExternalInput")
    wr = k.dram("wr", [128, 8, NEXP], F32, "ExternalInput")
    w1 = k.dram("w1", [NEXP, D, DFE], F32, "ExternalInput")
    w3 = k.dram("w3", [NEXP, D, DFE], F32, "ExternalInput")
    w2 = k.dram("w2", [NEXP, DFE, D], F32, "ExternalInput")
    lng = k.dram("lng", [128, 32], F32, "ExternalInput")
    lnb = k.dram("lnb", [128, 32], F32, "ExternalInput")
    ln4 = k.dram("ln4", [2, D], F32, "ExternalInput")
    ident_d = k.dram("ident", [128, 128], F32, "ExternalInput")
    ltri_d = k.dram("ltri", [128, 128], F32, "ExternalInput")
    o = k.dram("o", [tpc, D], F32, "ExternalOutput")
    cnt_o = k.dram("cnt", [128, NEXP], F32, "ExternalOutput")
    xg = k.dram("xg", [NEXP * cap + 1, D], BF16)
    ye = k.dram("ye", [NEXP * cap + 1, D], F32)
    x3tm = k.dram("x3tm", [tpc, D], F32)
    ntile = tpc // 128
    ntt = NT // 128
    nst = cap // 128

    ident = k.sb("ident", [128, 128], F32)
    k.dma("sp", ident, ident[:, :], ident_d, ident_d.t[:, :])
    ident_bf = k.sb("ident_bf", [128, 128], BF16)
    k.dma("pool", ident_bf, ident_bf[:, :], ident_d, ident_d.t[:, :])
    ltri = k.sb("ltri", [128, 128], BF16)
    k.dma("pool", ltri, ltri[:, :], ltri_d, ltri_d.t[:, :])
    ones_bf = k.sb("ones_bf", [128, 128], BF16)
    k.op("pool", lambda g, a=ones_bf[:, :]: g.memset(a, 1.0), [], [ones_bf])
    IDX = [k.sb("idx%d" % j, [128, ntile], U32) for j in range(2)]
    G = [k.sb("G%d" % j, [128, ntile], F32) for j in range(2)]
    base = k.sb("base", [128, NEXP], F32)
    k.op("pool", lambda g, a=base[:, :]: g.memset(a, 0.0), [], [base])
    iota_e = k.sb("iota_e", [128, ntt, NEXP], F32)
    for e in range(NEXP):
        k.op("pool", lambda g, e=e, a=iota_e[:, :, e:e + 1]: g.memset(a, float(e)), [], [iota_e])

    k.push_scope()
    cx = NS()
    cx.k, cx.NT = k, NT
    cx.pso = [k.ps("pso") for _ in range(2)]
    cx.pss = [k.ps("pss") for _ in range(2)]
    ptb = [k.ps("ptb", (128, 1024), BF16) for _ in range(2)]
    ptf = [k.ps("ptf") for _ in range(2)]
    cx.wa = [k.sb("wa", [128, 8, 512], BF16) for _ in range(2)]
    cx.z = k.sb("z", [128, 8, NT], F32)
    cx.xbf = k.sb("xbf", [128, 8, NT], BF16)
    abf = k.sb("abf", [128, 8, NT], BF16)
    cx.tmp = [k.sb("tmp", [128, 512], F32) for _ in range(2)]
    cx.ones = k.sb("ones", [128, 128], F32)
    k.op("pool", lambda g, a=cx.ones[:, :]: g.memset(a, 1.0), [], [cx.ones])
    cx.sq = [k.sb("sq", [128, 512], F32) for _ in range(2)]
    cx.mean = k.sb("mean", [128, 512], F32)
    cx.ex2 = k.sb("ex2", [128, 512], F32)
    cx.rstd = k.sb("rstd", [128, 512], F32)
    cx.nmr = k.sb("nmr", [128, 512], F32)
    cx.lng = k.sb("lng", [128, 32], F32)
    cx.lnb = k.sb("lnb", [128, 32], F32)
    cx.wi = cx.pi = cx.oi = 0
    k.dma("sp", cx.lng, cx.lng[:, :], lng, lng.t[:, :])
    k.dma("sp", cx.lnb, cx.lnb[:, :], lnb, lnb.t[:, :])
    wr_sb = k.sb("wr", [128, 8, NEXP], F32)
    k.dma("sp", wr_sb, wr_sb[:, :, :], wr, wr.t[:, :, :])
    lg = k.sb("lg", [128, ntt, 8], F32)
    top = k.sb("top", [128, ntt, 8], F32)
    nv1, e2, g1, g2 = [k.sb("gsm%d" % i, [128, ntt], F32) for i in range(4)]
    m = [k.sb("m%d" % j, [128, ntt, 8], F32) for j in range(2)]
    msel = k.sb("msel", [128, ntt, 8], BF16)
    Pw = k.sb("Pw", [128, ntt, 8], F32)
    cn = k.sb("cn", [128, ntt, 8], F32)
    offs = k.sb("offs", [128, ntt + 1, 8], F32)
    pos = k.sb("pos", [128, ntt, 8], F32)
    prod = k.sb("prod", [128, ntt, 8], F32)
    psel = [k.sb("psel%d" % j, [128, ntt], F32) for j in range(2)]
    esel = [k.sb("esel%d" % j, [128, ntt], F32) for j in range(2)]
    ovf = k.sb("ovf", [128, ntt], F32)
    flat = k.sb("flat", [128, ntt], F32)
    xtm = [k.sb("xtm", [128, D], BF16) for _ in range(2)]
    x3s = [k.sb("x3s", [128, D], F32) for _ in range(2)]
    z = cx.z
    for sb_ in range(tpc // NT):
        tsl = slice(sb_ * NT, (sb_ + 1) * NT)
        k.ses = SES
        for kt in range(8):
            k.dma("pool", abf, abf[:, kt, :], aT, aT.t[kt * 128:(kt + 1) * 128, tsl])
            k.dma("sp", z, z[:, kt, :], xT, xT.t[kt * 128:(kt + 1) * 128, tsl])
        for cb in range(2):
            wa = cx.wa[cx.wi % 2]
            cx.wi += 1
            load_wblock(k, wa, wo, 0, 8, cb * 512, 512)
            for fl in range(4):
                mt = cb * 4 + fl
                for tb in range(NT // 512):
                    po = cx.pso[cx.oi % 2]
                    cx.oi += 1
                    ts_ = slice(tb * 512, tb * 512 + 512)
                    for kt in range(8):
                        k.mm(po, po[:, :], wa, wa[:, kt, fl * 128:fl * 128 + 128], abf, abf[:, kt, ts_], kt == 0, kt == 7)
                    k.stt("dve", z, z[:, mt, ts_], z, z[:, mt, ts_], float(ALPHA), po, po[:, :], ALU.mult, ALU.add)
        layer_norm(k, cx, z, 2, cx.xbf)
        k.ses = True
        pl = cx.pss[0]
        for tt in range(ntt):
            for kt in range(8):
                k.mm(pl, pl[:, tt * 8:tt * 8 + 8], z, z[:, kt, tt * 128:(tt + 1) * 128], wr_sb, wr_sb[:, kt, :], kt == 0, kt == 7)
        k.act(lg, lg[:, :, :], pl, pl[:, 0:ntt * 8].rearrange("p (t e) -> p t e", e=8), AF.Copy)
        for tt in range(ntt):
            k.op("dve", lambda g, o_=top[:, tt, :], i=lg[:, tt, :]: g.max(out=o_, in_=i), [lg], [top])
        k.ts("dve", nv1, nv1[:, :], top, top[:, :, 0], -1.0, ALU.mult)
        k.tt("dve", e2, e2[:, :], top, top[:, :, 1], nv1, nv1[:, :], ALU.add)
        k.act(e2, e2[:, :], e2, e2[:, :], AF.Exp)
        k.ts("dve", g1, g1[:, :], e2, e2[:, :], 1.0, ALU.add)
        k.op("dve", lambda g, a=g1[:, :]: g.reciprocal(out=a, in_=a), [g1], [g1])
        k.tt("dve", g2, g2[:, :], e2, e2[:, :], g1, g1[:, :], ALU.mult)
        t0_ = sb_ * ntt
        k.op("dve", lambda g, o_=G[0][:, t0_:t0_ + ntt], i_=g1[:, :]: g.tensor_copy(out=o_, in_=i_), [g1], [G[0]])
        k.op("dve", lambda g, o_=G[1][:, t0_:t0_ + ntt], i_=g2[:, :]: g.tensor_copy(out=o_, in_=i_), [g2], [G[1]])
        for tt in range(ntt):
            k.ts("dve", m[0], m[0][:, tt, :], lg, lg[:, tt, :], top[:, tt, 0:1], ALU.is_equal, extra_reads=[top])
            k.ts("dve", m[1], m[1][:, tt, :], lg, lg[:, tt, :], top[:, tt, 1:2], ALU.is_equal, extra_reads=[top])
        k.tt("dve", msel, msel[:, :, :], m[0], m[0][:, :, :], m[1], m[1][:, :, :], ALU.add)
        pp = cx.pss[1]
        mflat = msel[:, :, :].rearrange("p t e -> p (t e)")
        k.mm(pp, pp[:, 0:ntt * 8], ltri, ltri[:, :], msel, mflat, True, True)
        k.mm(pp, pp[:, 256:256 + ntt * 8], ones_bf, ones_bf[:, :], msel, mflat, True, True)
        k.act(Pw, Pw[:, :, :], pp, pp[:, 0:ntt * 8].rearrange("p (t e) -> p t e", e=8), AF.Copy)
        k.act(cn, cn[:, :, :], pp, pp[:, 256:256 + ntt * 8].rearrange("p (t e) -> p t e", e=8), AF.Copy)
        k.op("dve", lambda g, o_=offs[:, 0, :], i_=base[:, :]: g.tensor_copy(out=o_, in_=i_), [base], [offs])
        for tt in range(ntt):
            k.tt("dve", offs, offs[:, tt + 1, :], offs, offs[:, tt, :], cn, cn[:, tt, :], ALU.add)
        k.op("dve", lambda g, o_=base[:, :], i_=offs[:, ntt, :]: g.tensor_copy(out=o_, in_=i_), [offs], [base])
        k.tt("dve", pos, pos[:, :, :], Pw, Pw[:, :, :], offs, offs[:, 0:ntt, :], ALU.add)
        for j in range(2):
            k.tt("dve", prod, prod[:, :, :], m[j], m[j][:, :, :], pos, pos[:, :, :], ALU.mult)
            k.op("dve", lambda g, o_=psel[j][:, :], i_=prod[:, :, :]: g.reduce_sum(out=o_, in_=i_, axis=AX.X), [prod], [psel[j]])
            k.tt("dve", prod, prod[:, :, :], m[j], m[j][:, :, :], iota_e, iota_e[:, :, :], ALU.mult)
            k.op("dve", lambda g, o_=esel[j][:, :], i_=prod[:, :, :]: g.reduce_sum(out=o_, in_=i_, axis=AX.X), [prod], [esel[j]])
            k.stt("dve", flat, flat[:, :], esel[j], esel[j][:, :], float(cap), psel[j], psel[j][:, :], ALU.mult, ALU.add)
            k.ts("dve", ovf, ovf[:, :], psel[j], psel[j][:, :], float(cap) - 0.5, ALU.is_gt)
            k.stt("dve", flat, flat[:, :], ovf, ovf[:, :], OOR, flat, flat[:, :], ALU.mult, ALU.add)
            k.ts("dve", flat, flat[:, :], flat, flat[:, :], float(NEXP * cap), ALU.min)
            k.op("dve", lambda g, o_=IDX[j][:, t0_:t0_ + ntt], i_=flat[:, :]: g.tensor_copy(out=o_, in_=i_), [flat], [IDX[j]])
        k.ses = SES
        for kt in range(8):
            k.ts("dve", z, z[:, kt, :], z, z[:, kt, :], float(ALPHA), ALU.mult)
        for tt in range(ntt):
            gt = t0_ + tt
            pb_, xt_ = ptb[tt % 2], xtm[tt % 2]
            for kt in range(8):
                k.tr(pb_, pb_[:, kt * 128:(kt + 1) * 128], cx.xbf, cx.xbf[:, kt, tt * 128:(tt + 1) * 128], ident_bf, ident_bf[:, :])
            k.act(xt_, xt_[:, :], pb_, pb_[:, :], AF.Copy)
            for j in range(2):
                k.idma(xg, xg.t[:, :], xt_, xt_[:, :], IDX[j], IDX[j][:, gt:gt + 1], True, NEXP * cap - 1)
            x3_ = x3s[tt % 2]
            for hf in range(2):
                pf_ = ptf[hf]
                for kq in range(4):
                    kt = hf * 4 + kq
                    k.tr(pf_, pf_[:, kq * 128:(kq + 1) * 128], z, z[:, kt, tt * 128:(tt + 1) * 128], ident, ident[:, :])
                k.op("dve", lambda g, o_=x3_[:, hf * 512:(hf + 1) * 512], i_=pf_[:, :]: g.tensor_copy(out=o_, in_=i_), [pf_], [x3_])
            k.dma("sp", x3tm, x3tm.t[gt * 128:(gt + 1) * 128, :], x3_, x3_[:, :])
    k.dma("sp", cnt_o, cnt_o.t[:, :], base, base[:, :])
    k.pop_scope()

    k.push_scope()
    cx = NS()
    cx.k, cx.NT = k, cap
    cx.ps1 = [k.ps("ps1") for _ in range(2)]
    cx.ps3 = [k.ps("ps3") for _ in range(2)]
    cx.pso = [k.ps("pso") for _ in range(2)]
    ptr = [k.ps("ptr", (128, 512), BF16) for _ in range(2)]
    cx.wa = [k.sb("wa", [128, 8, 512], BF16) for _ in range(2)]
    cx.wb = [k.sb("wb", [128, 8, 512], BF16) for _ in range(2)]
    cx.w2h = k.sb("w2h", [128, 14, 1024], BF16)
    cx.hmid = k.sb("hmid", [128, 14, cap], BF16)
    cx.sact = [k.sb("sact", [128, 512], F32) for _ in range(2)]
    cx.wi = cx.pi = cx.oi = 0
    xe = k.sb("xge", [128, 8, cap], BF16)
    xs = [k.sb("xs", [128, 4, D], BF16) for _ in range(2)]
    yacc = k.sb("yacc", [128, nst, D], F32)
    xi = 0
    ti = 0
    for e in range(NEXP):
        for sg in range((nst + 3) // 4):
            n_ = min(4, nst - 4 * sg)
            xs_ = xs[xi % 2]
            xi += 1
            r0 = e * cap + sg * 512
            k.dma("sp", xs_, xs_[:, 0:n_, :], xg, xg.t[r0:r0 + n_ * 128, :].rearrange("(t p) f -> p t f", p=128))
            for kt in range(8):
                pt_ = ptr[ti % 2]
                for t in range(n_):
                    k.tr(pt_, pt_[:, t * 128:(t + 1) * 128], xs_, xs_[:, t, kt * 128:(kt + 1) * 128], ident_bf, ident_bf[:, :])
                if ti % 2 == 0:
                    k.act(xe, xe[:, kt, sg * 512:sg * 512 + n_ * 128], pt_, pt_[:, 0:n_ * 128], AF.Copy)
                else:
                    k.op("dve", lambda g, o_=xe[:, kt, sg * 512:sg * 512 + n_ * 128], i_=pt_[:, 0:n_ * 128]:
                         g.tensor_copy(out=o_, in_=i_), [pt_], [xe])
                ti += 1
        w1e, w3e, w2e = Buf("w1e", w1.t[e]), Buf("w3e", w3.t[e]), Buf("w2e", w2.t[e])
        for half in range(2):
            f0 = half * 14

            def emit_h(ft, t0, tw, sa, pB, cx=cx):
                k.tt("dve", cx.hmid, cx.hmid[:, ft, t0:t0 + tw], pB, pB[:, 0:tw], sa, sa[:, 0:tw], ALU.mult)

            glu_phase(k, cx, xe, w1e, f0 * 128, w3e, f0 * 128, 14 * 128, AF.Silu, emit_h,
                      hook=lambda f0=f0, w2e=w2e, cx=cx: down_load(k, cx, w2e, f0, 14), NT=cap)
            for st in range(nst):
                for fh in range(2):
                    po = cx.pso[cx.oi % 2]
                    cx.oi += 1
                    for kt in range(14):
                        k.mm(po, po[:, :], cx.hmid, cx.hmid[:, kt, st * 128:(st + 1) * 128], cx.w2h, cx.w2h[:, kt, fh * 512:(fh + 1) * 512],
                             kt == 0, kt == 13)
                    ysl = yacc[:, st, fh * 512:(fh + 1) * 512]
                    if half == 0:
                        k.act(yacc, ysl, po, po[:, :], AF.Copy)
                    else:
                        k.tt("dve", yacc, ysl, po, po[:, :], yacc, ysl, ALU.add)
        k.dma("sp", ye, ye.t[e * cap:(e + 1) * cap, :].rearrange("(t p) f -> p t f", p=128), yacc, yacc[:, :, :])
    k.pop_scope()

    k.push_scope()
    gB = k.sb("gB", [128, D], F32)
    bB = k.sb("bB", [128, D], F32)
    k.dma("sp", gB, gB[:, :], ln4, ln4.t[0:1, :].to_broadcast([128, D]))
    k.dma("sp", bB, bB[:, :], ln4, ln4.t[1:2, :].to_broadcast([128, D]))
    NB3 = 3
    r1 = [k.sb("r1", [128, D], F32) for _ in range(NB3)]
    r2 = [k.sb("r2", [128, D], F32) for _ in range(NB3)]
    xa = [k.sb("xa", [128, D], F32) for _ in range(NB3)]
    zt = [k.sb("zt", [128, D], F32) for _ in range(NB3)]
    st6 = [k.sb("st6", [128, 2, 6], F32) for _ in range(NB3)]
    mv = [k.sb("mv", [128, 2], F32) for _ in range(NB3)]
    sd = [k.sb("sd", [128, 1], F32) for _ in range(NB3)]
    rs_l = [k.sb("rs", [128, 1], F32) for _ in range(NB3)]
    nm_l = [k.sb("nm", [128, 1], F32) for _ in range(NB3)]
    for t in range(ntile):
        i3 = t % NB3
        a_, b_, x_, z_ = r1[i3], r2[i3], xa[i3], zt[i3]
        s6, mv_, sd_, rs_, nm_ = st6[i3], mv[i3], sd[i3], rs_l[i3], nm_l[i3]
        k.ses = SES
        k.idma(a_, a_[:, :], ye, ye.t[:, :], IDX[0], IDX[0][:, t:t + 1], False, NEXP * cap - 1)
        k.idma(b_, b_[:, :], ye, ye.t[:, :], IDX[1], IDX[1][:, t:t + 1], False, NEXP * cap - 1)
        k.dma("sp", x_, x_[:, :], x3tm, x3tm.t[t * 128:(t + 1) * 128, :])
        k.stt("dve", z_, z_[:, :], a_, a_[:, :], G[0][:, t:t + 1], x_, x_[:, :], ALU.mult, ALU.add, extra_reads=[G[0]])
        k.stt("dve", z_, z_[:, :], b_, b_[:, :], G[1][:, t:t + 1], z_, z_[:, :], ALU.mult, ALU.add, extra_reads=[G[1]])
        k.ses = True
        for hf in range(2):
            k.op("dve", lambda g, o_=s6[:, hf, :], i_=z_[:, hf * 512:(hf + 1) * 512]: g.bn_stats(out=o_, in_=i_), [z_], [s6])
        k.op("dve", lambda g, o_=mv_[:, :], i_=s6[:, :, :].rearrange("p a b -> p (a b)"): g.bn_aggr(out=o_, in_=i_), [s6], [mv_])
        k.act(sd_, sd_[:, :], mv_, mv_[:, 1:2], AF.Sqrt, bias=LN_EPS)
        k.op("dve", lambda g, o_=rs_[:, :], i_=sd_[:, :]: g.reciprocal(out=o_, in_=i_), [sd_], [rs_])
        k.stt("dve", nm_, nm_[:, :], mv_, mv_[:, 0:1], -1.0, rs_, rs_[:, :], ALU.mult, ALU.mult)
        k.op("act", lambda g, a=z_[:, :], sc=rs_[:, 0:1], bi=nm_[:, 0:1]: g.activation(
            out=a, in_=a, func=AF.Identity, scale=sc, bias=bi), [z_, rs_, nm_], [z_])
        k.ses = SES
        k.tt("dve", z_, z_[:, :], z_, z_[:, :], gB, gB[:, :], ALU.mult)
        k.tt("pool", z_, z_[:, :], z_, z_[:, :], bB, bB[:, :], ALU.add)
        k.dma("sp", o, o.t[t * 128:(t + 1) * 128, :], z_, z_[:, :])
    k.pop_scope()
    return k.finish()
```
